# Optimizing a Trainium2 kernel written in Bass

```python
import jax, jax.numpy as jnp
from jax import lax
import numpy as np

D_MODEL = 2048
BATCH = 4
SEQ = 4096
DEPTH = 1

A_HEADS = 8
A_HEAD_DIM = 128
A_WIDTH = A_HEADS * A_HEAD_DIM
MOBA_BLOCK = 256
MOBA_TOPK = 3
Q_CHUNK = 16
R_HEADS = 8
R_KEY_DIM = 128
R_VAL_DIM = 128
R_WIDTH = R_HEADS * R_KEY_DIM
R_CHUNK = 64
N_GROUPS = 4
EXPERTS_PER_GROUP = 8
N_EXPERTS = N_GROUPS * EXPERTS_PER_GROUP
TOP_K_INNER = 2
D_EXPERT = 512
MOE_BLOCK = 256
EPS = 1e-6
IN_SPLITS = (A_WIDTH, A_WIDTH, A_WIDTH, R_WIDTH, R_WIDTH, R_WIDTH, R_WIDTH, D_MODEL, D_MODEL)
IN_COLS = sum(IN_SPLITS)

kernel_name = "hybrid_moba_hgrn2_hiermoe_adaln"


def rms_norm(x, g):
    xf = x.astype(jnp.float32)
    y = xf * lax.rsqrt(jnp.mean(xf * xf, axis=-1, keepdims=True) + EPS)
    return (y * g.astype(jnp.float32)).astype(x.dtype)


def split_heads(t, n_heads):
    b, s, w = t.shape
    return t.reshape(b, s, n_heads, w // n_heads).transpose(0, 2, 1, 3)


def merge_heads(t):
    b, h, s, d = t.shape
    return t.transpose(0, 2, 1, 3).reshape(b, s, h * d)


def moba_attention(q, k, v):
    B, H, S, Dh = q.shape
    L = MOBA_BLOCK
    s_pad = -(-S // L) * L
    pad = ((0, 0), (0, 0), (0, s_pad - S), (0, 0))
    q, k, v = jnp.pad(q, pad), jnp.pad(k, pad), jnp.pad(v, pad)
    nb = s_pad // L
    kb = k.reshape(B, H, nb, L, Dh)
    vb = v.reshape(B, H, nb, L, Dh)
    k_mean = jnp.mean(kb.astype(jnp.float32), axis=3)
    gate = jnp.einsum('bhsd,bhnd->bhsn', q.astype(jnp.float32), k_mean)
    q_blk = jnp.arange(s_pad) // L
    past = jnp.arange(nb)[None, :] < q_blk[:, None]
    gate = jnp.where(past, gate, -jnp.inf)
    if nb < MOBA_TOPK:
        gate = jnp.pad(gate, ((0, 0), (0, 0), (0, 0), (0, MOBA_TOPK - nb)), constant_values=-jnp.inf)
    _, sel = lax.top_k(gate, MOBA_TOPK)
    valid = sel < q_blk[:, None]
    sel = jnp.minimum(sel, nb - 1)
    nc = s_pad // Q_CHUNK
    scale = Dh ** -0.5

    def chunks(t):
        return jnp.moveaxis(t.reshape(B, H, nc, Q_CHUNK, t.shape[-1]), 2, 0)

    gather = jax.vmap(jax.vmap(lambda blocks, idx: blocks[idx]))

    def one_chunk(args):
        qc, selc, validc, ci = args
        kg = gather(kb, selc)
        vg = gather(vb, selc)
        s_sel = jnp.einsum('bhqd,bhqkld->bhqkl', qc, kg, preferred_element_type=jnp.float32) * scale
        s_sel = jnp.where(validc[..., None], s_sel, -jnp.inf)
        q0 = ci * Q_CHUNK
        blk = q0 // L
        k_own = lax.dynamic_slice_in_dim(k, blk * L, L, axis=2)
        v_own = lax.dynamic_slice_in_dim(v, blk * L, L, axis=2)
        q_pos = q0 + jnp.arange(Q_CHUNK)
        k_pos = blk * L + jnp.arange(L)
        s_own = jnp.einsum('bhqd,bhld->bhql', qc, k_own, preferred_element_type=jnp.float32) * scale
        s_own = jnp.where(k_pos[None, :] <= q_pos[:, None], s_own, -jnp.inf)
        s_all = jnp.concatenate([s_sel.reshape(B, H, Q_CHUNK, MOBA_TOPK * L), s_own], axis=-1)
        p = jax.nn.softmax(s_all, axis=-1).astype(v.dtype)
        p_sel = p[..., :MOBA_TOPK * L].reshape(B, H, Q_CHUNK, MOBA_TOPK, L)
        p_own = p[..., MOBA_TOPK * L:]
        return (jnp.einsum('bhqkl,bhqkld->bhqd', p_sel, vg)
                + jnp.einsum('bhql,bhld->bhqd', p_own, v_own))

    out = lax.map(one_chunk, (chunks(q), chunks(sel), chunks(valid), jnp.arange(nc)))
    out = jnp.moveaxis(out, 0, 2).reshape(B, H, s_pad, Dh)
    return out[:, :, :S]


def hgrn2_recurrence(q, k, v, log_f):
    B, H, S, Dk = q.shape
    Dv = v.shape[-1]
    C = R_CHUNK
    nc = S // C

    def chunks(t):
        return jnp.moveaxis(t.astype(jnp.float32).reshape(B, H, nc, C, t.shape[-1]), 2, 0)

    causal = jnp.tril(jnp.ones((C, C), dtype=bool))

    def step(state, inp):
        qc, kc, vc, gc = inp
        b = jnp.cumsum(gc, axis=2)
        diff = b[:, :, :, None, :] - b[:, :, None, :, :]
        decay = jnp.exp(jnp.where(causal[:, :, None], diff, -jnp.inf))
        attn = jnp.einsum('bhtd,bhsd,bhtsd->bhts', qc, kc, decay)
        o = (jnp.einsum('bhts,bhse->bhte', attn, vc)
             + jnp.einsum('bhtd,bhde->bhte', qc * jnp.exp(b), state))
        b_last = b[:, :, -1:, :]
        state = (jnp.exp(b_last)[:, :, 0, :, None] * state
                 + jnp.einsum('bhsd,bhse->bhde', kc * jnp.exp(b_last - b), vc))
        return state, o

    state0 = jnp.zeros((B, H, Dk, Dv), jnp.float32)
    _, o = lax.scan(step, state0, (chunks(q), chunks(k), chunks(v), chunks(log_f)))
    return jnp.moveaxis(o, 0, 2).reshape(B, H, S, Dv)


def hybrid_mixer(h, w_in, lb, r_norm_g, w_up_a, w_up_r, w_out):
    proj = h @ w_in
    idx = np.cumsum(IN_SPLITS)[:-1].tolist()
    a_q, a_k, a_v, r_q, r_f, r_i, r_og, g_a, g_r = jnp.split(proj, idx, axis=-1)
    att = moba_attention(split_heads(a_q, A_HEADS), split_heads(a_k, A_HEADS), split_heads(a_v, A_HEADS))
    y_a = merge_heads(att.astype(h.dtype)) @ w_up_a
    lb_h = lb.reshape(1, R_HEADS, 1, R_KEY_DIM)
    f_logit = split_heads(r_f, R_HEADS).astype(jnp.float32)
    f = lb_h + (1.0 - lb_h) * jax.nn.sigmoid(f_logit)
    k_in = (1.0 - lb_h) * jax.nn.sigmoid(-f_logit)
    o = hgrn2_recurrence(split_heads(r_q, R_HEADS), k_in, split_heads(r_i, R_HEADS), jnp.log(f))
    o = rms_norm(o, r_norm_g).astype(h.dtype)
    o = merge_heads(o) * jax.nn.silu(r_og)
    y_r = o @ w_up_r
    merged = jax.nn.sigmoid(g_a) * y_a + jax.nn.sigmoid(g_r) * y_r
    return merged @ w_out


def hier_moe(h, w_rg, b_rg, w_re, b_re, w1, w3, w2):
    B, S, D = h.shape
    T = B * S
    ht = h.reshape(T, D)
    lg = (ht @ w_rg + b_rg).astype(jnp.float32)
    pg = jax.nn.softmax(lg, axis=-1)
    grp = jnp.argmax(lg, axis=-1)
    pg_top = jnp.take_along_axis(pg, grp[:, None], axis=-1)
    le = (ht @ w_re + b_re).astype(jnp.float32).reshape(T, N_GROUPS, EXPERTS_PER_GROUP)
    le_g = jnp.take_along_axis(le, grp[:, None, None], axis=1)[:, 0]
    pe = jax.nn.softmax(le_g, axis=-1)
    top_p, top_e = lax.top_k(pe, TOP_K_INNER)
    wts = pg_top * top_p / jnp.sum(top_p, axis=-1, keepdims=True)
    eid = grp[:, None] * EXPERTS_PER_GROUP + top_e
    TK = T * TOP_K_INNER
    flat_e = eid.reshape(TK)
    flat_tok = jnp.repeat(jnp.arange(T, dtype=jnp.int32), TOP_K_INNER)
    flat_w = wts.reshape(TK)
    order = jnp.argsort(flat_e)
    se = flat_e[order]
    counts = jnp.bincount(flat_e, length=N_EXPERTS)
    padded = (counts + MOE_BLOCK - 1) // MOE_BLOCK * MOE_BLOCK
    pad_end = jnp.cumsum(padded)
    pad_start = pad_end - padded
    start = jnp.cumsum(counts) - counts
    dest = pad_start[se] + (jnp.arange(TK) - start[se])
    R = TK + N_EXPERTS * MOE_BLOCK
    row_tok = jnp.zeros((R,), jnp.int32).at[dest].set(flat_tok[order])
    row_w = jnp.zeros((R,), jnp.float32).at[dest].set(flat_w[order])
    nblk = R // MOE_BLOCK
    blk_e = jnp.minimum(jnp.searchsorted(pad_end, jnp.arange(nblk) * MOE_BLOCK, side='right'), N_EXPERTS - 1)
    xr = ht[row_tok].reshape(nblk, MOE_BLOCK, D)

    def expert_block(args):
        xb, e = args
        hid = jax.nn.silu(xb @ w1[e]) * (xb @ w3[e])
        return hid @ w2[e]

    yr = lax.map(expert_block, (xr, blk_e)).reshape(R, D)
    y = jnp.zeros((T, D), h.dtype).at[row_tok].add(yr * row_w[:, None].astype(h.dtype))
    return y.reshape(B, S, D)


def setup_inputs(seed: int = 0) -> dict:
    key = jax.random.key(seed)
    ks = jax.random.split(key, 21)
    f32 = jnp.float32
    nrm = lambda k, shape, s: jax.random.normal(k, shape, f32) * s
    D = D_MODEL
    return {
        "x": nrm(ks[0], (BATCH, SEQ, D), 1.0),
        "c": nrm(ks[1], (BATCH, D), 1.0),
        "norm1_g": 1.0 + nrm(ks[2], (DEPTH, D), 0.02),
        "norm2_g": 1.0 + nrm(ks[3], (DEPTH, D), 0.02),
        "final_g": 1.0 + nrm(ks[4], (D,), 0.02),
        "w_ada": nrm(ks[5], (DEPTH, D, 6 * D), 0.5 * D ** -0.5),
        "b_ada": nrm(ks[6], (DEPTH, 6 * D), 0.02),
        "w_in": nrm(ks[7], (DEPTH, D, IN_COLS), D ** -0.5),
        "r_lower": nrm(ks[8], (DEPTH + 1, R_WIDTH), 0.5),
        "r_norm_g": 1.0 + nrm(ks[9], (DEPTH, R_VAL_DIM), 0.02),
        "w_up_a": nrm(ks[10], (DEPTH, A_WIDTH, D), A_WIDTH ** -0.5),
        "w_up_r": nrm(ks[11], (DEPTH, R_WIDTH, D), R_WIDTH ** -0.5),
        "w_out": nrm(ks[12], (DEPTH, D, D), D ** -0.5),
        "w_rg": nrm(ks[13], (DEPTH, D, N_GROUPS), D ** -0.5),
        "b_rg": nrm(ks[14], (DEPTH, N_GROUPS), 0.01),
        "w_re": nrm(ks[15], (DEPTH, D, N_EXPERTS), D ** -0.5),
        "b_re": nrm(ks[16], (DEPTH, N_EXPERTS), 0.01),
        "w1": nrm(ks[17], (DEPTH, N_EXPERTS, D, D_EXPERT), D ** -0.5),
        "w3": nrm(ks[18], (DEPTH, N_EXPERTS, D, D_EXPERT), D ** -0.5),
        "w2": nrm(ks[19], (DEPTH, N_EXPERTS, D_EXPERT, D), D_EXPERT ** -0.5),
    }


def reference(x, c, norm1_g, norm2_g, final_g, w_ada, b_ada, w_in, r_lower, r_norm_g,
              w_up_a, w_up_r, w_out, w_rg, b_rg, w_re, b_re, w1, w3, w2):
    lb_all = jnp.cumsum(jax.nn.softmax(r_lower.astype(jnp.float32), axis=0), axis=0)
    c_act = jax.nn.silu(c)
    for l in range(DEPTH):
        mod = (c_act @ w_ada[l] + b_ada[l])[:, None, :]
        sh1, sc1, gt1, sh2, sc2, gt2 = jnp.split(mod, 6, axis=-1)
        h = rms_norm(x, norm1_g[l]) * (1.0 + sc1) + sh1
        x = x + gt1 * hybrid_mixer(h, w_in[l], lb_all[l], r_norm_g[l], w_up_a[l], w_up_r[l], w_out[l])
        h = rms_norm(x, norm2_g[l]) * (1.0 + sc2) + sh2
        x = x + gt2 * hier_moe(h, w_rg[l], b_rg[l], w_re[l], b_re[l], w1[l], w3[l], w2[l])
    return rms_norm(x, final_g)
```

```python
import contextlib
import numpy as np
import ml_dtypes
import concourse.bass as bass
import concourse.mybir as mybir
from concourse.bass_utils import run_bass_kernel_spmd

F32 = mybir.dt.float32
BF16 = mybir.dt.bfloat16
I32 = mybir.dt.int32
CAP = 512
NR = 32 * CAP
ALU = mybir.AluOpType
AF = mybir.ActivationFunctionType
AX = mybir.AxisListType

D = 2048
NT = 2048
KC = 16
EPS = 1e-6
NEG = -30000.0
NEGBIG = -1.0e30
ENGS = ("pe", "act", "dve", "pool", "sp")
SAME_ENGINE_SYNC = {"pe": False, "act": True, "dve": True, "pool": True, "sp": False}


class Buf:
    __slots__ = ("name", "last_w", "readers", "dsem", "dcount", "const", "last_dma", "excl")

    def __init__(self, name, const=False, excl=False):
        self.excl = excl
        self.name = name
        self.last_w = None
        self.readers = []
        self.dsem = None
        self.dcount = 0
        self.const = const
        self.last_dma = None


class Op:
    __slots__ = ("eng", "fn", "deps", "sig", "sem", "val", "dbuf")

    def __init__(self, eng, fn, dbuf=None):
        self.eng = eng
        self.fn = fn
        self.deps = []
        self.sig = dbuf is not None
        self.sem = None
        self.val = 0
        self.dbuf = dbuf


class Sched:
    def __init__(self, nc):
        self.nc = nc
        self.ops = {e: [] for e in ENGS}
        self.dma_bufs = []
        self.epoch = None
        self.all_ops = []

    def barrier(self, fn):
        o = Op("dve", fn, None)
        deps = {}
        for e in ENGS:
            if self.ops[e]:
                d = self.ops[e][-1]
                deps[id(d)] = d
        for b in self.dma_bufs:
            if b.last_dma is not None:
                deps[id(b.last_dma)] = b.last_dma
        o.deps = list(deps.values())
        self.ops["dve"].append(o)
        self.all_ops.append(o)
        self.epoch = o
        return o

    def op(self, eng, fn, reads=(), writes=(), dbuf=None, extra=()):
        o = Op(eng, fn, dbuf)
        deps = {}
        for d in extra:
            if d is not None:
                deps[id(d)] = d
        if self.epoch is not None:
            deps[id(self.epoch)] = self.epoch
        xr = [b for b in reads if b.excl]
        if xr:
            reads = [b for b in reads if not b.excl]
            writes = list(writes) + [b for b in xr if b not in writes]
        for b in reads:
            if b.last_w is not None:
                deps[id(b.last_w)] = b.last_w
        for b in writes:
            if b.last_w is not None:
                deps[id(b.last_w)] = b.last_w
            for r in b.readers:
                deps[id(r)] = r
        o.deps = list(deps.values())
        for b in writes:
            b.last_w = o
            b.readers = []
        for b in reads:
            if not b.const and b.last_w is not o:
                b.readers.append(o)
        if dbuf is not None:
            dbuf.last_dma = o
            if dbuf.dsem is None:
                dbuf.dsem = True
                self.dma_bufs.append(dbuf)
        self.ops[eng].append(o)
        self.all_ops.append(o)
        return o

    def emit(self, stack):
        nc = self.nc
        for e in ENGS:
            for o in self.ops[e]:
                keep = []
                for d in o.deps:
                    if d.dbuf is None and d.eng == o.eng and not SAME_ENGINE_SYNC[o.eng]:
                        continue
                    keep.append(d)
                    d.sig = True
                o.deps = keep
        esem = {}
        for e in ENGS:
            if any(o.sig and o.dbuf is None for o in self.ops[e]):
                esem[e] = stack.enter_context(nc.semaphore("s_" + e))
        for i, b in enumerate(self.dma_bufs):
            b.dsem = stack.enter_context(nc.semaphore("d%d" % i))
            b.dcount = 0
        cnt = {e: 0 for e in ENGS}
        for o in self.all_ops:
            if o.dbuf is not None:
                o.dbuf.dcount += 16
                o.sem = o.dbuf.dsem
                o.val = o.dbuf.dcount
            elif o.sig:
                cnt[o.eng] += 1
                o.sem = esem[o.eng]
                o.val = cnt[o.eng]
        block = stack.enter_context(nc.Block())
        handles = {"pe": block.tensor, "act": block.scalar, "dve": block.vector,
                   "pool": block.gpsimd, "sp": block.sync}
        for e in ENGS:
            ops = self.ops[e]
            if not ops:
                continue

            def body(eng, ops=ops):
                waited = {}
                for o in ops:
                    need = {}
                    for d in o.deps:
                        k = id(d.sem)
                        if d.val > need.get(k, (0, None))[0]:
                            need[k] = (d.val, d.sem)
                    for k, (v, sem) in need.items():
                        if waited.get(k, 0) >= v:
                            continue
                        waited[k] = v
                        eng.wait_ge(sem, v)
                    ins = o.fn(eng)
                    if o.sig:
                        ins.then_inc(o.sem, 16 if o.dbuf is not None else 1)

            handles[e](body)


class SbAlloc:
    def __init__(self, nc, base=17408, limit=229000):
        self.nc = nc
        self.top = base
        self.limit = limit
        self.n = 0
        self.marks = []

    def push(self):
        self.marks.append(self.top)

    def pop(self):
        self.top = self.marks.pop()

    def alloc(self, name, shape, dtype):
        sz = int(np.prod(shape[1:])) * (4 if dtype in (F32, I32) else 2)
        off = (self.top + 63) // 64 * 64
        assert off + sz <= self.limit, (name, off, sz, self.limit)
        self.top = off + sz
        self.n += 1
        return self.nc.alloc_sbuf_tensor_at("%s_%d" % (name, self.n), list(shape), dtype, offset=off)


class K:
    def __init__(self, nc, debug=False, stages="012345"):
        self.nc = nc
        self.S = Sched(nc)
        self.sb = SbAlloc(nc)
        self.debug = debug
        self.stages = stages
        self.dpool = {}
        self.bkind = {}
        self.dram = {}
        self.sbufs = {}
        self.scr_w = {}

    def din(self, name, shape, dt=F32):
        return self.nc.dram_tensor(name, list(shape), dt, kind="ExternalInput").ap()

    def dscr(self, name, shape, dt):
        kind = "ExternalOutput" if self.debug else "Internal"
        t = self.nc.dram_tensor(name, list(shape), dt, kind=kind).ap()
        self.dram[name] = t
        return t

    def dbuf(self, name, kind="sp"):
        pool = self.dpool.setdefault(kind, [])
        if pool:
            return pool.pop()
        b = Buf(name)
        self.bkind[id(b)] = kind
        return b

    def release(self, bufs):
        for b in bufs:
            self.dpool[self.bkind[id(b)]].append(b)

    def dma(self, q, out, in_, reads, writes, dbuf, **kw):
        k = self.bkind.setdefault(id(dbuf), q)
        assert k == q, (dbuf.name, k, q)
        return self.S.op(q, lambda e: e.dma_start(out=out, in_=in_, **kw), reads, writes, dbuf=dbuf)

    def mm(self, out, lhsT, rhs, start, stop, reads, writes, tp=None):
        if tp is not None:
            return self.S.op("pe", lambda e: e.matmul(out, lhsT=lhsT, rhs=rhs, start=start, stop=stop, tile_position=tp), reads, writes)
        return self.S.op("pe", lambda e: e.matmul(out, lhsT=lhsT, rhs=rhs, start=start, stop=stop), reads, writes)

    def tr(self, out, in_, reads, writes):
        idn = self.identb
        n = in_.shape[0]
        return self.S.op("pe", lambda e: e.transpose(out=out, in_=in_, identity=idn[0:n, 0:n]),
                         list(reads) + [self.B_c0], writes)

    def act(self, out, in_, func, reads, writes, scale=1.0, bias=None, accum=None):
        kw = {}
        if bias is not None:
            kw["bias"] = bias
        if accum is not None:
            kw["accum_out"] = accum
        return self.S.op("act", lambda e: e.activation(out=out, in_=in_, func=func, scale=scale, **kw), reads, writes)

    def ts(self, out, in0, s1, s2, op0, op1, reads, writes, eng="dve"):
        if s2 is None:
            return self.S.op(eng, lambda e: e.tensor_scalar(out=out, in0=in0, scalar1=s1, scalar2=None, op0=op0), reads, writes)
        return self.S.op(eng, lambda e: e.tensor_scalar(out=out, in0=in0, scalar1=s1, scalar2=s2, op0=op0, op1=op1), reads, writes)

    def tt(self, out, in0, in1, op, reads, writes, eng="dve"):
        return self.S.op(eng, lambda e: e.tensor_tensor(out=out, in0=in0, in1=in1, op=op), reads, writes)

    def stt(self, out, in0, scalar, in1, op0, op1, reads, writes, eng="dve"):
        return self.S.op(eng, lambda e: e.scalar_tensor_tensor(out=out, in0=in0, scalar=scalar, in1=in1, op0=op0, op1=op1), reads, writes)

    def cp(self, eng, out, in_, reads, writes):
        if eng == "act":
            return self.S.op("act", lambda e: e.copy(out=out, in_=in_), reads, writes)
        return self.S.op(eng, lambda e: e.tensor_copy(out=out, in_=in_), reads, writes)

    def rstd(self, out, ssq, n, reads, writes, tmp):
        eps = self.epsc
        self.act(tmp, ssq, AF.Sqrt, list(reads) + [self.B_c0], [writes[1]], scale=1.0 / n, bias=eps[:, 0:1])
        self.S.op("dve", lambda e: e.reciprocal(out=out, in_=tmp), [writes[1]], [writes[0]])

    def build(self):
        nc = self.nc
        sb = self.sb
        S = self.S
        I = {}
        I["xm"] = self.din("xm", [NT, D])
        I["xp"] = self.din("xp", [NT, D])
        I["c"] = self.din("c", [D])
        I["pm"] = self.din("pm", [128, 1])
        I["norm1_g"] = self.din("norm1_g", [D])
        I["norm2_g"] = self.din("norm2_g", [D])
        I["final_g"] = self.din("final_g", [D])
        I["w_ada"] = self.din("w_ada", [D, 6 * D])
        I["b_ada"] = self.din("b_ada", [6 * D])
        I["w_in"] = self.din("w_in", [D, 11264])
        I["r_lower"] = self.din("r_lower", [2, 1024])
        I["r_norm_g"] = self.din("r_norm_g", [128])
        I["w_up_a"] = self.din("w_up_a", [1024, D])
        I["w_up_r"] = self.din("w_up_r", [1024, D])
        I["w_out"] = self.din("w_out", [D, D])
        I["w_rg"] = self.din("w_rg", [D, 4])
        I["b_rg"] = self.din("b_rg", [4])
        I["w_re"] = self.din("w_re", [D, 32])
        I["b_re"] = self.din("b_re", [32])
        I["w1"] = self.din("w1", [32, D, 512])
        I["w3"] = self.din("w3", [32, D, 512])
        I["w2"] = self.din("w2", [32, 512, D])
        I["k_ident"] = self.din("k_ident", [128, 128])
        I["k_cm"] = self.din("k_cm", [128, 512])
        I["k_cpen"] = self.din("k_cpen", [128, 256])
        I["k_sel"] = self.din("k_sel", [16, 2048])
        I["k_wall"] = self.din("k_wall", [128, 136])
        I["k_wl"] = self.din("k_wl", [128, 128])
        I["k_bd"] = self.din("k_bd", [128, 128])
        I["k_slt"] = self.din("k_slt", [128, 128])
        I["k_ones"] = self.din("k_ones", [128, 128])
        I["k_ebase"] = self.din("k_ebase", [128, 32])
        self.I = I
        self.out = nc.dram_tensor("out", [NT, D], F32, kind="ExternalOutput").ap()
        self.dscr("MOD", [6 * D], F32)
        self.dscr("QT", [8, 128, NT], BF16)
        self.dscr("KT", [8, 128, 2 * NT], BF16)
        self.dscr("V", [2 * NT, 1024], BF16)
        self.dscr("RQT", [8, 128, NT], BF16)
        self.dscr("RF", [2 * NT, 1024], F32)
        self.dscr("RI", [2 * NT, 1024], BF16)
        self.dscr("ROG", [NT, 1024], BF16)
        self.dscr("GAT", [16, 128, NT], BF16)
        self.dscr("GRT", [16, 128, NT], BF16)
        self.dscr("MT", [16, 128, NT], BF16)
        self.dscr("X1", [NT, D], F32)
        self.dscr("H2T", [16, 128, NT], BF16)
        self.dscr("XD", [NR, D], BF16)
        self.dscr("YD0", [NR, 1024], F32)
        self.dscr("YD1", [NR, 1024], F32)
        if self.debug:
            self.dscr("ATT", [8, 128, NT], BF16)
            self.dscr("OT", [8, 128, NT], BF16)
            self.dscr("WF", [128, 16 * 32], F32)
        self.scrB = {}

        with contextlib.ExitStack() as st:
            self.F = [st.enter_context(nc.psum_tensor("F%d" % i, [128, 512], F32)) for i in range(6)]
            self.T = [st.enter_context(nc.psum_tensor("T%d" % i, [128, 1024], BF16)) for i in range(2)]
            self.BF = [Buf("F%d" % i, excl=True) for i in range(6)]
            self.BFs = [[self.BF[i]] * 4 for i in range(6)]
            _bt = [Buf("T%d" % i, excl=True) for i in range(2)]
            self.BT = [[_bt[i]] * 8 for i in range(2)]
            self.dummy = sb.alloc("dummy", [128, 8], F32)
            self.stage0()
            self.bar()
            if "1" in self.stages:
                self.stage1()
                self.bar()
            sb.push()
            self.attT = sb.alloc("attT", [128, 8, NT], BF16)
            self.oT = sb.alloc("oT", [128, 8, NT], BF16)
            self.B_attT = [Buf("attT%d" % h) for h in range(8)]
            self.B_oT = [Buf("oT%d" % t) for t in range(16)]
            if "2" in self.stages:
                self.stage2()
                self.bar()
            if "3" in self.stages:
                self.stage3()
                self.bar()
            if "4" in self.stages:
                self.stage4a()
                self.bar()
            sb.pop()
            if "4" in self.stages:
                self.stage4b()
                self.bar()
            if "5" in self.stages:
                self.stage5()
            allb = [b for b in self.S.dma_bufs]
            S.op("sp", lambda e: e.nop(), [], allb)
            S.emit(st)
        return nc

    def bar(self):
        d = self.dummy
        self.S.barrier(lambda e: e.memset(d[:], 0.0))

    def sB(self, name, key):
        k = (name, key)
        if k not in self.scrB:
            self.scrB[k] = Buf("%s%s" % (name, key))
        return self.scrB[k]

    def stage0(self):
        S, sb, I = self.S, self.sb, self.I
        cbs = {"sp": [Buf("c%d" % i) for i in range(4)], "pool": [Buf("cp%d" % i) for i in range(2)]}
        cb = cbs["sp"] + cbs["pool"]
        self.B_c0 = Buf("constall")
        ci = [0]

        def cload(q, dst, src, **kw):
            lst = cbs[q]
            b = lst[ci[0] % len(lst)]
            ci[0] += 1
            self.dma(q, dst, src, [], [b], b, **kw)

        def A(name, shape, dt=F32):
            t = sb.alloc(name, shape, dt)
            return t

        self.identb = A("identb", [128, 128], BF16)
        self.ident32 = A("ident32", [128, 128])
        self.rows = A("rows", [48, 128])
        self.cm = A("cm", [128, 512], BF16)
        self.cpen = A("cpen", [128, 256])
        self.selc = A("selc", [16, 2048], BF16)
        self.wall = A("wall", [128, 136])
        self.wl = A("wl", [128, 128])
        self.bd = A("bd", [128, 128])
        self.epsc = A("epsc", [128, 1])
        self.pm = A("pm", [128, 1])
        self.ppen = A("ppen", [128, 1])
        self.g1c = A("g1c", [128, 16])
        self.g2c = A("g2c", [128, 16])
        self.ccol = A("ccol", [128, 16])
        self.cact = A("cact", [128, 16], BF16)
        self.modc = A("modc", [128, 6, 16])
        self.a1 = A("a1", [128, 16])
        self.a2 = A("a2", [128, 16])
        self.gt1b = A("gt1b", [128, D])
        self.gt2b = A("gt2b", [128, D])
        self.fgb = A("fgb", [128, D])
        self.lb = A("lb", [128, 1024])
        self.oml = A("oml", [128, 1024])
        self.rngb = A("rngb", [128, 128])
        self.wr = A("wr", [128, 16, 36], BF16)
        self.br = A("br", [128, 36])
        self.wfull = A("wfull", [128, 16, 32])
        self.slt = A("slt", [128, 128], BF16)
        self.ones = A("ones", [128, 128], BF16)
        self.ebase = A("ebase", [128, 32])
        self.tot = A("tot", [128, 32])
        self.desti = A("desti", [128, 16, 2], I32)
        self.wk = A("wk", [128, 16, 2])
        self.B_tot = Buf("tot")
        self.B_ind = Buf("ind")
        self.six = [A("six", [128, 2], I32) for _ in range(2)]
        self.gix = [A("gix", [128, 2], I32) for _ in range(2)]
        self.B_six = [[Buf("six%d_%d" % (i, k)) for k in range(2)] for i in range(2)]
        self.B_gix = [[Buf("gix%d_%d" % (i, k)) for k in range(2)] for i in range(2)]
        self.B_dest = [Buf("dest%d" % t) for t in range(16)]
        self.B_wfull = [Buf("wfull%d" % t) for t in range(16)]
        cload("pool", self.identb[:], I["k_ident"])
        cload("sp", self.ident32[:], I["k_ident"])
        cload("pool", self.cm[:], I["k_cm"])
        cload("sp", self.cpen[:], I["k_cpen"])
        cload("pool", self.selc[:], I["k_sel"])
        cload("sp", self.wall[:], I["k_wall"])
        cload("sp", self.wl[:], I["k_wl"])
        cload("sp", self.bd[:], I["k_bd"])
        cload("sp", self.pm[:], I["pm"])
        cload("sp", self.rows[0:16, :], I["norm1_g"].rearrange("(k p) -> k p", p=128))
        cload("sp", self.rows[16:32, :], I["norm2_g"].rearrange("(k p) -> k p", p=128))
        cload("sp", self.rows[32:48, :], I["c"].rearrange("(k p) -> k p", p=128))
        cload("sp", self.fgb[:], I["final_g"].partition_broadcast(128))
        cload("sp", self.lb[:], I["r_lower"][0, :].partition_broadcast(128))
        cload("sp", self.oml[:], I["r_lower"][1, :].partition_broadcast(128))
        cload("sp", self.rngb[:], I["r_norm_g"].partition_broadcast(128))
        cload("pool", self.wr[:, :, 0:4], I["w_rg"].rearrange("(k p) n -> p k n", p=128))
        cload("pool", self.wr[:, :, 4:36], I["w_re"].rearrange("(k p) n -> p k n", p=128))
        cload("pool", self.slt[:], I["k_slt"])
        cload("pool", self.ones[:], I["k_ones"])
        cload("sp", self.ebase[:], I["k_ebase"])
        cload("sp", self.br[:, 0:4], I["b_rg"].partition_broadcast(128))
        cload("sp", self.br[:, 4:36], I["b_re"].partition_broadcast(128))
        cs = Buf("cs")
        S.op("dve", lambda e: e.memset(self.epsc[:], EPS), [], [cs])
        S.op("pe", lambda e: e.transpose(out=self.F[2][:, 0:48], in_=self.rows[:, :], identity=self.ident32[0:48, 0:48]), cb, [self.BF[2]])
        self.cp("dve", self.g1c[:], self.F[2][:, 0:16], [self.BF[2]], [cs])
        self.cp("dve", self.g2c[:], self.F[2][:, 16:32], [self.BF[2]], [cs])
        self.cp("dve", self.ccol[:], self.F[2][:, 32:48], [self.BF[2]], [cs])
        S.op("dve", lambda e: e.memset(self.tot[:], 0.0), [], [self.B_tot])
        self.ts(self.ppen[:], self.pm[:], -1.0, -NEGBIG, ALU.add, ALU.mult, cb, [cs])
        self.tt(self.lb[:], self.lb[:], self.oml[:], ALU.subtract, cb, [cs])
        self.act(self.lb[:], self.lb[:], AF.Sigmoid, [cs], [cs])
        self.ts(self.oml[:], self.lb[:], -1.0, 1.0, ALU.mult, ALU.add, [cs], [cs])
        self.act(self.cact[:], self.ccol[:], AF.Silu, [cs], [cs])
        sb.push()
        MOD = self.dram["MOD"]
        wsl = [sb.alloc("wsl0", [128, KC, 512], BF16) for _ in range(3)]
        Bw = [self.dbuf("wsl", "pool") for _ in range(3)]
        bsl = [sb.alloc("bsl", [1, 512], F32) for _ in range(2)]
        Bb = [self.dbuf("bsl") for _ in range(2)]
        msl = [sb.alloc("msl", [1, 512], F32) for _ in range(2)]
        Bm = [self.dbuf("msl") for _ in range(2)]
        wsrc = I["w_ada"].rearrange("(k p) n -> p k n", p=128)
        Bmod = Buf("MOD")
        for j in range(24):
            w = wsl[j % 3]
            bw = Bw[j % 3]
            self.dma("pool", w[:], wsrc[:, :, j * 512:(j + 1) * 512], [], [bw], bw)
            bb = Bb[j % 2]
            self.dma("sp", bsl[j % 2][:], I["b_ada"][j * 512:(j + 1) * 512].rearrange("(o n) -> o n", o=1), [], [bb], bb)
            pf = self.F[j % 2]
            bpf = self.BF[j % 2]
            for kc in range(KC):
                self.mm(pf[0:1, :], self.cact[:, kc:kc + 1], w[:, kc, :], kc == 0, kc == KC - 1, [bw, cs], [bpf])
            bm = Bm[j % 2]
            self.tt(msl[j % 2][:], pf[0:1, :], bsl[j % 2][:], ALU.add, [bpf, bb], [bm])
            self.dma("sp", MOD[j * 512:(j + 1) * 512].rearrange("(o n) -> o n", o=1), msl[j % 2][:], [bm], [Bmod], bm)
        sb.pop()
        self.release(Bw + Bb + Bm)
        bmc = self.dbuf("modc")
        modr = sb.alloc("modr", [96, 128], F32)
        self.dma("sp", modr[:], MOD.rearrange("(r p) -> r p", p=128), [Bmod], [bmc], bmc)
        S.op("pe", lambda e: e.transpose(out=self.F[3][:, 0:96], in_=modr[:, :], identity=self.ident32[0:96, 0:96]), [bmc] + cb, [self.BF[3]])
        self.cp("dve", self.modc[:].rearrange("p s k -> p (s k)"), self.F[3][:, 0:96], [self.BF[3]], [cs])
        bg1 = self.dbuf("gt1b")
        self.dma("sp", self.gt1b[:], MOD[2 * D:3 * D].partition_broadcast(128), [Bmod], [bg1], bg1)
        bg2 = self.dbuf("gt2b")
        self.dma("sp", self.gt2b[:], MOD[5 * D:6 * D].partition_broadcast(128), [Bmod], [bg2], bg2)
        self.stt(self.a1[:], self.modc[:, 1, :], 1.0, self.g1c[:], ALU.add, ALU.mult, [cs], [cs])
        self.stt(self.a2[:], self.modc[:, 4, :], 1.0, self.g2c[:], ALU.add, ALU.mult, [cs], [cs])
        S.op("dve", lambda e: e.memset(self.epsc[:], EPS), cb + [cs, bmc, bg1, bg2], [self.B_c0])
        self.B_c0.const = True
        self.B_modc = bmc

    def norm_to_hT(self, xt, bx, hT_dst, bh, a_col, sh_col, ring):
        junk, ssq, tmp, rs, xn, bj, bs, bxn = ring
        import os
        lvl = int(os.environ.get("N1", "9"))
        self.act(junk[:], xt, AF.Square, [bx], [bj, bs], accum=ssq[:, 0:1])
        if lvl < 2:
            return
        self.rstd(rs[:, 0:1], ssq[:, 0:1], float(D), [bs], [bs, bj], tmp[:, 0:1])
        if lvl < 3:
            return
        self.ts(xn[:], xt, rs[:, 0:1], None, ALU.mult, None, [bx, bs], [bxn])
        if lvl < 4:
            return
        for kc in range(KC):
            tb, ts_ = kc // 8, kc % 8
            pt = self.T[tb][:, ts_ * 128:(ts_ + 1) * 128]
            bt = self.BT[tb][ts_]
            self.tr(pt, xn[:, kc * 128:(kc + 1) * 128], [bxn], [bt])
            if os.environ.get("NOEV"):
                continue
            evm = os.environ.get("EVM", "")
            if evm == "f":
                self.act(hT_dst[:, kc, :], pt, AF.Identity, [bt, self.B_c0], [bh], scale=2.0)
            elif evm == "s":
                self.act(hT_dst[:, kc, :], pt, AF.Identity, [bt, self.B_c0], [bh], scale=a_col[:, kc:kc + 1])
            elif evm == "j":
                self.cp("dve", junk[:, 0:128], pt, [bt, self.B_c0], [bh])
            elif evm == "d":
                self.cp("dve", hT_dst[:, kc, :], pt, [bt, self.B_c0], [bh])
            elif evm == "c":
                self.act(hT_dst[:, kc, :], pt, AF.Copy, [bt, self.B_c0], [bh])
            elif tb == 0 or lvl < 5:
                self.act(hT_dst[:, kc, :], pt, AF.Identity, [bt, self.B_c0], [bh], scale=a_col[:, kc:kc + 1], bias=sh_col[:, kc:kc + 1])
            else:
                self.ts(hT_dst[:, kc, :], pt, a_col[:, kc:kc + 1], sh_col[:, kc:kc + 1], ALU.mult, ALU.add, [bt, self.B_c0], [bh])

    def norm_ring(self, sb):
        junk = sb.alloc("junk", [128, D], BF16)
        ssq = sb.alloc("ssq", [128, 1], F32)
        tmp = sb.alloc("tmp", [128, 1], F32)
        rs = sb.alloc("rs", [128, 1], F32)
        xn = sb.alloc("xn", [128, D], BF16)
        return (junk, ssq, tmp, rs, xn, Buf("junk"), Buf("ssq"), Buf("xn"))

    def stage1(self):
        S, sb, I = self.S, self.sb, self.I
        sb.push()
        G = 1024
        hT = [sb.alloc("hT", [128, KC, G], BF16) for _ in range(2)]
        BhT = [[Buf("hT%d_%d" % (s, t)) for t in range(8)] for s in range(2)]
        wsl = [sb.alloc("wsl", [128, KC, 512], BF16) for _ in range(3)]
        Bw = [self.dbuf("wsl", "pool") for _ in range(3)]
        xs = [sb.alloc("xs", [128, D], F32) for _ in range(2)]
        Bx = [self.dbuf("xs") for _ in range(2)]
        rings = [self.norm_ring(sb) for _ in range(2)]
        stg = []
        stgb = []
        for _ in range(4):
            sb.push()
            stgb.append(sb.alloc("stgb", [128, 512], BF16))
            sb.pop()
            stg.append(sb.alloc("stg", [128, 512], F32))
        Bs = [self.dbuf("stg") for _ in range(4)]
        wsrc = I["w_in"].rearrange("(k p) n -> p k n", p=128)
        D_ = self.dram
        zt = sb.alloc("zt", [128, D], BF16)
        Bz = self.dbuf("zt")
        S.op("dve", lambda e: e.memset(zt[:], 0.0), [], [Bz])
        for i in range(NR // 128):
            self.dma("sp", D_["XD"][i * 128:(i + 1) * 128, :], zt[:], [Bz], [Buf("z")], Bz)
        groups = [("p", 0), ("p", 1), ("m", 0), ("m", 1)]
        pblocks = [2, 3, 4, 5, 8, 9, 10, 11]
        import os
        if os.environ.get("S1G"):
            groups = groups[:int(os.environ["S1G"])]
        if os.environ.get("S1B"):
            pblocks = pblocks[:int(os.environ["S1B"])]
        wi = [0]
        si = [0]
        xi = [0]
        for gi, (kind, gidx) in enumerate(groups):
            slot = gi % 2
            src = I["xp"] if kind == "p" else I["xm"]
            ltok0 = gidx * G + (0 if kind == "p" else NT)
            mtok0 = gidx * G
            for t in range(int(os.environ.get("S1T", "8"))):
                xsl = xs[xi[0] % 2]
                bx = Bx[xi[0] % 2]
                ring = rings[xi[0] % 2]
                xi[0] += 1
                r0 = gidx * G + t * 128
                self.dma("sp", xsl[:], src[r0:r0 + 128, :], [], [bx], bx)
                self.norm_to_hT(xsl[:], bx, hT[slot][:, :, t * 128:(t + 1) * 128], BhT[slot][t], self.a1, self.modc[:, 0, :], ring)
            blocks = pblocks if kind == "p" else list(range(22))
            for blk in blocks:
                w = wsl[wi[0] % 3]
                bw = Bw[wi[0] % 3]
                wi[0] += 1
                self.dma("pool", w[:], wsrc[:, :, blk * 512:(blk + 1) * 512], [], [bw], bw)
                fm = blk in (0, 1, 2, 3, 6, 7) or blk >= 14
                for u in range(8):
                    fb = u % 4
                    pf = self.F[fb]
                    bpf = self.BF[fb]
                    if fm:
                        cs_, th = u // 2, u % 2
                        for kc in range(KC):
                            self.mm(pf[:, :], w[:, kc, cs_ * 128:(cs_ + 1) * 128], hT[slot][:, kc, th * 512:(th + 1) * 512],
                                    kc == 0, kc == KC - 1, [bw] + BhT[slot][th * 4:(th + 1) * 4], [bpf])
                    else:
                        for kc in range(KC):
                            self.mm(pf[:, :], hT[slot][:, kc, u * 128:(u + 1) * 128], w[:, kc, :],
                                    kc == 0, kc == KC - 1, [bw, BhT[slot][u]], [bpf])
                    sg = stg[si[0] % 4]
                    sgb = stgb[si[0] % 4]
                    bs = Bs[si[0] % 4]
                    si[0] += 1
                    if fm:
                        if blk < 2:
                            dst = D_["QT"][blk * 4 + cs_, :, mtok0 + th * 512: mtok0 + (th + 1) * 512]
                            key = ("QT", blk * 4 + cs_)
                            fn = AF.Copy
                        elif blk < 4:
                            dst = D_["KT"][(blk - 2) * 4 + cs_, :, ltok0 + th * 512: ltok0 + (th + 1) * 512]
                            key = ("KT", (blk - 2) * 4 + cs_)
                            fn = AF.Copy
                        elif blk < 8:
                            dst = D_["RQT"][(blk - 6) * 4 + cs_, :, mtok0 + th * 512: mtok0 + (th + 1) * 512]
                            key = ("RQT", (mtok0 + th * 512) // 128)
                            fn = AF.Copy
                        elif blk < 18:
                            dst = D_["GAT"][(blk - 14) * 4 + cs_, :, mtok0 + th * 512: mtok0 + (th + 1) * 512]
                            key = ("GAT", (blk - 14) * 4 + cs_)
                            fn = AF.Sigmoid
                        else:
                            dst = D_["GRT"][(blk - 18) * 4 + cs_, :, mtok0 + th * 512: mtok0 + (th + 1) * 512]
                            key = ("GRT", (blk - 18) * 4 + cs_)
                            fn = AF.Sigmoid
                        odt = BF16
                    else:
                        if blk < 6:
                            dst = D_["V"][ltok0 + u * 128: ltok0 + (u + 1) * 128, (blk - 4) * 512:(blk - 3) * 512]
                            key = ("V", (blk - 4) * 4)
                            fn, odt = AF.Copy, BF16
                        elif blk < 10:
                            dst = D_["RF"][ltok0 + u * 128: ltok0 + (u + 1) * 128, (blk - 8) * 512:(blk - 7) * 512]
                            key = ("RF", (ltok0 + u * 128) // 128)
                            fn, odt = AF.Copy, F32
                        elif blk < 12:
                            dst = D_["RI"][ltok0 + u * 128: ltok0 + (u + 1) * 128, (blk - 10) * 512:(blk - 9) * 512]
                            key = ("RI", (ltok0 + u * 128) // 128)
                            fn, odt = AF.Copy, BF16
                        else:
                            dst = D_["ROG"][mtok0 + u * 128: mtok0 + (u + 1) * 128, (blk - 12) * 512:(blk - 11) * 512]
                            key = ("ROG", (mtok0 + u * 128) // 128)
                            fn, odt = AF.Silu, BF16
                    so = sg[:] if odt == F32 else sgb[:]
                    if fn == AF.Copy and (u % 2 == 1):
                        self.cp("dve", so, pf[:, :], [bpf], [bs])
                    else:
                        self.act(so, pf[:, :], fn, [bpf], [bs])
                    wl_ = self.scr_w.setdefault(key, [])
                    tok = Buf("w")
                    wl_.append(tok)
                    self.dma("sp", dst, so, [bs], [tok], bs)
        sb.pop()
        self.release(Bw + Bx + Bs + [Bz])

    def scr_reads(self, name, keys):
        out = []
        for k in keys:
            out.extend(self.scr_w.get((name, k), []))
        return out

    def stage2(self):
        S, sb = self.S, self.sb
        D_ = self.dram
        sb.push()
        QTh = [sb.alloc("QTh", [128, NT], BF16) for _ in range(2)]
        KTh = [sb.alloc("KTh", [128, 2 * NT], BF16) for _ in range(2)]
        Vh = [sb.alloc("Vh", [128, 32, 129], BF16) for _ in range(2)]
        Bq = [self.dbuf("q") for _ in range(2)]
        Bk = [self.dbuf("k") for _ in range(2)]
        Bv = [self.dbuf("v") for _ in range(2)]
        km32 = sb.alloc("km32", [128, 16], F32)
        kmb = sb.alloc("kmb", [128, 16], BF16)
        gm = sb.alloc("gm", [128, 16, 16], F32)
        top8 = sb.alloc("top8", [128, 16, 8], F32)
        pen = sb.alloc("pen", [128, 16, 16], F32)
        penb = sb.alloc("penb", [128, 256], BF16)
        penT = sb.alloc("penT", [16, NT], BF16)
        Bkm, Bgm, Bpen, BpenT = Buf("km"), Buf("gm"), Buf("pen"), Buf("penT")
        pT = [sb.alloc("pT", [128, 256], BF16) for _ in range(4)]
        BpT = [Buf("pT%d" % i) for i in range(4)]
        rsum = [sb.alloc("rsum", [128, 1], F32) for _ in range(2)]
        atts = [sb.alloc("atts", [128, 128], BF16) for _ in range(2)]
        Brs = [Buf("rsum%d" % i) for i in range(2)]
        Bat = [Buf("atts%d" % i) for i in range(2)]
        scale = 1.0 / np.sqrt(128.0)
        pi = [0]
        ei = [0]
        for sl in range(2):
            S.op("dve", lambda e, t=Vh[sl]: e.memset(t[:, :, 128:129], 1.0), [], [Bv[sl]])
        for h in range(8):
            sl = h % 2
            q, k, v = QTh[sl], KTh[sl], Vh[sl]
            bq, bk, bv = Bq[sl], Bk[sl], Bv[sl]
            self.dma("sp", q[:], D_["QT"][h], self.scr_reads("QT", [h]), [bq], bq)
            self.dma("sp", k[:], D_["KT"][h], self.scr_reads("KT", [h]), [bk], bk)
            self.dma("sp", v[:, :, 0:128], D_["V"].rearrange("(t p) c -> p t c", p=128)[:, :, h * 128:(h + 1) * 128],
                     self.scr_reads("V", [(h // 4) * 4]), [bv], bv)
            S.op("dve", lambda e, k=k: e.tensor_reduce(out=km32[:], in_=k[:].rearrange("p (b l) -> p b l", l=256), axis=AX.X, op=ALU.add),
                 [bk], [Bkm])
            self.ts(kmb[:], km32[:], 1.0 / 256, None, ALU.mult, None, [Bkm], [Bkm])
            pg = self.F[5]
            bpg = self.BF[5]
            for qt in range(16):
                self.mm(pg[:, qt * 16:(qt + 1) * 16], q[:, qt * 128:(qt + 1) * 128], kmb[:, :], True, True, [bq, Bkm], [bpg])
            gmf = gm[:].rearrange("p a b -> p (a b)")
            self.tt(gmf, pg[:, 0:256], self.cpen[:], ALU.add, [bpg, self.B_c0], [Bgm])
            self.ts(gm[:, :, 0:8], gm[:, :, 0:8], self.ppen[:, 0:1], None, ALU.add, None, [Bgm, self.B_c0], [Bgm])
            for qt in range(16):
                S.op("dve", lambda e, qt=qt: e.max(out=top8[:, qt, :], in_=gm[:, qt, :]), [Bgm], [Bpen])
            for qt in range(16):
                self.ts(pen[:, qt, :], gm[:, qt, :], top8[:, qt, 2:3], None, ALU.is_ge, None, [Bgm, Bpen], [Bpen])
            penf = pen[:].rearrange("p a b -> p (a b)")
            self.ts(penf, penf, -1.0, -NEG, ALU.add, ALU.mult, [Bpen], [Bpen])
            self.ts(pen[:, :, 0:8], pen[:, :, 0:8], self.ppen[:, 0:1], None, ALU.add, None, [Bpen, self.B_c0], [Bpen])
            self.cp("dve", penb[:], penf, [Bpen], [Bpen])
            for qt in range(16):
                tb, ts_ = qt // 8, qt % 8
                pt = self.T[tb][0:16, ts_ * 128:(ts_ + 1) * 128]
                bt = self.BT[tb][ts_]
                self.tr(pt, penb[:, qt * 16:(qt + 1) * 16], [Bpen], [bt])
                self.cp("act", penT[:, qt * 128:(qt + 1) * 128], pt, [bt], [BpenT])
            for qb in range(8):
                Bl = 8 + qb
                nkt = 2 * Bl + 2
                po = [self.F[3], self.F[4]]
                bpo = [self.BF[3], self.BF[4]]
                LAG = 2
                pend = []
                for kk in range(nkt + LAG):
                    if kk < nkt:
                        kt = kk
                        fb = kt % 3
                        ps = self.F[fb]
                        bps = self.BF[fb]
                        self.mm(ps[:, 0:256], k[:, kt * 128:(kt + 1) * 128], q[:, qb * 256:(qb + 1) * 256], True, False, [bk, bq], [bps])
                        if kt < 2 * Bl:
                            i = kt // 2
                            self.mm(ps[:, 0:256], self.selc[0:16, i * 128:(i + 1) * 128], penT[0:16, qb * 256:(qb + 1) * 256],
                                    False, True, [BpenT, self.B_c0], [bps])
                        else:
                            j = kt - 2 * Bl
                            self.mm(ps[:, 0:256], self.identb[:, :], self.cm[:, j * 256:(j + 1) * 256], False, True, [self.B_c0], [bps])
                        p = pT[pi[0] % 4]
                        bp = BpT[pi[0] % 4]
                        pi[0] += 1
                        self.act(p[:], ps[:, 0:256], AF.Exp, [bps], [bp], scale=scale)
                        pend.append((kt, p, bp))
                    if kk >= LAG:
                        kt, p, bp = pend.pop(0)
                        for qh in range(2):
                            self.mm(po[qh][:, 0:129], p[:, qh * 128:(qh + 1) * 128], v[:, kt, :], kt == 0, kt == nkt - 1, [bp, bv], [bpo[qh]])
                for qh in range(2):
                    e_ = ei[0] % 2
                    ei[0] += 1
                    S.op("dve", lambda e, o=rsum[e_], i=po[qh]: e.reciprocal(out=o[:, 0:1], in_=i[:, 128:129]), [bpo[qh]], [Brs[e_]])
                    self.act(atts[e_][:], po[qh][:, 0:128], AF.Copy, [bpo[qh], Brs[e_]], [Bat[e_]], scale=rsum[e_][:, 0:1])
                    qt = qb * 2 + qh
                    tb, ts_ = qt // 8, qt % 8
                    pt = self.T[tb][:, ts_ * 128:(ts_ + 1) * 128]
                    bt = self.BT[tb][ts_]
                    self.tr(pt, atts[e_][:], [Bat[e_]], [bt])
                    self.cp("dve", self.attT[:, h, qt * 128:(qt + 1) * 128], pt, [bt], [self.B_attT[h]])
            if self.debug:
                self.dma("sp", D_["ATT"][h], self.attT[:, h, :], [self.B_attT[h]], [Buf("x")], self.B_attT[h])
        sb.pop()
        self.release(Bq + Bk + Bv)

    def stage3(self):
        S, sb = self.S, self.sb
        D_ = self.dram
        sb.push()
        rf = [sb.alloc("rf", [128, 1024], F32) for _ in range(2)]
        ri = [sb.alloc("ri", [128, 1024], BF16) for _ in range(2)]
        rq = [sb.alloc("rq", [128, 8, 128], BF16) for _ in range(2)]
        rog = [sb.alloc("rog", [128, 1024], BF16) for _ in range(2)]
        Brf = [self.dbuf("rf") for _ in range(2)]
        Bri = [self.dbuf("ri") for _ in range(2)]
        Brq = [self.dbuf("rq") for _ in range(2)]
        Brog = [self.dbuf("rog") for _ in range(2)]
        logf = [sb.alloc("logf", [128, 1024], F32) for _ in range(2)]
        kin = [sb.alloc("kin", [128, 1024], F32) for _ in range(2)]
        Blf = [Buf("logf%d" % i) for i in range(2)]
        Bkin = [Buf("kin%d" % i) for i in range(2)]
        R = 3
        ebl = [sb.alloc("ebl", [128, 128], F32) for _ in range(R)]
        khat = [sb.alloc("khat", [128, 128], BF16) for _ in range(R)]
        eb8 = [sb.alloc("eb8", [128, 8], F32) for _ in range(R)]
        ebm = [sb.alloc("ebm", [128, 128], F32) for _ in range(R)]
        qtl = [sb.alloc("qtl", [128, 128], BF16) for _ in range(R)]
        enb = [sb.alloc("enb", [128, 128], F32) for _ in range(R)]
        ktl = [sb.alloc("ktl", [128, 128], BF16) for _ in range(R)]
        ktlT = [sb.alloc("ktlT", [128, 128], BF16) for _ in range(R)]
        khc = [sb.alloc("khc", [128, 4, 128], BF16) for _ in range(R)]
        qtlz = [sb.alloc("qtlz", [128, 4, 128], BF16) for _ in range(R)]
        Bkhc = [Buf("khc%d" % i) for i in range(R)]
        Bqz = [Buf("qtlz%d" % i) for i in range(R)]
        for i in range(R):
            S.op("dve", lambda e, t=qtlz[i]: e.memset(t[:], 0.0), [], [Bqz[i]])
        at = [sb.alloc("at", [128, 128], BF16) for _ in range(R)]
        Sb = [sb.alloc("Sb", [128, 4, 128], BF16) for _ in range(R)]
        ot = [sb.alloc("ot", [128, 128], F32) for _ in range(R)]
        of_ = [sb.alloc("of", [128, 128], BF16) for _ in range(R)]
        sq = [sb.alloc("sq", [128, 4], F32) for _ in range(R)]
        jk = [sb.alloc("jk", [128, 128], BF16) for _ in range(R)]
        names = "ebl khat eb8 ebm qtl enb ktl ktlT at ot of sq".split()
        Bn = {n: [Buf(n + str(i)) for i in range(R)] for n in names}
        BSb = [[Buf("Sb%d_%d" % (i, c)) for c in range(4)] for i in range(R)]
        St = sb.alloc("St", [128, 8, 128], F32)
        BSt = [Buf("St%d" % h) for h in range(8)]
        for h in range(8):
            S.op("dve", lambda e, h=h: e.memset(St[:, h, :], 0.0), [], [BSt[h]])
        F, BFs = self.F, self.BFs
        hi = [0]
        import os
        tl = list(range(32))
        if os.environ.get("S3T"):
            tl = [int(v) for v in os.environ["S3T"].split(",")]
        s3c = int(os.environ.get("S3C", "4"))
        s3m = int(os.environ.get("S3M", "9"))
        for tile in tl:
            main = tile >= 16
            mt = tile - 16
            sl = tile % 2
            self.dma("sp", rf[sl][:], D_["RF"][tile * 128:(tile + 1) * 128, :], self.scr_reads("RF", [tile]), [Brf[sl]], Brf[sl])
            self.dma("sp", ri[sl][:], D_["RI"][tile * 128:(tile + 1) * 128, :], self.scr_reads("RI", [tile]), [Bri[sl]], Bri[sl])
            if main:
                self.dma("sp", rq[sl][:], D_["RQT"][:, :, mt * 128:(mt + 1) * 128].rearrange("h p t -> p h t"),
                         self.scr_reads("RQT", [(mt // 4) * 4]), [Brq[sl]], Brq[sl])
                self.dma("sp", rog[sl][:], D_["ROG"][mt * 128:(mt + 1) * 128, :], self.scr_reads("ROG", [mt]), [Brog[sl]], Brog[sl])
            lf, kn = logf[sl], kin[sl]
            self.act(kn[:], rf[sl][:], AF.Sigmoid, [Brf[sl]], [Bkin[sl]])
            self.tt(kn[:], kn[:], self.oml[:], ALU.mult, [Bkin[sl], self.B_c0], [Bkin[sl]])
            self.tt(kn[:], kn[:], self.lb[:], ALU.add, [Bkin[sl], self.B_c0], [Bkin[sl]])
            self.act(lf[:], kn[:], AF.Ln, [Bkin[sl]], [Blf[sl]])
            self.ts(kn[:], kn[:], -1.0, 1.0, ALU.mult, ALU.add, [Bkin[sl]], [Bkin[sl]])
            for h in range(8):
                r = hi[0] % R
                hi[0] += 1
                hs = slice(h * 128, (h + 1) * 128)
                s4 = h % 4
                self.mm(F[0][:, s4 * 128:(s4 + 1) * 128], self.wl[:, :], lf[:, hs], True, True, [Blf[sl], self.B_c0], [BFs[0][s4]])
                self.act(ebl[r][:], F[0][:, s4 * 128:(s4 + 1) * 128], AF.Exp, [BFs[0][s4]], [Bn["ebl"][r]])
                for c in range(4):
                    self.stt(khc[r][:, c, :], kn[:, hs], self.wall[:, 132 + c:133 + c], ebl[r][:], ALU.mult, ALU.mult,
                             [Bkin[sl], Bn["ebl"][r], self.B_c0], [Bkhc[r]])
                s3 = h % 3
                pb = F[1][:, s3 * 136:(s3 + 1) * 136]
                bpb = BFs[1][s3]
                if main:
                    self.mm(pb, lf[:, hs], self.wall[:, :], True, True, [Blf[sl], self.B_c0], [bpb])
                else:
                    self.mm(pb[:, 128:136], lf[:, hs], self.wall[:, 128:136], True, True, [Blf[sl], self.B_c0], [bpb])
                self.act(eb8[r][:], pb[:, 128:136], AF.Exp, [bpb], [Bn["eb8"][r]])
                if main:
                    self.act(ebm[r][:], pb[:, 0:128], AF.Exp, [bpb], [Bn["ebm"][r]])
                    self.tt(qtl[r][:], rq[sl][:, h, :], ebm[r][:], ALU.mult, [Brq[sl], Bn["ebm"][r]], [Bn["qtl"][r]])
                    for c in range(4):
                        cc = slice(32 * c, 32 * c + 32)
                        self.tt(qtlz[r][:, c, cc], rq[sl][:, h, cc], ebm[r][:, cc], ALU.mult, [Brq[sl], Bn["ebm"][r]], [Bqz[r]])
                if main and s3m >= 2:
                    self.mm(F[2][:, s4 * 128:(s4 + 1) * 128], self.wall[:, 0:128], lf[:, hs], True, True, [Blf[sl], self.B_c0], [BFs[2][s4]])
                    self.act(enb[r][:], F[2][:, s4 * 128:(s4 + 1) * 128], AF.Exp, [BFs[2][s4]], [Bn["enb"][r]], scale=-1.0)
                    self.tt(ktl[r][:], kn[:, hs], enb[r][:], ALU.mult, [Bkin[sl], Bn["enb"][r]], [Bn["ktl"][r]])
                    tsl = h % 8
                    pt = self.T[0][:, tsl * 128:(tsl + 1) * 128]
                    bt = self.BT[0][tsl]
                    self.tr(pt, ktl[r][:], [Bn["ktl"][r]], [bt])
                    self.cp("act", ktlT[r][:], pt, [bt], [Bn["ktlT"][r]])
                if main and s3m >= 3:
                    self.mm(F[3][:, s4 * 128:(s4 + 1) * 128], ktlT[r][:], qtl[r][:], True, True, [Bn["ktlT"][r], Bn["qtl"][r]], [BFs[3][s4]])
                    self.tt(at[r][:], F[3][:, s4 * 128:(s4 + 1) * 128], self.bd[:], ALU.mult, [BFs[3][s4], self.B_c0], [Bn["at"][r]])
                po = F[4][:, 0:128]
                bpo = self.BF[4]
                main4 = main and s3m >= 4
                if main4:
                    self.mm(po, at[r][:], ri[sl][:, hs], True, False, [Bn["at"][r], Bri[sl]], [bpo])
                for c in range(s3c):
                    cs_ = slice(32 * c, 32 * c + 32)
                    pd = F[5][:, c * 128:(c + 1) * 128]
                    self.mm(pd, khc[r][:, c, :], ri[sl][:, hs], True, True, [Bkhc[r], Bri[sl]], [BFs[5][c]])
                for c in range(s3c):
                    cs_ = slice(32 * c, 32 * c + 32)
                    if main4:
                        self.act(Sb[r][:, c, :], St[:, h, :], AF.Copy, [BSt[h], Bn["eb8"][r]], [BSb[r][c]], scale=eb8[r][:, c:c + 1])
                        self.mm(po, qtlz[r][:, c, :], Sb[r][:, c, :], False, c == 3, [Bqz[r], BSb[r][c]], [bpo])
                    pd = F[5][:, c * 128:(c + 1) * 128]
                    bpd = BFs[5][c]
                    if os.environ.get("S3NOSTT"):
                        self.cp("dve", ebl[r][:], pd, [bpd], [Bn["ebl"][r]])
                        continue
                    self.stt(St[:, h, :], St[:, h, :], eb8[r][:, 4 + c:5 + c], pd, ALU.mult, ALU.add, [BSt[h], Bn["eb8"][r], bpd], [BSt[h]])
                if main and s3m >= 5:
                    s3n = int(os.environ.get("S3N", "9"))
                    self.act(jk[r][:], po, AF.Square, [bpo], [Bn["sq"][r]], accum=sq[r][:, 0:1])
                    if s3n < 2:
                        continue
                    self.tt(ot[r][:], po, self.rngb[:], ALU.mult, [bpo, self.B_c0], [Bn["ot"][r]])
                    if s3n < 3:
                        continue
                    self.rstd(sq[r][:, 2:3], sq[r][:, 0:1], 128.0, [Bn["sq"][r]], [Bn["sq"][r], Bn["sq"][r]], sq[r][:, 1:2])
                    if s3n < 4:
                        continue
                    self.stt(of_[r][:], ot[r][:], sq[r][:, 2:3], rog[sl][:, hs], ALU.mult, ALU.mult, [Bn["ot"][r], Bn["sq"][r], Brog[sl]], [Bn["of"][r]])
                    if s3n < 5:
                        continue
                    tsl = h % 8
                    pt = self.T[1][:, tsl * 128:(tsl + 1) * 128]
                    bt = self.BT[1][tsl]
                    self.tr(pt, of_[r][:], [Bn["of"][r]], [bt])
                    self.cp("act", self.oT[:, h, mt * 128:(mt + 1) * 128], pt, [bt], [self.B_oT[mt]])
            if tile == 15:
                for h in range(8):
                    self.ts(St[:, h, :], St[:, h, :], self.pm[:, 0:1], None, ALU.mult, None, [BSt[h], self.B_c0], [BSt[h]])
        if self.debug:
            for h in range(8):
                self.dma("sp", D_["OT"][h], self.oT[:, h, :], self.B_oT, [Buf("x")], self.B_oT[0])
        sb.pop()
        self.release(Brf + Bri + Brq + Brog)

    def stage4a(self):
        S, sb, I = self.S, self.sb, self.I
        D_ = self.dram
        sb.push()
        wa = [sb.alloc("wa", [128, 8, 128], BF16) for _ in range(2)]
        wr_ = [sb.alloc("wr_", [128, 8, 128], BF16) for _ in range(2)]
        sga = [sb.alloc("sga", [128, NT], BF16) for _ in range(2)]
        sgr = [sb.alloc("sgr", [128, NT], BF16) for _ in range(2)]
        mT = [sb.alloc("mT", [128, NT], BF16) for _ in range(2)]
        t1 = [sb.alloc("t1", [128, 512], F32) for _ in range(2)]
        t2 = [sb.alloc("t2", [128, 512], F32) for _ in range(2)]
        Bwa = [self.dbuf("wa", "pool") for _ in range(2)]
        Bwr = [self.dbuf("wr", "pool") for _ in range(2)]
        Bsa = [self.dbuf("sga") for _ in range(2)]
        Bsr = [self.dbuf("sgr") for _ in range(2)]
        BmT = [self.dbuf("mT") for _ in range(2)]
        Bt1 = [Buf("t1_%d" % i) for i in range(2)]
        Bt2 = [Buf("t2_%d" % i) for i in range(2)]
        wua = I["w_up_a"].rearrange("(k p) n -> p k n", p=128)
        wur = I["w_up_r"].rearrange("(k p) n -> p k n", p=128)
        ti = [0]
        for ch in range(16):
            sl = ch % 2
            self.dma("pool", wa[sl][:], wua[:, :, ch * 128:(ch + 1) * 128], [], [Bwa[sl]], Bwa[sl])
            self.dma("pool", wr_[sl][:], wur[:, :, ch * 128:(ch + 1) * 128], [], [Bwr[sl]], Bwr[sl])
            self.dma("sp", sga[sl][:], D_["GAT"][ch], self.scr_reads("GAT", [ch]), [Bsa[sl]], Bsa[sl])
            self.dma("sp", sgr[sl][:], D_["GRT"][ch], self.scr_reads("GRT", [ch]), [Bsr[sl]], Bsr[sl])
            for g in range(4):
                gs = slice(g * 512, (g + 1) * 512)
                fa, fr = (g % 2) * 2, (g % 2) * 2 + 1
                for wc in range(8):
                    self.mm(self.F[fa][:, :], wa[sl][:, wc, :], self.attT[:, wc, gs], wc == 0, wc == 7, [Bwa[sl], self.B_attT[wc]], [self.BF[fa]])
                for wc in range(8):
                    self.mm(self.F[fr][:, :], wr_[sl][:, wc, :], self.oT[:, wc, gs], wc == 0, wc == 7,
                            [Bwr[sl]] + self.B_oT[g * 4:(g + 1) * 4], [self.BF[fr]])
                i = ti[0] % 2
                ti[0] += 1
                self.tt(t1[i][:], self.F[fa][:, :], sga[sl][:, gs], ALU.mult, [self.BF[fa], Bsa[sl]], [Bt1[i]])
                self.tt(t2[i][:], self.F[fr][:, :], sgr[sl][:, gs], ALU.mult, [self.BF[fr], Bsr[sl]], [Bt2[i]], eng="dve")
                self.tt(mT[sl][:, gs], t1[i][:], t2[i][:], ALU.add, [Bt1[i], Bt2[i]], [BmT[sl]], eng="dve")
            tok = Buf("w")
            self.scr_w.setdefault(("MT", ch), []).append(tok)
            self.dma("sp", D_["MT"][ch], mT[sl][:], [BmT[sl]], [tok], BmT[sl])
        sb.pop()
        self.release(Bwa + Bwr + Bsa + Bsr + BmT)

    def stage4b(self):
        S, sb, I = self.S, self.sb, self.I
        D_ = self.dram
        sb.push()
        wo = sb.alloc("wo", [128, KC, D], BF16)
        Bwo = [self.dbuf("wo", "pool") for _ in range(4)]
        wsrc = I["w_out"].rearrange("(k p) n -> p k n", p=128)
        for i in range(4):
            self.dma("pool", wo[:, i * 4:(i + 1) * 4, :], wsrc[:, i * 4:(i + 1) * 4, :], [], [Bwo[i]], Bwo[i])
        mTg = [sb.alloc("mTg", [128, KC, 512], BF16) for _ in range(2)]
        Bmg = [self.dbuf("mTg") for _ in range(2)]
        xs = [sb.alloc("xs", [128, D], F32) for _ in range(2)]
        Bx = [self.dbuf("xs") for _ in range(2)]
        x1 = [sb.alloc("x1", [128, D], F32) for _ in range(2)]
        Bx1 = [self.dbuf("x1") for _ in range(2)]
        h2 = [sb.alloc("h2", [128, KC, 128], BF16) for _ in range(2)]
        Bh2 = [self.dbuf("h2") for _ in range(2)]
        rings = [self.norm_ring(sb) for _ in range(2)]
        tq = [sb.alloc("tq", [128, 512], F32) for _ in range(2)]
        Btq = [Buf("tq%d" % i) for i in range(2)]
        rt = sb.alloc("rt", [128, 160], F32)
        Brt = Buf("rt")
        r2 = sb.alloc("r2", [128, 112], F32)
        Br2 = Buf("r2")
        selb = sb.alloc("selb", [128, 32], BF16)
        Bselb = Buf("selb")
        mtsrc = D_["MT"].rearrange("k p t -> p k t")
        qi = [0]
        for g in range(4):
            sg = g % 2
            self.dma("sp", mTg[sg][:], mtsrc[:, :, g * 512:(g + 1) * 512], self.scr_reads("MT", range(16)), [Bmg[sg]], Bmg[sg])
            for tt_ in range(4):
                tile = g * 4 + tt_
                sl = tile % 2
                self.dma("sp", xs[sl][:], I["xm"][tile * 128:(tile + 1) * 128, :], [], [Bx[sl]], Bx[sl])
                for cg in range(4):
                    cs_ = slice(cg * 512, (cg + 1) * 512)
                    fb = cg % 4
                    for kc in range(KC):
                        self.mm(self.F[fb][:, :], mTg[sg][:, kc, tt_ * 128:(tt_ + 1) * 128], wo[:, kc, cs_], kc == 0, kc == KC - 1,
                                [Bmg[sg], Bwo[kc // 4]], [self.BF[fb]])
                    i = qi[0] % 2
                    qi[0] += 1
                    self.tt(tq[i][:], self.F[fb][:, :], self.gt1b[:, cs_], ALU.mult, [self.BF[fb], self.B_c0], [Btq[i]])
                    self.tt(x1[sl][:, cs_], tq[i][:], xs[sl][:, cs_], ALU.add, [Btq[i], Bx[sl]], [Bx1[sl]], eng="dve")
                tok = Buf("w")
                self.scr_w.setdefault(("X1", tile), []).append(tok)
                self.dma("sp", D_["X1"][tile * 128:(tile + 1) * 128, :], x1[sl][:], [Bx1[sl]], [tok], Bx1[sl])
                self.norm_to_hT(x1[sl][:], Bx1[sl], h2[sl][:, :, :], Bh2[sl], self.a2, self.modc[:, 3, :], rings[sl])
                tok = Buf("w")
                self.scr_w.setdefault(("H2T", tile), []).append(tok)
                self.dma("sp", D_["H2T"][:, :, tile * 128:(tile + 1) * 128].rearrange("k p t -> p k t"), h2[sl][:], [Bh2[sl]], [tok], Bh2[sl])
                pr = self.F[5][:, 0:36]
                bpr = self.BF[5]
                for kc in range(KC):
                    self.mm(pr, h2[sl][:, kc, :], self.wr[:, kc, :], kc == 0, kc == KC - 1, [Bh2[sl], self.B_c0], [bpr])
                lg = rt[:, 0:36]
                self.tt(lg, pr, self.br[:], ALU.add, [bpr, self.B_c0], [Brt])
                mx = rt[:, 36:37]
                S.op("dve", lambda e: e.tensor_reduce(out=rt[:, 36:37], in_=rt[:, 0:4], axis=AX.X, op=ALU.max), [Brt], [Brt])
                self.ts(rt[:, 37:38], mx, -1.0, None, ALU.mult, None, [Brt], [Brt])
                self.act(rt[:, 40:44], rt[:, 0:4], AF.Exp, [Brt], [Brt], bias=rt[:, 37:38], accum=rt[:, 38:39])
                self.ts(rt[:, 44:48], rt[:, 0:4], mx, None, ALU.is_ge, None, [Brt], [Brt])
                self.ts(rt[:, 44:48], rt[:, 44:48], -1.0, -NEGBIG, ALU.add, ALU.mult, [Brt], [Brt])
                for gi in range(4):
                    self.ts(rt[:, 48 + gi * 8:56 + gi * 8], rt[:, 4 + gi * 8:12 + gi * 8], rt[:, 44 + gi:45 + gi], None, ALU.add, None, [Brt], [Brt])
                S.op("dve", lambda e: e.max(out=rt[:, 80:88], in_=rt[:, 48:80]), [Brt], [Brt])
                self.ts(rt[:, 88:120], rt[:, 48:80], rt[:, 81:82], None, ALU.is_ge, None, [Brt], [Brt])
                self.ts(rt[:, 39:40], rt[:, 80:81], -1.0, None, ALU.mult, None, [Brt], [Brt])
                self.act(rt[:, 120:152], rt[:, 48:80], AF.Exp, [Brt], [Brt], bias=rt[:, 39:40])
                self.tt(rt[:, 120:152], rt[:, 120:152], rt[:, 88:120], ALU.mult, [Brt], [Brt])
                S.op("dve", lambda e: e.reduce_sum(out=rt[:, 152:153], in_=rt[:, 120:152], axis=AX.X), [Brt], [Brt])
                self.tt(rt[:, 152:153], rt[:, 152:153], rt[:, 38:39], ALU.mult, [Brt], [Brt])
                S.op("dve", lambda e: e.reciprocal(out=rt[:, 153:154], in_=rt[:, 152:153]), [Brt], [Brt])
                self.ts(self.wfull[:, tile, :], rt[:, 120:152], rt[:, 153:154], None, ALU.mult, None, [Brt], [self.B_wfull[tile]])
                self.cp("dve", selb[:], rt[:, 88:120], [Brt], [Bselb])
                pc = self.F[4]
                self.mm(pc[:, 0:32], self.slt[:, :], selb[:], True, True, [Bselb, self.B_c0], [self.BF[4]])
                self.mm(pc[:, 32:64], self.ones[:, :], selb[:], True, True, [Bselb, self.B_c0], [self.BF[4]])
                self.tt(r2[:, 0:32], pc[:, 0:32], self.tot[:], ALU.add, [self.BF[4], self.B_tot], [Br2])
                self.tt(self.tot[:], pc[:, 32:64], self.tot[:], ALU.add, [self.BF[4], self.B_tot], [self.B_tot])
                self.ts(r2[:, 0:32], r2[:, 0:32], float(CAP - 1), None, ALU.min, None, [Br2], [Br2])
                self.tt(r2[:, 0:32], r2[:, 0:32], self.ebase[:], ALU.add, [Br2, self.B_c0], [Br2])
                self.stt(r2[:, 32:64], r2[:, 0:32], 1.0e6, rt[:, 88:120], ALU.add, ALU.mult, [Br2, Brt], [Br2])
                S.op("dve", lambda e: e.max(out=r2[:, 64:72], in_=r2[:, 32:64]), [Br2], [Br2])
                self.ts(r2[:, 72:74], r2[:, 64:66], -1.0e6, None, ALU.add, None, [Br2], [Br2])
                self.cp("dve", self.desti[:, tile, :], r2[:, 72:74], [Br2], [self.B_dest[tile]])
                for k_ in range(2):
                    self.ts(r2[:, 80:112], r2[:, 32:64], r2[:, 64 + k_:65 + k_], None, ALU.is_equal, None, [Br2], [Br2])
                    self.tt(r2[:, 80:112], r2[:, 80:112], self.wfull[:, tile, :], ALU.mult, [Br2, self.B_wfull[tile]], [Br2])
                    S.op("dve", lambda e, k_=k_, tile=tile: e.reduce_sum(out=self.wk[:, tile, k_:k_ + 1], in_=r2[:, 80:112], axis=AX.X),
                         [Br2], [self.B_dest[tile]])
                xn_t, bxn_t = rings[sl][4], rings[sl][7]
                for k_ in range(2):
                    self.cp("dve", self.six[sl][:, k_:k_ + 1], self.desti[:, tile, k_:k_ + 1], [self.B_dest[tile]], [self.B_six[sl][k_]])
                    S.op("pool", lambda e, k_=k_, ix=self.six[sl], xn_t=xn_t: e.indirect_dma_start(
                        out=D_["XD"], out_offset=bass.IndirectOffsetOnAxis(ap=ix[:, k_:k_ + 1], axis=0),
                        in_=xn_t[:], in_offset=None),
                        [bxn_t, self.B_six[sl][k_]], [Buf("sc"), self.B_ind], dbuf=self.B_ind)
        if self.debug:
            self.dma("sp", D_["WF"], self.wfull[:].rearrange("p a b -> p (a b)"), self.B_wfull, [Buf("x")], self.B_wfull[0])
        sb.pop()
        self.release(Bwo + Bmg + Bx + Bx1 + Bh2)

    def stage5(self):
        S, sb, I = self.S, self.sb, self.I
        D_ = self.dram
        sb.push()
        w1 = [sb.alloc("w1", [128, KC, 512], BF16) for _ in range(2)]
        w3 = [sb.alloc("w3", [128, KC, 512], BF16) for _ in range(2)]
        w2 = sb.alloc("w2", [128, 4, D], BF16)
        Bw1 = [self.dbuf("w1", "pool") for _ in range(2)]
        Bw3 = [self.dbuf("w3", "pool") for _ in range(2)]
        Bw2 = self.dbuf("w2", "pool")
        nrt = CAP // 128
        xr = [sb.alloc("xr", [128, D], BF16) for _ in range(nrt)]
        Bxr = [self.dbuf("xr") for _ in range(nrt)]
        XT = [sb.alloc("XT", [128, KC, CAP], BF16) for _ in range(2)]
        BXT = [[Buf("XT%d_%d" % (i, j)) for j in range(nrt)] for i in range(2)]
        s1 = [sb.alloc("s1", [128, CAP], F32) for _ in range(2)]
        Bs1 = [Buf("s1_%d" % i) for i in range(2)]
        hid = [sb.alloc("hid", [128, 4, CAP], BF16) for _ in range(2)]
        Bhid = [[Buf("hid%d_%d" % (i, f)) for f in range(4)] for i in range(2)]
        ys = [sb.alloc("ys", [128, D], F32) for _ in range(2)]
        Bys = [self.dbuf("ys") for _ in range(2)]
        si = [0]
        yi = [0]

        def loads(ex):
            for r_ in range(nrt):
                row0 = ex * CAP + r_ * 128
                self.dma("sp", xr[r_][:], D_["XD"][row0:row0 + 128, :], [], [Bxr[r_]], Bxr[r_])

        def trans(ex):
            sl = ex % 2
            for r_ in range(nrt):
                for kc in range(KC):
                    tb, ts_ = kc // 8, kc % 8
                    pt = self.T[tb][:, ts_ * 128:(ts_ + 1) * 128]
                    bt = self.BT[tb][ts_]
                    self.tr(pt, xr[r_][:, kc * 128:(kc + 1) * 128], [Bxr[r_]], [bt])
                    dst = XT[sl][:, kc, r_ * 128:(r_ + 1) * 128]
                    if tb == 0:
                        self.act(dst, pt, AF.Identity, [bt, self.B_c0], [BXT[sl][r_]], scale=self.a2[:, kc:kc + 1], bias=self.modc[:, 3, kc:kc + 1])
                    else:
                        self.ts(dst, pt, self.a2[:, kc:kc + 1], self.modc[:, 3, kc:kc + 1], ALU.mult, ALU.add, [bt, self.B_c0], [BXT[sl][r_]])

        loads(0)
        trans(0)
        for ex in range(32):
            sl = ex % 2
            self.dma("pool", w1[sl][:], I["w1"][ex].rearrange("(k p) n -> p k n", p=128), [], [Bw1[sl]], Bw1[sl])
            self.dma("pool", w3[sl][:], I["w3"][ex].rearrange("(k p) n -> p k n", p=128), [], [Bw3[sl]], Bw3[sl])
            self.dma("pool", w2[:], I["w2"][ex].rearrange("(k p) n -> p k n", p=128), [], [Bw2], Bw2)
            if ex + 1 < 32:
                loads(ex + 1)
            for fc in range(4):
                fs = slice(fc * 128, (fc + 1) * 128)
                f1, f3 = (fc % 2) * 2, (fc % 2) * 2 + 1
                for kc in range(KC):
                    self.mm(self.F[f1][:, 0:CAP], w1[sl][:, kc, fs], XT[sl][:, kc, :], kc == 0, kc == KC - 1, [Bw1[sl]] + BXT[sl], [self.BF[f1]])
                for kc in range(KC):
                    self.mm(self.F[f3][:, 0:CAP], w3[sl][:, kc, fs], XT[sl][:, kc, :], kc == 0, kc == KC - 1, [Bw3[sl]] + BXT[sl], [self.BF[f3]])
                i = si[0] % 2
                si[0] += 1
                self.act(s1[i][:], self.F[f1][:, 0:CAP], AF.Silu, [self.BF[f1]], [Bs1[i]])
                self.tt(hid[sl][:, fc, :], s1[i][:], self.F[f3][:, 0:CAP], ALU.mult, [Bs1[i], self.BF[f3]], [Bhid[sl][fc]])
            if ex + 1 < 32:
                trans(ex + 1)
            for r_ in range(nrt):
                y_ = ys[yi[0] % 2]
                by = Bys[yi[0] % 2]
                yi[0] += 1
                for cg in range(4):
                    cs_ = slice(cg * 512, (cg + 1) * 512)
                    fb = 4 + (cg % 2)
                    for fc in range(4):
                        self.mm(self.F[fb][:, :], hid[sl][:, fc, r_ * 128:(r_ + 1) * 128], w2[:, fc, cs_], fc == 0, fc == 3,
                                [Bhid[sl][fc], Bw2], [self.BF[fb]])
                    if fb == 4:
                        self.cp("act", y_[:, cs_], self.F[fb][:, :], [self.BF[fb]], [by])
                    else:
                        self.cp("dve", y_[:, cs_], self.F[fb][:, :], [self.BF[fb]], [by])
                row0 = ex * CAP + r_ * 128
                for hf in range(2):
                    self.dma("sp", D_["YD%d" % hf][row0:row0 + 128, :], y_[:, hf * 1024:(hf + 1) * 1024], [by], [Buf("y")], by)
        sb.pop()
        self.release(Bw1 + Bw3 + [Bw2] + Bxr + Bys)
        self.bar()
        sb.push()
        xs = [sb.alloc("xs", [128, D], F32) for _ in range(2)]
        Bx = [self.dbuf("xs") for _ in range(2)]
        yg = [[[sb.alloc("yg", [128, 1024], F32) for _ in range(2)] for _ in range(2)] for _ in range(2)]
        Byg = [[[Buf("yg") for _ in range(2)] for _ in range(2)] for _ in range(2)]
        jk = sb.alloc("jk5", [128, D], BF16)
        st5 = sb.alloc("st5", [128, 4], F32)
        Bst = Buf("st5")
        for tile in range(16):
            sl = tile % 2
            self.dma("sp", xs[sl][:], D_["X1"][tile * 128:(tile + 1) * 128, :], self.scr_reads("X1", [tile]), [Bx[sl]], Bx[sl])
            for k_ in range(2):
                self.cp("dve", self.gix[sl][:, k_:k_ + 1], self.desti[:, tile, k_:k_ + 1], [self.B_dest[tile]], [self.B_gix[sl][k_]])
                for hf in range(2):
                    S.op("pool", lambda e, k_=k_, ix=self.gix[sl], dst=yg[sl][k_][hf], hf=hf: e.indirect_dma_start(
                        out=dst[:], out_offset=None, in_=D_["YD%d" % hf],
                        in_offset=bass.IndirectOffsetOnAxis(ap=ix[:, k_:k_ + 1], axis=0)),
                        [self.B_gix[sl][k_]], [Byg[sl][k_][hf], self.B_ind], dbuf=self.B_ind)
            for hf in range(2):
                a, b = yg[sl][0][hf], yg[sl][1][hf]
                ba, bb = Byg[sl][0][hf], Byg[sl][1][hf]
                hs_ = slice(hf * 1024, (hf + 1) * 1024)
                self.ts(a[:], a[:], self.wk[:, tile, 0:1], None, ALU.mult, None, [ba, self.B_dest[tile]], [ba])
                self.stt(a[:], b[:], self.wk[:, tile, 1:2], a[:], ALU.mult, ALU.add, [bb, ba, self.B_dest[tile]], [ba])
                self.tt(a[:], a[:], self.gt2b[:, hs_], ALU.mult, [ba, self.B_c0], [ba])
                self.tt(xs[sl][:, hs_], xs[sl][:, hs_], a[:], ALU.add, [Bx[sl], ba], [Bx[sl]])
            self.act(jk[:], xs[sl][:], AF.Square, [Bx[sl]], [Bst], accum=st5[:, 0:1])
            self.rstd(st5[:, 2:3], st5[:, 0:1], float(D), [Bst], [Bst, Bst], st5[:, 1:2])
            self.stt(xs[sl][:], xs[sl][:], st5[:, 2:3], self.fgb[:], ALU.mult, ALU.mult, [Bx[sl], Bst, self.B_c0], [Bx[sl]])
            self.dma("sp", self.out[tile * 128:(tile + 1) * 128, :], xs[sl][:], [Bx[sl]], [Buf("o")], Bx[sl])
        sb.pop()
        self.release(Bx)


def host_consts():
    bf = ml_dtypes.bfloat16
    c = {}
    c["k_ident"] = np.eye(128, dtype=np.float32)
    cm = np.zeros((128, 512), np.float32)
    for j in range(2):
        kp = np.arange(128)[:, None] + j * 128
        q = np.arange(256)[None, :]
        cm[:, j * 256:(j + 1) * 256] = np.where(kp <= q, 0.0, NEG)
    c["k_cm"] = cm
    cp = np.zeros((128, 16, 16), np.float32)
    for qt in range(16):
        cp[:, qt, 8 + qt // 2:] = NEGBIG
    c["k_cpen"] = cp.reshape(128, 256)
    sel = np.zeros((16, 16, 128), np.float32)
    for i in range(16):
        sel[i, i, :] = 1.0
    c["k_sel"] = sel.reshape(16, 2048)
    s = np.arange(128)[:, None]
    t = np.arange(128)[None, :]
    same = (s // 32) == (t // 32)
    mid = (t // 32) * 32 + 15
    wm = np.where(same & (s > mid) & (s <= t), 1.0, 0.0) - np.where(same & (s > t) & (s <= mid), 1.0, 0.0)
    wmid = np.zeros((128, 4), np.float32)
    wtot = np.zeros((128, 4), np.float32)
    for cc in range(4):
        sl = np.arange(128)
        wmid[:, cc] = ((sl // 32) == cc) & (sl <= cc * 32 + 15)
        wtot[:, cc] = (sl // 32) == cc
    c["k_wall"] = np.concatenate([wm, wmid, wtot], axis=1).astype(np.float32)
    c["k_wl"] = np.where(same & (s > t), 1.0, 0.0).astype(np.float32)
    c["k_bd"] = np.where(same & (s <= t), 1.0, 0.0).astype(np.float32)
    c["k_slt"] = np.where(s < t, 1.0, 0.0).astype(np.float32)
    c["k_ones"] = np.ones((128, 128), np.float32)
    c["k_ebase"] = np.tile((np.arange(32) * CAP).astype(np.float32)[None, :], (128, 1))
    return c


_NC_CACHE = {}


def get_nc(debug=False, stages="012345"):
    key = (debug, stages)
    if key not in _NC_CACHE:
        nc = bass.Bass("TRN2", target_bir_lowering=False)
        kb = K(nc, debug=debug, stages=stages)
        kb.build()
        _NC_CACHE[key] = (nc, kb)
    return _NC_CACHE[key]


def make_in_maps(inputs, cores=range(8)):
    f = lambda a: np.ascontiguousarray(np.asarray(a, dtype=np.float32))
    x = f(inputs["x"])
    c = f(inputs["c"])
    shared = {
        "norm1_g": f(inputs["norm1_g"])[0], "norm2_g": f(inputs["norm2_g"])[0], "final_g": f(inputs["final_g"]),
        "w_ada": f(inputs["w_ada"])[0], "b_ada": f(inputs["b_ada"])[0], "w_in": f(inputs["w_in"])[0],
        "r_lower": f(inputs["r_lower"]), "r_norm_g": f(inputs["r_norm_g"])[0],
        "w_up_a": f(inputs["w_up_a"])[0], "w_up_r": f(inputs["w_up_r"])[0], "w_out": f(inputs["w_out"])[0],
        "w_rg": f(inputs["w_rg"])[0], "b_rg": f(inputs["b_rg"])[0], "w_re": f(inputs["w_re"])[0], "b_re": f(inputs["b_re"])[0],
        "w1": f(inputs["w1"])[0], "w3": f(inputs["w3"])[0], "w2": f(inputs["w2"])[0],
    }
    shared.update(host_consts())
    maps = []
    for core in cores:
        b, half = core // 2, core % 2
        m = dict(shared)
        m["xm"] = np.ascontiguousarray(x[b, half * NT:(half + 1) * NT])
        m["xp"] = np.ascontiguousarray(x[b, 0:NT])
        m["c"] = np.ascontiguousarray(c[b])
        m["pm"] = np.full((128, 1), float(half), np.float32)
        maps.append(m)
    return maps


def kernel(**inputs):
    nc, _ = get_nc()
    maps = make_in_maps(inputs)
    res = run_bass_kernel_spmd(nc, maps, core_ids=list(range(8)))
    out = np.zeros((4, 4096, D), np.float32)
    for core in range(8):
        b, half = core // 2, core % 2
        out[b, half * NT:(half + 1) * NT] = np.asarray(res.results[core]["out"], dtype=np.float32)
    return out
```

```python
import contextlib
import numpy as np
import ml_dtypes
import concourse.bass as bass
import concourse.mybir as mybir
from concourse.bass_utils import run_bass_kernel_spmd

F32 = mybir.dt.float32
BF16 = mybir.dt.bfloat16
I32 = mybir.dt.int32
CAP = 512
NR = 32 * CAP
ALU = mybir.AluOpType
AF = mybir.ActivationFunctionType
AX = mybir.AxisListType

D = 2048
NT = 2048
KC = 16
EPS = 1e-6
NEG = -30000.0
NEGBIG = -1.0e30
ENGS = ("pe", "act", "dve", "pool", "sp")
SAME_ENGINE_SYNC = {"pe": False, "act": True, "dve": True, "pool": True, "sp": False}


class Buf:
    __slots__ = ("name", "last_w", "readers", "dsem", "dcount", "const", "last_dma", "excl")

    def __init__(self, name, const=False, excl=False):
        self.excl = excl
        self.name = name
        self.last_w = None
        self.readers = []
        self.dsem = None
        self.dcount = 0
        self.const = const
        self.last_dma = None


class Op:
    __slots__ = ("eng", "fn", "deps", "sig", "sem", "val", "dbuf")

    def __init__(self, eng, fn, dbuf=None):
        self.eng = eng
        self.fn = fn
        self.deps = []
        self.sig = dbuf is not None
        self.sem = None
        self.val = 0
        self.dbuf = dbuf


class Sched:
    def __init__(self, nc):
        self.nc = nc
        self.ops = {e: [] for e in ENGS}
        self.dma_bufs = []
        self.epoch = None
        self.all_ops = []

    def barrier(self, fn):
        o = Op("dve", fn, None)
        deps = {}
        for e in ENGS:
            if self.ops[e]:
                d = self.ops[e][-1]
                deps[id(d)] = d
        for b in self.dma_bufs:
            if b.last_dma is not None:
                deps[id(b.last_dma)] = b.last_dma
        o.deps = list(deps.values())
        self.ops["dve"].append(o)
        self.all_ops.append(o)
        self.epoch = o
        return o

    def op(self, eng, fn, reads=(), writes=(), dbuf=None, extra=()):
        o = Op(eng, fn, dbuf)
        deps = {}
        for d in extra:
            if d is not None:
                deps[id(d)] = d
        if self.epoch is not None:
            deps[id(self.epoch)] = self.epoch
        xr = [b for b in reads if b.excl]
        if xr:
            reads = [b for b in reads if not b.excl]
            writes = list(writes) + [b for b in xr if b not in writes]
        for b in reads:
            if b.last_w is not None:
                deps[id(b.last_w)] = b.last_w
        for b in writes:
            if b.last_w is not None:
                deps[id(b.last_w)] = b.last_w
            for r in b.readers:
                deps[id(r)] = r
        o.deps = list(deps.values())
        for b in writes:
            b.last_w = o
            b.readers = []
        for b in reads:
            if not b.const and b.last_w is not o:
                b.readers.append(o)
        if dbuf is not None:
            dbuf.last_dma = o
            if dbuf.dsem is None:
                dbuf.dsem = True
                self.dma_bufs.append(dbuf)
        self.ops[eng].append(o)
        self.all_ops.append(o)
        return o

    def emit(self, stack):
        nc = self.nc
        for e in ENGS:
            for o in self.ops[e]:
                keep = []
                for d in o.deps:
                    if d.dbuf is None and d.eng == o.eng and not SAME_ENGINE_SYNC[o.eng]:
                        continue
                    keep.append(d)
                    d.sig = True
                o.deps = keep
        esem = {}
        for e in ENGS:
            if any(o.sig and o.dbuf is None for o in self.ops[e]):
                esem[e] = stack.enter_context(nc.semaphore("s_" + e))
        for i, b in enumerate(self.dma_bufs):
            b.dsem = stack.enter_context(nc.semaphore("d%d" % i))
            b.dcount = 0
        cnt = {e: 0 for e in ENGS}
        for o in self.all_ops:
            if o.dbuf is not None:
                o.dbuf.dcount += 16
                o.sem = o.dbuf.dsem
                o.val = o.dbuf.dcount
            elif o.sig:
                cnt[o.eng] += 1
                o.sem = esem[o.eng]
                o.val = cnt[o.eng]
        block = stack.enter_context(nc.Block())
        handles = {"pe": block.tensor, "act": block.scalar, "dve": block.vector,
                   "pool": block.gpsimd, "sp": block.sync}
        for e in ENGS:
            ops = self.ops[e]
            if not ops:
                continue

            def body(eng, ops=ops):
                waited = {}
                for o in ops:
                    need = {}
                    for d in o.deps:
                        k = id(d.sem)
                        if d.val > need.get(k, (0, None))[0]:
                            need[k] = (d.val, d.sem)
                    for k, (v, sem) in need.items():
                        if waited.get(k, 0) >= v:
                            continue
                        waited[k] = v
                        eng.wait_ge(sem, v)
                    ins = o.fn(eng)
                    if o.sig:
                        ins.then_inc(o.sem, 16 if o.dbuf is not None else 1)

            handles[e](body)


class SbAlloc:
    def __init__(self, nc, base=17408, limit=229000):
        self.nc = nc
        self.top = base
        self.limit = limit
        self.n = 0
        self.marks = []

    def push(self):
        self.marks.append(self.top)

    def pop(self):
        self.top = self.marks.pop()

    def alloc(self, name, shape, dtype):
        sz = int(np.prod(shape[1:])) * (4 if dtype in (F32, I32) else 2)
        off = (self.top + 63) // 64 * 64
        assert off + sz <= self.limit, (name, off, sz, self.limit)
        self.top = off + sz
        self.n += 1
        return self.nc.alloc_sbuf_tensor_at("%s_%d" % (name, self.n), list(shape), dtype, offset=off)


class K:
    def __init__(self, nc, debug=False, stages="012345"):
        self.nc = nc
        self.S = Sched(nc)
        self.sb = SbAlloc(nc)
        self.debug = debug
        self.stages = stages
        self.dpool = {}
        self.bkind = {}
        self.dram = {}
        self.sbufs = {}
        self.scr_w = {}

    def din(self, name, shape, dt=F32):
        return self.nc.dram_tensor(name, list(shape), dt, kind="ExternalInput").ap()

    def dscr(self, name, shape, dt):
        kind = "ExternalOutput" if self.debug else "Internal"
        t = self.nc.dram_tensor(name, list(shape), dt, kind=kind).ap()
        self.dram[name] = t
        return t

    def dbuf(self, name, kind="sp"):
        pool = self.dpool.setdefault(kind, [])
        if pool:
            return pool.pop()
        b = Buf(name)
        self.bkind[id(b)] = kind
        return b

    def release(self, bufs):
        for b in bufs:
            self.dpool[self.bkind[id(b)]].append(b)

    def dma(self, q, out, in_, reads, writes, dbuf, **kw):
        k = self.bkind.setdefault(id(dbuf), q)
        assert k == q, (dbuf.name, k, q)
        return self.S.op(q, lambda e: e.dma_start(out=out, in_=in_, **kw), reads, writes, dbuf=dbuf)

    def mm(self, out, lhsT, rhs, start, stop, reads, writes, tp=None):
        if tp is not None:
            return self.S.op("pe", lambda e: e.matmul(out, lhsT=lhsT, rhs=rhs, start=start, stop=stop, tile_position=tp), reads, writes)
        return self.S.op("pe", lambda e: e.matmul(out, lhsT=lhsT, rhs=rhs, start=start, stop=stop), reads, writes)

    def tr(self, out, in_, reads, writes):
        idn = self.identb
        n = in_.shape[0]
        return self.S.op("pe", lambda e: e.transpose(out=out, in_=in_, identity=idn[0:n, 0:n]),
                         list(reads) + [self.B_c0], writes)

    def act(self, out, in_, func, reads, writes, scale=1.0, bias=None, accum=None):
        kw = {}
        if bias is not None:
            kw["bias"] = bias
        if accum is not None:
            kw["accum_out"] = accum
        return self.S.op("act", lambda e: e.activation(out=out, in_=in_, func=func, scale=scale, **kw), reads, writes)

    def ts(self, out, in0, s1, s2, op0, op1, reads, writes, eng="dve"):
        if s2 is None:
            return self.S.op(eng, lambda e: e.tensor_scalar(out=out, in0=in0, scalar1=s1, scalar2=None, op0=op0), reads, writes)
        return self.S.op(eng, lambda e: e.tensor_scalar(out=out, in0=in0, scalar1=s1, scalar2=s2, op0=op0, op1=op1), reads, writes)

    def tt(self, out, in0, in1, op, reads, writes, eng="dve"):
        return self.S.op(eng, lambda e: e.tensor_tensor(out=out, in0=in0, in1=in1, op=op), reads, writes)

    def stt(self, out, in0, scalar, in1, op0, op1, reads, writes, eng="dve"):
        return self.S.op(eng, lambda e: e.scalar_tensor_tensor(out=out, in0=in0, scalar=scalar, in1=in1, op0=op0, op1=op1), reads, writes)

    def cp(self, eng, out, in_, reads, writes):
        if eng == "act":
            return self.S.op("act", lambda e: e.copy(out=out, in_=in_), reads, writes)
        return self.S.op(eng, lambda e: e.tensor_copy(out=out, in_=in_), reads, writes)

    def rstd(self, out, ssq, n, reads, writes, tmp):
        eps = self.epsc
        self.act(tmp, ssq, AF.Sqrt, list(reads) + [self.B_c0], [writes[1]], scale=1.0 / n, bias=eps[:, 0:1])
        self.S.op("dve", lambda e: e.reciprocal(out=out, in_=tmp), [writes[1]], [writes[0]])

    def build(self):
        nc = self.nc
        sb = self.sb
        S = self.S
        I = {}
        I["xm"] = self.din("xm", [NT, D])
        I["xp"] = self.din("xp", [NT, D])
        I["c"] = self.din("c", [D])
        I["pm"] = self.din("pm", [128, 1])
        I["norm1_g"] = self.din("norm1_g", [D])
        I["norm2_g"] = self.din("norm2_g", [D])
        I["final_g"] = self.din("final_g", [D])
        I["w_ada"] = self.din("w_ada", [D, 6 * D])
        I["b_ada"] = self.din("b_ada", [6 * D])
        I["w_in"] = self.din("w_in", [D, 11264])
        I["r_lower"] = self.din("r_lower", [2, 1024])
        I["r_norm_g"] = self.din("r_norm_g", [128])
        I["w_up_a"] = self.din("w_up_a", [1024, D])
        I["w_up_r"] = self.din("w_up_r", [1024, D])
        I["w_out"] = self.din("w_out", [D, D])
        I["w_rg"] = self.din("w_rg", [D, 4])
        I["b_rg"] = self.din("b_rg", [4])
        I["w_re"] = self.din("w_re", [D, 32])
        I["b_re"] = self.din("b_re", [32])
        I["w1"] = self.din("w1", [32, D, 512])
        I["w3"] = self.din("w3", [32, D, 512])
        I["w2"] = self.din("w2", [32, 512, D])
        I["k_ident"] = self.din("k_ident", [128, 128])
        I["k_cm"] = self.din("k_cm", [128, 512])
        I["k_cpen"] = self.din("k_cpen", [128, 256])
        I["k_sel"] = self.din("k_sel", [16, 2048])
        I["k_wall"] = self.din("k_wall", [128, 136])
        I["k_wl"] = self.din("k_wl", [128, 128])
        I["k_bd"] = self.din("k_bd", [128, 128])
        I["k_slt"] = self.din("k_slt", [128, 128])
        I["k_ones"] = self.din("k_ones", [128, 128])
        I["k_ebase"] = self.din("k_ebase", [128, 32])
        self.I = I
        self.out = nc.dram_tensor("out", [NT, D], F32, kind="ExternalOutput").ap()
        self.dscr("MOD", [6 * D], F32)
        self.dscr("QT", [8, 128, NT], BF16)
        self.dscr("KT", [8, 128, 2 * NT], BF16)
        self.dscr("V", [2 * NT, 1024], BF16)
        self.dscr("RQT", [8, 128, NT], BF16)
        self.dscr("RF", [2 * NT, 1024], F32)
        self.dscr("RI", [2 * NT, 1024], BF16)
        self.dscr("ROG", [NT, 1024], BF16)
        self.dscr("GAT", [16, 128, NT], BF16)
        self.dscr("GRT", [16, 128, NT], BF16)
        self.dscr("MT", [16, 128, NT], BF16)
        self.dscr("X1", [NT, D], F32)
        self.dscr("H2T", [16, 128, NT], BF16)
        self.dscr("XD", [NR, D], BF16)
        self.dscr("YD0", [NR, 1024], F32)
        self.dscr("YD1", [NR, 1024], F32)
        if self.debug:
            self.dscr("ATT", [8, 128, NT], BF16)
            self.dscr("OT", [8, 128, NT], BF16)
            self.dscr("WF", [128, 16 * 32], F32)
        self.scrB = {}

        with contextlib.ExitStack() as st:
            self.F = [st.enter_context(nc.psum_tensor("F%d" % i, [128, 512], F32)) for i in range(6)]
            self.T = [st.enter_context(nc.psum_tensor("T%d" % i, [128, 1024], BF16)) for i in range(2)]
            self.BF = [Buf("F%d" % i, excl=True) for i in range(6)]
            self.BFs = [[self.BF[i]] * 4 for i in range(6)]
            _bt = [Buf("T%d" % i, excl=True) for i in range(2)]
            self.BT = [[_bt[i]] * 8 for i in range(2)]
            self.dummy = sb.alloc("dummy", [128, 8], F32)
            self.stage0()
            self.bar()
            if "1" in self.stages:
                self.stage1()
                self.bar()
            sb.push()
            self.attT = sb.alloc("attT", [128, 8, NT], BF16)
            self.oT = sb.alloc("oT", [128, 8, NT], BF16)
            self.B_attT = [Buf("attT%d" % h) for h in range(8)]
            self.B_oT = [Buf("oT%d" % t) for t in range(16)]
            if "2" in self.stages:
                self.stage2()
                self.bar()
            if "3" in self.stages:
                self.stage3()
                self.bar()
            if "4" in self.stages:
                self.stage4a()
                self.bar()
            sb.pop()
            if "4" in self.stages:
                self.stage4b()
                self.bar()
            if "5" in self.stages:
                self.stage5()
            allb = [b for b in self.S.dma_bufs]
            S.op("sp", lambda e: e.nop(), [], allb)
            S.emit(st)
        return nc

    def bar(self):
        d = self.dummy
        self.S.barrier(lambda e: e.memset(d[:], 0.0))

    def sB(self, name, key):
        k = (name, key)
        if k not in self.scrB:
            self.scrB[k] = Buf("%s%s" % (name, key))
        return self.scrB[k]

    def stage0(self):
        S, sb, I = self.S, self.sb, self.I
        cbs = {"sp": [Buf("c%d" % i) for i in range(4)], "pool": [Buf("cp%d" % i) for i in range(2)]}
        cb = cbs["sp"] + cbs["pool"]
        self.B_c0 = Buf("constall")
        ci = [0]

        def cload(q, dst, src, **kw):
            lst = cbs[q]
            b = lst[ci[0] % len(lst)]
            ci[0] += 1
            self.dma(q, dst, src, [], [b], b, **kw)

        def A(name, shape, dt=F32):
            t = sb.alloc(name, shape, dt)
            return t

        self.identb = A("identb", [128, 128], BF16)
        self.ident32 = A("ident32", [128, 128])
        self.rows = A("rows", [48, 128])
        self.cm = A("cm", [128, 512], BF16)
        self.cpen = A("cpen", [128, 256])
        self.selc = A("selc", [16, 2048], BF16)
        self.wall = A("wall", [128, 136])
        self.wl = A("wl", [128, 128])
        self.bd = A("bd", [128, 128])
        self.epsc = A("epsc", [128, 1])
        self.pm = A("pm", [128, 1])
        self.ppen = A("ppen", [128, 1])
        self.g1c = A("g1c", [128, 16])
        self.g2c = A("g2c", [128, 16])
        self.ccol = A("ccol", [128, 16])
        self.cact = A("cact", [128, 16], BF16)
        self.modc = A("modc", [128, 6, 16])
        self.a1 = A("a1", [128, 16])
        self.a2 = A("a2", [128, 16])
        self.gt1b = A("gt1b", [128, D])
        self.gt2b = A("gt2b", [128, D])
        self.fgb = A("fgb", [128, D])
        self.lb = A("lb", [128, 1024])
        self.oml = A("oml", [128, 1024])
        self.rngb = A("rngb", [128, 128])
        self.wr = A("wr", [128, 16, 36], BF16)
        self.br = A("br", [128, 36])
        self.wfull = A("wfull", [128, 16, 32])
        self.slt = A("slt", [128, 128], BF16)
        self.ones = A("ones", [128, 128], BF16)
        self.ebase = A("ebase", [128, 32])
        self.tot = A("tot", [128, 32])
        self.desti = A("desti", [128, 16, 2], I32)
        self.wk = A("wk", [128, 16, 2])
        self.B_tot = Buf("tot")
        self.B_ind = Buf("ind")
        self.six = [A("six", [128, 2], I32) for _ in range(2)]
        self.gix = [A("gix", [128, 2], I32) for _ in range(2)]
        self.B_six = [[Buf("six%d_%d" % (i, k)) for k in range(2)] for i in range(2)]
        self.B_gix = [[Buf("gix%d_%d" % (i, k)) for k in range(2)] for i in range(2)]
        self.B_dest = [Buf("dest%d" % t) for t in range(16)]
        self.B_wfull = [Buf("wfull%d" % t) for t in range(16)]
        cload("pool", self.identb[:], I["k_ident"])
        cload("sp", self.ident32[:], I["k_ident"])
        cload("pool", self.cm[:], I["k_cm"])
        cload("sp", self.cpen[:], I["k_cpen"])
        cload("pool", self.selc[:], I["k_sel"])
        cload("sp", self.wall[:], I["k_wall"])
        cload("sp", self.wl[:], I["k_wl"])
        cload("sp", self.bd[:], I["k_bd"])
        cload("sp", self.pm[:], I["pm"])
        cload("sp", self.rows[0:16, :], I["norm1_g"].rearrange("(k p) -> k p", p=128))
        cload("sp", self.rows[16:32, :], I["norm2_g"].rearrange("(k p) -> k p", p=128))
        cload("sp", self.rows[32:48, :], I["c"].rearrange("(k p) -> k p", p=128))
        cload("sp", self.fgb[:], I["final_g"].partition_broadcast(128))
        cload("sp", self.lb[:], I["r_lower"][0, :].partition_broadcast(128))
        cload("sp", self.oml[:], I["r_lower"][1, :].partition_broadcast(128))
        cload("sp", self.rngb[:], I["r_norm_g"].partition_broadcast(128))
        cload("pool", self.wr[:, :, 0:4], I["w_rg"].rearrange("(k p) n -> p k n", p=128))
        cload("pool", self.wr[:, :, 4:36], I["w_re"].rearrange("(k p) n -> p k n", p=128))
        cload("pool", self.slt[:], I["k_slt"])
        cload("pool", self.ones[:], I["k_ones"])
        cload("sp", self.ebase[:], I["k_ebase"])
        cload("sp", self.br[:, 0:4], I["b_rg"].partition_broadcast(128))
        cload("sp", self.br[:, 4:36], I["b_re"].partition_broadcast(128))
        cs = Buf("cs")
        S.op("dve", lambda e: e.memset(self.epsc[:], EPS), [], [cs])
        S.op("pe", lambda e: e.transpose(out=self.F[2][:, 0:48], in_=self.rows[:, :], identity=self.ident32[0:48, 0:48]), cb, [self.BF[2]])
        self.cp("dve", self.g1c[:], self.F[2][:, 0:16], [self.BF[2]], [cs])
        self.cp("dve", self.g2c[:], self.F[2][:, 16:32], [self.BF[2]], [cs])
        self.cp("dve", self.ccol[:], self.F[2][:, 32:48], [self.BF[2]], [cs])
        S.op("dve", lambda e: e.memset(self.tot[:], 0.0), [], [self.B_tot])
        self.ts(self.ppen[:], self.pm[:], -1.0, -NEGBIG, ALU.add, ALU.mult, cb, [cs])
        self.tt(self.lb[:], self.lb[:], self.oml[:], ALU.subtract, cb, [cs])
        self.act(self.lb[:], self.lb[:], AF.Sigmoid, [cs], [cs])
        self.ts(self.oml[:], self.lb[:], -1.0, 1.0, ALU.mult, ALU.add, [cs], [cs])
        self.act(self.cact[:], self.ccol[:], AF.Silu, [cs], [cs])
        sb.push()
        MOD = self.dram["MOD"]
        wsl = [sb.alloc("wsl0", [128, KC, 512], BF16) for _ in range(3)]
        Bw = [self.dbuf("wsl", "pool") for _ in range(3)]
        bsl = [sb.alloc("bsl", [1, 512], F32) for _ in range(2)]
        Bb = [self.dbuf("bsl") for _ in range(2)]
        msl = [sb.alloc("msl", [1, 512], F32) for _ in range(2)]
        Bm = [self.dbuf("msl") for _ in range(2)]
        wsrc = I["w_ada"].rearrange("(k p) n -> p k n", p=128)
        Bmod = Buf("MOD")
        for j in range(24):
            w = wsl[j % 3]
            bw = Bw[j % 3]
            self.dma("pool", w[:], wsrc[:, :, j * 512:(j + 1) * 512], [], [bw], bw)
            bb = Bb[j % 2]
            self.dma("sp", bsl[j % 2][:], I["b_ada"][j * 512:(j + 1) * 512].rearrange("(o n) -> o n", o=1), [], [bb], bb)
            pf = self.F[j % 2]
            bpf = self.BF[j % 2]
            for kc in range(KC):
                self.mm(pf[0:1, :], self.cact[:, kc:kc + 1], w[:, kc, :], kc == 0, kc == KC - 1, [bw, cs], [bpf])
            bm = Bm[j % 2]
            self.tt(msl[j % 2][:], pf[0:1, :], bsl[j % 2][:], ALU.add, [bpf, bb], [bm])
            self.dma("sp", MOD[j * 512:(j + 1) * 512].rearrange("(o n) -> o n", o=1), msl[j % 2][:], [bm], [Bmod], bm)
        sb.pop()
        self.release(Bw + Bb + Bm)
        bmc = self.dbuf("modc")
        modr = sb.alloc("modr", [96, 128], F32)
        self.dma("sp", modr[:], MOD.rearrange("(r p) -> r p", p=128), [Bmod], [bmc], bmc)
        S.op("pe", lambda e: e.transpose(out=self.F[3][:, 0:96], in_=modr[:, :], identity=self.ident32[0:96, 0:96]), [bmc] + cb, [self.BF[3]])
        self.cp("dve", self.modc[:].rearrange("p s k -> p (s k)"), self.F[3][:, 0:96], [self.BF[3]], [cs])
        bg1 = self.dbuf("gt1b")
        self.dma("sp", self.gt1b[:], MOD[2 * D:3 * D].partition_broadcast(128), [Bmod], [bg1], bg1)
        bg2 = self.dbuf("gt2b")
        self.dma("sp", self.gt2b[:], MOD[5 * D:6 * D].partition_broadcast(128), [Bmod], [bg2], bg2)
        self.stt(self.a1[:], self.modc[:, 1, :], 1.0, self.g1c[:], ALU.add, ALU.mult, [cs], [cs])
        self.stt(self.a2[:], self.modc[:, 4, :], 1.0, self.g2c[:], ALU.add, ALU.mult, [cs], [cs])
        S.op("dve", lambda e: e.memset(self.epsc[:], EPS), cb + [cs, bmc, bg1, bg2], [self.B_c0])
        self.B_c0.const = True
        self.B_modc = bmc

    def norm_to_hT(self, xt, bx, hT_dst, bh, a_col, sh_col, ring):
        junk, ssq, tmp, rs, xn, bj, bs, bxn = ring
        import os
        lvl = int(os.environ.get("N1", "9"))
        self.act(junk[:], xt, AF.Square, [bx], [bj, bs], accum=ssq[:, 0:1])
        if lvl < 2:
            return
        self.rstd(rs[:, 0:1], ssq[:, 0:1], float(D), [bs], [bs, bj], tmp[:, 0:1])
        if lvl < 3:
            return
        self.ts(xn[:], xt, rs[:, 0:1], None, ALU.mult, None, [bx, bs], [bxn])
        if lvl < 4:
            return
        for kc in range(KC):
            tb, ts_ = kc // 8, kc % 8
            pt = self.T[tb][:, ts_ * 128:(ts_ + 1) * 128]
            bt = self.BT[tb][ts_]
            self.tr(pt, xn[:, kc * 128:(kc + 1) * 128], [bxn], [bt])
            if os.environ.get("NOEV"):
                continue
            evm = os.environ.get("EVM", "")
            if evm == "f":
                self.act(hT_dst[:, kc, :], pt, AF.Identity, [bt, self.B_c0], [bh], scale=2.0)
            elif evm == "s":
                self.act(hT_dst[:, kc, :], pt, AF.Identity, [bt, self.B_c0], [bh], scale=a_col[:, kc:kc + 1])
            elif evm == "j":
                self.cp("dve", junk[:, 0:128], pt, [bt, self.B_c0], [bh])
            elif evm == "d":
                self.cp("dve", hT_dst[:, kc, :], pt, [bt, self.B_c0], [bh])
            elif evm == "c":
                self.act(hT_dst[:, kc, :], pt, AF.Copy, [bt, self.B_c0], [bh])
            elif tb == 0 or lvl < 5:
                self.act(hT_dst[:, kc, :], pt, AF.Identity, [bt, self.B_c0], [bh], scale=a_col[:, kc:kc + 1], bias=sh_col[:, kc:kc + 1])
            else:
                self.ts(hT_dst[:, kc, :], pt, a_col[:, kc:kc + 1], sh_col[:, kc:kc + 1], ALU.mult, ALU.add, [bt, self.B_c0], [bh])

    def norm_ring(self, sb):
        junk = sb.alloc("junk", [128, D], BF16)
        ssq = sb.alloc("ssq", [128, 1], F32)
        tmp = sb.alloc("tmp", [128, 1], F32)
        rs = sb.alloc("rs", [128, 1], F32)
        xn = sb.alloc("xn", [128, D], BF16)
        return (junk, ssq, tmp, rs, xn, Buf("junk"), Buf("ssq"), Buf("xn"))

    def stage1(self):
        S, sb, I = self.S, self.sb, self.I
        sb.push()
        G = 1024
        hT = [sb.alloc("hT", [128, KC, G], BF16) for _ in range(2)]
        BhT = [[Buf("hT%d_%d" % (s, t)) for t in range(8)] for s in range(2)]
        wsl = [sb.alloc("wsl", [128, KC, 512], BF16) for _ in range(3)]
        Bw = [self.dbuf("wsl", "pool") for _ in range(3)]
        xs = [sb.alloc("xs", [128, D], F32) for _ in range(2)]
        Bx = [self.dbuf("xs") for _ in range(2)]
        rings = [self.norm_ring(sb) for _ in range(2)]
        stg = []
        stgb = []
        for _ in range(4):
            sb.push()
            stgb.append(sb.alloc("stgb", [128, 512], BF16))
            sb.pop()
            stg.append(sb.alloc("stg", [128, 512], F32))
        Bs = [self.dbuf("stg") for _ in range(4)]
        wsrc = I["w_in"].rearrange("(k p) n -> p k n", p=128)
        D_ = self.dram
        zt = sb.alloc("zt", [128, D], BF16)
        Bz = self.dbuf("zt")
        S.op("dve", lambda e: e.memset(zt[:], 0.0), [], [Bz])
        for i in range(NR // 128):
            self.dma("sp", D_["XD"][i * 128:(i + 1) * 128, :], zt[:], [Bz], [Buf("z")], Bz)
        groups = [("p", 0), ("p", 1), ("m", 0), ("m", 1)]
        pblocks = [2, 3, 4, 5, 8, 9, 10, 11]
        import os
        if os.environ.get("S1G"):
            groups = groups[:int(os.environ["S1G"])]
        if os.environ.get("S1B"):
            pblocks = pblocks[:int(os.environ["S1B"])]
        wi = [0]
        si = [0]
        xi = [0]
        for gi, (kind, gidx) in enumerate(groups):
            slot = gi % 2
            src = I["xp"] if kind == "p" else I["xm"]
            ltok0 = gidx * G + (0 if kind == "p" else NT)
            mtok0 = gidx * G
            for t in range(int(os.environ.get("S1T", "8"))):
                xsl = xs[xi[0] % 2]
                bx = Bx[xi[0] % 2]
                ring = rings[xi[0] % 2]
                xi[0] += 1
                r0 = gidx * G + t * 128
                self.dma("sp", xsl[:], src[r0:r0 + 128, :], [], [bx], bx)
                self.norm_to_hT(xsl[:], bx, hT[slot][:, :, t * 128:(t + 1) * 128], BhT[slot][t], self.a1, self.modc[:, 0, :], ring)
            blocks = pblocks if kind == "p" else list(range(22))
            for blk in blocks:
                w = wsl[wi[0] % 3]
                bw = Bw[wi[0] % 3]
                wi[0] += 1
                self.dma("pool", w[:], wsrc[:, :, blk * 512:(blk + 1) * 512], [], [bw], bw)
                fm = blk in (0, 1, 2, 3, 6, 7) or blk >= 14
                for u in range(8):
                    fb = u % 4
                    pf = self.F[fb]
                    bpf = self.BF[fb]
                    if fm:
                        cs_, th = u // 2, u % 2
                        for kc in range(KC):
                            self.mm(pf[:, :], w[:, kc, cs_ * 128:(cs_ + 1) * 128], hT[slot][:, kc, th * 512:(th + 1) * 512],
                                    kc == 0, kc == KC - 1, [bw] + BhT[slot][th * 4:(th + 1) * 4], [bpf])
                    else:
                        for kc in range(KC):
                            self.mm(pf[:, :], hT[slot][:, kc, u * 128:(u + 1) * 128], w[:, kc, :],
                                    kc == 0, kc == KC - 1, [bw, BhT[slot][u]], [bpf])
                    sg = stg[si[0] % 4]
                    sgb = stgb[si[0] % 4]
                    bs = Bs[si[0] % 4]
                    si[0] += 1
                    if fm:
                        if blk < 2:
                            dst = D_["QT"][blk * 4 + cs_, :, mtok0 + th * 512: mtok0 + (th + 1) * 512]
                            key = ("QT", blk * 4 + cs_)
                            fn = AF.Copy
                        elif blk < 4:
                            dst = D_["KT"][(blk - 2) * 4 + cs_, :, ltok0 + th * 512: ltok0 + (th + 1) * 512]
                            key = ("KT", (blk - 2) * 4 + cs_)
                            fn = AF.Copy
                        elif blk < 8:
                            dst = D_["RQT"][(blk - 6) * 4 + cs_, :, mtok0 + th * 512: mtok0 + (th + 1) * 512]
                            key = ("RQT", (mtok0 + th * 512) // 128)
                            fn = AF.Copy
                        elif blk < 18:
                            dst = D_["GAT"][(blk - 14) * 4 + cs_, :, mtok0 + th * 512: mtok0 + (th + 1) * 512]
                            key = ("GAT", (blk - 14) * 4 + cs_)
                            fn = AF.Sigmoid
                        else:
                            dst = D_["GRT"][(blk - 18) * 4 + cs_, :, mtok0 + th * 512: mtok0 + (th + 1) * 512]
                            key = ("GRT", (blk - 18) * 4 + cs_)
                            fn = AF.Sigmoid
                        odt = BF16
                    else:
                        if blk < 6:
                            dst = D_["V"][ltok0 + u * 128: ltok0 + (u + 1) * 128, (blk - 4) * 512:(blk - 3) * 512]
                            key = ("V", (blk - 4) * 4)
                            fn, odt = AF.Copy, BF16
                        elif blk < 10:
                            dst = D_["RF"][ltok0 + u * 128: ltok0 + (u + 1) * 128, (blk - 8) * 512:(blk - 7) * 512]
                            key = ("RF", (ltok0 + u * 128) // 128)
                            fn, odt = AF.Copy, F32
                        elif blk < 12:
                            dst = D_["RI"][ltok0 + u * 128: ltok0 + (u + 1) * 128, (blk - 10) * 512:(blk - 9) * 512]
                            key = ("RI", (ltok0 + u * 128) // 128)
                            fn, odt = AF.Copy, BF16
                        else:
                            dst = D_["ROG"][mtok0 + u * 128: mtok0 + (u + 1) * 128, (blk - 12) * 512:(blk - 11) * 512]
                            key = ("ROG", (mtok0 + u * 128) // 128)
                            fn, odt = AF.Silu, BF16
                    so = sg[:] if odt == F32 else sgb[:]
                    if fn == AF.Copy and (u % 2 == 1):
                        self.cp("dve", so, pf[:, :], [bpf], [bs])
                    else:
                        self.act(so, pf[:, :], fn, [bpf], [bs])
                    wl_ = self.scr_w.setdefault(key, [])
                    tok = Buf("w")
                    wl_.append(tok)
                    self.dma("sp", dst, so, [bs], [tok], bs)
        sb.pop()
        self.release(Bw + Bx + Bs + [Bz])

    def scr_reads(self, name, keys):
        out = []
        for k in keys:
            out.extend(self.scr_w.get((name, k), []))
        return out

    def stage2(self):
        S, sb = self.S, self.sb
        D_ = self.dram
        sb.push()
        QTh = [sb.alloc("QTh", [128, NT], BF16) for _ in range(2)]
        KTh = [sb.alloc("KTh", [128, 2 * NT], BF16) for _ in range(2)]
        Vh = [sb.alloc("Vh", [128, 32, 129], BF16) for _ in range(2)]
        Bq = [self.dbuf("q") for _ in range(2)]
        Bk = [self.dbuf("k") for _ in range(2)]
        Bv = [self.dbuf("v") for _ in range(2)]
        km32 = sb.alloc("km32", [128, 16], F32)
        kmb = sb.alloc("kmb", [128, 16], BF16)
        gm = sb.alloc("gm", [128, 16, 16], F32)
        top8 = sb.alloc("top8", [128, 16, 8], F32)
        pen = sb.alloc("pen", [128, 16, 16], F32)
        penb = sb.alloc("penb", [128, 256], BF16)
        penT = sb.alloc("penT", [16, NT], BF16)
        Bkm, Bgm, Bpen, BpenT = Buf("km"), Buf("gm"), Buf("pen"), Buf("penT")
        pT = [sb.alloc("pT", [128, 256], BF16) for _ in range(4)]
        BpT = [Buf("pT%d" % i) for i in range(4)]
        rsum = [sb.alloc("rsum", [128, 1], F32) for _ in range(2)]
        atts = [sb.alloc("atts", [128, 128], BF16) for _ in range(2)]
        Brs = [Buf("rsum%d" % i) for i in range(2)]
        Bat = [Buf("atts%d" % i) for i in range(2)]
        scale = 1.0 / np.sqrt(128.0)
        pi = [0]
        ei = [0]
        for sl in range(2):
            S.op("dve", lambda e, t=Vh[sl]: e.memset(t[:, :, 128:129], 1.0), [], [Bv[sl]])
        for h in range(8):
            sl = h % 2
            q, k, v = QTh[sl], KTh[sl], Vh[sl]
            bq, bk, bv = Bq[sl], Bk[sl], Bv[sl]
            self.dma("sp", q[:], D_["QT"][h], self.scr_reads("QT", [h]), [bq], bq)
            self.dma("sp", k[:], D_["KT"][h], self.scr_reads("KT", [h]), [bk], bk)
            self.dma("sp", v[:, :, 0:128], D_["V"].rearrange("(t p) c -> p t c", p=128)[:, :, h * 128:(h + 1) * 128],
                     self.scr_reads("V", [(h // 4) * 4]), [bv], bv)
            S.op("dve", lambda e, k=k: e.tensor_reduce(out=km32[:], in_=k[:].rearrange("p (b l) -> p b l", l=256), axis=AX.X, op=ALU.add),
                 [bk], [Bkm])
            self.ts(kmb[:], km32[:], 1.0 / 256, None, ALU.mult, None, [Bkm], [Bkm])
            pg = self.F[5]
            bpg = self.BF[5]
            for qt in range(16):
                self.mm(pg[:, qt * 16:(qt + 1) * 16], q[:, qt * 128:(qt + 1) * 128], kmb[:, :], True, True, [bq, Bkm], [bpg])
            gmf = gm[:].rearrange("p a b -> p (a b)")
            self.tt(gmf, pg[:, 0:256], self.cpen[:], ALU.add, [bpg, self.B_c0], [Bgm])
            self.ts(gm[:, :, 0:8], gm[:, :, 0:8], self.ppen[:, 0:1], None, ALU.add, None, [Bgm, self.B_c0], [Bgm])
            for qt in range(16):
                S.op("dve", lambda e, qt=qt: e.max(out=top8[:, qt, :], in_=gm[:, qt, :]), [Bgm], [Bpen])
            for qt in range(16):
                self.ts(pen[:, qt, :], gm[:, qt, :], top8[:, qt, 2:3], None, ALU.is_ge, None, [Bgm, Bpen], [Bpen])
            penf = pen[:].rearrange("p a b -> p (a b)")
            self.ts(penf, penf, -1.0, -NEG, ALU.add, ALU.mult, [Bpen], [Bpen])
            self.ts(pen[:, :, 0:8], pen[:, :, 0:8], self.ppen[:, 0:1], None, ALU.add, None, [Bpen, self.B_c0], [Bpen])
            self.cp("dve", penb[:], penf, [Bpen], [Bpen])
            for qt in range(16):
                tb, ts_ = qt // 8, qt % 8
                pt = self.T[tb][0:16, ts_ * 128:(ts_ + 1) * 128]
                bt = self.BT[tb][ts_]
                self.tr(pt, penb[:, qt * 16:(qt + 1) * 16], [Bpen], [bt])
                self.cp("act", penT[:, qt * 128:(qt + 1) * 128], pt, [bt], [BpenT])
            for qb in range(8):
                Bl = 8 + qb
                nkt = 2 * Bl + 2
                po = [self.F[3], self.F[4]]
                bpo = [self.BF[3], self.BF[4]]
                LAG = 2
                pend = []
                for kk in range(nkt + LAG):
                    if kk < nkt:
                        kt = kk
                        fb = kt % 3
                        ps = self.F[fb]
                        bps = self.BF[fb]
                        self.mm(ps[:, 0:256], k[:, kt * 128:(kt + 1) * 128], q[:, qb * 256:(qb + 1) * 256], True, False, [bk, bq], [bps])
                        if kt < 2 * Bl:
                            i = kt // 2
                            self.mm(ps[:, 0:256], self.selc[0:16, i * 128:(i + 1) * 128], penT[0:16, qb * 256:(qb + 1) * 256],
                                    False, True, [BpenT, self.B_c0], [bps])
                        else:
                            j = kt - 2 * Bl
                            self.mm(ps[:, 0:256], self.identb[:, :], self.cm[:, j * 256:(j + 1) * 256], False, True, [self.B_c0], [bps])
                        p = pT[pi[0] % 4]
                        bp = BpT[pi[0] % 4]
                        pi[0] += 1
                        self.act(p[:], ps[:, 0:256], AF.Exp, [bps], [bp], scale=scale)
                        pend.append((kt, p, bp))
                    if kk >= LAG:
                        kt, p, bp = pend.pop(0)
                        for qh in range(2):
                            self.mm(po[qh][:, 0:129], p[:, qh * 128:(qh + 1) * 128], v[:, kt, :], kt == 0, kt == nkt - 1, [bp, bv], [bpo[qh]])
                for qh in range(2):
                    e_ = ei[0] % 2
                    ei[0] += 1
                    S.op("dve", lambda e, o=rsum[e_], i=po[qh]: e.reciprocal(out=o[:, 0:1], in_=i[:, 128:129]), [bpo[qh]], [Brs[e_]])
                    self.act(atts[e_][:], po[qh][:, 0:128], AF.Copy, [bpo[qh], Brs[e_]], [Bat[e_]], scale=rsum[e_][:, 0:1])
                    qt = qb * 2 + qh
                    tb, ts_ = qt // 8, qt % 8
                    pt = self.T[tb][:, ts_ * 128:(ts_ + 1) * 128]
                    bt = self.BT[tb][ts_]
                    self.tr(pt, atts[e_][:], [Bat[e_]], [bt])
                    self.cp("dve", self.attT[:, h, qt * 128:(qt + 1) * 128], pt, [bt], [self.B_attT[h]])
            if self.debug:
                self.dma("sp", D_["ATT"][h], self.attT[:, h, :], [self.B_attT[h]], [Buf("x")], self.B_attT[h])
        sb.pop()
        self.release(Bq + Bk + Bv)

    def stage3(self):
        S, sb = self.S, self.sb
        D_ = self.dram
        sb.push()
        rf = [sb.alloc("rf", [128, 1024], F32) for _ in range(2)]
        ri = [sb.alloc("ri", [128, 1024], BF16) for _ in range(2)]
        rq = [sb.alloc("rq", [128, 8, 128], BF16) for _ in range(2)]
        rog = [sb.alloc("rog", [128, 1024], BF16) for _ in range(2)]
        Brf = [self.dbuf("rf") for _ in range(2)]
        Bri = [self.dbuf("ri") for _ in range(2)]
        Brq = [self.dbuf("rq") for _ in range(2)]
        Brog = [self.dbuf("rog") for _ in range(2)]
        logf = [sb.alloc("logf", [128, 1024], F32) for _ in range(2)]
        kin = [sb.alloc("kin", [128, 1024], F32) for _ in range(2)]
        Blf = [Buf("logf%d" % i) for i in range(2)]
        Bkin = [Buf("kin%d" % i) for i in range(2)]
        R = 4
        ebl = [sb.alloc("ebl", [128, 128], F32) for _ in range(R)]
        khat = [sb.alloc("khat", [128, 128], BF16) for _ in range(R)]
        eb8 = [sb.alloc("eb8", [128, 8], F32) for _ in range(R)]
        ebm = [sb.alloc("ebm", [128, 128], F32) for _ in range(R)]
        qtl = [sb.alloc("qtl", [128, 128], BF16) for _ in range(R)]
        enb = [sb.alloc("enb", [128, 128], F32) for _ in range(R)]
        ktl = [sb.alloc("ktl", [128, 128], BF16) for _ in range(R)]
        ktlT = [sb.alloc("ktlT", [128, 128], BF16) for _ in range(R)]
        khc = [sb.alloc("khc", [128, 4, 128], BF16) for _ in range(R)]
        qtlz = [sb.alloc("qtlz", [128, 4, 128], BF16) for _ in range(R)]
        Bkhc = [Buf("khc%d" % i) for i in range(R)]
        Bqz = [Buf("qtlz%d" % i) for i in range(R)]
        for i in range(R):
            S.op("dve", lambda e, t=qtlz[i]: e.memset(t[:], 0.0), [], [Bqz[i]])
        at = [sb.alloc("at", [128, 128], BF16) for _ in range(R)]
        Sb = [sb.alloc("Sb", [128, 4, 128], BF16) for _ in range(R)]
        ot = [sb.alloc("ot", [128, 128], F32) for _ in range(R)]
        of_ = [sb.alloc("of", [128, 128], BF16) for _ in range(R)]
        sq = [sb.alloc("sq", [128, 4], F32) for _ in range(R)]
        jk = [sb.alloc("jk", [128, 128], BF16) for _ in range(R)]
        names = "ebl khat eb8 ebm qtl enb ktl ktlT at ot of sq".split()
        Bn = {n: [Buf(n + str(i)) for i in range(R)] for n in names}
        BSb = [[Buf("Sb%d_%d" % (i, c)) for c in range(4)] for i in range(R)]
        St = sb.alloc("St", [128, 8, 128], F32)
        BSt = [Buf("St%d" % h) for h in range(8)]
        for h in range(8):
            S.op("dve", lambda e, h=h: e.memset(St[:, h, :], 0.0), [], [BSt[h]])
        F, BFs = self.F, self.BFs
        hi = [0]
        import os
        tl = list(range(32))
        if os.environ.get("S3T"):
            tl = [int(v) for v in os.environ["S3T"].split(",")]
        s3c = int(os.environ.get("S3C", "4"))
        s3m = int(os.environ.get("S3M", "9"))
        for tile in tl:
            main = tile >= 16
            mt = tile - 16
            sl = tile % 2
            self.dma("sp", rf[sl][:], D_["RF"][tile * 128:(tile + 1) * 128, :], self.scr_reads("RF", [tile]), [Brf[sl]], Brf[sl])
            self.dma("sp", ri[sl][:], D_["RI"][tile * 128:(tile + 1) * 128, :], self.scr_reads("RI", [tile]), [Bri[sl]], Bri[sl])
            if main:
                self.dma("sp", rq[sl][:], D_["RQT"][:, :, mt * 128:(mt + 1) * 128].rearrange("h p t -> p h t"),
                         self.scr_reads("RQT", [(mt // 4) * 4]), [Brq[sl]], Brq[sl])
                self.dma("sp", rog[sl][:], D_["ROG"][mt * 128:(mt + 1) * 128, :], self.scr_reads("ROG", [mt]), [Brog[sl]], Brog[sl])
            lf, kn = logf[sl], kin[sl]
            self.act(kn[:], rf[sl][:], AF.Sigmoid, [Brf[sl]], [Bkin[sl]])
            self.tt(kn[:], kn[:], self.oml[:], ALU.mult, [Bkin[sl], self.B_c0], [Bkin[sl]])
            self.tt(kn[:], kn[:], self.lb[:], ALU.add, [Bkin[sl], self.B_c0], [Bkin[sl]])
            self.act(lf[:], kn[:], AF.Ln, [Bkin[sl]], [Blf[sl]])
            self.ts(kn[:], kn[:], -1.0, 1.0, ALU.mult, ALU.add, [Bkin[sl]], [Bkin[sl]])
            def hbody(h, r, main=main, sl=sl, lf=lf, kn=kn, mt=mt):
                hs = slice(h * 128, (h + 1) * 128)
                s2 = h % 2
                c128 = slice(s2 * 128, (s2 + 1) * 128)
                bl_ps = F[0][:, c128]
                b8_ps = F[0][:, 256 + s2 * 8:256 + (s2 + 1) * 8]
                self.mm(bl_ps, self.wl[:, :], lf[:, hs], True, True, [Blf[sl], self.B_c0], [self.BF[0]])
                self.mm(b8_ps, lf[:, hs], self.wall[:, 128:136], True, True, [Blf[sl], self.B_c0], [self.BF[0]])
                if main:
                    self.mm(F[1][:, c128], lf[:, hs], self.wall[:, 0:128], True, True, [Blf[sl], self.B_c0], [self.BF[1]])
                    bm_ps = F[1][:, 256 + s2 * 128:256 + (s2 + 1) * 128]
                    self.mm(bm_ps, self.wall[:, 0:128], lf[:, hs], True, True, [Blf[sl], self.B_c0], [self.BF[1]])
                yield
                self.act(ebl[r][:], bl_ps, AF.Exp, [self.BF[0]], [Bn["ebl"][r]])
                self.act(eb8[r][:], b8_ps, AF.Exp, [self.BF[0]], [Bn["eb8"][r]])
                if main:
                    self.act(ebm[r][:], F[1][:, c128], AF.Exp, [self.BF[1]], [Bn["ebm"][r]])
                    self.act(enb[r][:], bm_ps, AF.Exp, [self.BF[1]], [Bn["enb"][r]], scale=-1.0)
                yield
                for c in range(4):
                    self.stt(khc[r][:, c, :], kn[:, hs], self.wall[:, 132 + c:133 + c], ebl[r][:], ALU.mult, ALU.mult,
                             [Bkin[sl], Bn["ebl"][r], self.B_c0], [Bkhc[r]])
                if main:
                    self.tt(ktl[r][:], kn[:, hs], enb[r][:], ALU.mult, [Bkin[sl], Bn["enb"][r]], [Bn["ktl"][r]])
                    self.tt(qtl[r][:], rq[sl][:, h, :], ebm[r][:], ALU.mult, [Brq[sl], Bn["ebm"][r]], [Bn["qtl"][r]])
                    for c in range(4):
                        cc = slice(32 * c, 32 * c + 32)
                        self.tt(qtlz[r][:, c, cc], rq[sl][:, h, cc], ebm[r][:, cc], ALU.mult, [Brq[sl], Bn["ebm"][r]], [Bqz[r]])
                yield
                pds = [F[5][:, (s2 * 2 + (c % 2)) * 128:(s2 * 2 + (c % 2) + 1) * 128] for c in range(4)]
                if main:
                    pt = self.T[0][:, c128]
                    self.tr(pt, ktl[r][:], [Bn["ktl"][r]], [self.BT[0][0]])
                    yield
                    self.cp("act", ktlT[r][:], pt, [self.BT[0][0]], [Bn["ktlT"][r]])
                    yield
                    self.mm(F[3][:, c128], ktlT[r][:], qtl[r][:], True, True, [Bn["ktlT"][r], Bn["qtl"][r]], [self.BF[3]])
                    yield
                    self.tt(at[r][:], F[3][:, c128], self.bd[:], ALU.mult, [self.BF[3], self.B_c0], [Bn["at"][r]])
                    yield
                pob = 4 if s2 == 0 else 2
                po = F[pob][:, 0:128]
                bpo = self.BF[pob]
                if main:
                    self.mm(po, at[r][:], ri[sl][:, hs], True, False, [Bn["at"][r], Bri[sl]], [bpo])
                for c in range(4):
                    self.mm(pds[c], khc[r][:, c, :], ri[sl][:, hs], True, True, [Bkhc[r], Bri[sl]], [self.BF[5]])
                    if main:
                        self.act(Sb[r][:, c, :], St[:, h, :], AF.Copy, [BSt[h], Bn["eb8"][r]], [BSb[r][c]], scale=eb8[r][:, c:c + 1])
                    yield
                    if main:
                        self.mm(po, qtlz[r][:, c, :], Sb[r][:, c, :], False, c == 3, [Bqz[r], BSb[r][c]], [bpo])
                    self.stt(St[:, h, :], St[:, h, :], eb8[r][:, 4 + c:5 + c], pds[c], ALU.mult, ALU.add, [BSt[h], Bn["eb8"][r], self.BF[5]], [BSt[h]])
                    yield
                if main:
                    self.act(jk[r][:], po, AF.Square, [bpo], [Bn["sq"][r]], accum=sq[r][:, 0:1])
                    yield
                    self.tt(ot[r][:], po, self.rngb[:], ALU.mult, [bpo, self.B_c0], [Bn["ot"][r]])
                    self.rstd(sq[r][:, 2:3], sq[r][:, 0:1], 128.0, [Bn["sq"][r]], [Bn["sq"][r], Bn["sq"][r]], sq[r][:, 1:2])
                    yield
                    self.stt(of_[r][:], ot[r][:], sq[r][:, 2:3], rog[sl][:, hs], ALU.mult, ALU.mult, [Bn["ot"][r], Bn["sq"][r], Brog[sl]], [Bn["of"][r]])
                    yield
                    pt = self.T[1][:, c128]
                    self.tr(pt, of_[r][:], [Bn["of"][r]], [self.BT[1][0]])
                    yield
                    self.cp("act", self.oT[:, h, mt * 128:(mt + 1) * 128], pt, [self.BT[1][0]], [self.B_oT[mt]])

            G = 2
            for h0 in range(0, 8, G):
                gens = []
                for h in range(h0, h0 + G):
                    gens.append(hbody(h, hi[0] % R))
                    hi[0] += 1
                while gens:
                    for g_ in list(gens):
                        try:
                            next(g_)
                        except StopIteration:
                            gens.remove(g_)
            if tile == 15:
                for h in range(8):
                    self.ts(St[:, h, :], St[:, h, :], self.pm[:, 0:1], None, ALU.mult, None, [BSt[h], self.B_c0], [BSt[h]])
        if self.debug:
            for h in range(8):
                self.dma("sp", D_["OT"][h], self.oT[:, h, :], self.B_oT, [Buf("x")], self.B_oT[0])
        sb.pop()
        self.release(Brf + Bri + Brq + Brog)

    def stage4a(self):
        S, sb, I = self.S, self.sb, self.I
        D_ = self.dram
        sb.push()
        wa = [sb.alloc("wa", [128, 8, 128], BF16) for _ in range(2)]
        wr_ = [sb.alloc("wr_", [128, 8, 128], BF16) for _ in range(2)]
        sga = [sb.alloc("sga", [128, NT], BF16) for _ in range(2)]
        sgr = [sb.alloc("sgr", [128, NT], BF16) for _ in range(2)]
        mT = [sb.alloc("mT", [128, NT], BF16) for _ in range(2)]
        t1 = [sb.alloc("t1", [128, 512], F32) for _ in range(2)]
        t2 = [sb.alloc("t2", [128, 512], F32) for _ in range(2)]
        Bwa = [self.dbuf("wa", "pool") for _ in range(2)]
        Bwr = [self.dbuf("wr", "pool") for _ in range(2)]
        Bsa = [self.dbuf("sga") for _ in range(2)]
        Bsr = [self.dbuf("sgr") for _ in range(2)]
        BmT = [self.dbuf("mT") for _ in range(2)]
        Bt1 = [Buf("t1_%d" % i) for i in range(2)]
        Bt2 = [Buf("t2_%d" % i) for i in range(2)]
        wua = I["w_up_a"].rearrange("(k p) n -> p k n", p=128)
        wur = I["w_up_r"].rearrange("(k p) n -> p k n", p=128)
        ti = [0]
        for ch in range(16):
            sl = ch % 2
            self.dma("pool", wa[sl][:], wua[:, :, ch * 128:(ch + 1) * 128], [], [Bwa[sl]], Bwa[sl])
            self.dma("pool", wr_[sl][:], wur[:, :, ch * 128:(ch + 1) * 128], [], [Bwr[sl]], Bwr[sl])
            self.dma("sp", sga[sl][:], D_["GAT"][ch], self.scr_reads("GAT", [ch]), [Bsa[sl]], Bsa[sl])
            self.dma("sp", sgr[sl][:], D_["GRT"][ch], self.scr_reads("GRT", [ch]), [Bsr[sl]], Bsr[sl])
            for g in range(4):
                gs = slice(g * 512, (g + 1) * 512)
                fa, fr = (g % 2) * 2, (g % 2) * 2 + 1
                for wc in range(8):
                    self.mm(self.F[fa][:, :], wa[sl][:, wc, :], self.attT[:, wc, gs], wc == 0, wc == 7, [Bwa[sl], self.B_attT[wc]], [self.BF[fa]])
                for wc in range(8):
                    self.mm(self.F[fr][:, :], wr_[sl][:, wc, :], self.oT[:, wc, gs], wc == 0, wc == 7,
                            [Bwr[sl]] + self.B_oT[g * 4:(g + 1) * 4], [self.BF[fr]])
                i = ti[0] % 2
                ti[0] += 1
                self.tt(t1[i][:], self.F[fa][:, :], sga[sl][:, gs], ALU.mult, [self.BF[fa], Bsa[sl]], [Bt1[i]])
                self.tt(t2[i][:], self.F[fr][:, :], sgr[sl][:, gs], ALU.mult, [self.BF[fr], Bsr[sl]], [Bt2[i]], eng="dve")
                self.tt(mT[sl][:, gs], t1[i][:], t2[i][:], ALU.add, [Bt1[i], Bt2[i]], [BmT[sl]], eng="dve")
            tok = Buf("w")
            self.scr_w.setdefault(("MT", ch), []).append(tok)
            self.dma("sp", D_["MT"][ch], mT[sl][:], [BmT[sl]], [tok], BmT[sl])
        sb.pop()
        self.release(Bwa + Bwr + Bsa + Bsr + BmT)

    def stage4b(self):
        S, sb, I = self.S, self.sb, self.I
        D_ = self.dram
        sb.push()
        wo = sb.alloc("wo", [128, KC, D], BF16)
        Bwo = [self.dbuf("wo", "pool") for _ in range(4)]
        wsrc = I["w_out"].rearrange("(k p) n -> p k n", p=128)
        for i in range(4):
            self.dma("pool", wo[:, i * 4:(i + 1) * 4, :], wsrc[:, i * 4:(i + 1) * 4, :], [], [Bwo[i]], Bwo[i])
        mTg = [sb.alloc("mTg", [128, KC, 512], BF16) for _ in range(2)]
        Bmg = [self.dbuf("mTg") for _ in range(2)]
        xs = [sb.alloc("xs", [128, D], F32) for _ in range(2)]
        Bx = [self.dbuf("xs") for _ in range(2)]
        x1 = [sb.alloc("x1", [128, D], F32) for _ in range(2)]
        Bx1 = [self.dbuf("x1") for _ in range(2)]
        h2 = [sb.alloc("h2", [128, KC, 128], BF16) for _ in range(2)]
        Bh2 = [self.dbuf("h2") for _ in range(2)]
        rings = [self.norm_ring(sb) for _ in range(2)]
        tq = [sb.alloc("tq", [128, 512], F32) for _ in range(2)]
        Btq = [Buf("tq%d" % i) for i in range(2)]
        rt = sb.alloc("rt", [128, 160], F32)
        Brt = Buf("rt")
        r2 = sb.alloc("r2", [128, 112], F32)
        Br2 = Buf("r2")
        selb = sb.alloc("selb", [128, 32], BF16)
        Bselb = Buf("selb")
        mtsrc = D_["MT"].rearrange("k p t -> p k t")
        qi = [0]
        for g in range(4):
            sg = g % 2
            self.dma("sp", mTg[sg][:], mtsrc[:, :, g * 512:(g + 1) * 512], self.scr_reads("MT", range(16)), [Bmg[sg]], Bmg[sg])
            for tt_ in range(4):
                tile = g * 4 + tt_
                sl = tile % 2
                self.dma("sp", xs[sl][:], I["xm"][tile * 128:(tile + 1) * 128, :], [], [Bx[sl]], Bx[sl])
                for cg in range(4):
                    cs_ = slice(cg * 512, (cg + 1) * 512)
                    fb = cg % 4
                    for kc in range(KC):
                        self.mm(self.F[fb][:, :], mTg[sg][:, kc, tt_ * 128:(tt_ + 1) * 128], wo[:, kc, cs_], kc == 0, kc == KC - 1,
                                [Bmg[sg], Bwo[kc // 4]], [self.BF[fb]])
                    i = qi[0] % 2
                    qi[0] += 1
                    self.tt(tq[i][:], self.F[fb][:, :], self.gt1b[:, cs_], ALU.mult, [self.BF[fb], self.B_c0], [Btq[i]])
                    self.tt(x1[sl][:, cs_], tq[i][:], xs[sl][:, cs_], ALU.add, [Btq[i], Bx[sl]], [Bx1[sl]], eng="dve")
                tok = Buf("w")
                self.scr_w.setdefault(("X1", tile), []).append(tok)
                self.dma("sp", D_["X1"][tile * 128:(tile + 1) * 128, :], x1[sl][:], [Bx1[sl]], [tok], Bx1[sl])
                self.norm_to_hT(x1[sl][:], Bx1[sl], h2[sl][:, :, :], Bh2[sl], self.a2, self.modc[:, 3, :], rings[sl])
                tok = Buf("w")
                self.scr_w.setdefault(("H2T", tile), []).append(tok)
                self.dma("sp", D_["H2T"][:, :, tile * 128:(tile + 1) * 128].rearrange("k p t -> p k t"), h2[sl][:], [Bh2[sl]], [tok], Bh2[sl])
                pr = self.F[5][:, 0:36]
                bpr = self.BF[5]
                for kc in range(KC):
                    self.mm(pr, h2[sl][:, kc, :], self.wr[:, kc, :], kc == 0, kc == KC - 1, [Bh2[sl], self.B_c0], [bpr])
                lg = rt[:, 0:36]
                self.tt(lg, pr, self.br[:], ALU.add, [bpr, self.B_c0], [Brt])
                mx = rt[:, 36:37]
                S.op("dve", lambda e: e.tensor_reduce(out=rt[:, 36:37], in_=rt[:, 0:4], axis=AX.X, op=ALU.max), [Brt], [Brt])
                self.ts(rt[:, 37:38], mx, -1.0, None, ALU.mult, None, [Brt], [Brt])
                self.act(rt[:, 40:44], rt[:, 0:4], AF.Exp, [Brt], [Brt], bias=rt[:, 37:38], accum=rt[:, 38:39])
                self.ts(rt[:, 44:48], rt[:, 0:4], mx, None, ALU.is_ge, None, [Brt], [Brt])
                self.ts(rt[:, 44:48], rt[:, 44:48], -1.0, -NEGBIG, ALU.add, ALU.mult, [Brt], [Brt])
                for gi in range(4):
                    self.ts(rt[:, 48 + gi * 8:56 + gi * 8], rt[:, 4 + gi * 8:12 + gi * 8], rt[:, 44 + gi:45 + gi], None, ALU.add, None, [Brt], [Brt])
                S.op("dve", lambda e: e.max(out=rt[:, 80:88], in_=rt[:, 48:80]), [Brt], [Brt])
                self.ts(rt[:, 88:120], rt[:, 48:80], rt[:, 81:82], None, ALU.is_ge, None, [Brt], [Brt])
                self.ts(rt[:, 39:40], rt[:, 80:81], -1.0, None, ALU.mult, None, [Brt], [Brt])
                self.act(rt[:, 120:152], rt[:, 48:80], AF.Exp, [Brt], [Brt], bias=rt[:, 39:40])
                self.tt(rt[:, 120:152], rt[:, 120:152], rt[:, 88:120], ALU.mult, [Brt], [Brt])
                S.op("dve", lambda e: e.reduce_sum(out=rt[:, 152:153], in_=rt[:, 120:152], axis=AX.X), [Brt], [Brt])
                self.tt(rt[:, 152:153], rt[:, 152:153], rt[:, 38:39], ALU.mult, [Brt], [Brt])
                S.op("dve", lambda e: e.reciprocal(out=rt[:, 153:154], in_=rt[:, 152:153]), [Brt], [Brt])
                self.ts(self.wfull[:, tile, :], rt[:, 120:152], rt[:, 153:154], None, ALU.mult, None, [Brt], [self.B_wfull[tile]])
                self.cp("dve", selb[:], rt[:, 88:120], [Brt], [Bselb])
                pc = self.F[4]
                self.mm(pc[:, 0:32], self.slt[:, :], selb[:], True, True, [Bselb, self.B_c0], [self.BF[4]])
                self.mm(pc[:, 32:64], self.ones[:, :], selb[:], True, True, [Bselb, self.B_c0], [self.BF[4]])
                self.tt(r2[:, 0:32], pc[:, 0:32], self.tot[:], ALU.add, [self.BF[4], self.B_tot], [Br2])
                self.tt(self.tot[:], pc[:, 32:64], self.tot[:], ALU.add, [self.BF[4], self.B_tot], [self.B_tot])
                self.ts(r2[:, 0:32], r2[:, 0:32], float(CAP - 1), None, ALU.min, None, [Br2], [Br2])
                self.tt(r2[:, 0:32], r2[:, 0:32], self.ebase[:], ALU.add, [Br2, self.B_c0], [Br2])
                self.stt(r2[:, 32:64], r2[:, 0:32], 1.0e6, rt[:, 88:120], ALU.add, ALU.mult, [Br2, Brt], [Br2])
                S.op("dve", lambda e: e.max(out=r2[:, 64:72], in_=r2[:, 32:64]), [Br2], [Br2])
                self.ts(r2[:, 72:74], r2[:, 64:66], -1.0e6, None, ALU.add, None, [Br2], [Br2])
                self.cp("dve", self.desti[:, tile, :], r2[:, 72:74], [Br2], [self.B_dest[tile]])
                for k_ in range(2):
                    self.ts(r2[:, 80:112], r2[:, 32:64], r2[:, 64 + k_:65 + k_], None, ALU.is_equal, None, [Br2], [Br2])
                    self.tt(r2[:, 80:112], r2[:, 80:112], self.wfull[:, tile, :], ALU.mult, [Br2, self.B_wfull[tile]], [Br2])
                    S.op("dve", lambda e, k_=k_, tile=tile: e.reduce_sum(out=self.wk[:, tile, k_:k_ + 1], in_=r2[:, 80:112], axis=AX.X),
                         [Br2], [self.B_dest[tile]])
                xn_t, bxn_t = rings[sl][4], rings[sl][7]
                for k_ in range(2):
                    self.cp("dve", self.six[sl][:, k_:k_ + 1], self.desti[:, tile, k_:k_ + 1], [self.B_dest[tile]], [self.B_six[sl][k_]])
                    S.op("pool", lambda e, k_=k_, ix=self.six[sl], xn_t=xn_t: e.indirect_dma_start(
                        out=D_["XD"], out_offset=bass.IndirectOffsetOnAxis(ap=ix[:, k_:k_ + 1], axis=0),
                        in_=xn_t[:], in_offset=None),
                        [bxn_t, self.B_six[sl][k_]], [Buf("sc"), self.B_ind], dbuf=self.B_ind)
        if self.debug:
            self.dma("sp", D_["WF"], self.wfull[:].rearrange("p a b -> p (a b)"), self.B_wfull, [Buf("x")], self.B_wfull[0])
        sb.pop()
        self.release(Bwo + Bmg + Bx + Bx1 + Bh2)

    def stage5(self):
        S, sb, I = self.S, self.sb, self.I
        D_ = self.dram
        sb.push()
        w1 = [sb.alloc("w1", [128, KC, 512], BF16) for _ in range(2)]
        w3 = [sb.alloc("w3", [128, KC, 512], BF16) for _ in range(2)]
        w2 = sb.alloc("w2", [128, 4, D], BF16)
        Bw1 = [self.dbuf("w1", "pool") for _ in range(2)]
        Bw3 = [self.dbuf("w3", "pool") for _ in range(2)]
        Bw2 = self.dbuf("w2", "pool")
        nrt = CAP // 128
        xr = [sb.alloc("xr", [128, D], BF16) for _ in range(nrt)]
        Bxr = [self.dbuf("xr") for _ in range(nrt)]
        XT = [sb.alloc("XT", [128, KC, CAP], BF16) for _ in range(2)]
        BXT = [[Buf("XT%d_%d" % (i, j)) for j in range(nrt)] for i in range(2)]
        s1 = [sb.alloc("s1", [128, CAP], F32) for _ in range(2)]
        Bs1 = [Buf("s1_%d" % i) for i in range(2)]
        hid = [sb.alloc("hid", [128, 4, CAP], BF16) for _ in range(2)]
        Bhid = [[Buf("hid%d_%d" % (i, f)) for f in range(4)] for i in range(2)]
        ys = [sb.alloc("ys", [128, D], F32) for _ in range(2)]
        Bys = [self.dbuf("ys") for _ in range(2)]
        si = [0]
        yi = [0]

        def loads(ex):
            for r_ in range(nrt):
                row0 = ex * CAP + r_ * 128
                self.dma("sp", xr[r_][:], D_["XD"][row0:row0 + 128, :], [], [Bxr[r_]], Bxr[r_])

        def trans(ex):
            sl = ex % 2
            for r_ in range(nrt):
                for kc in range(KC):
                    tb, ts_ = kc // 8, kc % 8
                    pt = self.T[tb][:, ts_ * 128:(ts_ + 1) * 128]
                    bt = self.BT[tb][ts_]
                    self.tr(pt, xr[r_][:, kc * 128:(kc + 1) * 128], [Bxr[r_]], [bt])
                    dst = XT[sl][:, kc, r_ * 128:(r_ + 1) * 128]
                    if tb == 0:
                        self.act(dst, pt, AF.Identity, [bt, self.B_c0], [BXT[sl][r_]], scale=self.a2[:, kc:kc + 1], bias=self.modc[:, 3, kc:kc + 1])
                    else:
                        self.ts(dst, pt, self.a2[:, kc:kc + 1], self.modc[:, 3, kc:kc + 1], ALU.mult, ALU.add, [bt, self.B_c0], [BXT[sl][r_]])

        loads(0)
        trans(0)
        for ex in range(32):
            sl = ex % 2
            self.dma("pool", w1[sl][:], I["w1"][ex].rearrange("(k p) n -> p k n", p=128), [], [Bw1[sl]], Bw1[sl])
            self.dma("pool", w3[sl][:], I["w3"][ex].rearrange("(k p) n -> p k n", p=128), [], [Bw3[sl]], Bw3[sl])
            self.dma("pool", w2[:], I["w2"][ex].rearrange("(k p) n -> p k n", p=128), [], [Bw2], Bw2)
            if ex + 1 < 32:
                loads(ex + 1)
            for fc in range(4):
                fs = slice(fc * 128, (fc + 1) * 128)
                f1, f3 = (fc % 2) * 2, (fc % 2) * 2 + 1
                for kc in range(KC):
                    self.mm(self.F[f1][:, 0:CAP], w1[sl][:, kc, fs], XT[sl][:, kc, :], kc == 0, kc == KC - 1, [Bw1[sl]] + BXT[sl], [self.BF[f1]])
                for kc in range(KC):
                    self.mm(self.F[f3][:, 0:CAP], w3[sl][:, kc, fs], XT[sl][:, kc, :], kc == 0, kc == KC - 1, [Bw3[sl]] + BXT[sl], [self.BF[f3]])
                i = si[0] % 2
                si[0] += 1
                self.act(s1[i][:], self.F[f1][:, 0:CAP], AF.Silu, [self.BF[f1]], [Bs1[i]])
                self.tt(hid[sl][:, fc, :], s1[i][:], self.F[f3][:, 0:CAP], ALU.mult, [Bs1[i], self.BF[f3]], [Bhid[sl][fc]])
            if ex + 1 < 32:
                trans(ex + 1)
            for r_ in range(nrt):
                y_ = ys[yi[0] % 2]
                by = Bys[yi[0] % 2]
                yi[0] += 1
                for cg in range(4):
                    cs_ = slice(cg * 512, (cg + 1) * 512)
                    fb = 4 + (cg % 2)
                    for fc in range(4):
                        self.mm(self.F[fb][:, :], hid[sl][:, fc, r_ * 128:(r_ + 1) * 128], w2[:, fc, cs_], fc == 0, fc == 3,
                                [Bhid[sl][fc], Bw2], [self.BF[fb]])
                    if fb == 4:
                        self.cp("act", y_[:, cs_], self.F[fb][:, :], [self.BF[fb]], [by])
                    else:
                        self.cp("dve", y_[:, cs_], self.F[fb][:, :], [self.BF[fb]], [by])
                row0 = ex * CAP + r_ * 128
                for hf in range(2):
                    self.dma("sp", D_["YD%d" % hf][row0:row0 + 128, :], y_[:, hf * 1024:(hf + 1) * 1024], [by], [Buf("y")], by)
        sb.pop()
        self.release(Bw1 + Bw3 + [Bw2] + Bxr + Bys)
        self.bar()
        sb.push()
        xs = [sb.alloc("xs", [128, D], F32) for _ in range(2)]
        Bx = [self.dbuf("xs") for _ in range(2)]
        yg = [[[sb.alloc("yg", [128, 1024], F32) for _ in range(2)] for _ in range(2)] for _ in range(2)]
        Byg = [[[Buf("yg") for _ in range(2)] for _ in range(2)] for _ in range(2)]
        jk = sb.alloc("jk5", [128, D], BF16)
        st5 = sb.alloc("st5", [128, 4], F32)
        Bst = Buf("st5")
        for tile in range(16):
            sl = tile % 2
            self.dma("sp", xs[sl][:], D_["X1"][tile * 128:(tile + 1) * 128, :], self.scr_reads("X1", [tile]), [Bx[sl]], Bx[sl])
            for k_ in range(2):
                self.cp("dve", self.gix[sl][:, k_:k_ + 1], self.desti[:, tile, k_:k_ + 1], [self.B_dest[tile]], [self.B_gix[sl][k_]])
                for hf in range(2):
                    S.op("pool", lambda e, k_=k_, ix=self.gix[sl], dst=yg[sl][k_][hf], hf=hf: e.indirect_dma_start(
                        out=dst[:], out_offset=None, in_=D_["YD%d" % hf],
                        in_offset=bass.IndirectOffsetOnAxis(ap=ix[:, k_:k_ + 1], axis=0)),
                        [self.B_gix[sl][k_]], [Byg[sl][k_][hf], self.B_ind], dbuf=self.B_ind)
            for hf in range(2):
                a, b = yg[sl][0][hf], yg[sl][1][hf]
                ba, bb = Byg[sl][0][hf], Byg[sl][1][hf]
                hs_ = slice(hf * 1024, (hf + 1) * 1024)
                self.ts(a[:], a[:], self.wk[:, tile, 0:1], None, ALU.mult, None, [ba, self.B_dest[tile]], [ba])
                self.stt(a[:], b[:], self.wk[:, tile, 1:2], a[:], ALU.mult, ALU.add, [bb, ba, self.B_dest[tile]], [ba])
                self.tt(a[:], a[:], self.gt2b[:, hs_], ALU.mult, [ba, self.B_c0], [ba])
                self.tt(xs[sl][:, hs_], xs[sl][:, hs_], a[:], ALU.add, [Bx[sl], ba], [Bx[sl]])
            self.act(jk[:], xs[sl][:], AF.Square, [Bx[sl]], [Bst], accum=st5[:, 0:1])
            self.rstd(st5[:, 2:3], st5[:, 0:1], float(D), [Bst], [Bst, Bst], st5[:, 1:2])
            self.stt(xs[sl][:], xs[sl][:], st5[:, 2:3], self.fgb[:], ALU.mult, ALU.mult, [Bx[sl], Bst, self.B_c0], [Bx[sl]])
            self.dma("sp", self.out[tile * 128:(tile + 1) * 128, :], xs[sl][:], [Bx[sl]], [Buf("o")], Bx[sl])
        sb.pop()
        self.release(Bx)


def host_consts():
    bf = ml_dtypes.bfloat16
    c = {}
    c["k_ident"] = np.eye(128, dtype=np.float32)
    cm = np.zeros((128, 512), np.float32)
    for j in range(2):
        kp = np.arange(128)[:, None] + j * 128
        q = np.arange(256)[None, :]
        cm[:, j * 256:(j + 1) * 256] = np.where(kp <= q, 0.0, NEG)
    c["k_cm"] = cm
    cp = np.zeros((128, 16, 16), np.float32)
    for qt in range(16):
        cp[:, qt, 8 + qt // 2:] = NEGBIG
    c["k_cpen"] = cp.reshape(128, 256)
    sel = np.zeros((16, 16, 128), np.float32)
    for i in range(16):
        sel[i, i, :] = 1.0
    c["k_sel"] = sel.reshape(16, 2048)
    s = np.arange(128)[:, None]
    t = np.arange(128)[None, :]
    same = (s // 32) == (t // 32)
    mid = (t // 32) * 32 + 15
    wm = np.where(same & (s > mid) & (s <= t), 1.0, 0.0) - np.where(same & (s > t) & (s <= mid), 1.0, 0.0)
    wmid = np.zeros((128, 4), np.float32)
    wtot = np.zeros((128, 4), np.float32)
    for cc in range(4):
        sl = np.arange(128)
        wmid[:, cc] = ((sl // 32) == cc) & (sl <= cc * 32 + 15)
        wtot[:, cc] = (sl // 32) == cc
    c["k_wall"] = np.concatenate([wm, wmid, wtot], axis=1).astype(np.float32)
    c["k_wl"] = np.where(same & (s > t), 1.0, 0.0).astype(np.float32)
    c["k_bd"] = np.where(same & (s <= t), 1.0, 0.0).astype(np.float32)
    c["k_slt"] = np.where(s < t, 1.0, 0.0).astype(np.float32)
    c["k_ones"] = np.ones((128, 128), np.float32)
    c["k_ebase"] = np.tile((np.arange(32) * CAP).astype(np.float32)[None, :], (128, 1))
    return c


_NC_CACHE = {}


def get_nc(debug=False, stages="012345"):
    key = (debug, stages)
    if key not in _NC_CACHE:
        nc = bass.Bass("TRN2", target_bir_lowering=False)
        kb = K(nc, debug=debug, stages=stages)
        kb.build()
        _NC_CACHE[key] = (nc, kb)
    return _NC_CACHE[key]


def make_in_maps(inputs, cores=range(8)):
    f = lambda a: np.ascontiguousarray(np.asarray(a, dtype=np.float32))
    x = f(inputs["x"])
    c = f(inputs["c"])
    shared = {
        "norm1_g": f(inputs["norm1_g"])[0], "norm2_g": f(inputs["norm2_g"])[0], "final_g": f(inputs["final_g"]),
        "w_ada": f(inputs["w_ada"])[0], "b_ada": f(inputs["b_ada"])[0], "w_in": f(inputs["w_in"])[0],
        "r_lower": f(inputs["r_lower"]), "r_norm_g": f(inputs["r_norm_g"])[0],
        "w_up_a": f(inputs["w_up_a"])[0], "w_up_r": f(inputs["w_up_r"])[0], "w_out": f(inputs["w_out"])[0],
        "w_rg": f(inputs["w_rg"])[0], "b_rg": f(inputs["b_rg"])[0], "w_re": f(inputs["w_re"])[0], "b_re": f(inputs["b_re"])[0],
        "w1": f(inputs["w1"])[0], "w3": f(inputs["w3"])[0], "w2": f(inputs["w2"])[0],
    }
    shared.update(host_consts())
    maps = []
    for core in cores:
        b, half = core // 2, core % 2
        m = dict(shared)
        m["xm"] = np.ascontiguousarray(x[b, half * NT:(half + 1) * NT])
        m["xp"] = np.ascontiguousarray(x[b, 0:NT])
        m["c"] = np.ascontiguousarray(c[b])
        m["pm"] = np.full((128, 1), float(half), np.float32)
        maps.append(m)
    return maps


def kernel(**inputs):
    nc, _ = get_nc()
    maps = make_in_maps(inputs)
    res = run_bass_kernel_spmd(nc, maps, core_ids=list(range(8)))
    out = np.zeros((4, 4096, D), np.float32)
    for core in range(8):
        b, half = core // 2, core % 2
        out[b, half * NT:(half + 1) * NT] = np.asarray(res.results[core]["out"], dtype=np.float32)
    return out
```

```python
import contextlib
import numpy as np
import ml_dtypes
import concourse.bass as bass
import concourse.mybir as mybir
from concourse.bass_utils import run_bass_kernel_spmd

F32 = mybir.dt.float32
BF16 = mybir.dt.bfloat16
I32 = mybir.dt.int32
CAP = 512
NR = 32 * CAP
ALU = mybir.AluOpType
AF = mybir.ActivationFunctionType
AX = mybir.AxisListType

D = 2048
NT = 2048
KC = 16
EPS = 1e-6
NEG = -30000.0
NEGBIG = -1.0e30
ENGS = ("pe", "act", "dve", "pool", "sp")
SAME_ENGINE_SYNC = {"pe": False, "act": True, "dve": True, "pool": True, "sp": False}


class Buf:
    __slots__ = ("name", "last_w", "readers", "dsem", "dcount", "const", "last_dma", "excl")

    def __init__(self, name, const=False, excl=False):
        self.excl = excl
        self.name = name
        self.last_w = None
        self.readers = []
        self.dsem = None
        self.dcount = 0
        self.const = const
        self.last_dma = None


class Op:
    __slots__ = ("eng", "fn", "deps", "sig", "sem", "val", "dbuf")

    def __init__(self, eng, fn, dbuf=None):
        self.eng = eng
        self.fn = fn
        self.deps = []
        self.sig = dbuf is not None
        self.sem = None
        self.val = 0
        self.dbuf = dbuf


class Sched:
    def __init__(self, nc):
        self.nc = nc
        self.ops = {e: [] for e in ENGS}
        self.dma_bufs = []
        self.epoch = None
        self.all_ops = []

    def barrier(self, fn):
        o = Op("dve", fn, None)
        deps = {}
        for e in ENGS:
            if self.ops[e]:
                d = self.ops[e][-1]
                deps[id(d)] = d
        for b in self.dma_bufs:
            if b.last_dma is not None:
                deps[id(b.last_dma)] = b.last_dma
        o.deps = list(deps.values())
        self.ops["dve"].append(o)
        self.all_ops.append(o)
        self.epoch = o
        return o

    def op(self, eng, fn, reads=(), writes=(), dbuf=None, extra=()):
        o = Op(eng, fn, dbuf)
        deps = {}
        for d in extra:
            if d is not None:
                deps[id(d)] = d
        if self.epoch is not None:
            deps[id(self.epoch)] = self.epoch
        xr = [b for b in reads if b.excl]
        if xr:
            reads = [b for b in reads if not b.excl]
            writes = list(writes) + [b for b in xr if b not in writes]
        for b in reads:
            if b.last_w is not None:
                deps[id(b.last_w)] = b.last_w
        for b in writes:
            if b.last_w is not None:
                deps[id(b.last_w)] = b.last_w
            for r in b.readers:
                deps[id(r)] = r
        o.deps = list(deps.values())
        for b in writes:
            b.last_w = o
            b.readers = []
        for b in reads:
            if not b.const and b.last_w is not o:
                b.readers.append(o)
        if dbuf is not None:
            dbuf.last_dma = o
            if dbuf.dsem is None:
                dbuf.dsem = True
                self.dma_bufs.append(dbuf)
        self.ops[eng].append(o)
        self.all_ops.append(o)
        return o

    def emit(self, stack):
        nc = self.nc
        for e in ENGS:
            for o in self.ops[e]:
                keep = []
                for d in o.deps:
                    if d.dbuf is None and d.eng == o.eng and not SAME_ENGINE_SYNC[o.eng]:
                        continue
                    keep.append(d)
                    d.sig = True
                o.deps = keep
        esem = {}
        for e in ENGS:
            if any(o.sig and o.dbuf is None for o in self.ops[e]):
                esem[e] = stack.enter_context(nc.semaphore("s_" + e))
        for i, b in enumerate(self.dma_bufs):
            b.dsem = stack.enter_context(nc.semaphore("d%d" % i))
            b.dcount = 0
        cnt = {e: 0 for e in ENGS}
        for o in self.all_ops:
            if o.dbuf is not None:
                o.dbuf.dcount += 16
                o.sem = o.dbuf.dsem
                o.val = o.dbuf.dcount
            elif o.sig:
                cnt[o.eng] += 1
                o.sem = esem[o.eng]
                o.val = cnt[o.eng]
        block = stack.enter_context(nc.Block())
        handles = {"pe": block.tensor, "act": block.scalar, "dve": block.vector,
                   "pool": block.gpsimd, "sp": block.sync}
        for e in ENGS:
            ops = self.ops[e]
            if not ops:
                continue

            def body(eng, ops=ops):
                waited = {}
                for o in ops:
                    need = {}
                    for d in o.deps:
                        k = id(d.sem)
                        if d.val > need.get(k, (0, None))[0]:
                            need[k] = (d.val, d.sem)
                    for k, (v, sem) in need.items():
                        if waited.get(k, 0) >= v:
                            continue
                        waited[k] = v
                        eng.wait_ge(sem, v)
                    ins = o.fn(eng)
                    if o.sig:
                        ins.then_inc(o.sem, 16 if o.dbuf is not None else 1)

            handles[e](body)


class SbAlloc:
    def __init__(self, nc, base=17408, limit=229000):
        self.nc = nc
        self.top = base
        self.limit = limit
        self.n = 0
        self.marks = []

    def push(self):
        self.marks.append(self.top)

    def pop(self):
        self.top = self.marks.pop()

    def alloc(self, name, shape, dtype):
        sz = int(np.prod(shape[1:])) * (4 if dtype in (F32, I32) else 2)
        off = (self.top + 63) // 64 * 64
        assert off + sz <= self.limit, (name, off, sz, self.limit)
        self.top = off + sz
        self.n += 1
        return self.nc.alloc_sbuf_tensor_at("%s_%d" % (name, self.n), list(shape), dtype, offset=off)


class K:
    def __init__(self, nc, debug=False, stages="012345"):
        self.nc = nc
        self.S = Sched(nc)
        self.sb = SbAlloc(nc)
        self.debug = debug
        self.stages = stages
        self.dpool = {}
        self.bkind = {}
        self.dram = {}
        self.sbufs = {}
        self.scr_w = {}

    def din(self, name, shape, dt=F32):
        return self.nc.dram_tensor(name, list(shape), dt, kind="ExternalInput").ap()

    def dscr(self, name, shape, dt):
        kind = "ExternalOutput" if self.debug else "Internal"
        t = self.nc.dram_tensor(name, list(shape), dt, kind=kind).ap()
        self.dram[name] = t
        return t

    def dbuf(self, name, kind="sp"):
        pool = self.dpool.setdefault(kind, [])
        if pool:
            return pool.pop()
        b = Buf(name)
        self.bkind[id(b)] = kind
        return b

    def release(self, bufs):
        for b in bufs:
            self.dpool[self.bkind[id(b)]].append(b)

    def dma(self, q, out, in_, reads, writes, dbuf, **kw):
        k = self.bkind.setdefault(id(dbuf), q)
        assert k == q, (dbuf.name, k, q)
        return self.S.op(q, lambda e: e.dma_start(out=out, in_=in_, **kw), reads, writes, dbuf=dbuf)

    def mm(self, out, lhsT, rhs, start, stop, reads, writes, tp=None):
        if tp is not None:
            return self.S.op("pe", lambda e: e.matmul(out, lhsT=lhsT, rhs=rhs, start=start, stop=stop, tile_position=tp), reads, writes)
        return self.S.op("pe", lambda e: e.matmul(out, lhsT=lhsT, rhs=rhs, start=start, stop=stop), reads, writes)

    def tr(self, out, in_, reads, writes):
        idn = self.identb
        n = in_.shape[0]
        return self.S.op("pe", lambda e: e.transpose(out=out, in_=in_, identity=idn[0:n, 0:n]),
                         list(reads) + [self.B_c0], writes)

    def act(self, out, in_, func, reads, writes, scale=1.0, bias=None, accum=None):
        kw = {}
        if bias is not None:
            kw["bias"] = bias
        if accum is not None:
            kw["accum_out"] = accum
        return self.S.op("act", lambda e: e.activation(out=out, in_=in_, func=func, scale=scale, **kw), reads, writes)

    def ts(self, out, in0, s1, s2, op0, op1, reads, writes, eng="dve"):
        if s2 is None:
            return self.S.op(eng, lambda e: e.tensor_scalar(out=out, in0=in0, scalar1=s1, scalar2=None, op0=op0), reads, writes)
        return self.S.op(eng, lambda e: e.tensor_scalar(out=out, in0=in0, scalar1=s1, scalar2=s2, op0=op0, op1=op1), reads, writes)

    def tt(self, out, in0, in1, op, reads, writes, eng="dve"):
        return self.S.op(eng, lambda e: e.tensor_tensor(out=out, in0=in0, in1=in1, op=op), reads, writes)

    def stt(self, out, in0, scalar, in1, op0, op1, reads, writes, eng="dve"):
        return self.S.op(eng, lambda e: e.scalar_tensor_tensor(out=out, in0=in0, scalar=scalar, in1=in1, op0=op0, op1=op1), reads, writes)

    def cp(self, eng, out, in_, reads, writes):
        if eng == "act":
            return self.S.op("act", lambda e: e.copy(out=out, in_=in_), reads, writes)
        return self.S.op(eng, lambda e: e.tensor_copy(out=out, in_=in_), reads, writes)

    def rstd(self, out, ssq, n, reads, writes, tmp):
        eps = self.epsc
        self.act(tmp, ssq, AF.Sqrt, list(reads) + [self.B_c0], [writes[1]], scale=1.0 / n, bias=eps[:, 0:1])
        self.S.op("dve", lambda e: e.reciprocal(out=out, in_=tmp), [writes[1]], [writes[0]])

    def build(self):
        nc = self.nc
        sb = self.sb
        S = self.S
        I = {}
        I["xm"] = self.din("xm", [NT, D])
        I["xp"] = self.din("xp", [NT, D])
        I["c"] = self.din("c", [D])
        I["pm"] = self.din("pm", [128, 1])
        I["norm1_g"] = self.din("norm1_g", [D])
        I["norm2_g"] = self.din("norm2_g", [D])
        I["final_g"] = self.din("final_g", [D])
        I["w_ada"] = self.din("w_ada", [D, 6 * D])
        I["b_ada"] = self.din("b_ada", [6 * D])
        I["w_in"] = self.din("w_in", [D, 11264])
        I["r_lower"] = self.din("r_lower", [2, 1024])
        I["r_norm_g"] = self.din("r_norm_g", [128])
        I["w_up_a"] = self.din("w_up_a", [1024, D])
        I["w_up_r"] = self.din("w_up_r", [1024, D])
        I["w_out"] = self.din("w_out", [D, D])
        I["w_rg"] = self.din("w_rg", [D, 4])
        I["b_rg"] = self.din("b_rg", [4])
        I["w_re"] = self.din("w_re", [D, 32])
        I["b_re"] = self.din("b_re", [32])
        I["w1"] = self.din("w1", [32, D, 512])
        I["w3"] = self.din("w3", [32, D, 512])
        I["w2"] = self.din("w2", [32, 512, D])
        I["k_ident"] = self.din("k_ident", [128, 128])
        I["k_cm"] = self.din("k_cm", [128, 512])
        I["k_cpen"] = self.din("k_cpen", [128, 256])
        I["k_sel"] = self.din("k_sel", [16, 2048])
        I["k_wall"] = self.din("k_wall", [128, 136])
        I["k_wl"] = self.din("k_wl", [128, 128])
        I["k_bd"] = self.din("k_bd", [128, 128])
        I["k_slt"] = self.din("k_slt", [128, 128])
        I["k_ones"] = self.din("k_ones", [128, 128])
        I["k_ebase"] = self.din("k_ebase", [128, 32])
        self.I = I
        self.out = nc.dram_tensor("out", [NT, D], F32, kind="ExternalOutput").ap()
        self.dscr("MOD", [6 * D], F32)
        self.dscr("QT", [8, 128, NT], BF16)
        self.dscr("KT", [8, 128, 2 * NT], BF16)
        self.dscr("V", [2 * NT, 1024], BF16)
        self.dscr("RQT", [8, 128, NT], BF16)
        self.dscr("RF", [2 * NT, 1024], F32)
        self.dscr("RI", [2 * NT, 1024], BF16)
        self.dscr("ROG", [NT, 1024], BF16)
        self.dscr("GAT", [16, 128, NT], BF16)
        self.dscr("GRT", [16, 128, NT], BF16)
        self.dscr("MT", [16, 128, NT], BF16)
        self.dscr("X1", [NT, D], F32)
        self.dscr("H2T", [16, 128, NT], BF16)
        self.dscr("XD", [NR, D], BF16)
        self.dscr("YD0", [NR, 1024], F32)
        self.dscr("YD1", [NR, 1024], F32)
        if self.debug:
            self.dscr("ATT", [8, 128, NT], BF16)
            self.dscr("OT", [8, 128, NT], BF16)
            self.dscr("WF", [128, 16 * 32], F32)
        self.scrB = {}

        with contextlib.ExitStack() as st:
            self.F = [st.enter_context(nc.psum_tensor("F%d" % i, [128, 512], F32)) for i in range(6)]
            self.T = [st.enter_context(nc.psum_tensor("T%d" % i, [128, 1024], BF16)) for i in range(2)]
            self.BF = [Buf("F%d" % i, excl=True) for i in range(6)]
            self.BFs = [[self.BF[i]] * 4 for i in range(6)]
            _bt = [Buf("T%d" % i, excl=True) for i in range(2)]
            self.BT = [[_bt[i]] * 8 for i in range(2)]
            self.dummy = sb.alloc("dummy", [128, 8], F32)
            self.stage0()
            self.bar()
            if "1" in self.stages:
                self.stage1()
                self.bar()
            sb.push()
            self.attT = sb.alloc("attT", [128, 8, NT], BF16)
            self.oT = sb.alloc("oT", [128, 8, NT], BF16)
            self.B_attT = [Buf("attT%d" % h) for h in range(8)]
            self.B_oT = [Buf("oT%d" % t) for t in range(16)]
            if "2" in self.stages:
                self.stage2()
                self.bar()
            if "3" in self.stages:
                self.stage3()
                self.bar()
            if "4" in self.stages:
                self.stage4a()
                self.bar()
            sb.pop()
            if "4" in self.stages:
                self.stage4b()
                self.bar()
            if "5" in self.stages:
                self.stage5()
            allb = [b for b in self.S.dma_bufs]
            S.op("sp", lambda e: e.nop(), [], allb)
            S.emit(st)
        return nc

    def bar(self):
        d = self.dummy
        self.S.barrier(lambda e: e.memset(d[:], 0.0))

    def sB(self, name, key):
        k = (name, key)
        if k not in self.scrB:
            self.scrB[k] = Buf("%s%s" % (name, key))
        return self.scrB[k]

    def stage0(self):
        S, sb, I = self.S, self.sb, self.I
        cbs = {"sp": [Buf("c%d" % i) for i in range(4)], "pool": [Buf("cp%d" % i) for i in range(2)]}
        cb = cbs["sp"] + cbs["pool"]
        self.B_c0 = Buf("constall")
        ci = [0]

        def cload(q, dst, src, **kw):
            lst = cbs[q]
            b = lst[ci[0] % len(lst)]
            ci[0] += 1
            self.dma(q, dst, src, [], [b], b, **kw)

        def A(name, shape, dt=F32):
            t = sb.alloc(name, shape, dt)
            return t

        self.identb = A("identb", [128, 128], BF16)
        self.ident32 = A("ident32", [128, 128])
        self.rows = A("rows", [48, 128])
        self.cm = A("cm", [128, 512], BF16)
        self.cpen = A("cpen", [128, 256])
        self.selc = A("selc", [16, 2048], BF16)
        self.wall = A("wall", [128, 136])
        self.wl = A("wl", [128, 128])
        self.bd = A("bd", [128, 128])
        self.epsc = A("epsc", [128, 1])
        self.pm = A("pm", [128, 1])
        self.ppen = A("ppen", [128, 1])
        self.g1c = A("g1c", [128, 16])
        self.g2c = A("g2c", [128, 16])
        self.ccol = A("ccol", [128, 16])
        self.cact = A("cact", [128, 16], BF16)
        self.modc = A("modc", [128, 6, 16])
        self.a1 = A("a1", [128, 16])
        self.a2 = A("a2", [128, 16])
        self.gt1b = A("gt1b", [128, D])
        self.gt2b = A("gt2b", [128, D])
        self.fgb = A("fgb", [128, D])
        self.lb = A("lb", [128, 1024])
        self.oml = A("oml", [128, 1024])
        self.rngb = A("rngb", [128, 128])
        self.wr = A("wr", [128, 16, 36], BF16)
        self.br = A("br", [128, 36])
        self.wfull = A("wfull", [128, 16, 32])
        self.slt = A("slt", [128, 128], BF16)
        self.a2b = A("a2b", [128, D], BF16)
        self.sh2bb = A("sh2bb", [128, D], BF16)
        self.ones = A("ones", [128, 128], BF16)
        self.ebase = A("ebase", [128, 32])
        self.tot = A("tot", [128, 32])
        self.desti = A("desti", [128, 16, 2], I32)
        self.wk = A("wk", [128, 16, 2])
        self.B_tot = Buf("tot")
        self.B_ind = Buf("ind")
        self.six = [A("six", [128, 2], I32) for _ in range(2)]
        self.gix = [A("gix", [128, 2], I32) for _ in range(2)]
        self.B_six = [[Buf("six%d_%d" % (i, k)) for k in range(2)] for i in range(2)]
        self.B_gix = [[Buf("gix%d_%d" % (i, k)) for k in range(2)] for i in range(2)]
        self.B_dest = [Buf("dest%d" % t) for t in range(16)]
        self.B_wfull = [Buf("wfull%d" % t) for t in range(16)]
        cload("pool", self.identb[:], I["k_ident"])
        cload("sp", self.ident32[:], I["k_ident"])
        cload("pool", self.cm[:], I["k_cm"])
        cload("sp", self.cpen[:], I["k_cpen"])
        cload("pool", self.selc[:], I["k_sel"])
        cload("sp", self.wall[:], I["k_wall"])
        cload("sp", self.wl[:], I["k_wl"])
        cload("sp", self.bd[:], I["k_bd"])
        cload("sp", self.pm[:], I["pm"])
        cload("sp", self.rows[0:16, :], I["norm1_g"].rearrange("(k p) -> k p", p=128))
        cload("sp", self.rows[16:32, :], I["norm2_g"].rearrange("(k p) -> k p", p=128))
        cload("sp", self.rows[32:48, :], I["c"].rearrange("(k p) -> k p", p=128))
        cload("sp", self.fgb[:], I["final_g"].partition_broadcast(128))
        cload("sp", self.lb[:], I["r_lower"][0, :].partition_broadcast(128))
        cload("sp", self.oml[:], I["r_lower"][1, :].partition_broadcast(128))
        cload("sp", self.rngb[:], I["r_norm_g"].partition_broadcast(128))
        cload("pool", self.wr[:, :, 0:4], I["w_rg"].rearrange("(k p) n -> p k n", p=128))
        cload("pool", self.wr[:, :, 4:36], I["w_re"].rearrange("(k p) n -> p k n", p=128))
        cload("pool", self.slt[:], I["k_slt"])
        cload("pool", self.ones[:], I["k_ones"])
        cload("sp", self.ebase[:], I["k_ebase"])
        cload("sp", self.br[:, 0:4], I["b_rg"].partition_broadcast(128))
        cload("sp", self.br[:, 4:36], I["b_re"].partition_broadcast(128))
        cs = Buf("cs")
        S.op("dve", lambda e: e.memset(self.epsc[:], EPS), [], [cs])
        S.op("pe", lambda e: e.transpose(out=self.F[2][:, 0:48], in_=self.rows[:, :], identity=self.ident32[0:48, 0:48]), cb, [self.BF[2]])
        self.cp("dve", self.g1c[:], self.F[2][:, 0:16], [self.BF[2]], [cs])
        self.cp("dve", self.g2c[:], self.F[2][:, 16:32], [self.BF[2]], [cs])
        self.cp("dve", self.ccol[:], self.F[2][:, 32:48], [self.BF[2]], [cs])
        S.op("dve", lambda e: e.memset(self.tot[:], 0.0), [], [self.B_tot])
        self.ts(self.ppen[:], self.pm[:], -1.0, -NEGBIG, ALU.add, ALU.mult, cb, [cs])
        self.tt(self.lb[:], self.lb[:], self.oml[:], ALU.subtract, cb, [cs])
        self.act(self.lb[:], self.lb[:], AF.Sigmoid, [cs], [cs])
        self.ts(self.oml[:], self.lb[:], -1.0, 1.0, ALU.mult, ALU.add, [cs], [cs])
        self.act(self.cact[:], self.ccol[:], AF.Silu, [cs], [cs])
        sb.push()
        MOD = self.dram["MOD"]
        wsl = [sb.alloc("wsl0", [128, KC, 512], BF16) for _ in range(3)]
        Bw = [self.dbuf("wsl", "pool") for _ in range(3)]
        bsl = [sb.alloc("bsl", [1, 512], F32) for _ in range(2)]
        Bb = [self.dbuf("bsl") for _ in range(2)]
        msl = [sb.alloc("msl", [1, 512], F32) for _ in range(2)]
        Bm = [self.dbuf("msl") for _ in range(2)]
        wsrc = I["w_ada"].rearrange("(k p) n -> p k n", p=128)
        Bmod = Buf("MOD")
        for j in range(24):
            w = wsl[j % 3]
            bw = Bw[j % 3]
            self.dma("pool", w[:], wsrc[:, :, j * 512:(j + 1) * 512], [], [bw], bw)
            bb = Bb[j % 2]
            self.dma("sp", bsl[j % 2][:], I["b_ada"][j * 512:(j + 1) * 512].rearrange("(o n) -> o n", o=1), [], [bb], bb)
            pf = self.F[j % 2]
            bpf = self.BF[j % 2]
            for kc in range(KC):
                self.mm(pf[0:1, :], self.cact[:, kc:kc + 1], w[:, kc, :], kc == 0, kc == KC - 1, [bw, cs], [bpf])
            bm = Bm[j % 2]
            self.tt(msl[j % 2][:], pf[0:1, :], bsl[j % 2][:], ALU.add, [bpf, bb], [bm])
            self.dma("sp", MOD[j * 512:(j + 1) * 512].rearrange("(o n) -> o n", o=1), msl[j % 2][:], [bm], [Bmod], bm)
        sb.pop()
        self.release(Bw + Bb + Bm)
        bmc = self.dbuf("modc")
        modr = sb.alloc("modr", [96, 128], F32)
        self.dma("sp", modr[:], MOD.rearrange("(r p) -> r p", p=128), [Bmod], [bmc], bmc)
        S.op("pe", lambda e: e.transpose(out=self.F[3][:, 0:96], in_=modr[:, :], identity=self.ident32[0:96, 0:96]), [bmc] + cb, [self.BF[3]])
        self.cp("dve", self.modc[:].rearrange("p s k -> p (s k)"), self.F[3][:, 0:96], [self.BF[3]], [cs])
        bg1 = self.dbuf("gt1b")
        self.dma("sp", self.gt1b[:], MOD[2 * D:3 * D].partition_broadcast(128), [Bmod], [bg1], bg1)
        bg2 = self.dbuf("gt2b")
        self.dma("sp", self.gt2b[:], MOD[5 * D:6 * D].partition_broadcast(128), [Bmod], [bg2], bg2)
        sb.push()
        g2b = sb.alloc("g2b", [128, D], F32)
        sc2b = sb.alloc("sc2b", [128, D], F32)
        sh2b = sb.alloc("sh2b", [128, D], F32)
        bq1, bq2, bq3 = self.dbuf("g2b"), self.dbuf("sc2b"), self.dbuf("sh2b")
        self.dma("sp", g2b[:], I["norm2_g"].partition_broadcast(128), [], [bq1], bq1)
        self.dma("sp", sc2b[:], MOD[4 * D:5 * D].partition_broadcast(128), [Bmod], [bq2], bq2)
        self.dma("sp", sh2b[:], MOD[3 * D:4 * D].partition_broadcast(128), [Bmod], [bq3], bq3)
        self.stt(self.a2b[:], sc2b[:], 1.0, g2b[:], ALU.add, ALU.mult, [bq1, bq2], [cs])
        self.cp("dve", self.sh2bb[:], sh2b[:], [bq3], [cs])
        sb.pop()
        self.release([bq1, bq2, bq3])
        self.stt(self.a1[:], self.modc[:, 1, :], 1.0, self.g1c[:], ALU.add, ALU.mult, [cs], [cs])
        self.stt(self.a2[:], self.modc[:, 4, :], 1.0, self.g2c[:], ALU.add, ALU.mult, [cs], [cs])
        S.op("dve", lambda e: e.memset(self.epsc[:], EPS), cb + [cs, bmc, bg1, bg2], [self.B_c0])
        self.B_c0.const = True
        self.B_modc = bmc

    def norm_to_hT(self, xt, bx, hT_dst, bh, a_col, sh_col, ring):
        junk, ssq, tmp, rs, xn, bj, bs, bxn = ring
        import os
        lvl = int(os.environ.get("N1", "9"))
        self.act(junk[:], xt, AF.Square, [bx], [bj, bs], accum=ssq[:, 0:1])
        if lvl < 2:
            return
        self.rstd(rs[:, 0:1], ssq[:, 0:1], float(D), [bs], [bs, bj], tmp[:, 0:1])
        if lvl < 3:
            return
        self.ts(xn[:], xt, rs[:, 0:1], None, ALU.mult, None, [bx, bs], [bxn])
        if lvl < 4:
            return
        for kc in range(KC):
            tb, ts_ = kc // 8, kc % 8
            pt = self.T[tb][:, ts_ * 128:(ts_ + 1) * 128]
            bt = self.BT[tb][ts_]
            self.tr(pt, xn[:, kc * 128:(kc + 1) * 128], [bxn], [bt])
            if os.environ.get("NOEV"):
                continue
            evm = os.environ.get("EVM", "")
            if evm == "f":
                self.act(hT_dst[:, kc, :], pt, AF.Identity, [bt, self.B_c0], [bh], scale=2.0)
            elif evm == "s":
                self.act(hT_dst[:, kc, :], pt, AF.Identity, [bt, self.B_c0], [bh], scale=a_col[:, kc:kc + 1])
            elif evm == "j":
                self.cp("dve", junk[:, 0:128], pt, [bt, self.B_c0], [bh])
            elif evm == "d":
                self.cp("dve", hT_dst[:, kc, :], pt, [bt, self.B_c0], [bh])
            elif evm == "c":
                self.act(hT_dst[:, kc, :], pt, AF.Copy, [bt, self.B_c0], [bh])
            elif tb == 0 or lvl < 5:
                self.act(hT_dst[:, kc, :], pt, AF.Identity, [bt, self.B_c0], [bh], scale=a_col[:, kc:kc + 1], bias=sh_col[:, kc:kc + 1])
            else:
                self.ts(hT_dst[:, kc, :], pt, a_col[:, kc:kc + 1], sh_col[:, kc:kc + 1], ALU.mult, ALU.add, [bt, self.B_c0], [bh])

    def norm_ring(self, sb, junk=None):
        if junk is None:
            junk = sb.alloc("junk", [128, D], BF16)
        ssq = sb.alloc("ssq", [128, 1], F32)
        tmp = sb.alloc("tmp", [128, 1], F32)
        rs = sb.alloc("rs", [128, 1], F32)
        xn = sb.alloc("xn", [128, D], BF16)
        return (junk, ssq, tmp, rs, xn, Buf("junk"), Buf("ssq"), Buf("xn"))

    def stage1(self):
        S, sb, I = self.S, self.sb, self.I
        sb.push()
        G = 1024
        hT = [sb.alloc("hT", [128, KC, G], BF16) for _ in range(2)]
        BhT = [[Buf("hT%d_%d" % (s, t)) for t in range(8)] for s in range(2)]
        wsl = [sb.alloc("wsl", [128, KC, 512], BF16) for _ in range(3)]
        Bw = [self.dbuf("wsl", "pool") for _ in range(3)]
        xs = [sb.alloc("xs", [128, D], F32) for _ in range(2)]
        Bx = [self.dbuf("xs") for _ in range(2)]
        rings = [self.norm_ring(sb)]
        rings.append(self.norm_ring(sb, junk=rings[0][0]))
        stg = []
        stgb = []
        for _ in range(4):
            sb.push()
            stgb.append(sb.alloc("stgb", [128, 512], BF16))
            sb.pop()
            stg.append(sb.alloc("stg", [128, 512], F32))
        Bs = [self.dbuf("stg") for _ in range(4)]
        wsrc = I["w_in"].rearrange("(k p) n -> p k n", p=128)
        D_ = self.dram
        zt = sb.alloc("zt", [128, 1024], BF16)
        Bz = self.dbuf("zt")
        S.op("dve", lambda e: e.memset(zt[:], 0.0), [], [Bz])
        for i in range(NR // 128):
            for hf in range(2):
                self.dma("sp", D_["XD"][i * 128:(i + 1) * 128, hf * 1024:(hf + 1) * 1024], zt[:], [Bz], [Buf("z")], Bz)
        groups = [("p", 0), ("p", 1), ("m", 0), ("m", 1)]
        pblocks = [2, 3, 4, 5, 8, 9, 10, 11]
        import os
        if os.environ.get("S1G"):
            groups = groups[:int(os.environ["S1G"])]
        if os.environ.get("S1B"):
            pblocks = pblocks[:int(os.environ["S1B"])]
        wi = [0]
        si = [0]
        xi = [0]
        for gi, (kind, gidx) in enumerate(groups):
            slot = gi % 2
            src = I["xp"] if kind == "p" else I["xm"]
            ltok0 = gidx * G + (0 if kind == "p" else NT)
            mtok0 = gidx * G
            for t in range(int(os.environ.get("S1T", "8"))):
                xsl = xs[xi[0] % 2]
                bx = Bx[xi[0] % 2]
                ring = rings[xi[0] % 2]
                xi[0] += 1
                r0 = gidx * G + t * 128
                self.dma("sp", xsl[:], src[r0:r0 + 128, :], [], [bx], bx)
                self.norm_to_hT(xsl[:], bx, hT[slot][:, :, t * 128:(t + 1) * 128], BhT[slot][t], self.a1, self.modc[:, 0, :], ring)
            blocks = pblocks if kind == "p" else list(range(22))
            for blk in blocks:
                w = wsl[wi[0] % 3]
                bw = Bw[wi[0] % 3]
                wi[0] += 1
                self.dma("pool", w[:], wsrc[:, :, blk * 512:(blk + 1) * 512], [], [bw], bw)
                fm = blk in (0, 1, 2, 3, 6, 7) or blk >= 14
                for u in range(8):
                    fb = u % 4
                    pf = self.F[fb]
                    bpf = self.BF[fb]
                    if fm:
                        cs_, th = u // 2, u % 2
                        for kc in range(KC):
                            self.mm(pf[:, :], w[:, kc, cs_ * 128:(cs_ + 1) * 128], hT[slot][:, kc, th * 512:(th + 1) * 512],
                                    kc == 0, kc == KC - 1, [bw] + BhT[slot][th * 4:(th + 1) * 4], [bpf])
                    else:
                        for kc in range(KC):
                            self.mm(pf[:, :], hT[slot][:, kc, u * 128:(u + 1) * 128], w[:, kc, :],
                                    kc == 0, kc == KC - 1, [bw, BhT[slot][u]], [bpf])
                    sg = stg[si[0] % 4]
                    sgb = stgb[si[0] % 4]
                    bs = Bs[si[0] % 4]
                    si[0] += 1
                    if fm:
                        if blk < 2:
                            dst = D_["QT"][blk * 4 + cs_, :, mtok0 + th * 512: mtok0 + (th + 1) * 512]
                            key = ("QT", blk * 4 + cs_)
                            fn = AF.Copy
                        elif blk < 4:
                            dst = D_["KT"][(blk - 2) * 4 + cs_, :, ltok0 + th * 512: ltok0 + (th + 1) * 512]
                            key = ("KT", (blk - 2) * 4 + cs_)
                            fn = AF.Copy
                        elif blk < 8:
                            dst = D_["RQT"][(blk - 6) * 4 + cs_, :, mtok0 + th * 512: mtok0 + (th + 1) * 512]
                            key = ("RQT", (mtok0 + th * 512) // 128)
                            fn = AF.Copy
                        elif blk < 18:
                            dst = D_["GAT"][(blk - 14) * 4 + cs_, :, mtok0 + th * 512: mtok0 + (th + 1) * 512]
                            key = ("GAT", (blk - 14) * 4 + cs_)
                            fn = AF.Sigmoid
                        else:
                            dst = D_["GRT"][(blk - 18) * 4 + cs_, :, mtok0 + th * 512: mtok0 + (th + 1) * 512]
                            key = ("GRT", (blk - 18) * 4 + cs_)
                            fn = AF.Sigmoid
                        odt = BF16
                    else:
                        if blk < 6:
                            dst = D_["V"][ltok0 + u * 128: ltok0 + (u + 1) * 128, (blk - 4) * 512:(blk - 3) * 512]
                            key = ("V", (blk - 4) * 4)
                            fn, odt = AF.Copy, BF16
                        elif blk < 10:
                            dst = D_["RF"][ltok0 + u * 128: ltok0 + (u + 1) * 128, (blk - 8) * 512:(blk - 7) * 512]
                            key = ("RF", (ltok0 + u * 128) // 128)
                            fn, odt = AF.Copy, F32
                        elif blk < 12:
                            dst = D_["RI"][ltok0 + u * 128: ltok0 + (u + 1) * 128, (blk - 10) * 512:(blk - 9) * 512]
                            key = ("RI", (ltok0 + u * 128) // 128)
                            fn, odt = AF.Copy, BF16
                        else:
                            dst = D_["ROG"][mtok0 + u * 128: mtok0 + (u + 1) * 128, (blk - 12) * 512:(blk - 11) * 512]
                            key = ("ROG", (mtok0 + u * 128) // 128)
                            fn, odt = AF.Silu, BF16
                    so = sg[:] if odt == F32 else sgb[:]
                    if fn == AF.Copy and (u % 2 == 1):
                        self.cp("dve", so, pf[:, :], [bpf], [bs])
                    else:
                        self.act(so, pf[:, :], fn, [bpf], [bs])
                    wl_ = self.scr_w.setdefault(key, [])
                    tok = Buf("w")
                    wl_.append(tok)
                    self.dma("sp", dst, so, [bs], [tok], bs)
        sb.pop()
        self.release(Bw + Bx + Bs + [Bz])

    def scr_reads(self, name, keys):
        out = []
        for k in keys:
            out.extend(self.scr_w.get((name, k), []))
        return out

    def stage2(self):
        S, sb = self.S, self.sb
        D_ = self.dram
        sb.push()
        QTh = [sb.alloc("QTh", [128, NT], BF16) for _ in range(2)]
        KTh = [sb.alloc("KTh", [128, 2 * NT], BF16) for _ in range(2)]
        Vh = [sb.alloc("Vh", [128, 32, 129], BF16) for _ in range(2)]
        Bq = [self.dbuf("q") for _ in range(2)]
        Bk = [self.dbuf("k") for _ in range(2)]
        Bv = [self.dbuf("v") for _ in range(2)]
        km32 = [sb.alloc("km32", [128, 16], F32) for _ in range(2)]
        kmb = [sb.alloc("kmb", [128, 16], BF16) for _ in range(2)]
        gm = [sb.alloc("gm", [128, 16, 16], F32) for _ in range(2)]
        top8 = [sb.alloc("top8", [128, 16, 8], F32) for _ in range(2)]
        pen = [sb.alloc("pen", [128, 16, 16], F32) for _ in range(2)]
        penb = [sb.alloc("penb", [128, 256], BF16) for _ in range(2)]
        penT = [sb.alloc("penT", [16, NT], BF16) for _ in range(2)]
        Bkm = [Buf("km%d" % i) for i in range(2)]
        Bgm = [Buf("gm%d" % i) for i in range(2)]
        Bpen = [Buf("pen%d" % i) for i in range(2)]
        BpenT = [Buf("penT%d" % i) for i in range(2)]
        pT = [sb.alloc("pT", [128, 256], BF16) for _ in range(4)]
        BpT = [Buf("pT%d" % i) for i in range(4)]
        rsum = [sb.alloc("rsum", [128, 1], F32) for _ in range(2)]
        atts = [sb.alloc("atts", [128, 128], BF16) for _ in range(2)]
        Brs = [Buf("rsum%d" % i) for i in range(2)]
        Bat = [Buf("atts%d" % i) for i in range(2)]
        scale = 1.0 / np.sqrt(128.0)
        pi = [0]
        ei = [0]
        for sl in range(2):
            S.op("dve", lambda e, t=Vh[sl]: e.memset(t[:, :, 128:129], 1.0), [], [Bv[sl]])
        def preamble(h):
            sl = h % 2
            q, k, v = QTh[sl], KTh[sl], Vh[sl]
            bq, bk, bv = Bq[sl], Bk[sl], Bv[sl]
            self.dma("sp", q[:], D_["QT"][h], self.scr_reads("QT", [h]), [bq], bq)
            self.dma("sp", k[:], D_["KT"][h], self.scr_reads("KT", [h]), [bk], bk)
            self.dma("sp", v[:, :, 0:128], D_["V"].rearrange("(t p) c -> p t c", p=128)[:, :, h * 128:(h + 1) * 128],
                     self.scr_reads("V", [(h // 4) * 4]), [bv], bv)
            yield
            S.op("dve", lambda e, k=k: e.tensor_reduce(out=km32[sl][:], in_=k[:].rearrange("p (b l) -> p b l", l=256), axis=AX.X, op=ALU.add),
                 [bk], [Bkm[sl]])
            yield
            self.ts(kmb[sl][:], km32[sl][:], 1.0 / 256, None, ALU.mult, None, [Bkm[sl]], [Bkm[sl]])
            yield
            pg = self.F[5]
            bpg = self.BF[5]
            for qt in range(16):
                self.mm(pg[:, qt * 16:(qt + 1) * 16], q[:, qt * 128:(qt + 1) * 128], kmb[sl][:, :], True, True, [bq, Bkm[sl]], [bpg])
            yield
            gmf = gm[sl][:].rearrange("p a b -> p (a b)")
            self.tt(gmf, pg[:, 0:256], self.cpen[:], ALU.add, [bpg, self.B_c0], [Bgm[sl]])
            yield
            self.ts(gm[sl][:, :, 0:8], gm[sl][:, :, 0:8], self.ppen[:, 0:1], None, ALU.add, None, [Bgm[sl], self.B_c0], [Bgm[sl]])
            yield
            for qt in range(16):
                S.op("dve", lambda e, qt=qt: e.max(out=top8[sl][:, qt, :], in_=gm[sl][:, qt, :]), [Bgm[sl]], [Bpen[sl]])
                yield
            for qt in range(16):
                self.ts(pen[sl][:, qt, :], gm[sl][:, qt, :], top8[sl][:, qt, 2:3], None, ALU.is_ge, None, [Bgm[sl], Bpen[sl]], [Bpen[sl]])
                yield
            penf = pen[sl][:].rearrange("p a b -> p (a b)")
            self.ts(penf, penf, -1.0, -NEG, ALU.add, ALU.mult, [Bpen[sl]], [Bpen[sl]])
            yield
            self.ts(pen[sl][:, :, 0:8], pen[sl][:, :, 0:8], self.ppen[:, 0:1], None, ALU.add, None, [Bpen[sl], self.B_c0], [Bpen[sl]])
            yield
            self.cp("dve", penb[sl][:], penf, [Bpen[sl]], [Bpen[sl]])
            yield
            for half in range(2):
                for qt in range(half * 8, half * 8 + 8):
                    tb, ts_ = qt // 8, qt % 8
                    pt = self.T[tb][0:16, ts_ * 128:(ts_ + 1) * 128]
                    self.tr(pt, penb[sl][:, qt * 16:(qt + 1) * 16], [Bpen[sl]], [self.BT[tb][ts_]])
                yield
                for qt in range(half * 8, half * 8 + 8):
                    tb, ts_ = qt // 8, qt % 8
                    pt = self.T[tb][0:16, ts_ * 128:(ts_ + 1) * 128]
                    self.cp("act", penT[sl][:, qt * 128:(qt + 1) * 128], pt, [self.BT[tb][ts_]], [BpenT[sl]])
                yield

        for _ in preamble(0):
            pass
        for h in range(8):
            sl = h % 2
            q, k, v = QTh[sl], KTh[sl], Vh[sl]
            bq, bk, bv = Bq[sl], Bk[sl], Bv[sl]
            nxt = preamble(h + 1) if h + 1 < 8 else iter(())
            itc = [0]
            for qb in range(8):
                Bl = 8 + qb
                nkt = 2 * Bl + 2
                po = [self.F[3], self.F[4]]
                bpo = [self.BF[3], self.BF[4]]
                LAG = 2
                pend = []
                for kk in range(nkt + LAG):
                    if kk < nkt:
                        kt = kk
                        fb = kt % 3
                        ps = self.F[fb]
                        bps = self.BF[fb]
                        self.mm(ps[:, 0:256], k[:, kt * 128:(kt + 1) * 128], q[:, qb * 256:(qb + 1) * 256], True, False, [bk, bq], [bps])
                        if kt < 2 * Bl:
                            i = kt // 2
                            self.mm(ps[:, 0:256], self.selc[0:16, i * 128:(i + 1) * 128], penT[sl][0:16, qb * 256:(qb + 1) * 256],
                                    False, True, [BpenT[sl], self.B_c0], [bps])
                        else:
                            j = kt - 2 * Bl
                            self.mm(ps[:, 0:256], self.identb[:, :], self.cm[:, j * 256:(j + 1) * 256], False, True, [self.B_c0], [bps])
                        p = pT[pi[0] % 4]
                        bp = BpT[pi[0] % 4]
                        pi[0] += 1
                        self.act(p[:], ps[:, 0:256], AF.Exp, [bps], [bp], scale=scale)
                        pend.append((kt, p, bp))
                    if kk >= LAG:
                        kt, p, bp = pend.pop(0)
                        for qh in range(2):
                            self.mm(po[qh][:, 0:129], p[:, qh * 128:(qh + 1) * 128], v[:, kt, :], kt == 0, kt == nkt - 1, [bp, bv], [bpo[qh]])
                    itc[0] += 1
                    if itc[0] >= 24:
                        next(nxt, None)
                for qh in range(2):
                    e_ = ei[0] % 2
                    ei[0] += 1
                    S.op("dve", lambda e, o=rsum[e_], i=po[qh]: e.reciprocal(out=o[:, 0:1], in_=i[:, 128:129]), [bpo[qh]], [Brs[e_]])
                    self.act(atts[e_][:], po[qh][:, 0:128], AF.Copy, [bpo[qh], Brs[e_]], [Bat[e_]], scale=rsum[e_][:, 0:1])
                    qt = qb * 2 + qh
                    tb, ts_ = qt // 8, qt % 8
                    pt = self.T[tb][:, ts_ * 128:(ts_ + 1) * 128]
                    bt = self.BT[tb][ts_]
                    self.tr(pt, atts[e_][:], [Bat[e_]], [bt])
                    self.cp("dve", self.attT[:, h, qt * 128:(qt + 1) * 128], pt, [bt], [self.B_attT[h]])
            for _ in nxt:
                pass
            if self.debug:
                self.dma("sp", D_["ATT"][h], self.attT[:, h, :], [self.B_attT[h]], [Buf("x")], self.B_attT[h])
        sb.pop()
        self.release(Bq + Bk + Bv)

    def stage3(self):
        S, sb = self.S, self.sb
        D_ = self.dram
        sb.push()
        rf = [sb.alloc("rf", [128, 1024], F32) for _ in range(2)]
        ri = [sb.alloc("ri", [128, 1024], BF16) for _ in range(2)]
        rq = [sb.alloc("rq", [128, 8, 128], BF16) for _ in range(2)]
        rog = [sb.alloc("rog", [128, 1024], BF16) for _ in range(2)]
        Brf = [self.dbuf("rf") for _ in range(2)]
        Bri = [self.dbuf("ri") for _ in range(2)]
        Brq = [self.dbuf("rq") for _ in range(2)]
        Brog = [self.dbuf("rog") for _ in range(2)]
        logf = [sb.alloc("logf", [128, 1024], F32) for _ in range(2)]
        kin = [sb.alloc("kin", [128, 1024], F32) for _ in range(2)]
        Blf = [Buf("logf%d" % i) for i in range(2)]
        Bkin = [Buf("kin%d" % i) for i in range(2)]
        R = 4
        ebl = [sb.alloc("ebl", [128, 128], F32) for _ in range(R)]
        khat = [sb.alloc("khat", [128, 128], BF16) for _ in range(R)]
        eb8 = [sb.alloc("eb8", [128, 8], F32) for _ in range(R)]
        ebm = [sb.alloc("ebm", [128, 128], F32) for _ in range(R)]
        qtl = [sb.alloc("qtl", [128, 128], BF16) for _ in range(R)]
        enb = [sb.alloc("enb", [128, 128], F32) for _ in range(R)]
        ktl = [sb.alloc("ktl", [128, 128], BF16) for _ in range(R)]
        ktlT = [sb.alloc("ktlT", [128, 128], BF16) for _ in range(R)]
        khc = [sb.alloc("khc", [128, 4, 128], BF16) for _ in range(R)]
        qtlz = [sb.alloc("qtlz", [128, 4, 128], BF16) for _ in range(R)]
        Bkhc = [Buf("khc%d" % i) for i in range(R)]
        Bqz = [Buf("qtlz%d" % i) for i in range(R)]
        for i in range(R):
            S.op("dve", lambda e, t=qtlz[i]: e.memset(t[:], 0.0), [], [Bqz[i]])
        at = [sb.alloc("at", [128, 128], BF16) for _ in range(R)]
        Sb = [sb.alloc("Sb", [128, 4, 128], BF16) for _ in range(R)]
        ot = [sb.alloc("ot", [128, 128], F32) for _ in range(R)]
        of_ = [sb.alloc("of", [128, 128], BF16) for _ in range(R)]
        sq = [sb.alloc("sq", [128, 4], F32) for _ in range(R)]
        jk = [sb.alloc("jk", [128, 128], BF16) for _ in range(R)]
        names = "ebl khat eb8 ebm qtl enb ktl ktlT at ot of sq".split()
        Bn = {n: [Buf(n + str(i)) for i in range(R)] for n in names}
        BSb = [[Buf("Sb%d_%d" % (i, c)) for c in range(4)] for i in range(R)]
        St = sb.alloc("St", [128, 8, 128], F32)
        BSt = [Buf("St%d" % h) for h in range(8)]
        for h in range(8):
            S.op("dve", lambda e, h=h: e.memset(St[:, h, :], 0.0), [], [BSt[h]])
        F, BFs = self.F, self.BFs
        hi = [0]
        import os
        tl = list(range(32))
        if os.environ.get("S3T"):
            tl = [int(v) for v in os.environ["S3T"].split(",")]
        s3c = int(os.environ.get("S3C", "4"))
        s3m = int(os.environ.get("S3M", "9"))
        for tile in tl:
            main = tile >= 16
            mt = tile - 16
            sl = tile % 2
            self.dma("sp", rf[sl][:], D_["RF"][tile * 128:(tile + 1) * 128, :], self.scr_reads("RF", [tile]), [Brf[sl]], Brf[sl])
            self.dma("sp", ri[sl][:], D_["RI"][tile * 128:(tile + 1) * 128, :], self.scr_reads("RI", [tile]), [Bri[sl]], Bri[sl])
            if main:
                self.dma("sp", rq[sl][:], D_["RQT"][:, :, mt * 128:(mt + 1) * 128].rearrange("h p t -> p h t"),
                         self.scr_reads("RQT", [(mt // 4) * 4]), [Brq[sl]], Brq[sl])
                self.dma("sp", rog[sl][:], D_["ROG"][mt * 128:(mt + 1) * 128, :], self.scr_reads("ROG", [mt]), [Brog[sl]], Brog[sl])
            lf, kn = logf[sl], kin[sl]
            self.act(kn[:], rf[sl][:], AF.Sigmoid, [Brf[sl]], [Bkin[sl]])
            self.tt(kn[:], kn[:], self.oml[:], ALU.mult, [Bkin[sl], self.B_c0], [Bkin[sl]])
            self.tt(kn[:], kn[:], self.lb[:], ALU.add, [Bkin[sl], self.B_c0], [Bkin[sl]])
            self.act(lf[:], kn[:], AF.Ln, [Bkin[sl]], [Blf[sl]])
            self.ts(kn[:], kn[:], -1.0, 1.0, ALU.mult, ALU.add, [Bkin[sl]], [Bkin[sl]])
            def hbody(h, r, main=main, sl=sl, lf=lf, kn=kn, mt=mt):
                hs = slice(h * 128, (h + 1) * 128)
                s2 = h % 2
                c128 = slice(s2 * 128, (s2 + 1) * 128)
                bl_ps = F[0][:, c128]
                b8_ps = F[0][:, 256 + s2 * 8:256 + (s2 + 1) * 8]
                self.mm(bl_ps, self.wl[:, :], lf[:, hs], True, True, [Blf[sl], self.B_c0], [self.BF[0]])
                self.mm(b8_ps, lf[:, hs], self.wall[:, 128:136], True, True, [Blf[sl], self.B_c0], [self.BF[0]])
                if main:
                    self.mm(F[1][:, c128], lf[:, hs], self.wall[:, 0:128], True, True, [Blf[sl], self.B_c0], [self.BF[1]])
                    bm_ps = F[1][:, 256 + s2 * 128:256 + (s2 + 1) * 128]
                    self.mm(bm_ps, self.wall[:, 0:128], lf[:, hs], True, True, [Blf[sl], self.B_c0], [self.BF[1]])
                yield
                self.act(ebl[r][:], bl_ps, AF.Exp, [self.BF[0]], [Bn["ebl"][r]])
                self.act(eb8[r][:], b8_ps, AF.Exp, [self.BF[0]], [Bn["eb8"][r]])
                if main:
                    self.act(ebm[r][:], F[1][:, c128], AF.Exp, [self.BF[1]], [Bn["ebm"][r]])
                    self.act(enb[r][:], bm_ps, AF.Exp, [self.BF[1]], [Bn["enb"][r]], scale=-1.0)
                yield
                for c in range(4):
                    self.stt(khc[r][:, c, :], kn[:, hs], self.wall[:, 132 + c:133 + c], ebl[r][:], ALU.mult, ALU.mult,
                             [Bkin[sl], Bn["ebl"][r], self.B_c0], [Bkhc[r]])
                if main:
                    self.tt(ktl[r][:], kn[:, hs], enb[r][:], ALU.mult, [Bkin[sl], Bn["enb"][r]], [Bn["ktl"][r]])
                    self.tt(qtl[r][:], rq[sl][:, h, :], ebm[r][:], ALU.mult, [Brq[sl], Bn["ebm"][r]], [Bn["qtl"][r]])
                    for c in range(4):
                        cc = slice(32 * c, 32 * c + 32)
                        self.tt(qtlz[r][:, c, cc], rq[sl][:, h, cc], ebm[r][:, cc], ALU.mult, [Brq[sl], Bn["ebm"][r]], [Bqz[r]])
                yield
                pds = [F[5][:, (s2 * 2 + (c % 2)) * 128:(s2 * 2 + (c % 2) + 1) * 128] for c in range(4)]
                if main:
                    pt = self.T[0][:, c128]
                    self.tr(pt, ktl[r][:], [Bn["ktl"][r]], [self.BT[0][0]])
                    yield
                    self.cp("act", ktlT[r][:], pt, [self.BT[0][0]], [Bn["ktlT"][r]])
                    yield
                    self.mm(F[3][:, c128], ktlT[r][:], qtl[r][:], True, True, [Bn["ktlT"][r], Bn["qtl"][r]], [self.BF[3]])
                    yield
                    self.tt(at[r][:], F[3][:, c128], self.bd[:], ALU.mult, [self.BF[3], self.B_c0], [Bn["at"][r]])
                    yield
                pob = 4 if s2 == 0 else 2
                po = F[pob][:, 0:128]
                bpo = self.BF[pob]
                if main:
                    self.mm(po, at[r][:], ri[sl][:, hs], True, False, [Bn["at"][r], Bri[sl]], [bpo])
                for c in range(4):
                    self.mm(pds[c], khc[r][:, c, :], ri[sl][:, hs], True, True, [Bkhc[r], Bri[sl]], [self.BF[5]])
                    if main:
                        self.act(Sb[r][:, c, :], St[:, h, :], AF.Copy, [BSt[h], Bn["eb8"][r]], [BSb[r][c]], scale=eb8[r][:, c:c + 1])
                    yield
                    if main:
                        self.mm(po, qtlz[r][:, c, :], Sb[r][:, c, :], False, c == 3, [Bqz[r], BSb[r][c]], [bpo])
                    self.stt(St[:, h, :], St[:, h, :], eb8[r][:, 4 + c:5 + c], pds[c], ALU.mult, ALU.add, [BSt[h], Bn["eb8"][r], self.BF[5]], [BSt[h]])
                    yield
                if main:
                    self.act(jk[r][:], po, AF.Square, [bpo], [Bn["sq"][r]], accum=sq[r][:, 0:1])
                    yield
                    self.tt(ot[r][:], po, self.rngb[:], ALU.mult, [bpo, self.B_c0], [Bn["ot"][r]])
                    self.rstd(sq[r][:, 2:3], sq[r][:, 0:1], 128.0, [Bn["sq"][r]], [Bn["sq"][r], Bn["sq"][r]], sq[r][:, 1:2])
                    yield
                    self.stt(of_[r][:], ot[r][:], sq[r][:, 2:3], rog[sl][:, hs], ALU.mult, ALU.mult, [Bn["ot"][r], Bn["sq"][r], Brog[sl]], [Bn["of"][r]])
                    yield
                    pt = self.T[1][:, c128]
                    self.tr(pt, of_[r][:], [Bn["of"][r]], [self.BT[1][0]])
                    yield
                    self.cp("act", self.oT[:, h, mt * 128:(mt + 1) * 128], pt, [self.BT[1][0]], [self.B_oT[mt]])

            G = 2
            for h0 in range(0, 8, G):
                gens = []
                for h in range(h0, h0 + G):
                    gens.append(hbody(h, hi[0] % R))
                    hi[0] += 1
                while gens:
                    for g_ in list(gens):
                        try:
                            next(g_)
                        except StopIteration:
                            gens.remove(g_)
            if tile == 15:
                for h in range(8):
                    self.ts(St[:, h, :], St[:, h, :], self.pm[:, 0:1], None, ALU.mult, None, [BSt[h], self.B_c0], [BSt[h]])
        if self.debug:
            for h in range(8):
                self.dma("sp", D_["OT"][h], self.oT[:, h, :], self.B_oT, [Buf("x")], self.B_oT[0])
        sb.pop()
        self.release(Brf + Bri + Brq + Brog)

    def stage4a(self):
        S, sb, I = self.S, self.sb, self.I
        D_ = self.dram
        sb.push()
        wa = [sb.alloc("wa", [128, 8, 128], BF16) for _ in range(2)]
        wr_ = [sb.alloc("wr_", [128, 8, 128], BF16) for _ in range(2)]
        sga = [sb.alloc("sga", [128, NT], BF16) for _ in range(2)]
        sgr = [sb.alloc("sgr", [128, NT], BF16) for _ in range(2)]
        mT = [sb.alloc("mT", [128, NT], BF16) for _ in range(2)]
        t1 = [sb.alloc("t1", [128, 512], F32) for _ in range(2)]
        t2 = [sb.alloc("t2", [128, 512], F32) for _ in range(2)]
        Bwa = [self.dbuf("wa", "pool") for _ in range(2)]
        Bwr = [self.dbuf("wr", "pool") for _ in range(2)]
        Bsa = [self.dbuf("sga") for _ in range(2)]
        Bsr = [self.dbuf("sgr") for _ in range(2)]
        BmT = [self.dbuf("mT") for _ in range(2)]
        Bt1 = [Buf("t1_%d" % i) for i in range(2)]
        Bt2 = [Buf("t2_%d" % i) for i in range(2)]
        wua = I["w_up_a"].rearrange("(k p) n -> p k n", p=128)
        wur = I["w_up_r"].rearrange("(k p) n -> p k n", p=128)
        ti = [0]
        for ch in range(16):
            sl = ch % 2
            self.dma("pool", wa[sl][:], wua[:, :, ch * 128:(ch + 1) * 128], [], [Bwa[sl]], Bwa[sl])
            self.dma("pool", wr_[sl][:], wur[:, :, ch * 128:(ch + 1) * 128], [], [Bwr[sl]], Bwr[sl])
            self.dma("sp", sga[sl][:], D_["GAT"][ch], self.scr_reads("GAT", [ch]), [Bsa[sl]], Bsa[sl])
            self.dma("sp", sgr[sl][:], D_["GRT"][ch], self.scr_reads("GRT", [ch]), [Bsr[sl]], Bsr[sl])
            for g in range(4):
                gs = slice(g * 512, (g + 1) * 512)
                fa, fr = (g % 2) * 2, (g % 2) * 2 + 1
                for wc in range(8):
                    self.mm(self.F[fa][:, :], wa[sl][:, wc, :], self.attT[:, wc, gs], wc == 0, wc == 7, [Bwa[sl], self.B_attT[wc]], [self.BF[fa]])
                for wc in range(8):
                    self.mm(self.F[fr][:, :], wr_[sl][:, wc, :], self.oT[:, wc, gs], wc == 0, wc == 7,
                            [Bwr[sl]] + self.B_oT[g * 4:(g + 1) * 4], [self.BF[fr]])
                i = ti[0] % 2
                ti[0] += 1
                self.tt(t1[i][:], self.F[fa][:, :], sga[sl][:, gs], ALU.mult, [self.BF[fa], Bsa[sl]], [Bt1[i]])
                self.tt(t2[i][:], self.F[fr][:, :], sgr[sl][:, gs], ALU.mult, [self.BF[fr], Bsr[sl]], [Bt2[i]], eng="dve")
                self.tt(mT[sl][:, gs], t1[i][:], t2[i][:], ALU.add, [Bt1[i], Bt2[i]], [BmT[sl]], eng="dve")
            tok = Buf("w")
            self.scr_w.setdefault(("MT", ch), []).append(tok)
            self.dma("sp", D_["MT"][ch], mT[sl][:], [BmT[sl]], [tok], BmT[sl])
        sb.pop()
        self.release(Bwa + Bwr + Bsa + Bsr + BmT)

    def stage4b(self):
        S, sb, I = self.S, self.sb, self.I
        D_ = self.dram
        sb.push()
        wo = sb.alloc("wo", [128, KC, D], BF16)
        Bwo = [self.dbuf("wo", "pool") for _ in range(4)]
        wsrc = I["w_out"].rearrange("(k p) n -> p k n", p=128)
        for i in range(4):
            self.dma("pool", wo[:, i * 4:(i + 1) * 4, :], wsrc[:, i * 4:(i + 1) * 4, :], [], [Bwo[i]], Bwo[i])
        mTg = [sb.alloc("mTg", [128, KC, 512], BF16)] * 2
        Bmg = [self.dbuf("mTg")] * 2
        xs = [sb.alloc("xs", [128, D], F32) for _ in range(2)]
        Bx = [self.dbuf("xs") for _ in range(2)]
        x1 = [sb.alloc("x1", [128, D], F32) for _ in range(2)]
        Bx1 = [self.dbuf("x1") for _ in range(2)]
        h2 = [sb.alloc("h2", [128, KC, 128], BF16) for _ in range(2)]
        Bh2 = [self.dbuf("h2") for _ in range(2)]
        rings = [self.norm_ring(sb)]
        rings.append(self.norm_ring(sb, junk=rings[0][0]))
        tq = [sb.alloc("tq", [128, 512], F32) for _ in range(2)]
        Btq = [Buf("tq%d" % i) for i in range(2)]
        rt = sb.alloc("rt", [128, 160], F32)
        Brt = Buf("rt")
        r2 = sb.alloc("r2", [128, 112], F32)
        Br2 = Buf("r2")
        selb = sb.alloc("selb", [128, 32], BF16)
        Bselb = Buf("selb")
        h2k = [sb.alloc("h2k", [128, D], BF16) for _ in range(2)]
        Bh2k = [Buf("h2k%d" % i) for i in range(2)]
        mtsrc = D_["MT"].rearrange("k p t -> p k t")
        qi = [0]
        for g in range(4):
            sg = g % 2
            self.dma("sp", mTg[sg][:], mtsrc[:, :, g * 512:(g + 1) * 512], self.scr_reads("MT", range(16)), [Bmg[sg]], Bmg[sg])
            for tt_ in range(4):
                tile = g * 4 + tt_
                sl = tile % 2
                self.dma("sp", xs[sl][:], I["xm"][tile * 128:(tile + 1) * 128, :], [], [Bx[sl]], Bx[sl])
                for cg in range(4):
                    cs_ = slice(cg * 512, (cg + 1) * 512)
                    fb = cg % 4
                    for kc in range(KC):
                        self.mm(self.F[fb][:, :], mTg[sg][:, kc, tt_ * 128:(tt_ + 1) * 128], wo[:, kc, cs_], kc == 0, kc == KC - 1,
                                [Bmg[sg], Bwo[kc // 4]], [self.BF[fb]])
                    i = qi[0] % 2
                    qi[0] += 1
                    self.tt(tq[i][:], self.F[fb][:, :], self.gt1b[:, cs_], ALU.mult, [self.BF[fb], self.B_c0], [Btq[i]])
                    self.tt(x1[sl][:, cs_], tq[i][:], xs[sl][:, cs_], ALU.add, [Btq[i], Bx[sl]], [Bx1[sl]], eng="dve")
                tok = Buf("w")
                self.scr_w.setdefault(("X1", tile), []).append(tok)
                self.dma("sp", D_["X1"][tile * 128:(tile + 1) * 128, :], x1[sl][:], [Bx1[sl]], [tok], Bx1[sl])
                self.norm_to_hT(x1[sl][:], Bx1[sl], h2[sl][:, :, :], Bh2[sl], self.a2, self.modc[:, 3, :], rings[sl])
                tok = Buf("w")
                self.scr_w.setdefault(("H2T", tile), []).append(tok)
                self.dma("sp", D_["H2T"][:, :, tile * 128:(tile + 1) * 128].rearrange("k p t -> p k t"), h2[sl][:], [Bh2[sl]], [tok], Bh2[sl])
                pr = self.F[5][:, 0:36]
                bpr = self.BF[5]
                for kc in range(KC):
                    self.mm(pr, h2[sl][:, kc, :], self.wr[:, kc, :], kc == 0, kc == KC - 1, [Bh2[sl], self.B_c0], [bpr])
                lg = rt[:, 0:36]
                self.tt(lg, pr, self.br[:], ALU.add, [bpr, self.B_c0], [Brt])
                mx = rt[:, 36:37]
                S.op("dve", lambda e: e.tensor_reduce(out=rt[:, 36:37], in_=rt[:, 0:4], axis=AX.X, op=ALU.max), [Brt], [Brt])
                self.ts(rt[:, 37:38], mx, -1.0, None, ALU.mult, None, [Brt], [Brt])
                self.act(rt[:, 40:44], rt[:, 0:4], AF.Exp, [Brt], [Brt], bias=rt[:, 37:38], accum=rt[:, 38:39])
                self.ts(rt[:, 44:48], rt[:, 0:4], mx, None, ALU.is_ge, None, [Brt], [Brt])
                self.ts(rt[:, 44:48], rt[:, 44:48], -1.0, -NEGBIG, ALU.add, ALU.mult, [Brt], [Brt])
                for gi in range(4):
                    self.ts(rt[:, 48 + gi * 8:56 + gi * 8], rt[:, 4 + gi * 8:12 + gi * 8], rt[:, 44 + gi:45 + gi], None, ALU.add, None, [Brt], [Brt])
                S.op("dve", lambda e: e.max(out=rt[:, 80:88], in_=rt[:, 48:80]), [Brt], [Brt])
                self.ts(rt[:, 88:120], rt[:, 48:80], rt[:, 81:82], None, ALU.is_ge, None, [Brt], [Brt])
                self.ts(rt[:, 39:40], rt[:, 80:81], -1.0, None, ALU.mult, None, [Brt], [Brt])
                self.act(rt[:, 120:152], rt[:, 48:80], AF.Exp, [Brt], [Brt], bias=rt[:, 39:40])
                self.tt(rt[:, 120:152], rt[:, 120:152], rt[:, 88:120], ALU.mult, [Brt], [Brt])
                S.op("dve", lambda e: e.reduce_sum(out=rt[:, 152:153], in_=rt[:, 120:152], axis=AX.X), [Brt], [Brt])
                self.tt(rt[:, 152:153], rt[:, 152:153], rt[:, 38:39], ALU.mult, [Brt], [Brt])
                S.op("dve", lambda e: e.reciprocal(out=rt[:, 153:154], in_=rt[:, 152:153]), [Brt], [Brt])
                self.ts(self.wfull[:, tile, :], rt[:, 120:152], rt[:, 153:154], None, ALU.mult, None, [Brt], [self.B_wfull[tile]])
                self.cp("dve", selb[:], rt[:, 88:120], [Brt], [Bselb])
                pc = self.F[4]
                self.mm(pc[:, 0:32], self.slt[:, :], selb[:], True, True, [Bselb, self.B_c0], [self.BF[4]])
                self.mm(pc[:, 32:64], self.ones[:, :], selb[:], True, True, [Bselb, self.B_c0], [self.BF[4]])
                self.tt(r2[:, 0:32], pc[:, 0:32], self.tot[:], ALU.add, [self.BF[4], self.B_tot], [Br2])
                self.tt(self.tot[:], pc[:, 32:64], self.tot[:], ALU.add, [self.BF[4], self.B_tot], [self.B_tot])
                self.ts(r2[:, 0:32], r2[:, 0:32], float(CAP - 1), None, ALU.min, None, [Br2], [Br2])
                self.tt(r2[:, 0:32], r2[:, 0:32], self.ebase[:], ALU.add, [Br2, self.B_c0], [Br2])
                self.stt(r2[:, 32:64], r2[:, 0:32], 1.0e6, rt[:, 88:120], ALU.add, ALU.mult, [Br2, Brt], [Br2])
                S.op("dve", lambda e: e.max(out=r2[:, 64:72], in_=r2[:, 32:64]), [Br2], [Br2])
                self.ts(r2[:, 72:74], r2[:, 64:66], -1.0e6, None, ALU.add, None, [Br2], [Br2])
                self.cp("dve", self.desti[:, tile, :], r2[:, 72:74], [Br2], [self.B_dest[tile]])
                for k_ in range(2):
                    self.ts(r2[:, 80:112], r2[:, 32:64], r2[:, 64 + k_:65 + k_], None, ALU.is_equal, None, [Br2], [Br2])
                    self.tt(r2[:, 80:112], r2[:, 80:112], self.wfull[:, tile, :], ALU.mult, [Br2, self.B_wfull[tile]], [Br2])
                    S.op("dve", lambda e, k_=k_, tile=tile: e.reduce_sum(out=self.wk[:, tile, k_:k_ + 1], in_=r2[:, 80:112], axis=AX.X),
                         [Br2], [self.B_dest[tile]])
                xn_t, bxn_t = h2k[sl], Bh2k[sl]
                self.tt(xn_t[:], rings[sl][4][:], self.a2b[:], ALU.mult, [rings[sl][7], self.B_c0], [bxn_t])
                self.tt(xn_t[:], xn_t[:], self.sh2bb[:], ALU.add, [bxn_t, self.B_c0], [bxn_t])
                for k_ in range(2):
                    self.cp("dve", self.six[sl][:, k_:k_ + 1], self.desti[:, tile, k_:k_ + 1], [self.B_dest[tile]], [self.B_six[sl][k_]])
                    S.op("pool", lambda e, k_=k_, ix=self.six[sl], xn_t=xn_t: e.indirect_dma_start(
                        out=D_["XD"], out_offset=bass.IndirectOffsetOnAxis(ap=ix[:, k_:k_ + 1], axis=0),
                        in_=xn_t[:], in_offset=None),
                        [bxn_t, self.B_six[sl][k_]], [Buf("sc"), self.B_ind], dbuf=self.B_ind)
        if self.debug:
            self.dma("sp", D_["WF"], self.wfull[:].rearrange("p a b -> p (a b)"), self.B_wfull, [Buf("x")], self.B_wfull[0])
        sb.pop()
        self.release(Bwo + Bmg[:1] + Bx + Bx1 + Bh2)

    def stage5(self):
        S, sb, I = self.S, self.sb, self.I
        D_ = self.dram
        sb.push()
        w1 = [sb.alloc("w1", [128, KC, 512], BF16) for _ in range(2)]
        w3 = [sb.alloc("w3", [128, KC, 512], BF16) for _ in range(2)]
        w2 = sb.alloc("w2", [128, 4, D], BF16)
        Bw1 = [self.dbuf("w1", "pool") for _ in range(2)]
        Bw3 = [self.dbuf("w3", "pool") for _ in range(2)]
        Bw2 = self.dbuf("w2", "pool")
        nrt = CAP // 128
        xr = [sb.alloc("xr", [128, D], BF16) for _ in range(nrt)]
        Bxr = [self.dbuf("xr") for _ in range(nrt)]
        XT = [sb.alloc("XT", [128, KC, CAP], BF16) for _ in range(2)]
        BXT = [[Buf("XT%d_%d" % (i, j)) for j in range(nrt)] for i in range(2)]
        s1 = [sb.alloc("s1", [128, CAP], F32) for _ in range(2)]
        Bs1 = [Buf("s1_%d" % i) for i in range(2)]
        hid = [sb.alloc("hid", [128, 4, CAP], BF16) for _ in range(2)]
        Bhid = [[Buf("hid%d_%d" % (i, f)) for f in range(4)] for i in range(2)]
        ys = [sb.alloc("ys", [128, D], F32)] * 2
        Bys = [self.dbuf("ys")] * 2
        si = [0]
        yi = [0]

        def loads(ex):
            for r_ in range(nrt):
                row0 = ex * CAP + r_ * 128
                self.dma("sp", xr[r_][:], D_["XD"][row0:row0 + 128, :], [], [Bxr[r_]], Bxr[r_])

        def trans(ex):
            sl = ex % 2
            for r_ in range(nrt):
                for tb in range(2):
                    for ts_ in range(8):
                        kc = tb * 8 + ts_
                        self.tr(self.T[tb][:, ts_ * 128:(ts_ + 1) * 128], xr[r_][:, kc * 128:(kc + 1) * 128], [Bxr[r_]], [self.BT[tb][0]])
                    src = self.T[tb][:, :].rearrange("p (k t) -> p k t", t=128)
                    dst = XT[sl][:, tb * 8:(tb + 1) * 8, r_ * 128:(r_ + 1) * 128]
                    self.cp("act" if tb == 0 else "dve", dst, src, [self.BT[tb][0]], [BXT[sl][r_]])

        loads(0)
        trans(0)
        for ex in range(32):
            sl = ex % 2
            self.dma("pool", w1[sl][:], I["w1"][ex].rearrange("(k p) n -> p k n", p=128), [], [Bw1[sl]], Bw1[sl])
            self.dma("pool", w3[sl][:], I["w3"][ex].rearrange("(k p) n -> p k n", p=128), [], [Bw3[sl]], Bw3[sl])
            self.dma("pool", w2[:], I["w2"][ex].rearrange("(k p) n -> p k n", p=128), [], [Bw2], Bw2)
            if ex + 1 < 32:
                loads(ex + 1)
            for fc in range(4):
                fs = slice(fc * 128, (fc + 1) * 128)
                f1, f3 = (fc % 2) * 2, (fc % 2) * 2 + 1
                for kc in range(KC):
                    self.mm(self.F[f1][:, 0:CAP], w1[sl][:, kc, fs], XT[sl][:, kc, :], kc == 0, kc == KC - 1, [Bw1[sl]] + BXT[sl], [self.BF[f1]])
                for kc in range(KC):
                    self.mm(self.F[f3][:, 0:CAP], w3[sl][:, kc, fs], XT[sl][:, kc, :], kc == 0, kc == KC - 1, [Bw3[sl]] + BXT[sl], [self.BF[f3]])
                i = si[0] % 2
                si[0] += 1
                self.act(s1[i][:], self.F[f1][:, 0:CAP], AF.Silu, [self.BF[f1]], [Bs1[i]])
                self.tt(hid[sl][:, fc, :], s1[i][:], self.F[f3][:, 0:CAP], ALU.mult, [Bs1[i], self.BF[f3]], [Bhid[sl][fc]])
            if ex + 1 < 32:
                trans(ex + 1)
            for r_ in range(nrt):
                y_ = ys[yi[0] % 2]
                by = Bys[yi[0] % 2]
                yi[0] += 1
                for cg in range(4):
                    cs_ = slice(cg * 512, (cg + 1) * 512)
                    fb = 4 + (cg % 2)
                    for fc in range(4):
                        self.mm(self.F[fb][:, :], hid[sl][:, fc, r_ * 128:(r_ + 1) * 128], w2[:, fc, cs_], fc == 0, fc == 3,
                                [Bhid[sl][fc], Bw2], [self.BF[fb]])
                    if fb == 4:
                        self.cp("act", y_[:, cs_], self.F[fb][:, :], [self.BF[fb]], [by])
                    else:
                        self.cp("dve", y_[:, cs_], self.F[fb][:, :], [self.BF[fb]], [by])
                row0 = ex * CAP + r_ * 128
                for hf in range(2):
                    self.dma("sp", D_["YD%d" % hf][row0:row0 + 128, :], y_[:, hf * 1024:(hf + 1) * 1024], [by], [Buf("y")], by)
        sb.pop()
        self.release(Bw1 + Bw3 + [Bw2] + Bxr + Bys[:1])
        self.bar()
        sb.push()
        xs = [sb.alloc("xs", [128, D], F32) for _ in range(2)]
        Bx = [self.dbuf("xs") for _ in range(2)]
        yg = [[[sb.alloc("yg", [128, 1024], F32) for _ in range(2)] for _ in range(2)] for _ in range(2)]
        Byg = [[[Buf("yg") for _ in range(2)] for _ in range(2)] for _ in range(2)]
        jk = sb.alloc("jk5", [128, D], BF16)
        st5 = sb.alloc("st5", [128, 4], F32)
        Bst = Buf("st5")
        for tile in range(16):
            sl = tile % 2
            self.dma("sp", xs[sl][:], D_["X1"][tile * 128:(tile + 1) * 128, :], self.scr_reads("X1", [tile]), [Bx[sl]], Bx[sl])
            for k_ in range(2):
                self.cp("dve", self.gix[sl][:, k_:k_ + 1], self.desti[:, tile, k_:k_ + 1], [self.B_dest[tile]], [self.B_gix[sl][k_]])
                for hf in range(2):
                    S.op("pool", lambda e, k_=k_, ix=self.gix[sl], dst=yg[sl][k_][hf], hf=hf: e.indirect_dma_start(
                        out=dst[:], out_offset=None, in_=D_["YD%d" % hf],
                        in_offset=bass.IndirectOffsetOnAxis(ap=ix[:, k_:k_ + 1], axis=0)),
                        [self.B_gix[sl][k_]], [Byg[sl][k_][hf], self.B_ind], dbuf=self.B_ind)
            for hf in range(2):
                a, b = yg[sl][0][hf], yg[sl][1][hf]
                ba, bb = Byg[sl][0][hf], Byg[sl][1][hf]
                hs_ = slice(hf * 1024, (hf + 1) * 1024)
                self.ts(a[:], a[:], self.wk[:, tile, 0:1], None, ALU.mult, None, [ba, self.B_dest[tile]], [ba])
                self.stt(a[:], b[:], self.wk[:, tile, 1:2], a[:], ALU.mult, ALU.add, [bb, ba, self.B_dest[tile]], [ba])
                self.tt(a[:], a[:], self.gt2b[:, hs_], ALU.mult, [ba, self.B_c0], [ba])
                self.tt(xs[sl][:, hs_], xs[sl][:, hs_], a[:], ALU.add, [Bx[sl], ba], [Bx[sl]])
            self.act(jk[:], xs[sl][:], AF.Square, [Bx[sl]], [Bst], accum=st5[:, 0:1])
            self.rstd(st5[:, 2:3], st5[:, 0:1], float(D), [Bst], [Bst, Bst], st5[:, 1:2])
            self.stt(xs[sl][:], xs[sl][:], st5[:, 2:3], self.fgb[:], ALU.mult, ALU.mult, [Bx[sl], Bst, self.B_c0], [Bx[sl]])
            self.dma("sp", self.out[tile * 128:(tile + 1) * 128, :], xs[sl][:], [Bx[sl]], [Buf("o")], Bx[sl])
        sb.pop()
        self.release(Bx)


def host_consts():
    bf = ml_dtypes.bfloat16
    c = {}
    c["k_ident"] = np.eye(128, dtype=np.float32)
    cm = np.zeros((128, 512), np.float32)
    for j in range(2):
        kp = np.arange(128)[:, None] + j * 128
        q = np.arange(256)[None, :]
        cm[:, j * 256:(j + 1) * 256] = np.where(kp <= q, 0.0, NEG)
    c["k_cm"] = cm
    cp = np.zeros((128, 16, 16), np.float32)
    for qt in range(16):
        cp[:, qt, 8 + qt // 2:] = NEGBIG
    c["k_cpen"] = cp.reshape(128, 256)
    sel = np.zeros((16, 16, 128), np.float32)
    for i in range(16):
        sel[i, i, :] = 1.0
    c["k_sel"] = sel.reshape(16, 2048)
    s = np.arange(128)[:, None]
    t = np.arange(128)[None, :]
    same = (s // 32) == (t // 32)
    mid = (t // 32) * 32 + 15
    wm = np.where(same & (s > mid) & (s <= t), 1.0, 0.0) - np.where(same & (s > t) & (s <= mid), 1.0, 0.0)
    wmid = np.zeros((128, 4), np.float32)
    wtot = np.zeros((128, 4), np.float32)
    for cc in range(4):
        sl = np.arange(128)
        wmid[:, cc] = ((sl // 32) == cc) & (sl <= cc * 32 + 15)
        wtot[:, cc] = (sl // 32) == cc
    c["k_wall"] = np.concatenate([wm, wmid, wtot], axis=1).astype(np.float32)
    c["k_wl"] = np.where(same & (s > t), 1.0, 0.0).astype(np.float32)
    c["k_bd"] = np.where(same & (s <= t), 1.0, 0.0).astype(np.float32)
    c["k_slt"] = np.where(s < t, 1.0, 0.0).astype(np.float32)
    c["k_ones"] = np.ones((128, 128), np.float32)
    c["k_ebase"] = np.tile((np.arange(32) * CAP).astype(np.float32)[None, :], (128, 1))
    return c


_NC_CACHE = {}


def get_nc(debug=False, stages="012345"):
    key = (debug, stages)
    if key not in _NC_CACHE:
        nc = bass.Bass("TRN2", target_bir_lowering=False)
        kb = K(nc, debug=debug, stages=stages)
        kb.build()
        _NC_CACHE[key] = (nc, kb)
    return _NC_CACHE[key]


def make_in_maps(inputs, cores=range(8)):
    f = lambda a: np.ascontiguousarray(np.asarray(a, dtype=np.float32))
    x = f(inputs["x"])
    c = f(inputs["c"])
    shared = {
        "norm1_g": f(inputs["norm1_g"])[0], "norm2_g": f(inputs["norm2_g"])[0], "final_g": f(inputs["final_g"]),
        "w_ada": f(inputs["w_ada"])[0], "b_ada": f(inputs["b_ada"])[0], "w_in": f(inputs["w_in"])[0],
        "r_lower": f(inputs["r_lower"]), "r_norm_g": f(inputs["r_norm_g"])[0],
        "w_up_a": f(inputs["w_up_a"])[0], "w_up_r": f(inputs["w_up_r"])[0], "w_out": f(inputs["w_out"])[0],
        "w_rg": f(inputs["w_rg"])[0], "b_rg": f(inputs["b_rg"])[0], "w_re": f(inputs["w_re"])[0], "b_re": f(inputs["b_re"])[0],
        "w1": f(inputs["w1"])[0], "w3": f(inputs["w3"])[0], "w2": f(inputs["w2"])[0],
    }
    shared.update(host_consts())
    maps = []
    for core in cores:
        b, half = core // 2, core % 2
        m = dict(shared)
        m["xm"] = np.ascontiguousarray(x[b, half * NT:(half + 1) * NT])
        m["xp"] = np.ascontiguousarray(x[b, 0:NT])
        m["c"] = np.ascontiguousarray(c[b])
        m["pm"] = np.full((128, 1), float(half), np.float32)
        maps.append(m)
    return maps


def kernel(**inputs):
    nc, _ = get_nc()
    maps = make_in_maps(inputs)
    res = run_bass_kernel_spmd(nc, maps, core_ids=list(range(8)))
    out = np.zeros((4, 4096, D), np.float32)
    for core in range(8):
        b, half = core // 2, core % 2
        out[b, half * NT:(half + 1) * NT] = np.asarray(res.results[core]["out"], dtype=np.float32)
    return out
```

```python
import contextlib
import numpy as np
import ml_dtypes
import concourse.bass as bass
import concourse.mybir as mybir
from concourse.bass_utils import run_bass_kernel_spmd

F32 = mybir.dt.float32
BF16 = mybir.dt.bfloat16
I32 = mybir.dt.int32
CAP = 512
NR = 32 * CAP
ALU = mybir.AluOpType
AF = mybir.ActivationFunctionType
AX = mybir.AxisListType

D = 2048
NT = 2048
KC = 16
EPS = 1e-6
NEG = -30000.0
NEGBIG = -1.0e30
ENGS = ("pe", "act", "dve", "pool", "sp")
SAME_ENGINE_SYNC = {"pe": False, "act": True, "dve": True, "pool": True, "sp": False}


class Buf:
    __slots__ = ("name", "last_w", "readers", "dsem", "dcount", "const", "last_dma", "excl")

    def __init__(self, name, const=False, excl=False):
        self.excl = excl
        self.name = name
        self.last_w = None
        self.readers = []
        self.dsem = None
        self.dcount = 0
        self.const = const
        self.last_dma = None


class Op:
    __slots__ = ("eng", "fn", "deps", "sig", "sem", "val", "dbuf")

    def __init__(self, eng, fn, dbuf=None):
        self.eng = eng
        self.fn = fn
        self.deps = []
        self.sig = dbuf is not None
        self.sem = None
        self.val = 0
        self.dbuf = dbuf


class Sched:
    def __init__(self, nc):
        self.nc = nc
        self.ops = {e: [] for e in ENGS}
        self.dma_bufs = []
        self.epoch = None
        self.all_ops = []

    def barrier(self, fn):
        o = Op("dve", fn, None)
        deps = {}
        for e in ENGS:
            if self.ops[e]:
                d = self.ops[e][-1]
                deps[id(d)] = d
        for b in self.dma_bufs:
            if b.last_dma is not None:
                deps[id(b.last_dma)] = b.last_dma
        o.deps = list(deps.values())
        self.ops["dve"].append(o)
        self.all_ops.append(o)
        self.epoch = o
        return o

    def op(self, eng, fn, reads=(), writes=(), dbuf=None, extra=()):
        o = Op(eng, fn, dbuf)
        deps = {}
        for d in extra:
            if d is not None:
                deps[id(d)] = d
        if self.epoch is not None:
            deps[id(self.epoch)] = self.epoch
        xr = [b for b in reads if b.excl]
        if xr:
            reads = [b for b in reads if not b.excl]
            writes = list(writes) + [b for b in xr if b not in writes]
        for b in reads:
            if b.last_w is not None:
                deps[id(b.last_w)] = b.last_w
        for b in writes:
            if b.last_w is not None:
                deps[id(b.last_w)] = b.last_w
            for r in b.readers:
                deps[id(r)] = r
        o.deps = list(deps.values())
        for b in writes:
            b.last_w = o
            b.readers = []
        for b in reads:
            if not b.const and b.last_w is not o:
                b.readers.append(o)
        if dbuf is not None:
            dbuf.last_dma = o
            if dbuf.dsem is None:
                dbuf.dsem = True
                self.dma_bufs.append(dbuf)
        self.ops[eng].append(o)
        self.all_ops.append(o)
        return o

    def emit(self, stack):
        nc = self.nc
        for e in ENGS:
            for o in self.ops[e]:
                keep = []
                for d in o.deps:
                    if d.dbuf is None and d.eng == o.eng and not SAME_ENGINE_SYNC[o.eng]:
                        continue
                    keep.append(d)
                    d.sig = True
                o.deps = keep
        esem = {}
        for e in ENGS:
            if any(o.sig and o.dbuf is None for o in self.ops[e]):
                esem[e] = stack.enter_context(nc.semaphore("s_" + e))
        for i, b in enumerate(self.dma_bufs):
            b.dsem = stack.enter_context(nc.semaphore("d%d" % i))
            b.dcount = 0
        cnt = {e: 0 for e in ENGS}
        for o in self.all_ops:
            if o.dbuf is not None:
                o.dbuf.dcount += 16
                o.sem = o.dbuf.dsem
                o.val = o.dbuf.dcount
            elif o.sig:
                cnt[o.eng] += 1
                o.sem = esem[o.eng]
                o.val = cnt[o.eng]
        block = stack.enter_context(nc.Block())
        handles = {"pe": block.tensor, "act": block.scalar, "dve": block.vector,
                   "pool": block.gpsimd, "sp": block.sync}
        for e in ENGS:
            ops = self.ops[e]
            if not ops:
                continue

            def body(eng, ops=ops):
                waited = {}
                for o in ops:
                    need = {}
                    for d in o.deps:
                        k = id(d.sem)
                        if d.val > need.get(k, (0, None))[0]:
                            need[k] = (d.val, d.sem)
                    for k, (v, sem) in need.items():
                        if waited.get(k, 0) >= v:
                            continue
                        waited[k] = v
                        eng.wait_ge(sem, v)
                    ins = o.fn(eng)
                    if o.sig:
                        ins.then_inc(o.sem, 16 if o.dbuf is not None else 1)

            handles[e](body)


class SbAlloc:
    def __init__(self, nc, base=17408, limit=229000):
        self.nc = nc
        self.top = base
        self.limit = limit
        self.n = 0
        self.marks = []

    def push(self):
        self.marks.append(self.top)

    def pop(self):
        self.top = self.marks.pop()

    def alloc(self, name, shape, dtype):
        sz = int(np.prod(shape[1:])) * (4 if dtype in (F32, I32) else 2)
        off = (self.top + 63) // 64 * 64
        assert off + sz <= self.limit, (name, off, sz, self.limit)
        self.top = off + sz
        self.n += 1
        return self.nc.alloc_sbuf_tensor_at("%s_%d" % (name, self.n), list(shape), dtype, offset=off)


class K:
    def __init__(self, nc, debug=False, stages="012345"):
        self.nc = nc
        self.S = Sched(nc)
        self.sb = SbAlloc(nc)
        self.debug = debug
        self.stages = stages
        self.dpool = {}
        self.bkind = {}
        self.dram = {}
        self.sbufs = {}
        self.scr_w = {}

    def din(self, name, shape, dt=F32):
        return self.nc.dram_tensor(name, list(shape), dt, kind="ExternalInput").ap()

    def dscr(self, name, shape, dt):
        kind = "ExternalOutput" if self.debug else "Internal"
        t = self.nc.dram_tensor(name, list(shape), dt, kind=kind).ap()
        self.dram[name] = t
        return t

    def dbuf(self, name, kind="sp"):
        pool = self.dpool.setdefault(kind, [])
        if pool:
            return pool.pop()
        b = Buf(name)
        self.bkind[id(b)] = kind
        return b

    def release(self, bufs):
        for b in bufs:
            self.dpool[self.bkind[id(b)]].append(b)

    def dma(self, q, out, in_, reads, writes, dbuf, **kw):
        k = self.bkind.setdefault(id(dbuf), q)
        assert k == q, (dbuf.name, k, q)
        return self.S.op(q, lambda e: e.dma_start(out=out, in_=in_, **kw), reads, writes, dbuf=dbuf)

    def mm(self, out, lhsT, rhs, start, stop, reads, writes, tp=None):
        if tp is not None:
            return self.S.op("pe", lambda e: e.matmul(out, lhsT=lhsT, rhs=rhs, start=start, stop=stop, tile_position=tp), reads, writes)
        return self.S.op("pe", lambda e: e.matmul(out, lhsT=lhsT, rhs=rhs, start=start, stop=stop), reads, writes)

    def tr(self, out, in_, reads, writes):
        idn = self.identb
        n = in_.shape[0]
        return self.S.op("pe", lambda e: e.transpose(out=out, in_=in_, identity=idn[0:n, 0:n]),
                         list(reads) + [self.B_c0], writes)

    def act(self, out, in_, func, reads, writes, scale=1.0, bias=None, accum=None):
        kw = {}
        if bias is not None:
            kw["bias"] = bias
        if accum is not None:
            kw["accum_out"] = accum
        return self.S.op("act", lambda e: e.activation(out=out, in_=in_, func=func, scale=scale, **kw), reads, writes)

    def ts(self, out, in0, s1, s2, op0, op1, reads, writes, eng="dve"):
        if s2 is None:
            return self.S.op(eng, lambda e: e.tensor_scalar(out=out, in0=in0, scalar1=s1, scalar2=None, op0=op0), reads, writes)
        return self.S.op(eng, lambda e: e.tensor_scalar(out=out, in0=in0, scalar1=s1, scalar2=s2, op0=op0, op1=op1), reads, writes)

    def tt(self, out, in0, in1, op, reads, writes, eng="dve"):
        return self.S.op(eng, lambda e: e.tensor_tensor(out=out, in0=in0, in1=in1, op=op), reads, writes)

    def stt(self, out, in0, scalar, in1, op0, op1, reads, writes, eng="dve"):
        return self.S.op(eng, lambda e: e.scalar_tensor_tensor(out=out, in0=in0, scalar=scalar, in1=in1, op0=op0, op1=op1), reads, writes)

    def cp(self, eng, out, in_, reads, writes):
        if eng == "act":
            return self.S.op("act", lambda e: e.copy(out=out, in_=in_), reads, writes)
        return self.S.op(eng, lambda e: e.tensor_copy(out=out, in_=in_), reads, writes)

    def rstd(self, out, ssq, n, reads, writes, tmp):
        eps = self.epsc
        self.act(tmp, ssq, AF.Sqrt, list(reads) + [self.B_c0], [writes[1]], scale=1.0 / n, bias=eps[:, 0:1])
        self.S.op("dve", lambda e: e.reciprocal(out=out, in_=tmp), [writes[1]], [writes[0]])

    def build(self):
        nc = self.nc
        sb = self.sb
        S = self.S
        I = {}
        I["xm"] = self.din("xm", [NT, D])
        I["xp"] = self.din("xp", [NT, D])
        I["c"] = self.din("c", [D])
        I["pm"] = self.din("pm", [128, 1])
        I["norm1_g"] = self.din("norm1_g", [D])
        I["norm2_g"] = self.din("norm2_g", [D])
        I["final_g"] = self.din("final_g", [D])
        I["w_ada"] = self.din("w_ada", [D, 6 * D])
        I["b_ada"] = self.din("b_ada", [6 * D])
        I["w_in"] = self.din("w_in", [D, 11264])
        I["r_lower"] = self.din("r_lower", [2, 1024])
        I["r_norm_g"] = self.din("r_norm_g", [128])
        I["w_up_a"] = self.din("w_up_a", [1024, D])
        I["w_up_r"] = self.din("w_up_r", [1024, D])
        I["w_out"] = self.din("w_out", [D, D])
        I["w_rg"] = self.din("w_rg", [D, 4])
        I["b_rg"] = self.din("b_rg", [4])
        I["w_re"] = self.din("w_re", [D, 32])
        I["b_re"] = self.din("b_re", [32])
        I["w1"] = self.din("w1", [32, D, 512])
        I["w3"] = self.din("w3", [32, D, 512])
        I["w2"] = self.din("w2", [32, 512, D])
        I["k_ident"] = self.din("k_ident", [128, 128])
        I["k_cm"] = self.din("k_cm", [128, 512])
        I["k_cpen"] = self.din("k_cpen", [128, 256])
        I["k_sel"] = self.din("k_sel", [16, 2048])
        I["k_wall"] = self.din("k_wall", [128, 136])
        I["k_wl"] = self.din("k_wl", [128, 128])
        I["k_bd"] = self.din("k_bd", [128, 128])
        I["k_slt"] = self.din("k_slt", [128, 128])
        I["k_ones"] = self.din("k_ones", [128, 128])
        I["k_ebase"] = self.din("k_ebase", [128, 32])
        self.I = I
        self.out = nc.dram_tensor("out", [NT, D], F32, kind="ExternalOutput").ap()
        self.dscr("MOD", [6 * D], F32)
        self.dscr("QT", [8, 128, NT], BF16)
        self.dscr("KT", [8, 128, 2 * NT], BF16)
        self.dscr("V", [2 * NT, 1024], BF16)
        self.dscr("RQT", [8, 128, NT], BF16)
        self.dscr("RF", [2 * NT, 1024], F32)
        self.dscr("RI", [2 * NT, 1024], BF16)
        self.dscr("ROG", [NT, 1024], BF16)
        self.dscr("GAT", [16, 128, NT], BF16)
        self.dscr("GRT", [16, 128, NT], BF16)
        self.dscr("MT", [16, 128, NT], BF16)
        self.dscr("X1", [NT, D], F32)
        self.dscr("H2T", [16, 128, NT], BF16)
        self.dscr("XD", [NR, D], BF16)
        self.dscr("YD0", [NR, 1024], F32)
        self.dscr("YD1", [NR, 1024], F32)
        if self.debug:
            self.dscr("ATT", [8, 128, NT], BF16)
            self.dscr("OT", [8, 128, NT], BF16)
            self.dscr("WF", [128, 16 * 32], F32)
        self.scrB = {}

        with contextlib.ExitStack() as st:
            self.F = [st.enter_context(nc.psum_tensor("F%d" % i, [128, 512], F32)) for i in range(6)]
            self.T = [st.enter_context(nc.psum_tensor("T%d" % i, [128, 1024], BF16)) for i in range(2)]
            self.BF = [Buf("F%d" % i, excl=True) for i in range(6)]
            self.BFs = [[self.BF[i]] * 4 for i in range(6)]
            _bt = [Buf("T%d" % i, excl=True) for i in range(2)]
            self.BT = [[_bt[i]] * 8 for i in range(2)]
            self.dummy = sb.alloc("dummy", [128, 8], F32)
            self.stage0()
            self.bar()
            if "1" in self.stages:
                self.stage1()
                self.bar()
            sb.push()
            self.attT = sb.alloc("attT", [128, 8, NT], BF16)
            self.oT = sb.alloc("oT", [128, 8, NT], BF16)
            self.B_attT = [Buf("attT%d" % h) for h in range(8)]
            self.B_oT = [Buf("oT%d" % t) for t in range(16)]
            if "2" in self.stages:
                self.stage2()
                self.bar()
            if "3" in self.stages:
                self.stage3()
                self.bar()
            if "4" in self.stages:
                self.stage4a()
                self.bar()
            sb.pop()
            if "4" in self.stages:
                self.stage4b()
                self.bar()
            if "5" in self.stages:
                self.stage5()
            allb = [b for b in self.S.dma_bufs]
            S.op("sp", lambda e: e.nop(), [], allb)
            S.emit(st)
        return nc

    def bar(self):
        d = self.dummy
        self.S.barrier(lambda e: e.memset(d[:], 0.0))

    def sB(self, name, key):
        k = (name, key)
        if k not in self.scrB:
            self.scrB[k] = Buf("%s%s" % (name, key))
        return self.scrB[k]

    def stage0(self):
        S, sb, I = self.S, self.sb, self.I
        cbs = {"sp": [Buf("c%d" % i) for i in range(4)], "pool": [Buf("cp%d" % i) for i in range(2)]}
        cb = cbs["sp"] + cbs["pool"]
        self.B_c0 = Buf("constall")
        ci = [0]

        def cload(q, dst, src, **kw):
            lst = cbs[q]
            b = lst[ci[0] % len(lst)]
            ci[0] += 1
            self.dma(q, dst, src, [], [b], b, **kw)

        def A(name, shape, dt=F32):
            t = sb.alloc(name, shape, dt)
            return t

        self.identb = A("identb", [128, 128], BF16)
        self.ident32 = A("ident32", [128, 128])
        self.rows = A("rows", [48, 128])
        self.cm = A("cm", [128, 512], BF16)
        self.cpen = A("cpen", [128, 256])
        self.selc = A("selc", [16, 2048], BF16)
        self.wall = A("wall", [128, 136])
        self.wl = A("wl", [128, 128])
        self.bd = A("bd", [128, 128])
        self.epsc = A("epsc", [128, 1])
        self.pm = A("pm", [128, 1])
        self.ppen = A("ppen", [128, 1])
        self.g1c = A("g1c", [128, 16])
        self.g2c = A("g2c", [128, 16])
        self.ccol = A("ccol", [128, 16])
        self.cact = A("cact", [128, 16], BF16)
        self.modc = A("modc", [128, 6, 16])
        self.a1 = A("a1", [128, 16])
        self.a2 = A("a2", [128, 16])
        self.gt1b = A("gt1b", [128, D])
        self.gt2b = A("gt2b", [128, D])
        self.fgb = A("fgb", [128, D])
        self.lb = A("lb", [128, 1024])
        self.oml = A("oml", [128, 1024])
        self.rngb = A("rngb", [128, 128])
        self.wr = A("wr", [128, 16, 36], BF16)
        self.br = A("br", [128, 36])
        self.wfull = A("wfull", [128, 16, 32])
        self.slt = A("slt", [128, 128], BF16)
        self.a2b = A("a2b", [128, D], BF16)
        self.sh2bb = A("sh2bb", [128, D], BF16)
        self.ones = A("ones", [128, 128], BF16)
        self.ebase = A("ebase", [128, 32])
        self.tot = A("tot", [128, 32])
        self.desti = A("desti", [128, 16, 2], I32)
        self.wk = A("wk", [128, 16, 2])
        self.B_tot = Buf("tot")
        self.B_ind = Buf("ind")
        self.six = [A("six", [128, 2], I32) for _ in range(2)]
        self.gix = [A("gix", [128, 2], I32) for _ in range(2)]
        self.B_six = [[Buf("six%d_%d" % (i, k)) for k in range(2)] for i in range(2)]
        self.B_gix = [[Buf("gix%d_%d" % (i, k)) for k in range(2)] for i in range(2)]
        self.B_dest = [Buf("dest%d" % t) for t in range(16)]
        self.B_wfull = [Buf("wfull%d" % t) for t in range(16)]
        cload("pool", self.identb[:], I["k_ident"])
        cload("sp", self.ident32[:], I["k_ident"])
        cload("pool", self.cm[:], I["k_cm"])
        cload("sp", self.cpen[:], I["k_cpen"])
        cload("pool", self.selc[:], I["k_sel"])
        cload("sp", self.wall[:], I["k_wall"])
        cload("sp", self.wl[:], I["k_wl"])
        cload("sp", self.bd[:], I["k_bd"])
        cload("sp", self.pm[:], I["pm"])
        cload("sp", self.rows[0:16, :], I["norm1_g"].rearrange("(k p) -> k p", p=128))
        cload("sp", self.rows[16:32, :], I["norm2_g"].rearrange("(k p) -> k p", p=128))
        cload("sp", self.rows[32:48, :], I["c"].rearrange("(k p) -> k p", p=128))
        cload("sp", self.fgb[:], I["final_g"].partition_broadcast(128))
        cload("sp", self.lb[:], I["r_lower"][0, :].partition_broadcast(128))
        cload("sp", self.oml[:], I["r_lower"][1, :].partition_broadcast(128))
        cload("sp", self.rngb[:], I["r_norm_g"].partition_broadcast(128))
        cload("pool", self.wr[:, :, 0:4], I["w_rg"].rearrange("(k p) n -> p k n", p=128))
        cload("pool", self.wr[:, :, 4:36], I["w_re"].rearrange("(k p) n -> p k n", p=128))
        cload("pool", self.slt[:], I["k_slt"])
        cload("pool", self.ones[:], I["k_ones"])
        cload("sp", self.ebase[:], I["k_ebase"])
        cload("sp", self.br[:, 0:4], I["b_rg"].partition_broadcast(128))
        cload("sp", self.br[:, 4:36], I["b_re"].partition_broadcast(128))
        cs = Buf("cs")
        S.op("dve", lambda e: e.memset(self.epsc[:], EPS), [], [cs])
        S.op("pe", lambda e: e.transpose(out=self.F[2][:, 0:48], in_=self.rows[:, :], identity=self.ident32[0:48, 0:48]), cb, [self.BF[2]])
        self.cp("dve", self.g1c[:], self.F[2][:, 0:16], [self.BF[2]], [cs])
        self.cp("dve", self.g2c[:], self.F[2][:, 16:32], [self.BF[2]], [cs])
        self.cp("dve", self.ccol[:], self.F[2][:, 32:48], [self.BF[2]], [cs])
        S.op("dve", lambda e: e.memset(self.tot[:], 0.0), [], [self.B_tot])
        self.ts(self.ppen[:], self.pm[:], -1.0, -NEGBIG, ALU.add, ALU.mult, cb, [cs])
        self.tt(self.lb[:], self.lb[:], self.oml[:], ALU.subtract, cb, [cs])
        self.act(self.lb[:], self.lb[:], AF.Sigmoid, [cs], [cs])
        self.ts(self.oml[:], self.lb[:], -1.0, 1.0, ALU.mult, ALU.add, [cs], [cs])
        self.act(self.cact[:], self.ccol[:], AF.Silu, [cs], [cs])
        sb.push()
        MOD = self.dram["MOD"]
        wsl = [sb.alloc("wsl0", [128, KC, 512], BF16) for _ in range(3)]
        Bw = [self.dbuf("wsl", "pool") for _ in range(3)]
        bsl = [sb.alloc("bsl", [1, 512], F32) for _ in range(2)]
        Bb = [self.dbuf("bsl") for _ in range(2)]
        msl = [sb.alloc("msl", [1, 512], F32) for _ in range(2)]
        Bm = [self.dbuf("msl") for _ in range(2)]
        wsrc = I["w_ada"].rearrange("(k p) n -> p k n", p=128)
        Bmod = Buf("MOD")
        for j in range(24):
            w = wsl[j % 3]
            bw = Bw[j % 3]
            self.dma("pool", w[:], wsrc[:, :, j * 512:(j + 1) * 512], [], [bw], bw)
            bb = Bb[j % 2]
            self.dma("sp", bsl[j % 2][:], I["b_ada"][j * 512:(j + 1) * 512].rearrange("(o n) -> o n", o=1), [], [bb], bb)
            pf = self.F[j % 2]
            bpf = self.BF[j % 2]
            for kc in range(KC):
                self.mm(pf[0:1, :], self.cact[:, kc:kc + 1], w[:, kc, :], kc == 0, kc == KC - 1, [bw, cs], [bpf])
            bm = Bm[j % 2]
            self.tt(msl[j % 2][:], pf[0:1, :], bsl[j % 2][:], ALU.add, [bpf, bb], [bm])
            self.dma("sp", MOD[j * 512:(j + 1) * 512].rearrange("(o n) -> o n", o=1), msl[j % 2][:], [bm], [Bmod], bm)
        sb.pop()
        self.release(Bw + Bb + Bm)
        bmc = self.dbuf("modc")
        modr = sb.alloc("modr", [96, 128], F32)
        self.dma("sp", modr[:], MOD.rearrange("(r p) -> r p", p=128), [Bmod], [bmc], bmc)
        S.op("pe", lambda e: e.transpose(out=self.F[3][:, 0:96], in_=modr[:, :], identity=self.ident32[0:96, 0:96]), [bmc] + cb, [self.BF[3]])
        self.cp("dve", self.modc[:].rearrange("p s k -> p (s k)"), self.F[3][:, 0:96], [self.BF[3]], [cs])
        bg1 = self.dbuf("gt1b")
        self.dma("sp", self.gt1b[:], MOD[2 * D:3 * D].partition_broadcast(128), [Bmod], [bg1], bg1)
        bg2 = self.dbuf("gt2b")
        self.dma("sp", self.gt2b[:], MOD[5 * D:6 * D].partition_broadcast(128), [Bmod], [bg2], bg2)
        sb.push()
        g2b = sb.alloc("g2b", [128, D], F32)
        sc2b = sb.alloc("sc2b", [128, D], F32)
        sh2b = sb.alloc("sh2b", [128, D], F32)
        bq1, bq2, bq3 = self.dbuf("g2b"), self.dbuf("sc2b"), self.dbuf("sh2b")
        self.dma("sp", g2b[:], I["norm2_g"].partition_broadcast(128), [], [bq1], bq1)
        self.dma("sp", sc2b[:], MOD[4 * D:5 * D].partition_broadcast(128), [Bmod], [bq2], bq2)
        self.dma("sp", sh2b[:], MOD[3 * D:4 * D].partition_broadcast(128), [Bmod], [bq3], bq3)
        self.stt(self.a2b[:], sc2b[:], 1.0, g2b[:], ALU.add, ALU.mult, [bq1, bq2], [cs])
        self.cp("dve", self.sh2bb[:], sh2b[:], [bq3], [cs])
        sb.pop()
        self.release([bq1, bq2, bq3])
        self.stt(self.a1[:], self.modc[:, 1, :], 1.0, self.g1c[:], ALU.add, ALU.mult, [cs], [cs])
        self.stt(self.a2[:], self.modc[:, 4, :], 1.0, self.g2c[:], ALU.add, ALU.mult, [cs], [cs])
        S.op("dve", lambda e: e.memset(self.epsc[:], EPS), cb + [cs, bmc, bg1, bg2], [self.B_c0])
        self.B_c0.const = True
        self.B_modc = bmc

    def norm_to_hT(self, xt, bx, hT_dst, bh, a_col, sh_col, ring):
        junk, ssq, tmp, rs, xn, bj, bs, bxn = ring
        self.act(junk[:], xt, AF.Square, [bx], [bj, bs], accum=ssq[:, 0:1])
        self.rstd(rs[:, 0:1], ssq[:, 0:1], float(D), [bs], [bs, bj], tmp[:, 0:1])
        yield
        self.ts(xn[:], xt, rs[:, 0:1], None, ALU.mult, None, [bx, bs], [bxn])
        yield
        for tb in range(2):
            for ts_ in range(8):
                kc = tb * 8 + ts_
                self.tr(self.T[tb][:, ts_ * 128:(ts_ + 1) * 128], xn[:, kc * 128:(kc + 1) * 128], [bxn], [self.BT[tb][0]])
            yield
            for ts_ in range(8):
                kc = tb * 8 + ts_
                pt = self.T[tb][:, ts_ * 128:(ts_ + 1) * 128]
                if tb == 0:
                    self.act(hT_dst[:, kc, :], pt, AF.Identity, [self.BT[tb][0], self.B_c0], [bh], scale=a_col[:, kc:kc + 1], bias=sh_col[:, kc:kc + 1])
                else:
                    self.ts(hT_dst[:, kc, :], pt, a_col[:, kc:kc + 1], sh_col[:, kc:kc + 1], ALU.mult, ALU.add, [self.BT[tb][0], self.B_c0], [bh])
                if ts_ % 2 == 1:
                    yield

    def norm_ring(self, sb, junk=None):
        if junk is None:
            junk = sb.alloc("junk", [128, D], BF16)
        ssq = sb.alloc("ssq", [128, 1], F32)
        tmp = sb.alloc("tmp", [128, 1], F32)
        rs = sb.alloc("rs", [128, 1], F32)
        xn = sb.alloc("xn", [128, D], BF16)
        return (junk, ssq, tmp, rs, xn, Buf("junk"), Buf("ssq"), Buf("xn"))

    def stage1(self):
        S, sb, I = self.S, self.sb, self.I
        sb.push()
        G = 1024
        hT = [sb.alloc("hT", [128, KC, G], BF16) for _ in range(2)]
        BhT = [[Buf("hT%d_%d" % (s, t)) for t in range(8)] for s in range(2)]
        wsl = [sb.alloc("wsl", [128, KC, 512], BF16) for _ in range(3)]
        Bw = [self.dbuf("wsl", "pool") for _ in range(3)]
        xs = [sb.alloc("xs", [128, D], F32) for _ in range(2)]
        Bx = [self.dbuf("xs") for _ in range(2)]
        rings = [self.norm_ring(sb)]
        rings.append(self.norm_ring(sb, junk=rings[0][0]))
        stg = []
        stgb = []
        for _ in range(4):
            sb.push()
            stgb.append(sb.alloc("stgb", [128, 512], BF16))
            sb.pop()
            stg.append(sb.alloc("stg", [128, 512], F32))
        Bs = [self.dbuf("stg") for _ in range(4)]
        wsrc = I["w_in"].rearrange("(k p) n -> p k n", p=128)
        D_ = self.dram
        zt = sb.alloc("zt", [128, 1024], BF16)
        Bz = self.dbuf("zt")
        S.op("dve", lambda e: e.memset(zt[:], 0.0), [], [Bz])
        for i in range(NR // 128):
            for hf in range(2):
                self.dma("sp", D_["XD"][i * 128:(i + 1) * 128, hf * 1024:(hf + 1) * 1024], zt[:], [Bz], [Buf("z")], Bz)
        groups = [("p", 0), ("p", 1), ("m", 0), ("m", 1)]
        pblocks = [2, 3, 4, 5, 8, 9, 10, 11]
        import os
        if os.environ.get("S1G"):
            groups = groups[:int(os.environ["S1G"])]
        if os.environ.get("S1B"):
            pblocks = pblocks[:int(os.environ["S1B"])]
        wi = [0]
        si = [0]
        xi = [0]
        def hT_gen(gi):
            kind, gidx = groups[gi]
            slot = gi % 2
            src = I["xp"] if kind == "p" else I["xm"]
            for t in range(8):
                xsl = xs[xi[0] % 2]
                bx = Bx[xi[0] % 2]
                ring = rings[xi[0] % 2]
                xi[0] += 1
                r0 = gidx * G + t * 128
                self.dma("sp", xsl[:], src[r0:r0 + 128, :], [], [bx], bx)
                yield
                for _ in self.norm_to_hT(xsl[:], bx, hT[slot][:, :, t * 128:(t + 1) * 128], BhT[slot][t], self.a1, self.modc[:, 0, :], ring):
                    yield

        for _ in hT_gen(0):
            pass
        for gi, (kind, gidx) in enumerate(groups):
            slot = gi % 2
            ltok0 = gidx * G + (0 if kind == "p" else NT)
            mtok0 = gidx * G
            nxt = hT_gen(gi + 1) if gi + 1 < len(groups) else iter(())
            blocks = pblocks if kind == "p" else list(range(22))
            nstep = -(-8 * 16 // (len(blocks) * 8)) + 1
            for blk in blocks:
                w = wsl[wi[0] % 3]
                bw = Bw[wi[0] % 3]
                wi[0] += 1
                self.dma("pool", w[:], wsrc[:, :, blk * 512:(blk + 1) * 512], [], [bw], bw)
                fm = blk in (0, 1, 2, 3, 6, 7) or blk >= 14
                for u in range(8):
                    fb = u % 4
                    pf = self.F[fb]
                    bpf = self.BF[fb]
                    if fm:
                        cs_, th = u // 2, u % 2
                        for kc in range(KC):
                            self.mm(pf[:, :], w[:, kc, cs_ * 128:(cs_ + 1) * 128], hT[slot][:, kc, th * 512:(th + 1) * 512],
                                    kc == 0, kc == KC - 1, [bw] + BhT[slot][th * 4:(th + 1) * 4], [bpf])
                    else:
                        for kc in range(KC):
                            self.mm(pf[:, :], hT[slot][:, kc, u * 128:(u + 1) * 128], w[:, kc, :],
                                    kc == 0, kc == KC - 1, [bw, BhT[slot][u]], [bpf])
                    sg = stg[si[0] % 4]
                    sgb = stgb[si[0] % 4]
                    bs = Bs[si[0] % 4]
                    si[0] += 1
                    if fm:
                        if blk < 2:
                            dst = D_["QT"][blk * 4 + cs_, :, mtok0 + th * 512: mtok0 + (th + 1) * 512]
                            key = ("QT", blk * 4 + cs_)
                            fn = AF.Copy
                        elif blk < 4:
                            dst = D_["KT"][(blk - 2) * 4 + cs_, :, ltok0 + th * 512: ltok0 + (th + 1) * 512]
                            key = ("KT", (blk - 2) * 4 + cs_)
                            fn = AF.Copy
                        elif blk < 8:
                            dst = D_["RQT"][(blk - 6) * 4 + cs_, :, mtok0 + th * 512: mtok0 + (th + 1) * 512]
                            key = ("RQT", (mtok0 + th * 512) // 128)
                            fn = AF.Copy
                        elif blk < 18:
                            dst = D_["GAT"][(blk - 14) * 4 + cs_, :, mtok0 + th * 512: mtok0 + (th + 1) * 512]
                            key = ("GAT", (blk - 14) * 4 + cs_)
                            fn = AF.Sigmoid
                        else:
                            dst = D_["GRT"][(blk - 18) * 4 + cs_, :, mtok0 + th * 512: mtok0 + (th + 1) * 512]
                            key = ("GRT", (blk - 18) * 4 + cs_)
                            fn = AF.Sigmoid
                        odt = BF16
                    else:
                        if blk < 6:
                            dst = D_["V"][ltok0 + u * 128: ltok0 + (u + 1) * 128, (blk - 4) * 512:(blk - 3) * 512]
                            key = ("V", (blk - 4) * 4)
                            fn, odt = AF.Copy, BF16
                        elif blk < 10:
                            dst = D_["RF"][ltok0 + u * 128: ltok0 + (u + 1) * 128, (blk - 8) * 512:(blk - 7) * 512]
                            key = ("RF", (ltok0 + u * 128) // 128)
                            fn, odt = AF.Copy, F32
                        elif blk < 12:
                            dst = D_["RI"][ltok0 + u * 128: ltok0 + (u + 1) * 128, (blk - 10) * 512:(blk - 9) * 512]
                            key = ("RI", (ltok0 + u * 128) // 128)
                            fn, odt = AF.Copy, BF16
                        else:
                            dst = D_["ROG"][mtok0 + u * 128: mtok0 + (u + 1) * 128, (blk - 12) * 512:(blk - 11) * 512]
                            key = ("ROG", (mtok0 + u * 128) // 128)
                            fn, odt = AF.Silu, BF16
                    so = sg[:] if odt == F32 else sgb[:]
                    if fn == AF.Copy and (u % 2 == 1):
                        self.cp("dve", so, pf[:, :], [bpf], [bs])
                    else:
                        self.act(so, pf[:, :], fn, [bpf], [bs])
                    wl_ = self.scr_w.setdefault(key, [])
                    tok = Buf("w")
                    wl_.append(tok)
                    self.dma("sp", dst, so, [bs], [tok], bs)
                    for _ in range(nstep):
                        next(nxt, None)
            for _ in nxt:
                pass
        sb.pop()
        self.release(Bw + Bx + Bs + [Bz])

    def scr_reads(self, name, keys):
        out = []
        for k in keys:
            out.extend(self.scr_w.get((name, k), []))
        return out

    def stage2(self):
        S, sb = self.S, self.sb
        D_ = self.dram
        sb.push()
        QTh = [sb.alloc("QTh", [128, NT], BF16) for _ in range(2)]
        KTh = [sb.alloc("KTh", [128, 2 * NT], BF16) for _ in range(2)]
        Vh = [sb.alloc("Vh", [128, 32, 129], BF16) for _ in range(2)]
        Bq = [self.dbuf("q") for _ in range(2)]
        Bk = [self.dbuf("k") for _ in range(2)]
        Bv = [self.dbuf("v") for _ in range(2)]
        km32 = [sb.alloc("km32", [128, 16], F32) for _ in range(2)]
        kmb = [sb.alloc("kmb", [128, 16], BF16) for _ in range(2)]
        gm = [sb.alloc("gm", [128, 16, 16], F32) for _ in range(2)]
        top8 = [sb.alloc("top8", [128, 16, 8], F32) for _ in range(2)]
        pen = [sb.alloc("pen", [128, 16, 16], F32) for _ in range(2)]
        penb = [sb.alloc("penb", [128, 256], BF16) for _ in range(2)]
        penT = [sb.alloc("penT", [16, NT], BF16) for _ in range(2)]
        Bkm = [Buf("km%d" % i) for i in range(2)]
        Bgm = [Buf("gm%d" % i) for i in range(2)]
        Bpen = [Buf("pen%d" % i) for i in range(2)]
        BpenT = [Buf("penT%d" % i) for i in range(2)]
        pT = [sb.alloc("pT", [128, 256], BF16) for _ in range(4)]
        BpT = [Buf("pT%d" % i) for i in range(4)]
        rsum = [sb.alloc("rsum", [128, 1], F32) for _ in range(2)]
        atts = [sb.alloc("atts", [128, 128], BF16) for _ in range(2)]
        Brs = [Buf("rsum%d" % i) for i in range(2)]
        Bat = [Buf("atts%d" % i) for i in range(2)]
        scale = 1.0 / np.sqrt(128.0)
        pi = [0]
        ei = [0]
        for sl in range(2):
            S.op("dve", lambda e, t=Vh[sl]: e.memset(t[:, :, 128:129], 1.0), [], [Bv[sl]])
        def preamble(h):
            sl = h % 2
            q, k, v = QTh[sl], KTh[sl], Vh[sl]
            bq, bk, bv = Bq[sl], Bk[sl], Bv[sl]
            self.dma("sp", q[:], D_["QT"][h], self.scr_reads("QT", [h]), [bq], bq)
            self.dma("sp", k[:], D_["KT"][h], self.scr_reads("KT", [h]), [bk], bk)
            self.dma("sp", v[:, :, 0:128], D_["V"].rearrange("(t p) c -> p t c", p=128)[:, :, h * 128:(h + 1) * 128],
                     self.scr_reads("V", [(h // 4) * 4]), [bv], bv)
            yield
            S.op("dve", lambda e, k=k: e.tensor_reduce(out=km32[sl][:], in_=k[:].rearrange("p (b l) -> p b l", l=256), axis=AX.X, op=ALU.add),
                 [bk], [Bkm[sl]])
            yield
            self.ts(kmb[sl][:], km32[sl][:], 1.0 / 256, None, ALU.mult, None, [Bkm[sl]], [Bkm[sl]])
            yield
            pg = self.F[5]
            bpg = self.BF[5]
            for qt in range(16):
                self.mm(pg[:, qt * 16:(qt + 1) * 16], q[:, qt * 128:(qt + 1) * 128], kmb[sl][:, :], True, True, [bq, Bkm[sl]], [bpg])
            yield
            gmf = gm[sl][:].rearrange("p a b -> p (a b)")
            self.tt(gmf, pg[:, 0:256], self.cpen[:], ALU.add, [bpg, self.B_c0], [Bgm[sl]])
            yield
            self.ts(gm[sl][:, :, 0:8], gm[sl][:, :, 0:8], self.ppen[:, 0:1], None, ALU.add, None, [Bgm[sl], self.B_c0], [Bgm[sl]])
            yield
            for qt in range(16):
                S.op("dve", lambda e, qt=qt: e.max(out=top8[sl][:, qt, :], in_=gm[sl][:, qt, :]), [Bgm[sl]], [Bpen[sl]])
                yield
            for qt in range(16):
                self.ts(pen[sl][:, qt, :], gm[sl][:, qt, :], top8[sl][:, qt, 2:3], None, ALU.is_ge, None, [Bgm[sl], Bpen[sl]], [Bpen[sl]])
                yield
            penf = pen[sl][:].rearrange("p a b -> p (a b)")
            self.ts(penf, penf, -1.0, -NEG, ALU.add, ALU.mult, [Bpen[sl]], [Bpen[sl]])
            yield
            self.ts(pen[sl][:, :, 0:8], pen[sl][:, :, 0:8], self.ppen[:, 0:1], None, ALU.add, None, [Bpen[sl], self.B_c0], [Bpen[sl]])
            yield
            self.cp("dve", penb[sl][:], penf, [Bpen[sl]], [Bpen[sl]])
            yield
            for half in range(2):
                for qt in range(half * 8, half * 8 + 8):
                    tb, ts_ = qt // 8, qt % 8
                    pt = self.T[tb][0:16, ts_ * 128:(ts_ + 1) * 128]
                    self.tr(pt, penb[sl][:, qt * 16:(qt + 1) * 16], [Bpen[sl]], [self.BT[tb][ts_]])
                yield
                for qt in range(half * 8, half * 8 + 8):
                    tb, ts_ = qt // 8, qt % 8
                    pt = self.T[tb][0:16, ts_ * 128:(ts_ + 1) * 128]
                    self.cp("act", penT[sl][:, qt * 128:(qt + 1) * 128], pt, [self.BT[tb][ts_]], [BpenT[sl]])
                yield

        for _ in preamble(0):
            pass
        for h in range(8):
            sl = h % 2
            q, k, v = QTh[sl], KTh[sl], Vh[sl]
            bq, bk, bv = Bq[sl], Bk[sl], Bv[sl]
            nxt = preamble(h + 1) if h + 1 < 8 else iter(())
            itc = [0]
            for qb in range(8):
                Bl = 8 + qb
                nkt = 2 * Bl + 2
                po = [self.F[3], self.F[4]]
                bpo = [self.BF[3], self.BF[4]]
                LAG = 2
                pend = []
                for kk in range(nkt + LAG):
                    if kk < nkt:
                        kt = kk
                        fb = kt % 3
                        ps = self.F[fb]
                        bps = self.BF[fb]
                        self.mm(ps[:, 0:256], k[:, kt * 128:(kt + 1) * 128], q[:, qb * 256:(qb + 1) * 256], True, False, [bk, bq], [bps])
                        if kt < 2 * Bl:
                            i = kt // 2
                            self.mm(ps[:, 0:256], self.selc[0:16, i * 128:(i + 1) * 128], penT[sl][0:16, qb * 256:(qb + 1) * 256],
                                    False, True, [BpenT[sl], self.B_c0], [bps])
                        else:
                            j = kt - 2 * Bl
                            self.mm(ps[:, 0:256], self.identb[:, :], self.cm[:, j * 256:(j + 1) * 256], False, True, [self.B_c0], [bps])
                        p = pT[pi[0] % 4]
                        bp = BpT[pi[0] % 4]
                        pi[0] += 1
                        self.act(p[:], ps[:, 0:256], AF.Exp, [bps], [bp], scale=scale)
                        pend.append((kt, p, bp))
                    if kk >= LAG:
                        kt, p, bp = pend.pop(0)
                        for qh in range(2):
                            self.mm(po[qh][:, 0:129], p[:, qh * 128:(qh + 1) * 128], v[:, kt, :], kt == 0, kt == nkt - 1, [bp, bv], [bpo[qh]])
                    itc[0] += 1
                    if itc[0] >= 24:
                        next(nxt, None)
                for qh in range(2):
                    e_ = ei[0] % 2
                    ei[0] += 1
                    S.op("dve", lambda e, o=rsum[e_], i=po[qh]: e.reciprocal(out=o[:, 0:1], in_=i[:, 128:129]), [bpo[qh]], [Brs[e_]])
                    self.act(atts[e_][:], po[qh][:, 0:128], AF.Copy, [bpo[qh], Brs[e_]], [Bat[e_]], scale=rsum[e_][:, 0:1])
                    qt = qb * 2 + qh
                    tb, ts_ = qt // 8, qt % 8
                    pt = self.T[tb][:, ts_ * 128:(ts_ + 1) * 128]
                    bt = self.BT[tb][ts_]
                    self.tr(pt, atts[e_][:], [Bat[e_]], [bt])
                    self.cp("dve", self.attT[:, h, qt * 128:(qt + 1) * 128], pt, [bt], [self.B_attT[h]])
            for _ in nxt:
                pass
            if self.debug:
                self.dma("sp", D_["ATT"][h], self.attT[:, h, :], [self.B_attT[h]], [Buf("x")], self.B_attT[h])
        sb.pop()
        self.release(Bq + Bk + Bv)

    def stage3(self):
        S, sb = self.S, self.sb
        D_ = self.dram
        sb.push()
        rf = [sb.alloc("rf", [128, 1024], F32) for _ in range(2)]
        ri = [sb.alloc("ri", [128, 1024], BF16) for _ in range(2)]
        rq = [sb.alloc("rq", [128, 8, 128], BF16) for _ in range(2)]
        rog = [sb.alloc("rog", [128, 1024], BF16) for _ in range(2)]
        Brf = [self.dbuf("rf") for _ in range(2)]
        Bri = [self.dbuf("ri") for _ in range(2)]
        Brq = [self.dbuf("rq") for _ in range(2)]
        Brog = [self.dbuf("rog") for _ in range(2)]
        logf = [sb.alloc("logf", [128, 1024], F32) for _ in range(2)]
        kin = [sb.alloc("kin", [128, 1024], F32) for _ in range(2)]
        Blf = [Buf("logf%d" % i) for i in range(2)]
        Bkin = [Buf("kin%d" % i) for i in range(2)]
        R = 4
        ebl = [sb.alloc("ebl", [128, 128], F32) for _ in range(R)]
        khat = [sb.alloc("khat", [128, 128], BF16) for _ in range(R)]
        eb8 = [sb.alloc("eb8", [128, 8], F32) for _ in range(R)]
        ebm = [sb.alloc("ebm", [128, 128], F32) for _ in range(R)]
        qtl = [sb.alloc("qtl", [128, 128], BF16) for _ in range(R)]
        enb = [sb.alloc("enb", [128, 128], F32) for _ in range(R)]
        ktl = [sb.alloc("ktl", [128, 128], BF16) for _ in range(R)]
        ktlT = [sb.alloc("ktlT", [128, 128], BF16) for _ in range(R)]
        khc = [sb.alloc("khc", [128, 4, 128], BF16) for _ in range(R)]
        qtlz = [sb.alloc("qtlz", [128, 4, 128], BF16) for _ in range(R)]
        Bkhc = [Buf("khc%d" % i) for i in range(R)]
        Bqz = [Buf("qtlz%d" % i) for i in range(R)]
        for i in range(R):
            S.op("dve", lambda e, t=qtlz[i]: e.memset(t[:], 0.0), [], [Bqz[i]])
        at = [sb.alloc("at", [128, 128], BF16) for _ in range(R)]
        Sb = [sb.alloc("Sb", [128, 4, 128], BF16) for _ in range(R)]
        ot = [sb.alloc("ot", [128, 128], F32) for _ in range(R)]
        of_ = [sb.alloc("of", [128, 128], BF16) for _ in range(R)]
        sq = [sb.alloc("sq", [128, 4], F32) for _ in range(R)]
        jk = [sb.alloc("jk", [128, 128], BF16) for _ in range(R)]
        names = "ebl khat eb8 ebm qtl enb ktl ktlT at ot of sq".split()
        Bn = {n: [Buf(n + str(i)) for i in range(R)] for n in names}
        BSb = [[Buf("Sb%d_%d" % (i, c)) for c in range(4)] for i in range(R)]
        St = sb.alloc("St", [128, 8, 128], F32)
        BSt = [Buf("St%d" % h) for h in range(8)]
        for h in range(8):
            S.op("dve", lambda e, h=h: e.memset(St[:, h, :], 0.0), [], [BSt[h]])
        F, BFs = self.F, self.BFs
        hi = [0]
        import os
        tl = list(range(32))
        if os.environ.get("S3T"):
            tl = [int(v) for v in os.environ["S3T"].split(",")]
        s3c = int(os.environ.get("S3C", "4"))
        s3m = int(os.environ.get("S3M", "9"))
        for tile in tl:
            main = tile >= 16
            mt = tile - 16
            sl = tile % 2
            self.dma("sp", rf[sl][:], D_["RF"][tile * 128:(tile + 1) * 128, :], self.scr_reads("RF", [tile]), [Brf[sl]], Brf[sl])
            self.dma("sp", ri[sl][:], D_["RI"][tile * 128:(tile + 1) * 128, :], self.scr_reads("RI", [tile]), [Bri[sl]], Bri[sl])
            if main:
                self.dma("sp", rq[sl][:], D_["RQT"][:, :, mt * 128:(mt + 1) * 128].rearrange("h p t -> p h t"),
                         self.scr_reads("RQT", [(mt // 4) * 4]), [Brq[sl]], Brq[sl])
                self.dma("sp", rog[sl][:], D_["ROG"][mt * 128:(mt + 1) * 128, :], self.scr_reads("ROG", [mt]), [Brog[sl]], Brog[sl])
            lf, kn = logf[sl], kin[sl]
            self.act(kn[:], rf[sl][:], AF.Sigmoid, [Brf[sl]], [Bkin[sl]])
            self.tt(kn[:], kn[:], self.oml[:], ALU.mult, [Bkin[sl], self.B_c0], [Bkin[sl]])
            self.tt(kn[:], kn[:], self.lb[:], ALU.add, [Bkin[sl], self.B_c0], [Bkin[sl]])
            self.act(lf[:], kn[:], AF.Ln, [Bkin[sl]], [Blf[sl]])
            self.ts(kn[:], kn[:], -1.0, 1.0, ALU.mult, ALU.add, [Bkin[sl]], [Bkin[sl]])
            def hbody(h, r, main=main, sl=sl, lf=lf, kn=kn, mt=mt):
                hs = slice(h * 128, (h + 1) * 128)
                s2 = h % 2
                c128 = slice(s2 * 128, (s2 + 1) * 128)
                bl_ps = F[0][:, c128]
                b8_ps = F[0][:, 256 + s2 * 8:256 + (s2 + 1) * 8]
                self.mm(bl_ps, self.wl[:, :], lf[:, hs], True, True, [Blf[sl], self.B_c0], [self.BF[0]])
                self.mm(b8_ps, lf[:, hs], self.wall[:, 128:136], True, True, [Blf[sl], self.B_c0], [self.BF[0]])
                if main:
                    self.mm(F[1][:, c128], lf[:, hs], self.wall[:, 0:128], True, True, [Blf[sl], self.B_c0], [self.BF[1]])
                    bm_ps = F[1][:, 256 + s2 * 128:256 + (s2 + 1) * 128]
                    self.mm(bm_ps, self.wall[:, 0:128], lf[:, hs], True, True, [Blf[sl], self.B_c0], [self.BF[1]])
                yield
                self.act(ebl[r][:], bl_ps, AF.Exp, [self.BF[0]], [Bn["ebl"][r]])
                self.act(eb8[r][:], b8_ps, AF.Exp, [self.BF[0]], [Bn["eb8"][r]])
                if main:
                    self.act(ebm[r][:], F[1][:, c128], AF.Exp, [self.BF[1]], [Bn["ebm"][r]])
                    self.act(enb[r][:], bm_ps, AF.Exp, [self.BF[1]], [Bn["enb"][r]], scale=-1.0)
                yield
                for c in range(4):
                    self.stt(khc[r][:, c, :], kn[:, hs], self.wall[:, 132 + c:133 + c], ebl[r][:], ALU.mult, ALU.mult,
                             [Bkin[sl], Bn["ebl"][r], self.B_c0], [Bkhc[r]])
                if main:
                    self.tt(ktl[r][:], kn[:, hs], enb[r][:], ALU.mult, [Bkin[sl], Bn["enb"][r]], [Bn["ktl"][r]])
                    self.tt(qtl[r][:], rq[sl][:, h, :], ebm[r][:], ALU.mult, [Brq[sl], Bn["ebm"][r]], [Bn["qtl"][r]])
                    for c in range(4):
                        cc = slice(32 * c, 32 * c + 32)
                        self.tt(qtlz[r][:, c, cc], rq[sl][:, h, cc], ebm[r][:, cc], ALU.mult, [Brq[sl], Bn["ebm"][r]], [Bqz[r]])
                yield
                pds = [F[5][:, (s2 * 2 + (c % 2)) * 128:(s2 * 2 + (c % 2) + 1) * 128] for c in range(4)]
                if main:
                    pt = self.T[0][:, c128]
                    self.tr(pt, ktl[r][:], [Bn["ktl"][r]], [self.BT[0][0]])
                    yield
                    self.cp("act", ktlT[r][:], pt, [self.BT[0][0]], [Bn["ktlT"][r]])
                    yield
                    self.mm(F[3][:, c128], ktlT[r][:], qtl[r][:], True, True, [Bn["ktlT"][r], Bn["qtl"][r]], [self.BF[3]])
                    yield
                    self.tt(at[r][:], F[3][:, c128], self.bd[:], ALU.mult, [self.BF[3], self.B_c0], [Bn["at"][r]])
                    yield
                pob = 4 if s2 == 0 else 2
                po = F[pob][:, 0:128]
                bpo = self.BF[pob]
                if main:
                    self.mm(po, at[r][:], ri[sl][:, hs], True, False, [Bn["at"][r], Bri[sl]], [bpo])
                for c in range(4):
                    self.mm(pds[c], khc[r][:, c, :], ri[sl][:, hs], True, True, [Bkhc[r], Bri[sl]], [self.BF[5]])
                    if main:
                        self.act(Sb[r][:, c, :], St[:, h, :], AF.Copy, [BSt[h], Bn["eb8"][r]], [BSb[r][c]], scale=eb8[r][:, c:c + 1])
                    yield
                    if main:
                        self.mm(po, qtlz[r][:, c, :], Sb[r][:, c, :], False, c == 3, [Bqz[r], BSb[r][c]], [bpo])
                    self.stt(St[:, h, :], St[:, h, :], eb8[r][:, 4 + c:5 + c], pds[c], ALU.mult, ALU.add, [BSt[h], Bn["eb8"][r], self.BF[5]], [BSt[h]])
                    yield
                if main:
                    self.act(jk[r][:], po, AF.Square, [bpo], [Bn["sq"][r]], accum=sq[r][:, 0:1])
                    yield
                    self.tt(ot[r][:], po, self.rngb[:], ALU.mult, [bpo, self.B_c0], [Bn["ot"][r]])
                    self.rstd(sq[r][:, 2:3], sq[r][:, 0:1], 128.0, [Bn["sq"][r]], [Bn["sq"][r], Bn["sq"][r]], sq[r][:, 1:2])
                    yield
                    self.stt(of_[r][:], ot[r][:], sq[r][:, 2:3], rog[sl][:, hs], ALU.mult, ALU.mult, [Bn["ot"][r], Bn["sq"][r], Brog[sl]], [Bn["of"][r]])
                    yield
                    pt = self.T[1][:, c128]
                    self.tr(pt, of_[r][:], [Bn["of"][r]], [self.BT[1][0]])
                    yield
                    self.cp("act", self.oT[:, h, mt * 128:(mt + 1) * 128], pt, [self.BT[1][0]], [self.B_oT[mt]])

            G = 2
            for h0 in range(0, 8, G):
                gens = []
                for h in range(h0, h0 + G):
                    gens.append(hbody(h, hi[0] % R))
                    hi[0] += 1
                while gens:
                    for g_ in list(gens):
                        try:
                            next(g_)
                        except StopIteration:
                            gens.remove(g_)
            if tile == 15:
                for h in range(8):
                    self.ts(St[:, h, :], St[:, h, :], self.pm[:, 0:1], None, ALU.mult, None, [BSt[h], self.B_c0], [BSt[h]])
        if self.debug:
            for h in range(8):
                self.dma("sp", D_["OT"][h], self.oT[:, h, :], self.B_oT, [Buf("x")], self.B_oT[0])
        sb.pop()
        self.release(Brf + Bri + Brq + Brog)

    def stage4a(self):
        S, sb, I = self.S, self.sb, self.I
        D_ = self.dram
        sb.push()
        wa = [sb.alloc("wa", [128, 8, 128], BF16) for _ in range(2)]
        wr_ = [sb.alloc("wr_", [128, 8, 128], BF16) for _ in range(2)]
        sga = [sb.alloc("sga", [128, NT], BF16) for _ in range(2)]
        sgr = [sb.alloc("sgr", [128, NT], BF16) for _ in range(2)]
        mT = [sb.alloc("mT", [128, NT], BF16) for _ in range(2)]
        t1 = [sb.alloc("t1", [128, 512], F32) for _ in range(2)]
        t2 = [sb.alloc("t2", [128, 512], F32) for _ in range(2)]
        Bwa = [self.dbuf("wa", "pool") for _ in range(2)]
        Bwr = [self.dbuf("wr", "pool") for _ in range(2)]
        Bsa = [self.dbuf("sga") for _ in range(2)]
        Bsr = [self.dbuf("sgr") for _ in range(2)]
        BmT = [self.dbuf("mT") for _ in range(2)]
        Bt1 = [Buf("t1_%d" % i) for i in range(2)]
        Bt2 = [Buf("t2_%d" % i) for i in range(2)]
        wua = I["w_up_a"].rearrange("(k p) n -> p k n", p=128)
        wur = I["w_up_r"].rearrange("(k p) n -> p k n", p=128)
        ti = [0]
        for ch in range(16):
            sl = ch % 2
            self.dma("pool", wa[sl][:], wua[:, :, ch * 128:(ch + 1) * 128], [], [Bwa[sl]], Bwa[sl])
            self.dma("pool", wr_[sl][:], wur[:, :, ch * 128:(ch + 1) * 128], [], [Bwr[sl]], Bwr[sl])
            self.dma("sp", sga[sl][:], D_["GAT"][ch], self.scr_reads("GAT", [ch]), [Bsa[sl]], Bsa[sl])
            self.dma("sp", sgr[sl][:], D_["GRT"][ch], self.scr_reads("GRT", [ch]), [Bsr[sl]], Bsr[sl])
            for g in range(4):
                gs = slice(g * 512, (g + 1) * 512)
                fa, fr = (g % 2) * 2, (g % 2) * 2 + 1
                for wc in range(8):
                    self.mm(self.F[fa][:, :], wa[sl][:, wc, :], self.attT[:, wc, gs], wc == 0, wc == 7, [Bwa[sl], self.B_attT[wc]], [self.BF[fa]])
                for wc in range(8):
                    self.mm(self.F[fr][:, :], wr_[sl][:, wc, :], self.oT[:, wc, gs], wc == 0, wc == 7,
                            [Bwr[sl]] + self.B_oT[g * 4:(g + 1) * 4], [self.BF[fr]])
                i = ti[0] % 2
                ti[0] += 1
                self.tt(t1[i][:], self.F[fa][:, :], sga[sl][:, gs], ALU.mult, [self.BF[fa], Bsa[sl]], [Bt1[i]])
                self.tt(t2[i][:], self.F[fr][:, :], sgr[sl][:, gs], ALU.mult, [self.BF[fr], Bsr[sl]], [Bt2[i]], eng="dve")
                self.tt(mT[sl][:, gs], t1[i][:], t2[i][:], ALU.add, [Bt1[i], Bt2[i]], [BmT[sl]], eng="dve")
            tok = Buf("w")
            self.scr_w.setdefault(("MT", ch), []).append(tok)
            self.dma("sp", D_["MT"][ch], mT[sl][:], [BmT[sl]], [tok], BmT[sl])
        sb.pop()
        self.release(Bwa + Bwr + Bsa + Bsr + BmT)

    def stage4b(self):
        S, sb, I = self.S, self.sb, self.I
        D_ = self.dram
        sb.push()
        wo = sb.alloc("wo", [128, KC, D], BF16)
        Bwo = [self.dbuf("wo", "pool") for _ in range(4)]
        wsrc = I["w_out"].rearrange("(k p) n -> p k n", p=128)
        for i in range(4):
            self.dma("pool", wo[:, i * 4:(i + 1) * 4, :], wsrc[:, i * 4:(i + 1) * 4, :], [], [Bwo[i]], Bwo[i])
        mTg = [sb.alloc("mTg", [128, KC, 512], BF16)] * 2
        Bmg = [self.dbuf("mTg")] * 2
        xs = [sb.alloc("xs", [128, D], F32) for _ in range(2)]
        Bx = [self.dbuf("xs") for _ in range(2)]
        x1 = [sb.alloc("x1", [128, D], F32) for _ in range(2)]
        Bx1 = [self.dbuf("x1") for _ in range(2)]
        h2 = [sb.alloc("h2", [128, KC, 128], BF16) for _ in range(2)]
        Bh2 = [self.dbuf("h2") for _ in range(2)]
        rings = [self.norm_ring(sb)]
        rings.append(self.norm_ring(sb, junk=rings[0][0]))
        tq = [sb.alloc("tq", [128, 512], F32) for _ in range(2)]
        Btq = [Buf("tq%d" % i) for i in range(2)]
        rt = sb.alloc("rt", [128, 160], F32)
        Brt = Buf("rt")
        r2 = sb.alloc("r2", [128, 112], F32)
        Br2 = Buf("r2")
        selb = sb.alloc("selb", [128, 32], BF16)
        Bselb = Buf("selb")
        h2k = [sb.alloc("h2k", [128, D], BF16) for _ in range(2)]
        Bh2k = [Buf("h2k%d" % i) for i in range(2)]
        mtsrc = D_["MT"].rearrange("k p t -> p k t")
        qi = [0]
        for g in range(4):
            sg = g % 2
            self.dma("sp", mTg[sg][:], mtsrc[:, :, g * 512:(g + 1) * 512], self.scr_reads("MT", range(16)), [Bmg[sg]], Bmg[sg])
            for tt_ in range(4):
                tile = g * 4 + tt_
                sl = tile % 2
                self.dma("sp", xs[sl][:], I["xm"][tile * 128:(tile + 1) * 128, :], [], [Bx[sl]], Bx[sl])
                for cg in range(4):
                    cs_ = slice(cg * 512, (cg + 1) * 512)
                    fb = cg % 4
                    for kc in range(KC):
                        self.mm(self.F[fb][:, :], mTg[sg][:, kc, tt_ * 128:(tt_ + 1) * 128], wo[:, kc, cs_], kc == 0, kc == KC - 1,
                                [Bmg[sg], Bwo[kc // 4]], [self.BF[fb]])
                    i = qi[0] % 2
                    qi[0] += 1
                    self.tt(tq[i][:], self.F[fb][:, :], self.gt1b[:, cs_], ALU.mult, [self.BF[fb], self.B_c0], [Btq[i]])
                    self.tt(x1[sl][:, cs_], tq[i][:], xs[sl][:, cs_], ALU.add, [Btq[i], Bx[sl]], [Bx1[sl]], eng="dve")
                tok = Buf("w")
                self.scr_w.setdefault(("X1", tile), []).append(tok)
                self.dma("sp", D_["X1"][tile * 128:(tile + 1) * 128, :], x1[sl][:], [Bx1[sl]], [tok], Bx1[sl])
                for _ in self.norm_to_hT(x1[sl][:], Bx1[sl], h2[sl][:, :, :], Bh2[sl], self.a2, self.modc[:, 3, :], rings[sl]):
                    pass
                tok = Buf("w")
                self.scr_w.setdefault(("H2T", tile), []).append(tok)
                self.dma("sp", D_["H2T"][:, :, tile * 128:(tile + 1) * 128].rearrange("k p t -> p k t"), h2[sl][:], [Bh2[sl]], [tok], Bh2[sl])
                pr = self.F[5][:, 0:36]
                bpr = self.BF[5]
                for kc in range(KC):
                    self.mm(pr, h2[sl][:, kc, :], self.wr[:, kc, :], kc == 0, kc == KC - 1, [Bh2[sl], self.B_c0], [bpr])
                lg = rt[:, 0:36]
                self.tt(lg, pr, self.br[:], ALU.add, [bpr, self.B_c0], [Brt])
                mx = rt[:, 36:37]
                S.op("dve", lambda e: e.tensor_reduce(out=rt[:, 36:37], in_=rt[:, 0:4], axis=AX.X, op=ALU.max), [Brt], [Brt])
                self.ts(rt[:, 37:38], mx, -1.0, None, ALU.mult, None, [Brt], [Brt])
                self.act(rt[:, 40:44], rt[:, 0:4], AF.Exp, [Brt], [Brt], bias=rt[:, 37:38], accum=rt[:, 38:39])
                self.ts(rt[:, 44:48], rt[:, 0:4], mx, None, ALU.is_ge, None, [Brt], [Brt])
                self.ts(rt[:, 44:48], rt[:, 44:48], -1.0, -NEGBIG, ALU.add, ALU.mult, [Brt], [Brt])
                for gi in range(4):
                    self.ts(rt[:, 48 + gi * 8:56 + gi * 8], rt[:, 4 + gi * 8:12 + gi * 8], rt[:, 44 + gi:45 + gi], None, ALU.add, None, [Brt], [Brt])
                S.op("dve", lambda e: e.max(out=rt[:, 80:88], in_=rt[:, 48:80]), [Brt], [Brt])
                self.ts(rt[:, 88:120], rt[:, 48:80], rt[:, 81:82], None, ALU.is_ge, None, [Brt], [Brt])
                self.ts(rt[:, 39:40], rt[:, 80:81], -1.0, None, ALU.mult, None, [Brt], [Brt])
                self.act(rt[:, 120:152], rt[:, 48:80], AF.Exp, [Brt], [Brt], bias=rt[:, 39:40])
                self.tt(rt[:, 120:152], rt[:, 120:152], rt[:, 88:120], ALU.mult, [Brt], [Brt])
                S.op("dve", lambda e: e.reduce_sum(out=rt[:, 152:153], in_=rt[:, 120:152], axis=AX.X), [Brt], [Brt])
                self.tt(rt[:, 152:153], rt[:, 152:153], rt[:, 38:39], ALU.mult, [Brt], [Brt])
                S.op("dve", lambda e: e.reciprocal(out=rt[:, 153:154], in_=rt[:, 152:153]), [Brt], [Brt])
                self.ts(self.wfull[:, tile, :], rt[:, 120:152], rt[:, 153:154], None, ALU.mult, None, [Brt], [self.B_wfull[tile]])
                self.cp("dve", selb[:], rt[:, 88:120], [Brt], [Bselb])
                pc = self.F[4]
                self.mm(pc[:, 0:32], self.slt[:, :], selb[:], True, True, [Bselb, self.B_c0], [self.BF[4]])
                self.mm(pc[:, 32:64], self.ones[:, :], selb[:], True, True, [Bselb, self.B_c0], [self.BF[4]])
                self.tt(r2[:, 0:32], pc[:, 0:32], self.tot[:], ALU.add, [self.BF[4], self.B_tot], [Br2])
                self.tt(self.tot[:], pc[:, 32:64], self.tot[:], ALU.add, [self.BF[4], self.B_tot], [self.B_tot])
                self.ts(r2[:, 0:32], r2[:, 0:32], float(CAP - 1), None, ALU.min, None, [Br2], [Br2])
                self.tt(r2[:, 0:32], r2[:, 0:32], self.ebase[:], ALU.add, [Br2, self.B_c0], [Br2])
                self.stt(r2[:, 32:64], r2[:, 0:32], 1.0e6, rt[:, 88:120], ALU.add, ALU.mult, [Br2, Brt], [Br2])
                S.op("dve", lambda e: e.max(out=r2[:, 64:72], in_=r2[:, 32:64]), [Br2], [Br2])
                self.ts(r2[:, 72:74], r2[:, 64:66], -1.0e6, None, ALU.add, None, [Br2], [Br2])
                self.cp("dve", self.desti[:, tile, :], r2[:, 72:74], [Br2], [self.B_dest[tile]])
                for k_ in range(2):
                    self.ts(r2[:, 80:112], r2[:, 32:64], r2[:, 64 + k_:65 + k_], None, ALU.is_equal, None, [Br2], [Br2])
                    self.tt(r2[:, 80:112], r2[:, 80:112], self.wfull[:, tile, :], ALU.mult, [Br2, self.B_wfull[tile]], [Br2])
                    S.op("dve", lambda e, k_=k_, tile=tile: e.reduce_sum(out=self.wk[:, tile, k_:k_ + 1], in_=r2[:, 80:112], axis=AX.X),
                         [Br2], [self.B_dest[tile]])
                xn_t, bxn_t = h2k[sl], Bh2k[sl]
                self.tt(xn_t[:], rings[sl][4][:], self.a2b[:], ALU.mult, [rings[sl][7], self.B_c0], [bxn_t])
                self.tt(xn_t[:], xn_t[:], self.sh2bb[:], ALU.add, [bxn_t, self.B_c0], [bxn_t])
                for k_ in range(2):
                    self.cp("dve", self.six[sl][:, k_:k_ + 1], self.desti[:, tile, k_:k_ + 1], [self.B_dest[tile]], [self.B_six[sl][k_]])
                    S.op("pool", lambda e, k_=k_, ix=self.six[sl], xn_t=xn_t: e.indirect_dma_start(
                        out=D_["XD"], out_offset=bass.IndirectOffsetOnAxis(ap=ix[:, k_:k_ + 1], axis=0),
                        in_=xn_t[:], in_offset=None),
                        [bxn_t, self.B_six[sl][k_]], [Buf("sc"), self.B_ind], dbuf=self.B_ind)
        if self.debug:
            self.dma("sp", D_["WF"], self.wfull[:].rearrange("p a b -> p (a b)"), self.B_wfull, [Buf("x")], self.B_wfull[0])
        sb.pop()
        self.release(Bwo + Bmg[:1] + Bx + Bx1 + Bh2)

    def stage5(self):
        S, sb, I = self.S, self.sb, self.I
        D_ = self.dram
        sb.push()
        w1 = [sb.alloc("w1", [128, KC, 512], BF16) for _ in range(2)]
        w3 = [sb.alloc("w3", [128, KC, 512], BF16) for _ in range(2)]
        w2 = sb.alloc("w2", [128, 4, D], BF16)
        Bw1 = [self.dbuf("w1", "pool") for _ in range(2)]
        Bw3 = [self.dbuf("w3", "pool") for _ in range(2)]
        Bw2 = self.dbuf("w2", "pool")
        nrt = CAP // 128
        xr = [sb.alloc("xr", [128, D], BF16) for _ in range(nrt)]
        Bxr = [self.dbuf("xr") for _ in range(nrt)]
        XT = [sb.alloc("XT", [128, KC, CAP], BF16) for _ in range(2)]
        BXT = [[Buf("XT%d_%d" % (i, j)) for j in range(nrt)] for i in range(2)]
        s1 = [sb.alloc("s1", [128, CAP], F32) for _ in range(2)]
        Bs1 = [Buf("s1_%d" % i) for i in range(2)]
        hid = [sb.alloc("hid", [128, 4, CAP], BF16) for _ in range(2)]
        Bhid = [[Buf("hid%d_%d" % (i, f)) for f in range(4)] for i in range(2)]
        ys = [sb.alloc("ys", [128, D], F32)] * 2
        Bys = [self.dbuf("ys")] * 2
        si = [0]
        yi = [0]

        def loads(ex):
            for r_ in range(nrt):
                row0 = ex * CAP + r_ * 128
                self.dma("sp", xr[r_][:], D_["XD"][row0:row0 + 128, :], [], [Bxr[r_]], Bxr[r_])

        def trans(ex):
            sl = ex % 2
            for r_ in range(nrt):
                for tb in range(2):
                    for ts_ in range(8):
                        kc = tb * 8 + ts_
                        self.tr(self.T[tb][:, ts_ * 128:(ts_ + 1) * 128], xr[r_][:, kc * 128:(kc + 1) * 128], [Bxr[r_]], [self.BT[tb][0]])
                    src = self.T[tb][:, :].rearrange("p (k t) -> p k t", t=128)
                    dst = XT[sl][:, tb * 8:(tb + 1) * 8, r_ * 128:(r_ + 1) * 128]
                    self.cp("act" if tb == 0 else "dve", dst, src, [self.BT[tb][0]], [BXT[sl][r_]])

        loads(0)
        trans(0)
        for ex in range(32):
            sl = ex % 2
            self.dma("pool", w1[sl][:], I["w1"][ex].rearrange("(k p) n -> p k n", p=128), [], [Bw1[sl]], Bw1[sl])
            self.dma("pool", w3[sl][:], I["w3"][ex].rearrange("(k p) n -> p k n", p=128), [], [Bw3[sl]], Bw3[sl])
            self.dma("pool", w2[:], I["w2"][ex].rearrange("(k p) n -> p k n", p=128), [], [Bw2], Bw2)
            if ex + 1 < 32:
                loads(ex + 1)
            for fc in range(4):
                fs = slice(fc * 128, (fc + 1) * 128)
                f1, f3 = (fc % 2) * 2, (fc % 2) * 2 + 1
                for kc in range(KC):
                    self.mm(self.F[f1][:, 0:CAP], w1[sl][:, kc, fs], XT[sl][:, kc, :], kc == 0, kc == KC - 1, [Bw1[sl]] + BXT[sl], [self.BF[f1]])
                for kc in range(KC):
                    self.mm(self.F[f3][:, 0:CAP], w3[sl][:, kc, fs], XT[sl][:, kc, :], kc == 0, kc == KC - 1, [Bw3[sl]] + BXT[sl], [self.BF[f3]])
                i = si[0] % 2
                si[0] += 1
                self.act(s1[i][:], self.F[f1][:, 0:CAP], AF.Silu, [self.BF[f1]], [Bs1[i]])
                self.tt(hid[sl][:, fc, :], s1[i][:], self.F[f3][:, 0:CAP], ALU.mult, [Bs1[i], self.BF[f3]], [Bhid[sl][fc]])
            if ex + 1 < 32:
                trans(ex + 1)
            for r_ in range(nrt):
                y_ = ys[yi[0] % 2]
                by = Bys[yi[0] % 2]
                yi[0] += 1
                for cg in range(4):
                    cs_ = slice(cg * 512, (cg + 1) * 512)
                    fb = 4 + (cg % 2)
                    for fc in range(4):
                        self.mm(self.F[fb][:, :], hid[sl][:, fc, r_ * 128:(r_ + 1) * 128], w2[:, fc, cs_], fc == 0, fc == 3,
                                [Bhid[sl][fc], Bw2], [self.BF[fb]])
                    if fb == 4:
                        self.cp("act", y_[:, cs_], self.F[fb][:, :], [self.BF[fb]], [by])
                    else:
                        self.cp("dve", y_[:, cs_], self.F[fb][:, :], [self.BF[fb]], [by])
                row0 = ex * CAP + r_ * 128
                for hf in range(2):
                    self.dma("sp", D_["YD%d" % hf][row0:row0 + 128, :], y_[:, hf * 1024:(hf + 1) * 1024], [by], [Buf("y")], by)
        sb.pop()
        self.release(Bw1 + Bw3 + [Bw2] + Bxr + Bys[:1])
        self.bar()
        sb.push()
        xs = [sb.alloc("xs", [128, D], F32) for _ in range(2)]
        Bx = [self.dbuf("xs") for _ in range(2)]
        yg = [[[sb.alloc("yg", [128, 1024], F32) for _ in range(2)] for _ in range(2)] for _ in range(2)]
        Byg = [[[Buf("yg") for _ in range(2)] for _ in range(2)] for _ in range(2)]
        jk = sb.alloc("jk5", [128, D], BF16)
        st5 = sb.alloc("st5", [128, 4], F32)
        Bst = Buf("st5")
        for tile in range(16):
            sl = tile % 2
            self.dma("sp", xs[sl][:], D_["X1"][tile * 128:(tile + 1) * 128, :], self.scr_reads("X1", [tile]), [Bx[sl]], Bx[sl])
            for k_ in range(2):
                self.cp("dve", self.gix[sl][:, k_:k_ + 1], self.desti[:, tile, k_:k_ + 1], [self.B_dest[tile]], [self.B_gix[sl][k_]])
                for hf in range(2):
                    S.op("pool", lambda e, k_=k_, ix=self.gix[sl], dst=yg[sl][k_][hf], hf=hf: e.indirect_dma_start(
                        out=dst[:], out_offset=None, in_=D_["YD%d" % hf],
                        in_offset=bass.IndirectOffsetOnAxis(ap=ix[:, k_:k_ + 1], axis=0)),
                        [self.B_gix[sl][k_]], [Byg[sl][k_][hf], self.B_ind], dbuf=self.B_ind)
            for hf in range(2):
                a, b = yg[sl][0][hf], yg[sl][1][hf]
                ba, bb = Byg[sl][0][hf], Byg[sl][1][hf]
                hs_ = slice(hf * 1024, (hf + 1) * 1024)
                self.ts(a[:], a[:], self.wk[:, tile, 0:1], None, ALU.mult, None, [ba, self.B_dest[tile]], [ba])
                self.stt(a[:], b[:], self.wk[:, tile, 1:2], a[:], ALU.mult, ALU.add, [bb, ba, self.B_dest[tile]], [ba])
                self.tt(a[:], a[:], self.gt2b[:, hs_], ALU.mult, [ba, self.B_c0], [ba])
                self.tt(xs[sl][:, hs_], xs[sl][:, hs_], a[:], ALU.add, [Bx[sl], ba], [Bx[sl]])
            self.act(jk[:], xs[sl][:], AF.Square, [Bx[sl]], [Bst], accum=st5[:, 0:1])
            self.rstd(st5[:, 2:3], st5[:, 0:1], float(D), [Bst], [Bst, Bst], st5[:, 1:2])
            self.stt(xs[sl][:], xs[sl][:], st5[:, 2:3], self.fgb[:], ALU.mult, ALU.mult, [Bx[sl], Bst, self.B_c0], [Bx[sl]])
            self.dma("sp", self.out[tile * 128:(tile + 1) * 128, :], xs[sl][:], [Bx[sl]], [Buf("o")], Bx[sl])
        sb.pop()
        self.release(Bx)


def host_consts():
    bf = ml_dtypes.bfloat16
    c = {}
    c["k_ident"] = np.eye(128, dtype=np.float32)
    cm = np.zeros((128, 512), np.float32)
    for j in range(2):
        kp = np.arange(128)[:, None] + j * 128
        q = np.arange(256)[None, :]
        cm[:, j * 256:(j + 1) * 256] = np.where(kp <= q, 0.0, NEG)
    c["k_cm"] = cm
    cp = np.zeros((128, 16, 16), np.float32)
    for qt in range(16):
        cp[:, qt, 8 + qt // 2:] = NEGBIG
    c["k_cpen"] = cp.reshape(128, 256)
    sel = np.zeros((16, 16, 128), np.float32)
    for i in range(16):
        sel[i, i, :] = 1.0
    c["k_sel"] = sel.reshape(16, 2048)
    s = np.arange(128)[:, None]
    t = np.arange(128)[None, :]
    same = (s // 32) == (t // 32)
    mid = (t // 32) * 32 + 15
    wm = np.where(same & (s > mid) & (s <= t), 1.0, 0.0) - np.where(same & (s > t) & (s <= mid), 1.0, 0.0)
    wmid = np.zeros((128, 4), np.float32)
    wtot = np.zeros((128, 4), np.float32)
    for cc in range(4):
        sl = np.arange(128)
        wmid[:, cc] = ((sl // 32) == cc) & (sl <= cc * 32 + 15)
        wtot[:, cc] = (sl // 32) == cc
    c["k_wall"] = np.concatenate([wm, wmid, wtot], axis=1).astype(np.float32)
    c["k_wl"] = np.where(same & (s > t), 1.0, 0.0).astype(np.float32)
    c["k_bd"] = np.where(same & (s <= t), 1.0, 0.0).astype(np.float32)
    c["k_slt"] = np.where(s < t, 1.0, 0.0).astype(np.float32)
    c["k_ones"] = np.ones((128, 128), np.float32)
    c["k_ebase"] = np.tile((np.arange(32) * CAP).astype(np.float32)[None, :], (128, 1))
    return c


_NC_CACHE = {}


def get_nc(debug=False, stages="012345"):
    key = (debug, stages)
    if key not in _NC_CACHE:
        nc = bass.Bass("TRN2", target_bir_lowering=False)
        kb = K(nc, debug=debug, stages=stages)
        kb.build()
        _NC_CACHE[key] = (nc, kb)
    return _NC_CACHE[key]


def make_in_maps(inputs, cores=range(8)):
    f = lambda a: np.ascontiguousarray(np.asarray(a, dtype=np.float32))
    x = f(inputs["x"])
    c = f(inputs["c"])
    shared = {
        "norm1_g": f(inputs["norm1_g"])[0], "norm2_g": f(inputs["norm2_g"])[0], "final_g": f(inputs["final_g"]),
        "w_ada": f(inputs["w_ada"])[0], "b_ada": f(inputs["b_ada"])[0], "w_in": f(inputs["w_in"])[0],
        "r_lower": f(inputs["r_lower"]), "r_norm_g": f(inputs["r_norm_g"])[0],
        "w_up_a": f(inputs["w_up_a"])[0], "w_up_r": f(inputs["w_up_r"])[0], "w_out": f(inputs["w_out"])[0],
        "w_rg": f(inputs["w_rg"])[0], "b_rg": f(inputs["b_rg"])[0], "w_re": f(inputs["w_re"])[0], "b_re": f(inputs["b_re"])[0],
        "w1": f(inputs["w1"])[0], "w3": f(inputs["w3"])[0], "w2": f(inputs["w2"])[0],
    }
    shared.update(host_consts())
    maps = []
    for core in cores:
        b, half = core // 2, core % 2
        m = dict(shared)
        m["xm"] = np.ascontiguousarray(x[b, half * NT:(half + 1) * NT])
        m["xp"] = np.ascontiguousarray(x[b, 0:NT])
        m["c"] = np.ascontiguousarray(c[b])
        m["pm"] = np.full((128, 1), float(half), np.float32)
        maps.append(m)
    return maps


def kernel(**inputs):
    nc, _ = get_nc()
    maps = make_in_maps(inputs)
    res = run_bass_kernel_spmd(nc, maps, core_ids=list(range(8)))
    out = np.zeros((4, 4096, D), np.float32)
    for core in range(8):
        b, half = core // 2, core % 2
        out[b, half * NT:(half + 1) * NT] = np.asarray(res.results[core]["out"], dtype=np.float32)
    return out
```

```python
import contextlib
import numpy as np
import ml_dtypes
import concourse.bass as bass
import concourse.mybir as mybir
from concourse.bass_utils import run_bass_kernel_spmd

F32 = mybir.dt.float32
BF16 = mybir.dt.bfloat16
I32 = mybir.dt.int32
CAP = 512
NR = 32 * CAP
ALU = mybir.AluOpType
AF = mybir.ActivationFunctionType
AX = mybir.AxisListType

D = 2048
NT = 2048
KC = 16
EPS = 1e-6
NEG = -30000.0
NEGBIG = -1.0e30
ENGS = ("pe", "act", "dve", "pool", "sp")
SAME_ENGINE_SYNC = {"pe": False, "act": True, "dve": True, "pool": True, "sp": False}


class Buf:
    __slots__ = ("name", "last_w", "readers", "dsem", "dcount", "const", "last_dma", "excl")

    def __init__(self, name, const=False, excl=False):
        self.excl = excl
        self.name = name
        self.last_w = None
        self.readers = []
        self.dsem = None
        self.dcount = 0
        self.const = const
        self.last_dma = None


class Op:
    __slots__ = ("eng", "fn", "deps", "sig", "sem", "val", "dbuf")

    def __init__(self, eng, fn, dbuf=None):
        self.eng = eng
        self.fn = fn
        self.deps = []
        self.sig = dbuf is not None
        self.sem = None
        self.val = 0
        self.dbuf = dbuf


class Sched:
    def __init__(self, nc):
        self.nc = nc
        self.ops = {e: [] for e in ENGS}
        self.dma_bufs = []
        self.epoch = None
        self.all_ops = []

    def barrier(self, fn):
        o = Op("dve", fn, None)
        deps = {}
        for e in ENGS:
            if self.ops[e]:
                d = self.ops[e][-1]
                deps[id(d)] = d
        for b in self.dma_bufs:
            if b.last_dma is not None:
                deps[id(b.last_dma)] = b.last_dma
        o.deps = list(deps.values())
        self.ops["dve"].append(o)
        self.all_ops.append(o)
        self.epoch = o
        return o

    def op(self, eng, fn, reads=(), writes=(), dbuf=None, extra=()):
        o = Op(eng, fn, dbuf)
        deps = {}
        for d in extra:
            if d is not None:
                deps[id(d)] = d
        if self.epoch is not None:
            deps[id(self.epoch)] = self.epoch
        xr = [b for b in reads if b.excl]
        if xr:
            reads = [b for b in reads if not b.excl]
            writes = list(writes) + [b for b in xr if b not in writes]
        for b in reads:
            if b.last_w is not None:
                deps[id(b.last_w)] = b.last_w
        for b in writes:
            if b.last_w is not None:
                deps[id(b.last_w)] = b.last_w
            for r in b.readers:
                deps[id(r)] = r
        o.deps = list(deps.values())
        for b in writes:
            b.last_w = o
            b.readers = []
        for b in reads:
            if not b.const and b.last_w is not o:
                b.readers.append(o)
        if dbuf is not None:
            dbuf.last_dma = o
            if dbuf.dsem is None:
                dbuf.dsem = True
                self.dma_bufs.append(dbuf)
        self.ops[eng].append(o)
        self.all_ops.append(o)
        return o

    def emit(self, stack):
        nc = self.nc
        for e in ENGS:
            for o in self.ops[e]:
                keep = []
                for d in o.deps:
                    if d.dbuf is None and d.eng == o.eng and not SAME_ENGINE_SYNC[o.eng]:
                        continue
                    keep.append(d)
                    d.sig = True
                o.deps = keep
        esem = {}
        for e in ENGS:
            if any(o.sig and o.dbuf is None for o in self.ops[e]):
                esem[e] = stack.enter_context(nc.semaphore("s_" + e))
        for i, b in enumerate(self.dma_bufs):
            b.dsem = stack.enter_context(nc.semaphore("d%d" % i))
            b.dcount = 0
        cnt = {e: 0 for e in ENGS}
        for o in self.all_ops:
            if o.dbuf is not None:
                o.dbuf.dcount += 16
                o.sem = o.dbuf.dsem
                o.val = o.dbuf.dcount
            elif o.sig:
                cnt[o.eng] += 1
                o.sem = esem[o.eng]
                o.val = cnt[o.eng]
        block = stack.enter_context(nc.Block())
        handles = {"pe": block.tensor, "act": block.scalar, "dve": block.vector,
                   "pool": block.gpsimd, "sp": block.sync}
        for e in ENGS:
            ops = self.ops[e]
            if not ops:
                continue

            def body(eng, ops=ops):
                waited = {}
                for o in ops:
                    need = {}
                    for d in o.deps:
                        k = id(d.sem)
                        if d.val > need.get(k, (0, None))[0]:
                            need[k] = (d.val, d.sem)
                    for k, (v, sem) in need.items():
                        if waited.get(k, 0) >= v:
                            continue
                        waited[k] = v
                        eng.wait_ge(sem, v)
                    ins = o.fn(eng)
                    if o.sig:
                        ins.then_inc(o.sem, 16 if o.dbuf is not None else 1)

            handles[e](body)


class SbAlloc:
    def __init__(self, nc, base=17408, limit=229000):
        self.nc = nc
        self.top = base
        self.limit = limit
        self.n = 0
        self.marks = []

    def push(self):
        self.marks.append(self.top)

    def pop(self):
        self.top = self.marks.pop()

    def alloc(self, name, shape, dtype):
        sz = int(np.prod(shape[1:])) * (4 if dtype in (F32, I32) else 2)
        off = (self.top + 63) // 64 * 64
        assert off + sz <= self.limit, (name, off, sz, self.limit)
        self.top = off + sz
        self.n += 1
        return self.nc.alloc_sbuf_tensor_at("%s_%d" % (name, self.n), list(shape), dtype, offset=off)


class K:
    def __init__(self, nc, debug=False, stages="012345"):
        self.nc = nc
        self.S = Sched(nc)
        self.sb = SbAlloc(nc)
        self.debug = debug
        self.stages = stages
        self.dpool = {}
        self.bkind = {}
        self.dram = {}
        self.sbufs = {}
        self.scr_w = {}

    def din(self, name, shape, dt=F32):
        return self.nc.dram_tensor(name, list(shape), dt, kind="ExternalInput").ap()

    def dscr(self, name, shape, dt):
        kind = "ExternalOutput" if self.debug else "Internal"
        t = self.nc.dram_tensor(name, list(shape), dt, kind=kind).ap()
        self.dram[name] = t
        return t

    def dbuf(self, name, kind="sp"):
        pool = self.dpool.setdefault(kind, [])
        if pool:
            return pool.pop()
        b = Buf(name)
        self.bkind[id(b)] = kind
        return b

    def release(self, bufs):
        for b in bufs:
            self.dpool[self.bkind[id(b)]].append(b)

    def dma(self, q, out, in_, reads, writes, dbuf, **kw):
        k = self.bkind.setdefault(id(dbuf), q)
        assert k == q, (dbuf.name, k, q)
        return self.S.op(q, lambda e: e.dma_start(out=out, in_=in_, **kw), reads, writes, dbuf=dbuf)

    def mm(self, out, lhsT, rhs, start, stop, reads, writes, tp=None):
        if tp is not None:
            return self.S.op("pe", lambda e: e.matmul(out, lhsT=lhsT, rhs=rhs, start=start, stop=stop, tile_position=tp), reads, writes)
        return self.S.op("pe", lambda e: e.matmul(out, lhsT=lhsT, rhs=rhs, start=start, stop=stop), reads, writes)

    def tr(self, out, in_, reads, writes):
        idn = self.identb
        n = in_.shape[0]
        return self.S.op("pe", lambda e: e.transpose(out=out, in_=in_, identity=idn[0:n, 0:n]),
                         list(reads) + [self.B_c0], writes)

    def act(self, out, in_, func, reads, writes, scale=1.0, bias=None, accum=None):
        kw = {}
        if bias is not None:
            kw["bias"] = bias
        if accum is not None:
            kw["accum_out"] = accum
        return self.S.op("act", lambda e: e.activation(out=out, in_=in_, func=func, scale=scale, **kw), reads, writes)

    def ts(self, out, in0, s1, s2, op0, op1, reads, writes, eng="dve"):
        if s2 is None:
            return self.S.op(eng, lambda e: e.tensor_scalar(out=out, in0=in0, scalar1=s1, scalar2=None, op0=op0), reads, writes)
        return self.S.op(eng, lambda e: e.tensor_scalar(out=out, in0=in0, scalar1=s1, scalar2=s2, op0=op0, op1=op1), reads, writes)

    def tt(self, out, in0, in1, op, reads, writes, eng="dve"):
        return self.S.op(eng, lambda e: e.tensor_tensor(out=out, in0=in0, in1=in1, op=op), reads, writes)

    def stt(self, out, in0, scalar, in1, op0, op1, reads, writes, eng="dve"):
        return self.S.op(eng, lambda e: e.scalar_tensor_tensor(out=out, in0=in0, scalar=scalar, in1=in1, op0=op0, op1=op1), reads, writes)

    def cp(self, eng, out, in_, reads, writes):
        if eng == "act":
            return self.S.op("act", lambda e: e.copy(out=out, in_=in_), reads, writes)
        return self.S.op(eng, lambda e: e.tensor_copy(out=out, in_=in_), reads, writes)

    def rstd(self, out, ssq, n, reads, writes, tmp):
        eps = self.epsc
        self.act(tmp, ssq, AF.Sqrt, list(reads) + [self.B_c0], [writes[1]], scale=1.0 / n, bias=eps[:, 0:1])
        self.S.op("dve", lambda e: e.reciprocal(out=out, in_=tmp), [writes[1]], [writes[0]])

    def build(self):
        nc = self.nc
        sb = self.sb
        S = self.S
        I = {}
        I["xm"] = self.din("xm", [NT, D])
        I["xp"] = self.din("xp", [NT, D])
        I["c"] = self.din("c", [D])
        I["pm"] = self.din("pm", [128, 1])
        I["norm1_g"] = self.din("norm1_g", [D])
        I["norm2_g"] = self.din("norm2_g", [D])
        I["final_g"] = self.din("final_g", [D])
        I["w_ada"] = self.din("w_ada", [D, 6 * D])
        I["b_ada"] = self.din("b_ada", [6 * D])
        I["w_in"] = self.din("w_in", [D, 11264])
        I["r_lower"] = self.din("r_lower", [2, 1024])
        I["r_norm_g"] = self.din("r_norm_g", [128])
        I["w_up_a"] = self.din("w_up_a", [1024, D])
        I["w_up_r"] = self.din("w_up_r", [1024, D])
        I["w_out"] = self.din("w_out", [D, D])
        I["w_rg"] = self.din("w_rg", [D, 4])
        I["b_rg"] = self.din("b_rg", [4])
        I["w_re"] = self.din("w_re", [D, 32])
        I["b_re"] = self.din("b_re", [32])
        I["w1"] = self.din("w1", [32, D, 512])
        I["w3"] = self.din("w3", [32, D, 512])
        I["w2"] = self.din("w2", [32, 512, D])
        I["k_ident"] = self.din("k_ident", [128, 128])
        I["k_cm"] = self.din("k_cm", [128, 512])
        I["k_cpen"] = self.din("k_cpen", [128, 256])
        I["k_sel"] = self.din("k_sel", [16, 2048])
        I["k_wall"] = self.din("k_wall", [128, 136])
        I["k_wl"] = self.din("k_wl", [128, 128])
        I["k_bd"] = self.din("k_bd", [128, 128])
        I["k_slt"] = self.din("k_slt", [128, 128])
        I["k_ones"] = self.din("k_ones", [128, 128])
        I["k_ebase"] = self.din("k_ebase", [128, 32])
        self.I = I
        self.out = nc.dram_tensor("out", [NT, D], F32, kind="ExternalOutput").ap()
        self.dscr("MOD", [6 * D], F32)
        self.dscr("QT", [8, 128, NT], BF16)
        self.dscr("KT", [8, 128, 2 * NT], BF16)
        self.dscr("V", [2 * NT, 1024], BF16)
        self.dscr("RQT", [8, 128, NT], BF16)
        self.dscr("RF", [2 * NT, 1024], F32)
        self.dscr("RI", [2 * NT, 1024], BF16)
        self.dscr("ROG", [NT, 1024], BF16)
        self.dscr("GAT", [16, 128, NT], BF16)
        self.dscr("GRT", [16, 128, NT], BF16)
        self.dscr("MT", [16, 128, NT], BF16)
        self.dscr("X1", [NT, D], F32)
        self.dscr("H2T", [16, 128, NT], BF16)
        self.dscr("XD", [NR, D], BF16)
        self.dscr("YD0", [NR, 1024], F32)
        self.dscr("YD1", [NR, 1024], F32)
        if self.debug:
            self.dscr("ATT", [8, 128, NT], BF16)
            self.dscr("OT", [8, 128, NT], BF16)
            self.dscr("WF", [128, 16 * 32], F32)
        self.scrB = {}

        with contextlib.ExitStack() as st:
            self.F = [st.enter_context(nc.psum_tensor("F%d" % i, [128, 512], F32)) for i in range(6)]
            self.T = [st.enter_context(nc.psum_tensor("T%d" % i, [128, 1024], BF16)) for i in range(2)]
            self.BF = [Buf("F%d" % i, excl=True) for i in range(6)]
            self.BFs = [[self.BF[i]] * 4 for i in range(6)]
            _bt = [Buf("T%d" % i, excl=True) for i in range(2)]
            self.BT = [[_bt[i]] * 8 for i in range(2)]
            self.dummy = sb.alloc("dummy", [128, 8], F32)
            self.stage0()
            self.bar()
            if "1" in self.stages:
                self.stage1()
                self.bar()
            sb.push()
            self.attT = sb.alloc("attT", [128, 8, NT], BF16)
            self.oT = sb.alloc("oT", [128, 8, NT], BF16)
            self.B_attT = [Buf("attT%d" % h) for h in range(8)]
            self.B_oT = [Buf("oT%d" % t) for t in range(16)]
            if "2" in self.stages:
                self.stage2()
                self.bar()
            if "3" in self.stages:
                self.stage3()
                self.bar()
            if "4" in self.stages:
                self.stage4a()
                self.bar()
            sb.pop()
            if "4" in self.stages:
                self.stage4b()
                self.bar()
            if "5" in self.stages:
                self.stage5()
            allb = [b for b in self.S.dma_bufs]
            S.op("sp", lambda e: e.nop(), [], allb)
            S.emit(st)
        return nc

    def bar(self):
        d = self.dummy
        self.S.barrier(lambda e: e.memset(d[:], 0.0))

    def sB(self, name, key):
        k = (name, key)
        if k not in self.scrB:
            self.scrB[k] = Buf("%s%s" % (name, key))
        return self.scrB[k]

    def stage0(self):
        S, sb, I = self.S, self.sb, self.I
        cbs = {"sp": [Buf("c%d" % i) for i in range(4)], "pool": [Buf("cp%d" % i) for i in range(2)]}
        cb = cbs["sp"] + cbs["pool"]
        self.B_c0 = Buf("constall")
        ci = [0]

        def cload(q, dst, src, **kw):
            lst = cbs[q]
            b = lst[ci[0] % len(lst)]
            ci[0] += 1
            self.dma(q, dst, src, [], [b], b, **kw)

        def A(name, shape, dt=F32):
            t = sb.alloc(name, shape, dt)
            return t

        self.identb = A("identb", [128, 128], BF16)
        self.ident32 = A("ident32", [128, 128])
        self.rows = A("rows", [48, 128])
        self.cm = A("cm", [128, 512], BF16)
        self.cpen = A("cpen", [128, 256])
        self.selc = A("selc", [16, 2048], BF16)
        self.wall = A("wall", [128, 136])
        self.wl = A("wl", [128, 128])
        self.bd = A("bd", [128, 128])
        self.epsc = A("epsc", [128, 1])
        self.pm = A("pm", [128, 1])
        self.ppen = A("ppen", [128, 1])
        self.g1c = A("g1c", [128, 16])
        self.g2c = A("g2c", [128, 16])
        self.ccol = A("ccol", [128, 16])
        self.cact = A("cact", [128, 16], BF16)
        self.modc = A("modc", [128, 6, 16])
        self.a1 = A("a1", [128, 16])
        self.a2 = A("a2", [128, 16])
        self.gt1b = A("gt1b", [128, D])
        self.gt2b = A("gt2b", [128, D])
        self.fgb = A("fgb", [128, D])
        self.lb = A("lb", [128, 1024])
        self.oml = A("oml", [128, 1024])
        self.rngb = A("rngb", [128, 128])
        self.wr = A("wr", [128, 16, 36], BF16)
        self.br = A("br", [128, 36])
        self.wfull = A("wfull", [128, 16, 32])
        self.slt = A("slt", [128, 128], BF16)
        self.a2b = A("a2b", [128, D], BF16)
        self.sh2bb = A("sh2bb", [128, D], BF16)
        self.ones = A("ones", [128, 128], BF16)
        self.ebase = A("ebase", [128, 32])
        self.tot = A("tot", [128, 32])
        self.desti = A("desti", [128, 16, 2], I32)
        self.wk = A("wk", [128, 16, 2])
        self.B_tot = Buf("tot")
        self.B_ind = Buf("ind")
        self.six = [A("six", [128, 2], I32) for _ in range(2)]
        self.gix = [A("gix", [128, 2], I32) for _ in range(2)]
        self.B_six = [[Buf("six%d_%d" % (i, k)) for k in range(2)] for i in range(2)]
        self.B_gix = [[Buf("gix%d_%d" % (i, k)) for k in range(2)] for i in range(2)]
        self.B_dest = [Buf("dest%d" % t) for t in range(16)]
        self.B_wfull = [Buf("wfull%d" % t) for t in range(16)]
        cload("pool", self.identb[:], I["k_ident"])
        cload("sp", self.ident32[:], I["k_ident"])
        cload("pool", self.cm[:], I["k_cm"])
        cload("sp", self.cpen[:], I["k_cpen"])
        cload("pool", self.selc[:], I["k_sel"])
        cload("sp", self.wall[:], I["k_wall"])
        cload("sp", self.wl[:], I["k_wl"])
        cload("sp", self.bd[:], I["k_bd"])
        cload("sp", self.pm[:], I["pm"])
        cload("sp", self.rows[0:16, :], I["norm1_g"].rearrange("(k p) -> k p", p=128))
        cload("sp", self.rows[16:32, :], I["norm2_g"].rearrange("(k p) -> k p", p=128))
        cload("sp", self.rows[32:48, :], I["c"].rearrange("(k p) -> k p", p=128))
        cload("sp", self.fgb[:], I["final_g"].partition_broadcast(128))
        cload("sp", self.lb[:], I["r_lower"][0, :].partition_broadcast(128))
        cload("sp", self.oml[:], I["r_lower"][1, :].partition_broadcast(128))
        cload("sp", self.rngb[:], I["r_norm_g"].partition_broadcast(128))
        cload("pool", self.wr[:, :, 0:4], I["w_rg"].rearrange("(k p) n -> p k n", p=128))
        cload("pool", self.wr[:, :, 4:36], I["w_re"].rearrange("(k p) n -> p k n", p=128))
        cload("pool", self.slt[:], I["k_slt"])
        cload("pool", self.ones[:], I["k_ones"])
        cload("sp", self.ebase[:], I["k_ebase"])
        cload("sp", self.br[:, 0:4], I["b_rg"].partition_broadcast(128))
        cload("sp", self.br[:, 4:36], I["b_re"].partition_broadcast(128))
        cs = Buf("cs")
        S.op("dve", lambda e: e.memset(self.epsc[:], EPS), [], [cs])
        S.op("pe", lambda e: e.transpose(out=self.F[2][:, 0:48], in_=self.rows[:, :], identity=self.ident32[0:48, 0:48]), cb, [self.BF[2]])
        self.cp("dve", self.g1c[:], self.F[2][:, 0:16], [self.BF[2]], [cs])
        self.cp("dve", self.g2c[:], self.F[2][:, 16:32], [self.BF[2]], [cs])
        self.cp("dve", self.ccol[:], self.F[2][:, 32:48], [self.BF[2]], [cs])
        S.op("dve", lambda e: e.memset(self.tot[:], 0.0), [], [self.B_tot])
        self.ts(self.ppen[:], self.pm[:], -1.0, -NEGBIG, ALU.add, ALU.mult, cb, [cs])
        self.tt(self.lb[:], self.lb[:], self.oml[:], ALU.subtract, cb, [cs])
        self.act(self.lb[:], self.lb[:], AF.Sigmoid, [cs], [cs])
        self.ts(self.oml[:], self.lb[:], -1.0, 1.0, ALU.mult, ALU.add, [cs], [cs])
        self.act(self.cact[:], self.ccol[:], AF.Silu, [cs], [cs])
        sb.push()
        MOD = self.dram["MOD"]
        wsl = [sb.alloc("wsl0", [128, KC, 512], BF16) for _ in range(3)]
        Bw = [self.dbuf("wsl", "pool") for _ in range(3)]
        bsl = [sb.alloc("bsl", [1, 512], F32) for _ in range(2)]
        Bb = [self.dbuf("bsl") for _ in range(2)]
        msl = [sb.alloc("msl", [1, 512], F32) for _ in range(2)]
        Bm = [self.dbuf("msl") for _ in range(2)]
        wsrc = I["w_ada"].rearrange("(k p) n -> p k n", p=128)
        Bmod = Buf("MOD")
        for j in range(24):
            w = wsl[j % 3]
            bw = Bw[j % 3]
            self.dma("pool", w[:], wsrc[:, :, j * 512:(j + 1) * 512], [], [bw], bw)
            bb = Bb[j % 2]
            self.dma("sp", bsl[j % 2][:], I["b_ada"][j * 512:(j + 1) * 512].rearrange("(o n) -> o n", o=1), [], [bb], bb)
            pf = self.F[j % 2]
            bpf = self.BF[j % 2]
            for kc in range(KC):
                self.mm(pf[0:1, :], self.cact[:, kc:kc + 1], w[:, kc, :], kc == 0, kc == KC - 1, [bw, cs], [bpf])
            bm = Bm[j % 2]
            self.tt(msl[j % 2][:], pf[0:1, :], bsl[j % 2][:], ALU.add, [bpf, bb], [bm])
            self.dma("sp", MOD[j * 512:(j + 1) * 512].rearrange("(o n) -> o n", o=1), msl[j % 2][:], [bm], [Bmod], bm)
        sb.pop()
        self.release(Bw + Bb + Bm)
        bmc = self.dbuf("modc")
        modr = sb.alloc("modr", [96, 128], F32)
        self.dma("sp", modr[:], MOD.rearrange("(r p) -> r p", p=128), [Bmod], [bmc], bmc)
        S.op("pe", lambda e: e.transpose(out=self.F[3][:, 0:96], in_=modr[:, :], identity=self.ident32[0:96, 0:96]), [bmc] + cb, [self.BF[3]])
        self.cp("dve", self.modc[:].rearrange("p s k -> p (s k)"), self.F[3][:, 0:96], [self.BF[3]], [cs])
        bg1 = self.dbuf("gt1b")
        self.dma("sp", self.gt1b[:], MOD[2 * D:3 * D].partition_broadcast(128), [Bmod], [bg1], bg1)
        bg2 = self.dbuf("gt2b")
        self.dma("sp", self.gt2b[:], MOD[5 * D:6 * D].partition_broadcast(128), [Bmod], [bg2], bg2)
        sb.push()
        g2b = sb.alloc("g2b", [128, D], F32)
        sc2b = sb.alloc("sc2b", [128, D], F32)
        sh2b = sb.alloc("sh2b", [128, D], F32)
        bq1, bq2, bq3 = self.dbuf("g2b"), self.dbuf("sc2b"), self.dbuf("sh2b")
        self.dma("sp", g2b[:], I["norm2_g"].partition_broadcast(128), [], [bq1], bq1)
        self.dma("sp", sc2b[:], MOD[4 * D:5 * D].partition_broadcast(128), [Bmod], [bq2], bq2)
        self.dma("sp", sh2b[:], MOD[3 * D:4 * D].partition_broadcast(128), [Bmod], [bq3], bq3)
        self.stt(self.a2b[:], sc2b[:], 1.0, g2b[:], ALU.add, ALU.mult, [bq1, bq2], [cs])
        self.cp("dve", self.sh2bb[:], sh2b[:], [bq3], [cs])
        sb.pop()
        self.release([bq1, bq2, bq3])
        self.stt(self.a1[:], self.modc[:, 1, :], 1.0, self.g1c[:], ALU.add, ALU.mult, [cs], [cs])
        self.stt(self.a2[:], self.modc[:, 4, :], 1.0, self.g2c[:], ALU.add, ALU.mult, [cs], [cs])
        S.op("dve", lambda e: e.memset(self.epsc[:], EPS), cb + [cs, bmc, bg1, bg2], [self.B_c0])
        self.B_c0.const = True
        self.B_modc = bmc

    def norm_to_hT(self, xt, bx, hT_dst, bh, a_col, sh_col, ring):
        junk, ssq, tmp, rs, xn, bj, bs, bxn = ring
        self.act(junk[:], xt, AF.Square, [bx], [bj, bs], accum=ssq[:, 0:1])
        self.rstd(rs[:, 0:1], ssq[:, 0:1], float(D), [bs], [bs, bj], tmp[:, 0:1])
        yield
        self.ts(xn[:], xt, rs[:, 0:1], None, ALU.mult, None, [bx, bs], [bxn])
        yield
        for tb in range(2):
            for ts_ in range(8):
                kc = tb * 8 + ts_
                self.tr(self.T[tb][:, ts_ * 128:(ts_ + 1) * 128], xn[:, kc * 128:(kc + 1) * 128], [bxn], [self.BT[tb][0]])
            yield
            for ts_ in range(8):
                kc = tb * 8 + ts_
                pt = self.T[tb][:, ts_ * 128:(ts_ + 1) * 128]
                if tb == 0:
                    self.act(hT_dst[:, kc, :], pt, AF.Identity, [self.BT[tb][0], self.B_c0], [bh], scale=a_col[:, kc:kc + 1], bias=sh_col[:, kc:kc + 1])
                else:
                    self.ts(hT_dst[:, kc, :], pt, a_col[:, kc:kc + 1], sh_col[:, kc:kc + 1], ALU.mult, ALU.add, [self.BT[tb][0], self.B_c0], [bh])
                if ts_ % 2 == 1:
                    yield

    def norm_ring(self, sb, junk=None):
        if junk is None:
            junk = sb.alloc("junk", [128, D], BF16)
        ssq = sb.alloc("ssq", [128, 1], F32)
        tmp = sb.alloc("tmp", [128, 1], F32)
        rs = sb.alloc("rs", [128, 1], F32)
        xn = sb.alloc("xn", [128, D], BF16)
        return (junk, ssq, tmp, rs, xn, Buf("junk"), Buf("ssq"), Buf("xn"))

    def stage1(self):
        S, sb, I = self.S, self.sb, self.I
        sb.push()
        G = 1024
        hT = [sb.alloc("hT", [128, KC, G], BF16) for _ in range(2)]
        BhT = [[Buf("hT%d_%d" % (s, t)) for t in range(8)] for s in range(2)]
        wsl = [sb.alloc("wsl", [128, KC, 512], BF16) for _ in range(3)]
        Bw = [self.dbuf("wsl", "pool") for _ in range(3)]
        xs = [sb.alloc("xs", [128, D], F32) for _ in range(2)]
        Bx = [self.dbuf("xs") for _ in range(2)]
        rings = [self.norm_ring(sb)]
        rings.append(self.norm_ring(sb, junk=rings[0][0]))
        stg = []
        stgb = []
        for _ in range(4):
            sb.push()
            stgb.append(sb.alloc("stgb", [128, 512], BF16))
            sb.pop()
            stg.append(sb.alloc("stg", [128, 512], F32))
        Bs = [self.dbuf("stg") for _ in range(4)]
        wsrc = I["w_in"].rearrange("(k p) n -> p k n", p=128)
        D_ = self.dram
        zt = sb.alloc("zt", [128, 1024], BF16)
        Bz = self.dbuf("zt")
        S.op("dve", lambda e: e.memset(zt[:], 0.0), [], [Bz])
        for i in range(NR // 128):
            for hf in range(2):
                self.dma("sp", D_["XD"][i * 128:(i + 1) * 128, hf * 1024:(hf + 1) * 1024], zt[:], [Bz], [Buf("z")], Bz)
        groups = [("p", 0), ("p", 1), ("m", 0), ("m", 1)]
        pblocks = [2, 3, 4, 5, 8, 9, 10, 11]
        import os
        if os.environ.get("S1G"):
            groups = groups[:int(os.environ["S1G"])]
        if os.environ.get("S1B"):
            pblocks = pblocks[:int(os.environ["S1B"])]
        wi = [0]
        si = [0]
        xi = [0]
        def hT_gen(gi):
            kind, gidx = groups[gi]
            slot = gi % 2
            src = I["xp"] if kind == "p" else I["xm"]
            for t in range(8):
                xsl = xs[xi[0] % 2]
                bx = Bx[xi[0] % 2]
                ring = rings[xi[0] % 2]
                xi[0] += 1
                r0 = gidx * G + t * 128
                self.dma("sp", xsl[:], src[r0:r0 + 128, :], [], [bx], bx)
                yield
                for _ in self.norm_to_hT(xsl[:], bx, hT[slot][:, :, t * 128:(t + 1) * 128], BhT[slot][t], self.a1, self.modc[:, 0, :], ring):
                    yield

        for _ in hT_gen(0):
            pass
        for gi, (kind, gidx) in enumerate(groups):
            slot = gi % 2
            ltok0 = gidx * G + (0 if kind == "p" else NT)
            mtok0 = gidx * G
            nxt = hT_gen(gi + 1) if gi + 1 < len(groups) else iter(())
            blocks = pblocks if kind == "p" else list(range(22))
            nstep = -(-8 * 16 // (len(blocks) * 8)) + 1
            for blk in blocks:
                w = wsl[wi[0] % 3]
                bw = Bw[wi[0] % 3]
                wi[0] += 1
                self.dma("pool", w[:], wsrc[:, :, blk * 512:(blk + 1) * 512], [], [bw], bw)
                fm = blk in (0, 1, 2, 3, 6, 7) or blk >= 14
                for u in range(8):
                    fb = u % 4
                    pf = self.F[fb]
                    bpf = self.BF[fb]
                    if fm:
                        cs_, th = u // 2, u % 2
                        for kc in range(KC):
                            self.mm(pf[:, :], w[:, kc, cs_ * 128:(cs_ + 1) * 128], hT[slot][:, kc, th * 512:(th + 1) * 512],
                                    kc == 0, kc == KC - 1, [bw] + BhT[slot][th * 4:(th + 1) * 4], [bpf])
                    else:
                        for kc in range(KC):
                            self.mm(pf[:, :], hT[slot][:, kc, u * 128:(u + 1) * 128], w[:, kc, :],
                                    kc == 0, kc == KC - 1, [bw, BhT[slot][u]], [bpf])
                    sg = stg[si[0] % 4]
                    sgb = stgb[si[0] % 4]
                    bs = Bs[si[0] % 4]
                    si[0] += 1
                    if fm:
                        if blk < 2:
                            dst = D_["QT"][blk * 4 + cs_, :, mtok0 + th * 512: mtok0 + (th + 1) * 512]
                            key = ("QT", blk * 4 + cs_)
                            fn = AF.Copy
                        elif blk < 4:
                            dst = D_["KT"][(blk - 2) * 4 + cs_, :, ltok0 + th * 512: ltok0 + (th + 1) * 512]
                            key = ("KT", (blk - 2) * 4 + cs_)
                            fn = AF.Copy
                        elif blk < 8:
                            dst = D_["RQT"][(blk - 6) * 4 + cs_, :, mtok0 + th * 512: mtok0 + (th + 1) * 512]
                            key = ("RQT", (mtok0 + th * 512) // 128)
                            fn = AF.Copy
                        elif blk < 18:
                            dst = D_["GAT"][(blk - 14) * 4 + cs_, :, mtok0 + th * 512: mtok0 + (th + 1) * 512]
                            key = ("GAT", (blk - 14) * 4 + cs_)
                            fn = AF.Sigmoid
                        else:
                            dst = D_["GRT"][(blk - 18) * 4 + cs_, :, mtok0 + th * 512: mtok0 + (th + 1) * 512]
                            key = ("GRT", (blk - 18) * 4 + cs_)
                            fn = AF.Sigmoid
                        odt = BF16
                    else:
                        if blk < 6:
                            dst = D_["V"][ltok0 + u * 128: ltok0 + (u + 1) * 128, (blk - 4) * 512:(blk - 3) * 512]
                            key = ("V", (blk - 4) * 4)
                            fn, odt = AF.Copy, BF16
                        elif blk < 10:
                            dst = D_["RF"][ltok0 + u * 128: ltok0 + (u + 1) * 128, (blk - 8) * 512:(blk - 7) * 512]
                            key = ("RF", (ltok0 + u * 128) // 128)
                            fn, odt = AF.Copy, F32
                        elif blk < 12:
                            dst = D_["RI"][ltok0 + u * 128: ltok0 + (u + 1) * 128, (blk - 10) * 512:(blk - 9) * 512]
                            key = ("RI", (ltok0 + u * 128) // 128)
                            fn, odt = AF.Copy, BF16
                        else:
                            dst = D_["ROG"][mtok0 + u * 128: mtok0 + (u + 1) * 128, (blk - 12) * 512:(blk - 11) * 512]
                            key = ("ROG", (mtok0 + u * 128) // 128)
                            fn, odt = AF.Silu, BF16
                    so = sg[:] if odt == F32 else sgb[:]
                    if fn == AF.Copy and (u % 2 == 1):
                        self.cp("dve", so, pf[:, :], [bpf], [bs])
                    else:
                        self.act(so, pf[:, :], fn, [bpf], [bs])
                    wl_ = self.scr_w.setdefault(key, [])
                    tok = Buf("w")
                    wl_.append(tok)
                    self.dma("sp", dst, so, [bs], [tok], bs)
                    for _ in range(nstep):
                        next(nxt, None)
            for _ in nxt:
                pass
        sb.pop()
        self.release(Bw + Bx + Bs + [Bz])

    def scr_reads(self, name, keys):
        out = []
        for k in keys:
            out.extend(self.scr_w.get((name, k), []))
        return out

    def stage2(self):
        S, sb = self.S, self.sb
        D_ = self.dram
        sb.push()
        QTh = [sb.alloc("QTh", [128, NT], BF16) for _ in range(2)]
        KTh = [sb.alloc("KTh", [128, 2 * NT], BF16) for _ in range(2)]
        Vh = [sb.alloc("Vh", [128, 32, 129], BF16) for _ in range(2)]
        Bq = [self.dbuf("q") for _ in range(2)]
        Bk = [self.dbuf("k") for _ in range(2)]
        Bv = [self.dbuf("v") for _ in range(2)]
        km32 = [sb.alloc("km32", [128, 16], F32) for _ in range(2)]
        kmb = [sb.alloc("kmb", [128, 16], BF16) for _ in range(2)]
        gm = [sb.alloc("gm", [128, 16, 16], F32) for _ in range(2)]
        top8 = [sb.alloc("top8", [128, 16, 8], F32) for _ in range(2)]
        pen = [sb.alloc("pen", [128, 16, 16], F32) for _ in range(2)]
        penb = [sb.alloc("penb", [128, 256], BF16) for _ in range(2)]
        penT = [sb.alloc("penT", [16, NT], BF16) for _ in range(2)]
        Bkm = [Buf("km%d" % i) for i in range(2)]
        Bgm = [Buf("gm%d" % i) for i in range(2)]
        Bpen = [Buf("pen%d" % i) for i in range(2)]
        BpenT = [Buf("penT%d" % i) for i in range(2)]
        pT = [sb.alloc("pT", [128, 256], BF16) for _ in range(4)]
        BpT = [Buf("pT%d" % i) for i in range(4)]
        rsum = [sb.alloc("rsum", [128, 1], F32) for _ in range(2)]
        atts = [sb.alloc("atts", [128, 128], BF16) for _ in range(2)]
        Brs = [Buf("rsum%d" % i) for i in range(2)]
        Bat = [Buf("atts%d" % i) for i in range(2)]
        scale = 1.0 / np.sqrt(128.0)
        pi = [0]
        ei = [0]
        for sl in range(2):
            S.op("dve", lambda e, t=Vh[sl]: e.memset(t[:, :, 128:129], 1.0), [], [Bv[sl]])
        def preamble(h):
            sl = h % 2
            q, k, v = QTh[sl], KTh[sl], Vh[sl]
            bq, bk, bv = Bq[sl], Bk[sl], Bv[sl]
            self.dma("sp", q[:], D_["QT"][h], self.scr_reads("QT", [h]), [bq], bq)
            self.dma("sp", k[:], D_["KT"][h], self.scr_reads("KT", [h]), [bk], bk)
            self.dma("sp", v[:, :, 0:128], D_["V"].rearrange("(t p) c -> p t c", p=128)[:, :, h * 128:(h + 1) * 128],
                     self.scr_reads("V", [(h // 4) * 4]), [bv], bv)
            yield
            S.op("dve", lambda e, k=k: e.tensor_reduce(out=km32[sl][:], in_=k[:].rearrange("p (b l) -> p b l", l=256), axis=AX.X, op=ALU.add),
                 [bk], [Bkm[sl]])
            yield
            self.ts(kmb[sl][:], km32[sl][:], 1.0 / 256, None, ALU.mult, None, [Bkm[sl]], [Bkm[sl]])
            yield
            pg = self.F[5]
            bpg = self.BF[5]
            for qt in range(16):
                self.mm(pg[:, qt * 16:(qt + 1) * 16], q[:, qt * 128:(qt + 1) * 128], kmb[sl][:, :], True, True, [bq, Bkm[sl]], [bpg])
            yield
            gmf = gm[sl][:].rearrange("p a b -> p (a b)")
            self.tt(gmf, pg[:, 0:256], self.cpen[:], ALU.add, [bpg, self.B_c0], [Bgm[sl]])
            yield
            self.ts(gm[sl][:, :, 0:8], gm[sl][:, :, 0:8], self.ppen[:, 0:1], None, ALU.add, None, [Bgm[sl], self.B_c0], [Bgm[sl]])
            yield
            for qt in range(16):
                S.op("dve", lambda e, qt=qt: e.max(out=top8[sl][:, qt, :], in_=gm[sl][:, qt, :]), [Bgm[sl]], [Bpen[sl]])
                yield
            for qt in range(16):
                self.ts(pen[sl][:, qt, :], gm[sl][:, qt, :], top8[sl][:, qt, 2:3], None, ALU.is_ge, None, [Bgm[sl], Bpen[sl]], [Bpen[sl]])
                yield
            penf = pen[sl][:].rearrange("p a b -> p (a b)")
            self.ts(penf, penf, -1.0, -NEG, ALU.add, ALU.mult, [Bpen[sl]], [Bpen[sl]])
            yield
            self.ts(pen[sl][:, :, 0:8], pen[sl][:, :, 0:8], self.ppen[:, 0:1], None, ALU.add, None, [Bpen[sl], self.B_c0], [Bpen[sl]])
            yield
            self.cp("dve", penb[sl][:], penf, [Bpen[sl]], [Bpen[sl]])
            yield
            for half in range(2):
                for qt in range(half * 8, half * 8 + 8):
                    tb, ts_ = qt // 8, qt % 8
                    pt = self.T[tb][0:16, ts_ * 128:(ts_ + 1) * 128]
                    self.tr(pt, penb[sl][:, qt * 16:(qt + 1) * 16], [Bpen[sl]], [self.BT[tb][ts_]])
                yield
                for qt in range(half * 8, half * 8 + 8):
                    tb, ts_ = qt // 8, qt % 8
                    pt = self.T[tb][0:16, ts_ * 128:(ts_ + 1) * 128]
                    self.cp("act", penT[sl][:, qt * 128:(qt + 1) * 128], pt, [self.BT[tb][ts_]], [BpenT[sl]])
                yield

        for _ in preamble(0):
            pass
        for h in range(8):
            sl = h % 2
            q, k, v = QTh[sl], KTh[sl], Vh[sl]
            bq, bk, bv = Bq[sl], Bk[sl], Bv[sl]
            nxt = preamble(h + 1) if h + 1 < 8 else iter(())
            itc = [0]
            for qb in range(8):
                Bl = 8 + qb
                nkt = 2 * Bl + 2
                po = [self.F[3], self.F[4]]
                bpo = [self.BF[3], self.BF[4]]
                LAG = 2
                pend = []
                for kk in range(nkt + LAG):
                    if kk < nkt:
                        kt = kk
                        fb = kt % 3
                        ps = self.F[fb]
                        bps = self.BF[fb]
                        self.mm(ps[:, 0:256], k[:, kt * 128:(kt + 1) * 128], q[:, qb * 256:(qb + 1) * 256], True, False, [bk, bq], [bps])
                        if kt < 2 * Bl:
                            i = kt // 2
                            self.mm(ps[:, 0:256], self.selc[0:16, i * 128:(i + 1) * 128], penT[sl][0:16, qb * 256:(qb + 1) * 256],
                                    False, True, [BpenT[sl], self.B_c0], [bps])
                        else:
                            j = kt - 2 * Bl
                            self.mm(ps[:, 0:256], self.identb[:, :], self.cm[:, j * 256:(j + 1) * 256], False, True, [self.B_c0], [bps])
                        p = pT[pi[0] % 4]
                        bp = BpT[pi[0] % 4]
                        pi[0] += 1
                        self.act(p[:], ps[:, 0:256], AF.Exp, [bps], [bp], scale=scale)
                        pend.append((kt, p, bp))
                    if kk >= LAG:
                        kt, p, bp = pend.pop(0)
                        for qh in range(2):
                            self.mm(po[qh][:, 0:129], p[:, qh * 128:(qh + 1) * 128], v[:, kt, :], kt == 0, kt == nkt - 1, [bp, bv], [bpo[qh]])
                    itc[0] += 1
                    if itc[0] >= 24:
                        next(nxt, None)
                for qh in range(2):
                    e_ = ei[0] % 2
                    ei[0] += 1
                    S.op("dve", lambda e, o=rsum[e_], i=po[qh]: e.reciprocal(out=o[:, 0:1], in_=i[:, 128:129]), [bpo[qh]], [Brs[e_]])
                    self.act(atts[e_][:], po[qh][:, 0:128], AF.Copy, [bpo[qh], Brs[e_]], [Bat[e_]], scale=rsum[e_][:, 0:1])
                    qt = qb * 2 + qh
                    tb, ts_ = qt // 8, qt % 8
                    pt = self.T[tb][:, ts_ * 128:(ts_ + 1) * 128]
                    bt = self.BT[tb][ts_]
                    self.tr(pt, atts[e_][:], [Bat[e_]], [bt])
                    self.cp("dve", self.attT[:, h, qt * 128:(qt + 1) * 128], pt, [bt], [self.B_attT[h]])
            for _ in nxt:
                pass
            if self.debug:
                self.dma("sp", D_["ATT"][h], self.attT[:, h, :], [self.B_attT[h]], [Buf("x")], self.B_attT[h])
        sb.pop()
        self.release(Bq + Bk + Bv)

    def stage3(self):
        S, sb = self.S, self.sb
        D_ = self.dram
        sb.push()
        rf = [sb.alloc("rf", [128, 1024], F32) for _ in range(2)]
        ri = [sb.alloc("ri", [128, 1024], BF16) for _ in range(2)]
        rq = [sb.alloc("rq", [128, 8, 128], BF16) for _ in range(2)]
        rog = [sb.alloc("rog", [128, 1024], BF16) for _ in range(2)]
        Brf = [self.dbuf("rf") for _ in range(2)]
        Bri = [self.dbuf("ri") for _ in range(2)]
        Brq = [self.dbuf("rq") for _ in range(2)]
        Brog = [self.dbuf("rog") for _ in range(2)]
        logf = [sb.alloc("logf", [128, 1024], F32) for _ in range(2)]
        kin = [sb.alloc("kin", [128, 1024], F32) for _ in range(2)]
        Blf = [Buf("logf%d" % i) for i in range(2)]
        Bkin = [Buf("kin%d" % i) for i in range(2)]
        R = 4
        ebl = [sb.alloc("ebl", [128, 128], F32) for _ in range(R)]
        khat = [sb.alloc("khat", [128, 128], BF16) for _ in range(R)]
        eb8 = [sb.alloc("eb8", [128, 8], F32) for _ in range(R)]
        ebm = [sb.alloc("ebm", [128, 128], F32) for _ in range(R)]
        qtl = [sb.alloc("qtl", [128, 128], BF16) for _ in range(R)]
        enb = [sb.alloc("enb", [128, 128], F32) for _ in range(R)]
        ktl = [sb.alloc("ktl", [128, 128], BF16) for _ in range(R)]
        ktlT = [sb.alloc("ktlT", [128, 128], BF16) for _ in range(R)]
        khc = [sb.alloc("khc", [128, 4, 128], BF16) for _ in range(R)]
        qtlz = [sb.alloc("qtlz", [128, 4, 128], BF16) for _ in range(R)]
        Bkhc = [Buf("khc%d" % i) for i in range(R)]
        Bqz = [Buf("qtlz%d" % i) for i in range(R)]
        for i in range(R):
            S.op("dve", lambda e, t=qtlz[i]: e.memset(t[:], 0.0), [], [Bqz[i]])
        at = [sb.alloc("at", [128, 128], BF16) for _ in range(R)]
        Sb = [sb.alloc("Sb", [128, 4, 128], BF16) for _ in range(R)]
        ot = [sb.alloc("ot", [128, 128], F32) for _ in range(R)]
        of_ = [sb.alloc("of", [128, 128], BF16) for _ in range(R)]
        sq = [sb.alloc("sq", [128, 4], F32) for _ in range(R)]
        jk = [sb.alloc("jk", [128, 128], BF16) for _ in range(R)]
        names = "ebl khat eb8 ebm qtl enb ktl ktlT at ot of sq".split()
        Bn = {n: [Buf(n + str(i)) for i in range(R)] for n in names}
        BSb = [[Buf("Sb%d_%d" % (i, c)) for c in range(4)] for i in range(R)]
        St = sb.alloc("St", [128, 8, 128], F32)
        BSt = [Buf("St%d" % h) for h in range(8)]
        for h in range(8):
            S.op("dve", lambda e, h=h: e.memset(St[:, h, :], 0.0), [], [BSt[h]])
        F, BFs = self.F, self.BFs
        hi = [0]
        import os
        tl = list(range(32))
        if os.environ.get("S3T"):
            tl = [int(v) for v in os.environ["S3T"].split(",")]
        s3c = int(os.environ.get("S3C", "4"))
        s3m = int(os.environ.get("S3M", "9"))
        for tile in tl:
            main = tile >= 16
            mt = tile - 16
            sl = tile % 2
            self.dma("sp", rf[sl][:], D_["RF"][tile * 128:(tile + 1) * 128, :], self.scr_reads("RF", [tile]), [Brf[sl]], Brf[sl])
            self.dma("sp", ri[sl][:], D_["RI"][tile * 128:(tile + 1) * 128, :], self.scr_reads("RI", [tile]), [Bri[sl]], Bri[sl])
            if main:
                self.dma("sp", rq[sl][:], D_["RQT"][:, :, mt * 128:(mt + 1) * 128].rearrange("h p t -> p h t"),
                         self.scr_reads("RQT", [(mt // 4) * 4]), [Brq[sl]], Brq[sl])
                self.dma("sp", rog[sl][:], D_["ROG"][mt * 128:(mt + 1) * 128, :], self.scr_reads("ROG", [mt]), [Brog[sl]], Brog[sl])
            lf, kn = logf[sl], kin[sl]
            self.act(kn[:], rf[sl][:], AF.Sigmoid, [Brf[sl]], [Bkin[sl]])
            self.tt(kn[:], kn[:], self.oml[:], ALU.mult, [Bkin[sl], self.B_c0], [Bkin[sl]])
            self.tt(kn[:], kn[:], self.lb[:], ALU.add, [Bkin[sl], self.B_c0], [Bkin[sl]])
            self.act(lf[:], kn[:], AF.Ln, [Bkin[sl]], [Blf[sl]])
            self.ts(kn[:], kn[:], -1.0, 1.0, ALU.mult, ALU.add, [Bkin[sl]], [Bkin[sl]])
            def hbody(h, r, main=main, sl=sl, lf=lf, kn=kn, mt=mt):
                hs = slice(h * 128, (h + 1) * 128)
                s2 = h % 2
                c128 = slice(s2 * 128, (s2 + 1) * 128)
                bl_ps = F[0][:, c128]
                b8_ps = F[0][:, 256 + s2 * 8:256 + (s2 + 1) * 8]
                self.mm(bl_ps, self.wl[:, :], lf[:, hs], True, True, [Blf[sl], self.B_c0], [self.BF[0]])
                self.mm(b8_ps, lf[:, hs], self.wall[:, 128:136], True, True, [Blf[sl], self.B_c0], [self.BF[0]])
                if main:
                    self.mm(F[1][:, c128], lf[:, hs], self.wall[:, 0:128], True, True, [Blf[sl], self.B_c0], [self.BF[1]])
                    bm_ps = F[1][:, 256 + s2 * 128:256 + (s2 + 1) * 128]
                    self.mm(bm_ps, self.wall[:, 0:128], lf[:, hs], True, True, [Blf[sl], self.B_c0], [self.BF[1]])
                yield
                self.act(ebl[r][:], bl_ps, AF.Exp, [self.BF[0]], [Bn["ebl"][r]])
                self.act(eb8[r][:], b8_ps, AF.Exp, [self.BF[0]], [Bn["eb8"][r]])
                if main:
                    self.act(ebm[r][:], F[1][:, c128], AF.Exp, [self.BF[1]], [Bn["ebm"][r]])
                    self.act(enb[r][:], bm_ps, AF.Exp, [self.BF[1]], [Bn["enb"][r]], scale=-1.0)
                yield
                for c in range(4):
                    self.stt(khc[r][:, c, :], kn[:, hs], self.wall[:, 132 + c:133 + c], ebl[r][:], ALU.mult, ALU.mult,
                             [Bkin[sl], Bn["ebl"][r], self.B_c0], [Bkhc[r]])
                if main:
                    self.tt(ktl[r][:], kn[:, hs], enb[r][:], ALU.mult, [Bkin[sl], Bn["enb"][r]], [Bn["ktl"][r]])
                    self.tt(qtl[r][:], rq[sl][:, h, :], ebm[r][:], ALU.mult, [Brq[sl], Bn["ebm"][r]], [Bn["qtl"][r]])
                    for c in range(4):
                        cc = slice(32 * c, 32 * c + 32)
                        self.tt(qtlz[r][:, c, cc], rq[sl][:, h, cc], ebm[r][:, cc], ALU.mult, [Brq[sl], Bn["ebm"][r]], [Bqz[r]])
                yield
                pds = [F[5][:, (s2 * 2 + (c % 2)) * 128:(s2 * 2 + (c % 2) + 1) * 128] for c in range(4)]
                if main:
                    pt = self.T[0][:, c128]
                    self.tr(pt, ktl[r][:], [Bn["ktl"][r]], [self.BT[0][0]])
                    yield
                    self.cp("act", ktlT[r][:], pt, [self.BT[0][0]], [Bn["ktlT"][r]])
                    yield
                    self.mm(F[3][:, c128], ktlT[r][:], qtl[r][:], True, True, [Bn["ktlT"][r], Bn["qtl"][r]], [self.BF[3]])
                    yield
                    self.tt(at[r][:], F[3][:, c128], self.bd[:], ALU.mult, [self.BF[3], self.B_c0], [Bn["at"][r]])
                    yield
                pob = 4 if s2 == 0 else 2
                po = F[pob][:, 0:128]
                bpo = self.BF[pob]
                if main:
                    self.mm(po, at[r][:], ri[sl][:, hs], True, False, [Bn["at"][r], Bri[sl]], [bpo])
                for c in range(4):
                    self.mm(pds[c], khc[r][:, c, :], ri[sl][:, hs], True, True, [Bkhc[r], Bri[sl]], [self.BF[5]])
                    if main:
                        self.act(Sb[r][:, c, :], St[:, h, :], AF.Copy, [BSt[h], Bn["eb8"][r]], [BSb[r][c]], scale=eb8[r][:, c:c + 1])
                    yield
                    if main:
                        self.mm(po, qtlz[r][:, c, :], Sb[r][:, c, :], False, c == 3, [Bqz[r], BSb[r][c]], [bpo])
                    self.stt(St[:, h, :], St[:, h, :], eb8[r][:, 4 + c:5 + c], pds[c], ALU.mult, ALU.add, [BSt[h], Bn["eb8"][r], self.BF[5]], [BSt[h]])
                    yield
                if main:
                    self.act(jk[r][:], po, AF.Square, [bpo], [Bn["sq"][r]], accum=sq[r][:, 0:1])
                    yield
                    self.tt(ot[r][:], po, self.rngb[:], ALU.mult, [bpo, self.B_c0], [Bn["ot"][r]])
                    self.rstd(sq[r][:, 2:3], sq[r][:, 0:1], 128.0, [Bn["sq"][r]], [Bn["sq"][r], Bn["sq"][r]], sq[r][:, 1:2])
                    yield
                    self.stt(of_[r][:], ot[r][:], sq[r][:, 2:3], rog[sl][:, hs], ALU.mult, ALU.mult, [Bn["ot"][r], Bn["sq"][r], Brog[sl]], [Bn["of"][r]])
                    yield
                    pt = self.T[1][:, c128]
                    self.tr(pt, of_[r][:], [Bn["of"][r]], [self.BT[1][0]])
                    yield
                    self.cp("act", self.oT[:, h, mt * 128:(mt + 1) * 128], pt, [self.BT[1][0]], [self.B_oT[mt]])

            G = 2
            for h0 in range(0, 8, G):
                gens = []
                for h in range(h0, h0 + G):
                    gens.append(hbody(h, hi[0] % R))
                    hi[0] += 1
                while gens:
                    for g_ in list(gens):
                        try:
                            next(g_)
                        except StopIteration:
                            gens.remove(g_)
            if tile == 15:
                for h in range(8):
                    self.ts(St[:, h, :], St[:, h, :], self.pm[:, 0:1], None, ALU.mult, None, [BSt[h], self.B_c0], [BSt[h]])
        if self.debug:
            for h in range(8):
                self.dma("sp", D_["OT"][h], self.oT[:, h, :], self.B_oT, [Buf("x")], self.B_oT[0])
        sb.pop()
        self.release(Brf + Bri + Brq + Brog)

    def stage4a(self):
        S, sb, I = self.S, self.sb, self.I
        D_ = self.dram
        sb.push()
        wa = [sb.alloc("wa", [128, 8, 128], BF16) for _ in range(2)]
        wr_ = [sb.alloc("wr_", [128, 8, 128], BF16) for _ in range(2)]
        sga = [sb.alloc("sga", [128, NT], BF16) for _ in range(2)]
        sgr = [sb.alloc("sgr", [128, NT], BF16) for _ in range(2)]
        mT = [sb.alloc("mT", [128, NT], BF16) for _ in range(2)]
        t1 = [sb.alloc("t1", [128, 512], F32) for _ in range(2)]
        t2 = [sb.alloc("t2", [128, 512], F32) for _ in range(2)]
        Bwa = [self.dbuf("wa", "pool") for _ in range(2)]
        Bwr = [self.dbuf("wr", "pool") for _ in range(2)]
        Bsa = [self.dbuf("sga") for _ in range(2)]
        Bsr = [self.dbuf("sgr") for _ in range(2)]
        BmT = [self.dbuf("mT") for _ in range(2)]
        Bt1 = [Buf("t1_%d" % i) for i in range(2)]
        Bt2 = [Buf("t2_%d" % i) for i in range(2)]
        wua = I["w_up_a"].rearrange("(k p) n -> p k n", p=128)
        wur = I["w_up_r"].rearrange("(k p) n -> p k n", p=128)
        ti = [0]
        for ch in range(16):
            sl = ch % 2
            self.dma("pool", wa[sl][:], wua[:, :, ch * 128:(ch + 1) * 128], [], [Bwa[sl]], Bwa[sl])
            self.dma("pool", wr_[sl][:], wur[:, :, ch * 128:(ch + 1) * 128], [], [Bwr[sl]], Bwr[sl])
            self.dma("sp", sga[sl][:], D_["GAT"][ch], self.scr_reads("GAT", [ch]), [Bsa[sl]], Bsa[sl])
            self.dma("sp", sgr[sl][:], D_["GRT"][ch], self.scr_reads("GRT", [ch]), [Bsr[sl]], Bsr[sl])
            for g in range(4):
                gs = slice(g * 512, (g + 1) * 512)
                fa, fr = (g % 2) * 2, (g % 2) * 2 + 1
                for wc in range(8):
                    self.mm(self.F[fa][:, :], wa[sl][:, wc, :], self.attT[:, wc, gs], wc == 0, wc == 7, [Bwa[sl], self.B_attT[wc]], [self.BF[fa]])
                for wc in range(8):
                    self.mm(self.F[fr][:, :], wr_[sl][:, wc, :], self.oT[:, wc, gs], wc == 0, wc == 7,
                            [Bwr[sl]] + self.B_oT[g * 4:(g + 1) * 4], [self.BF[fr]])
                i = ti[0] % 2
                ti[0] += 1
                self.tt(t1[i][:], self.F[fa][:, :], sga[sl][:, gs], ALU.mult, [self.BF[fa], Bsa[sl]], [Bt1[i]])
                self.tt(t2[i][:], self.F[fr][:, :], sgr[sl][:, gs], ALU.mult, [self.BF[fr], Bsr[sl]], [Bt2[i]], eng="dve")
                self.tt(mT[sl][:, gs], t1[i][:], t2[i][:], ALU.add, [Bt1[i], Bt2[i]], [BmT[sl]], eng="dve")
            tok = Buf("w")
            self.scr_w.setdefault(("MT", ch), []).append(tok)
            self.dma("sp", D_["MT"][ch], mT[sl][:], [BmT[sl]], [tok], BmT[sl])
        sb.pop()
        self.release(Bwa + Bwr + Bsa + Bsr + BmT)

    def stage4b(self):
        S, sb, I = self.S, self.sb, self.I
        D_ = self.dram
        sb.push()
        wo = sb.alloc("wo", [128, KC, D], BF16)
        Bwo = [self.dbuf("wo", "pool") for _ in range(4)]
        wsrc = I["w_out"].rearrange("(k p) n -> p k n", p=128)
        for i in range(4):
            self.dma("pool", wo[:, i * 4:(i + 1) * 4, :], wsrc[:, i * 4:(i + 1) * 4, :], [], [Bwo[i]], Bwo[i])
        mTg = [sb.alloc("mTg", [128, KC, 512], BF16)] * 2
        Bmg = [self.dbuf("mTg")] * 2
        xs = [sb.alloc("xs", [128, D], F32) for _ in range(2)]
        Bx = [self.dbuf("xs") for _ in range(2)]
        x1 = [sb.alloc("x1", [128, D], F32) for _ in range(2)]
        Bx1 = [self.dbuf("x1") for _ in range(2)]
        h2 = [sb.alloc("h2", [128, KC, 128], BF16) for _ in range(2)]
        Bh2 = [self.dbuf("h2") for _ in range(2)]
        rings = [self.norm_ring(sb)]
        rings.append(self.norm_ring(sb, junk=rings[0][0]))
        tq = [sb.alloc("tq", [128, 512], F32) for _ in range(2)]
        Btq = [Buf("tq%d" % i) for i in range(2)]
        rt = sb.alloc("rt", [128, 160], F32)
        Brt = Buf("rt")
        r2 = sb.alloc("r2", [128, 112], F32)
        Br2 = Buf("r2")
        selb = sb.alloc("selb", [128, 32], BF16)
        Bselb = Buf("selb")
        h2k = [sb.alloc("h2k", [128, D], BF16) for _ in range(2)]
        Bh2k = [Buf("h2k%d" % i) for i in range(2)]
        mtsrc = D_["MT"].rearrange("k p t -> p k t")
        qi = [0]
        for g in range(4):
            sg = g % 2
            self.dma("sp", mTg[sg][:], mtsrc[:, :, g * 512:(g + 1) * 512], self.scr_reads("MT", range(16)), [Bmg[sg]], Bmg[sg])
            for tt_ in range(4):
                tile = g * 4 + tt_
                sl = tile % 2
                self.dma("sp", xs[sl][:], I["xm"][tile * 128:(tile + 1) * 128, :], [], [Bx[sl]], Bx[sl])
                for cg in range(4):
                    cs_ = slice(cg * 512, (cg + 1) * 512)
                    fb = cg % 4
                    for kc in range(KC):
                        self.mm(self.F[fb][:, :], mTg[sg][:, kc, tt_ * 128:(tt_ + 1) * 128], wo[:, kc, cs_], kc == 0, kc == KC - 1,
                                [Bmg[sg], Bwo[kc // 4]], [self.BF[fb]])
                    i = qi[0] % 2
                    qi[0] += 1
                    self.tt(tq[i][:], self.F[fb][:, :], self.gt1b[:, cs_], ALU.mult, [self.BF[fb], self.B_c0], [Btq[i]])
                    self.tt(x1[sl][:, cs_], tq[i][:], xs[sl][:, cs_], ALU.add, [Btq[i], Bx[sl]], [Bx1[sl]], eng="dve")
                tok = Buf("w")
                self.scr_w.setdefault(("X1", tile), []).append(tok)
                self.dma("sp", D_["X1"][tile * 128:(tile + 1) * 128, :], x1[sl][:], [Bx1[sl]], [tok], Bx1[sl])
                for _ in self.norm_to_hT(x1[sl][:], Bx1[sl], h2[sl][:, :, :], Bh2[sl], self.a2, self.modc[:, 3, :], rings[sl]):
                    pass
                tok = Buf("w")
                self.scr_w.setdefault(("H2T", tile), []).append(tok)
                self.dma("sp", D_["H2T"][:, :, tile * 128:(tile + 1) * 128].rearrange("k p t -> p k t"), h2[sl][:], [Bh2[sl]], [tok], Bh2[sl])
                pr = self.F[5][:, 0:36]
                bpr = self.BF[5]
                for kc in range(KC):
                    self.mm(pr, h2[sl][:, kc, :], self.wr[:, kc, :], kc == 0, kc == KC - 1, [Bh2[sl], self.B_c0], [bpr])
                lg = rt[:, 0:36]
                self.tt(lg, pr, self.br[:], ALU.add, [bpr, self.B_c0], [Brt])
                mx = rt[:, 36:37]
                S.op("dve", lambda e: e.tensor_reduce(out=rt[:, 36:37], in_=rt[:, 0:4], axis=AX.X, op=ALU.max), [Brt], [Brt])
                self.ts(rt[:, 37:38], mx, -1.0, None, ALU.mult, None, [Brt], [Brt])
                self.act(rt[:, 40:44], rt[:, 0:4], AF.Exp, [Brt], [Brt], bias=rt[:, 37:38], accum=rt[:, 38:39])
                self.ts(rt[:, 44:48], rt[:, 0:4], mx, None, ALU.is_ge, None, [Brt], [Brt])
                self.ts(rt[:, 44:48], rt[:, 44:48], -1.0, -NEGBIG, ALU.add, ALU.mult, [Brt], [Brt])
                for gi in range(4):
                    self.ts(rt[:, 48 + gi * 8:56 + gi * 8], rt[:, 4 + gi * 8:12 + gi * 8], rt[:, 44 + gi:45 + gi], None, ALU.add, None, [Brt], [Brt])
                S.op("dve", lambda e: e.max(out=rt[:, 80:88], in_=rt[:, 48:80]), [Brt], [Brt])
                self.ts(rt[:, 88:120], rt[:, 48:80], rt[:, 81:82], None, ALU.is_ge, None, [Brt], [Brt])
                self.ts(rt[:, 39:40], rt[:, 80:81], -1.0, None, ALU.mult, None, [Brt], [Brt])
                self.act(rt[:, 120:152], rt[:, 48:80], AF.Exp, [Brt], [Brt], bias=rt[:, 39:40])
                self.tt(rt[:, 120:152], rt[:, 120:152], rt[:, 88:120], ALU.mult, [Brt], [Brt])
                S.op("dve", lambda e: e.reduce_sum(out=rt[:, 152:153], in_=rt[:, 120:152], axis=AX.X), [Brt], [Brt])
                self.tt(rt[:, 152:153], rt[:, 152:153], rt[:, 38:39], ALU.mult, [Brt], [Brt])
                S.op("dve", lambda e: e.reciprocal(out=rt[:, 153:154], in_=rt[:, 152:153]), [Brt], [Brt])
                self.ts(self.wfull[:, tile, :], rt[:, 120:152], rt[:, 153:154], None, ALU.mult, None, [Brt], [self.B_wfull[tile]])
                self.cp("dve", selb[:], rt[:, 88:120], [Brt], [Bselb])
                pc = self.F[4]
                self.mm(pc[:, 0:32], self.slt[:, :], selb[:], True, True, [Bselb, self.B_c0], [self.BF[4]])
                self.mm(pc[:, 32:64], self.ones[:, :], selb[:], True, True, [Bselb, self.B_c0], [self.BF[4]])
                self.tt(r2[:, 0:32], pc[:, 0:32], self.tot[:], ALU.add, [self.BF[4], self.B_tot], [Br2])
                self.tt(self.tot[:], pc[:, 32:64], self.tot[:], ALU.add, [self.BF[4], self.B_tot], [self.B_tot])
                self.ts(r2[:, 0:32], r2[:, 0:32], float(CAP - 1), None, ALU.min, None, [Br2], [Br2])
                self.tt(r2[:, 0:32], r2[:, 0:32], self.ebase[:], ALU.add, [Br2, self.B_c0], [Br2])
                self.stt(r2[:, 32:64], r2[:, 0:32], 1.0e6, rt[:, 88:120], ALU.add, ALU.mult, [Br2, Brt], [Br2])
                S.op("dve", lambda e: e.max(out=r2[:, 64:72], in_=r2[:, 32:64]), [Br2], [Br2])
                self.ts(r2[:, 72:74], r2[:, 64:66], -1.0e6, None, ALU.add, None, [Br2], [Br2])
                self.cp("dve", self.desti[:, tile, :], r2[:, 72:74], [Br2], [self.B_dest[tile]])
                for k_ in range(2):
                    self.ts(r2[:, 80:112], r2[:, 32:64], r2[:, 64 + k_:65 + k_], None, ALU.is_equal, None, [Br2], [Br2])
                    self.tt(r2[:, 80:112], r2[:, 80:112], self.wfull[:, tile, :], ALU.mult, [Br2, self.B_wfull[tile]], [Br2])
                    S.op("dve", lambda e, k_=k_, tile=tile: e.reduce_sum(out=self.wk[:, tile, k_:k_ + 1], in_=r2[:, 80:112], axis=AX.X),
                         [Br2], [self.B_dest[tile]])
                xn_t, bxn_t = h2k[sl], Bh2k[sl]
                self.tt(xn_t[:], rings[sl][4][:], self.a2b[:], ALU.mult, [rings[sl][7], self.B_c0], [bxn_t])
                self.tt(xn_t[:], xn_t[:], self.sh2bb[:], ALU.add, [bxn_t, self.B_c0], [bxn_t])
                for k_ in range(2):
                    self.cp("dve", self.six[sl][:, k_:k_ + 1], self.desti[:, tile, k_:k_ + 1], [self.B_dest[tile]], [self.B_six[sl][k_]])
                    S.op("pool", lambda e, k_=k_, ix=self.six[sl], xn_t=xn_t: e.indirect_dma_start(
                        out=D_["XD"], out_offset=bass.IndirectOffsetOnAxis(ap=ix[:, k_:k_ + 1], axis=0),
                        in_=xn_t[:], in_offset=None),
                        [bxn_t, self.B_six[sl][k_]], [Buf("sc")], dbuf=bxn_t)
        if self.debug:
            self.dma("sp", D_["WF"], self.wfull[:].rearrange("p a b -> p (a b)"), self.B_wfull, [Buf("x")], self.B_wfull[0])
        sb.pop()
        self.release(Bwo + Bmg[:1] + Bx + Bx1 + Bh2)

    def stage5(self):
        S, sb, I = self.S, self.sb, self.I
        D_ = self.dram
        sb.push()
        w1 = [sb.alloc("w1", [128, KC, 512], BF16) for _ in range(2)]
        w3 = [sb.alloc("w3", [128, KC, 512], BF16) for _ in range(2)]
        w2 = sb.alloc("w2", [128, 4, D], BF16)
        Bw1 = [self.dbuf("w1", "pool") for _ in range(2)]
        Bw3 = [self.dbuf("w3", "pool") for _ in range(2)]
        Bw2 = self.dbuf("w2", "pool")
        nrt = CAP // 128
        xr = [sb.alloc("xr", [128, D], BF16) for _ in range(nrt)]
        Bxr = [self.dbuf("xr") for _ in range(nrt)]
        XT = [sb.alloc("XT", [128, KC, CAP], BF16) for _ in range(2)]
        BXT = [[Buf("XT%d_%d" % (i, j)) for j in range(nrt)] for i in range(2)]
        s1 = [sb.alloc("s1", [128, CAP], F32) for _ in range(2)]
        Bs1 = [Buf("s1_%d" % i) for i in range(2)]
        hid = [sb.alloc("hid", [128, 4, CAP], BF16) for _ in range(2)]
        Bhid = [[Buf("hid%d_%d" % (i, f)) for f in range(4)] for i in range(2)]
        ys = [sb.alloc("ys", [128, D], F32)] * 2
        Bys = [self.dbuf("ys")] * 2
        si = [0]
        yi = [0]

        def loads(ex):
            for r_ in range(nrt):
                row0 = ex * CAP + r_ * 128
                self.dma("sp", xr[r_][:], D_["XD"][row0:row0 + 128, :], [], [Bxr[r_]], Bxr[r_])

        def trans(ex):
            sl = ex % 2
            for r_ in range(nrt):
                for tb in range(2):
                    for ts_ in range(8):
                        kc = tb * 8 + ts_
                        self.tr(self.T[tb][:, ts_ * 128:(ts_ + 1) * 128], xr[r_][:, kc * 128:(kc + 1) * 128], [Bxr[r_]], [self.BT[tb][0]])
                    src = self.T[tb][:, :].rearrange("p (k t) -> p k t", t=128)
                    dst = XT[sl][:, tb * 8:(tb + 1) * 8, r_ * 128:(r_ + 1) * 128]
                    self.cp("act" if tb == 0 else "dve", dst, src, [self.BT[tb][0]], [BXT[sl][r_]])

        loads(0)
        trans(0)
        for ex in range(32):
            sl = ex % 2
            self.dma("pool", w1[sl][:], I["w1"][ex].rearrange("(k p) n -> p k n", p=128), [], [Bw1[sl]], Bw1[sl])
            self.dma("pool", w3[sl][:], I["w3"][ex].rearrange("(k p) n -> p k n", p=128), [], [Bw3[sl]], Bw3[sl])
            self.dma("pool", w2[:], I["w2"][ex].rearrange("(k p) n -> p k n", p=128), [], [Bw2], Bw2)
            if ex + 1 < 32:
                loads(ex + 1)
            for fc in range(4):
                fs = slice(fc * 128, (fc + 1) * 128)
                f1, f3 = (fc % 2) * 2, (fc % 2) * 2 + 1
                for kc in range(KC):
                    self.mm(self.F[f1][:, 0:CAP], w1[sl][:, kc, fs], XT[sl][:, kc, :], kc == 0, kc == KC - 1, [Bw1[sl]] + BXT[sl], [self.BF[f1]])
                for kc in range(KC):
                    self.mm(self.F[f3][:, 0:CAP], w3[sl][:, kc, fs], XT[sl][:, kc, :], kc == 0, kc == KC - 1, [Bw3[sl]] + BXT[sl], [self.BF[f3]])
                i = si[0] % 2
                si[0] += 1
                self.act(s1[i][:], self.F[f1][:, 0:CAP], AF.Silu, [self.BF[f1]], [Bs1[i]])
                self.tt(hid[sl][:, fc, :], s1[i][:], self.F[f3][:, 0:CAP], ALU.mult, [Bs1[i], self.BF[f3]], [Bhid[sl][fc]])
            if ex + 1 < 32:
                trans(ex + 1)
            for r_ in range(nrt):
                y_ = ys[yi[0] % 2]
                by = Bys[yi[0] % 2]
                yi[0] += 1
                for cg in range(4):
                    cs_ = slice(cg * 512, (cg + 1) * 512)
                    fb = 4 + (cg % 2)
                    for fc in range(4):
                        self.mm(self.F[fb][:, :], hid[sl][:, fc, r_ * 128:(r_ + 1) * 128], w2[:, fc, cs_], fc == 0, fc == 3,
                                [Bhid[sl][fc], Bw2], [self.BF[fb]])
                    if fb == 4:
                        self.cp("act", y_[:, cs_], self.F[fb][:, :], [self.BF[fb]], [by])
                    else:
                        self.cp("dve", y_[:, cs_], self.F[fb][:, :], [self.BF[fb]], [by])
                row0 = ex * CAP + r_ * 128
                for hf in range(2):
                    self.dma("sp", D_["YD%d" % hf][row0:row0 + 128, :], y_[:, hf * 1024:(hf + 1) * 1024], [by], [Buf("y")], by)
        sb.pop()
        self.release(Bw1 + Bw3 + [Bw2] + Bxr + Bys[:1])
        self.bar()
        sb.push()
        xs = [sb.alloc("xs", [128, D], F32) for _ in range(2)]
        Bx = [self.dbuf("xs") for _ in range(2)]
        yg = [[[sb.alloc("yg", [128, 1024], F32) for _ in range(2)] for _ in range(2)] for _ in range(2)]
        Byg = [[[Buf("yg") for _ in range(2)] for _ in range(2)] for _ in range(2)]
        jk = sb.alloc("jk5", [128, D], BF16)
        st5 = sb.alloc("st5", [128, 4], F32)
        Bst = Buf("st5")
        for tile in range(16):
            sl = tile % 2
            self.dma("sp", xs[sl][:], D_["X1"][tile * 128:(tile + 1) * 128, :], self.scr_reads("X1", [tile]), [Bx[sl]], Bx[sl])
            for k_ in range(2):
                self.cp("dve", self.gix[sl][:, k_:k_ + 1], self.desti[:, tile, k_:k_ + 1], [self.B_dest[tile]], [self.B_gix[sl][k_]])
                for hf in range(2):
                    S.op("pool", lambda e, k_=k_, ix=self.gix[sl], dst=yg[sl][k_][hf], hf=hf: e.indirect_dma_start(
                        out=dst[:], out_offset=None, in_=D_["YD%d" % hf],
                        in_offset=bass.IndirectOffsetOnAxis(ap=ix[:, k_:k_ + 1], axis=0)),
                        [self.B_gix[sl][k_]], [Byg[sl][k_][hf]], dbuf=Byg[sl][k_][hf])
            for hf in range(2):
                a, b = yg[sl][0][hf], yg[sl][1][hf]
                ba, bb = Byg[sl][0][hf], Byg[sl][1][hf]
                hs_ = slice(hf * 1024, (hf + 1) * 1024)
                self.ts(a[:], a[:], self.wk[:, tile, 0:1], None, ALU.mult, None, [ba, self.B_dest[tile]], [ba])
                self.stt(a[:], b[:], self.wk[:, tile, 1:2], a[:], ALU.mult, ALU.add, [bb, ba, self.B_dest[tile]], [ba])
                self.tt(a[:], a[:], self.gt2b[:, hs_], ALU.mult, [ba, self.B_c0], [ba])
                self.tt(xs[sl][:, hs_], xs[sl][:, hs_], a[:], ALU.add, [Bx[sl], ba], [Bx[sl]])
            self.act(jk[:], xs[sl][:], AF.Square, [Bx[sl]], [Bst], accum=st5[:, 0:1])
            self.rstd(st5[:, 2:3], st5[:, 0:1], float(D), [Bst], [Bst, Bst], st5[:, 1:2])
            self.stt(xs[sl][:], xs[sl][:], st5[:, 2:3], self.fgb[:], ALU.mult, ALU.mult, [Bx[sl], Bst, self.B_c0], [Bx[sl]])
            self.dma("sp", self.out[tile * 128:(tile + 1) * 128, :], xs[sl][:], [Bx[sl]], [Buf("o")], Bx[sl])
        sb.pop()
        self.release(Bx)


def host_consts():
    bf = ml_dtypes.bfloat16
    c = {}
    c["k_ident"] = np.eye(128, dtype=np.float32)
    cm = np.zeros((128, 512), np.float32)
    for j in range(2):
        kp = np.arange(128)[:, None] + j * 128
        q = np.arange(256)[None, :]
        cm[:, j * 256:(j + 1) * 256] = np.where(kp <= q, 0.0, NEG)
    c["k_cm"] = cm
    cp = np.zeros((128, 16, 16), np.float32)
    for qt in range(16):
        cp[:, qt, 8 + qt // 2:] = NEGBIG
    c["k_cpen"] = cp.reshape(128, 256)
    sel = np.zeros((16, 16, 128), np.float32)
    for i in range(16):
        sel[i, i, :] = 1.0
    c["k_sel"] = sel.reshape(16, 2048)
    s = np.arange(128)[:, None]
    t = np.arange(128)[None, :]
    same = (s // 32) == (t // 32)
    mid = (t // 32) * 32 + 15
    wm = np.where(same & (s > mid) & (s <= t), 1.0, 0.0) - np.where(same & (s > t) & (s <= mid), 1.0, 0.0)
    wmid = np.zeros((128, 4), np.float32)
    wtot = np.zeros((128, 4), np.float32)
    for cc in range(4):
        sl = np.arange(128)
        wmid[:, cc] = ((sl // 32) == cc) & (sl <= cc * 32 + 15)
        wtot[:, cc] = (sl // 32) == cc
    c["k_wall"] = np.concatenate([wm, wmid, wtot], axis=1).astype(np.float32)
    c["k_wl"] = np.where(same & (s > t), 1.0, 0.0).astype(np.float32)
    c["k_bd"] = np.where(same & (s <= t), 1.0, 0.0).astype(np.float32)
    c["k_slt"] = np.where(s < t, 1.0, 0.0).astype(np.float32)
    c["k_ones"] = np.ones((128, 128), np.float32)
    c["k_ebase"] = np.tile((np.arange(32) * CAP).astype(np.float32)[None, :], (128, 1))
    return c


_NC_CACHE = {}


def get_nc(debug=False, stages="012345"):
    key = (debug, stages)
    if key not in _NC_CACHE:
        nc = bass.Bass("TRN2", target_bir_lowering=False)
        kb = K(nc, debug=debug, stages=stages)
        kb.build()
        _NC_CACHE[key] = (nc, kb)
    return _NC_CACHE[key]


def make_in_maps(inputs, cores=range(8)):
    f = lambda a: np.ascontiguousarray(np.asarray(a, dtype=np.float32))
    x = f(inputs["x"])
    c = f(inputs["c"])
    shared = {
        "norm1_g": f(inputs["norm1_g"])[0], "norm2_g": f(inputs["norm2_g"])[0], "final_g": f(inputs["final_g"]),
        "w_ada": f(inputs["w_ada"])[0], "b_ada": f(inputs["b_ada"])[0], "w_in": f(inputs["w_in"])[0],
        "r_lower": f(inputs["r_lower"]), "r_norm_g": f(inputs["r_norm_g"])[0],
        "w_up_a": f(inputs["w_up_a"])[0], "w_up_r": f(inputs["w_up_r"])[0], "w_out": f(inputs["w_out"])[0],
        "w_rg": f(inputs["w_rg"])[0], "b_rg": f(inputs["b_rg"])[0], "w_re": f(inputs["w_re"])[0], "b_re": f(inputs["b_re"])[0],
        "w1": f(inputs["w1"])[0], "w3": f(inputs["w3"])[0], "w2": f(inputs["w2"])[0],
    }
    shared.update(host_consts())
    maps = []
    for core in cores:
        b, half = core // 2, core % 2
        m = dict(shared)
        m["xm"] = np.ascontiguousarray(x[b, half * NT:(half + 1) * NT])
        m["xp"] = np.ascontiguousarray(x[b, 0:NT])
        m["c"] = np.ascontiguousarray(c[b])
        m["pm"] = np.full((128, 1), float(half), np.float32)
        maps.append(m)
    return maps


def kernel(**inputs):
    nc, _ = get_nc()
    maps = make_in_maps(inputs)
    res = run_bass_kernel_spmd(nc, maps, core_ids=list(range(8)))
    out = np.zeros((4, 4096, D), np.float32)
    for core in range(8):
        b, half = core // 2, core % 2
        out[b, half * NT:(half + 1) * NT] = np.asarray(res.results[core]["out"], dtype=np.float32)
    return out
```

```python
import contextlib
import numpy as np
import ml_dtypes
import concourse.bass as bass
import concourse.mybir as mybir
from concourse.bass_utils import run_bass_kernel_spmd

F32 = mybir.dt.float32
BF16 = mybir.dt.bfloat16
I32 = mybir.dt.int32
CAP = 512
NR = 32 * CAP
ALU = mybir.AluOpType
AF = mybir.ActivationFunctionType
AX = mybir.AxisListType

D = 2048
NT = 2048
KC = 16
EPS = 1e-6
NEG = -30000.0
NEGBIG = -1.0e30
ENGS = ("pe", "act", "dve", "pool", "sp")
SAME_ENGINE_SYNC = {"pe": False, "act": True, "dve": True, "pool": True, "sp": False}


class Buf:
    __slots__ = ("name", "last_w", "readers", "dsem", "dcount", "const", "last_dma", "excl")

    def __init__(self, name, const=False, excl=False):
        self.excl = excl
        self.name = name
        self.last_w = None
        self.readers = []
        self.dsem = None
        self.dcount = 0
        self.const = const
        self.last_dma = None


class Op:
    __slots__ = ("eng", "fn", "deps", "sig", "sem", "val", "dbuf")

    def __init__(self, eng, fn, dbuf=None):
        self.eng = eng
        self.fn = fn
        self.deps = []
        self.sig = dbuf is not None
        self.sem = None
        self.val = 0
        self.dbuf = dbuf


class Sched:
    def __init__(self, nc):
        self.nc = nc
        self.ops = {e: [] for e in ENGS}
        self.dma_bufs = []
        self.epoch = None
        self.all_ops = []

    def barrier(self, fn):
        o = Op("dve", fn, None)
        deps = {}
        for e in ENGS:
            if self.ops[e]:
                d = self.ops[e][-1]
                deps[id(d)] = d
        for b in self.dma_bufs:
            if b.last_dma is not None:
                deps[id(b.last_dma)] = b.last_dma
        o.deps = list(deps.values())
        self.ops["dve"].append(o)
        self.all_ops.append(o)
        self.epoch = o
        return o

    def op(self, eng, fn, reads=(), writes=(), dbuf=None, extra=()):
        o = Op(eng, fn, dbuf)
        deps = {}
        for d in extra:
            if d is not None:
                deps[id(d)] = d
        if self.epoch is not None:
            deps[id(self.epoch)] = self.epoch
        xr = [b for b in reads if b.excl]
        if xr:
            reads = [b for b in reads if not b.excl]
            writes = list(writes) + [b for b in xr if b not in writes]
        for b in reads:
            if b.last_w is not None:
                deps[id(b.last_w)] = b.last_w
        for b in writes:
            if b.last_w is not None:
                deps[id(b.last_w)] = b.last_w
            for r in b.readers:
                deps[id(r)] = r
        o.deps = list(deps.values())
        for b in writes:
            b.last_w = o
            b.readers = []
        for b in reads:
            if not b.const and b.last_w is not o:
                b.readers.append(o)
        if dbuf is not None:
            dbuf.last_dma = o
            if dbuf.dsem is None:
                dbuf.dsem = True
                self.dma_bufs.append(dbuf)
        self.ops[eng].append(o)
        self.all_ops.append(o)
        return o

    def emit(self, stack):
        nc = self.nc
        for e in ENGS:
            for o in self.ops[e]:
                keep = []
                for d in o.deps:
                    if d.dbuf is None and d.eng == o.eng and not SAME_ENGINE_SYNC[o.eng]:
                        continue
                    keep.append(d)
                    d.sig = True
                o.deps = keep
        esem = {}
        for e in ENGS:
            if any(o.sig and o.dbuf is None for o in self.ops[e]):
                esem[e] = stack.enter_context(nc.semaphore("s_" + e))
        for i, b in enumerate(self.dma_bufs):
            b.dsem = stack.enter_context(nc.semaphore("d%d" % i))
            b.dcount = 0
        cnt = {e: 0 for e in ENGS}
        for o in self.all_ops:
            if o.dbuf is not None:
                o.dbuf.dcount += 16
                o.sem = o.dbuf.dsem
                o.val = o.dbuf.dcount
            elif o.sig:
                cnt[o.eng] += 1
                o.sem = esem[o.eng]
                o.val = cnt[o.eng]
        block = stack.enter_context(nc.Block())
        handles = {"pe": block.tensor, "act": block.scalar, "dve": block.vector,
                   "pool": block.gpsimd, "sp": block.sync}
        for e in ENGS:
            ops = self.ops[e]
            if not ops:
                continue

            def body(eng, ops=ops):
                waited = {}
                for o in ops:
                    need = {}
                    for d in o.deps:
                        k = id(d.sem)
                        if d.val > need.get(k, (0, None))[0]:
                            need[k] = (d.val, d.sem)
                    for k, (v, sem) in need.items():
                        if waited.get(k, 0) >= v:
                            continue
                        waited[k] = v
                        eng.wait_ge(sem, v)
                    ins = o.fn(eng)
                    if o.sig:
                        ins.then_inc(o.sem, 16 if o.dbuf is not None else 1)

            handles[e](body)


class SbAlloc:
    def __init__(self, nc, base=17408, limit=229000):
        self.nc = nc
        self.top = base
        self.limit = limit
        self.n = 0
        self.marks = []

    def push(self):
        self.marks.append(self.top)

    def pop(self):
        self.top = self.marks.pop()

    def alloc(self, name, shape, dtype):
        sz = int(np.prod(shape[1:])) * (4 if dtype in (F32, I32) else 2)
        off = (self.top + 63) // 64 * 64
        assert off + sz <= self.limit, (name, off, sz, self.limit)
        self.top = off + sz
        self.n += 1
        return self.nc.alloc_sbuf_tensor_at("%s_%d" % (name, self.n), list(shape), dtype, offset=off)


class K:
    def __init__(self, nc, debug=False, stages="012345"):
        self.nc = nc
        self.S = Sched(nc)
        self.sb = SbAlloc(nc)
        self.debug = debug
        self.stages = stages
        self.dpool = {}
        self.bkind = {}
        self.dram = {}
        self.sbufs = {}
        self.scr_w = {}

    def din(self, name, shape, dt=F32):
        return self.nc.dram_tensor(name, list(shape), dt, kind="ExternalInput").ap()

    def dscr(self, name, shape, dt):
        kind = "ExternalOutput" if self.debug else "Internal"
        t = self.nc.dram_tensor(name, list(shape), dt, kind=kind).ap()
        self.dram[name] = t
        return t

    def dbuf(self, name, kind="sp"):
        pool = self.dpool.setdefault(kind, [])
        if pool:
            return pool.pop()
        b = Buf(name)
        self.bkind[id(b)] = kind
        return b

    def release(self, bufs):
        for b in bufs:
            self.dpool[self.bkind[id(b)]].append(b)

    def dma(self, q, out, in_, reads, writes, dbuf, **kw):
        k = self.bkind.setdefault(id(dbuf), q)
        assert k == q, (dbuf.name, k, q)
        return self.S.op(q, lambda e: e.dma_start(out=out, in_=in_, **kw), reads, writes, dbuf=dbuf)

    def mm(self, out, lhsT, rhs, start, stop, reads, writes, tp=None):
        if tp is not None:
            return self.S.op("pe", lambda e: e.matmul(out, lhsT=lhsT, rhs=rhs, start=start, stop=stop, tile_position=tp), reads, writes)
        return self.S.op("pe", lambda e: e.matmul(out, lhsT=lhsT, rhs=rhs, start=start, stop=stop), reads, writes)

    def tr(self, out, in_, reads, writes):
        idn = self.identb
        n = in_.shape[0]
        return self.S.op("pe", lambda e: e.transpose(out=out, in_=in_, identity=idn[0:n, 0:n]),
                         list(reads) + [self.B_c0], writes)

    def act(self, out, in_, func, reads, writes, scale=1.0, bias=None, accum=None):
        kw = {}
        if bias is not None:
            kw["bias"] = bias
        if accum is not None:
            kw["accum_out"] = accum
        return self.S.op("act", lambda e: e.activation(out=out, in_=in_, func=func, scale=scale, **kw), reads, writes)

    def ts(self, out, in0, s1, s2, op0, op1, reads, writes, eng="dve"):
        if s2 is None:
            return self.S.op(eng, lambda e: e.tensor_scalar(out=out, in0=in0, scalar1=s1, scalar2=None, op0=op0), reads, writes)
        return self.S.op(eng, lambda e: e.tensor_scalar(out=out, in0=in0, scalar1=s1, scalar2=s2, op0=op0, op1=op1), reads, writes)

    def tt(self, out, in0, in1, op, reads, writes, eng="dve"):
        return self.S.op(eng, lambda e: e.tensor_tensor(out=out, in0=in0, in1=in1, op=op), reads, writes)

    def stt(self, out, in0, scalar, in1, op0, op1, reads, writes, eng="dve"):
        return self.S.op(eng, lambda e: e.scalar_tensor_tensor(out=out, in0=in0, scalar=scalar, in1=in1, op0=op0, op1=op1), reads, writes)

    def cp(self, eng, out, in_, reads, writes):
        if eng == "act":
            return self.S.op("act", lambda e: e.copy(out=out, in_=in_), reads, writes)
        return self.S.op(eng, lambda e: e.tensor_copy(out=out, in_=in_), reads, writes)

    def rstd(self, out, ssq, n, reads, writes, tmp):
        eps = self.epsc
        self.act(tmp, ssq, AF.Sqrt, list(reads) + [self.B_c0], [writes[1]], scale=1.0 / n, bias=eps[:, 0:1])
        self.S.op("dve", lambda e: e.reciprocal(out=out, in_=tmp), [writes[1]], [writes[0]])

    def build(self):
        nc = self.nc
        sb = self.sb
        S = self.S
        I = {}
        I["xm"] = self.din("xm", [NT, D])
        I["xp"] = self.din("xp", [NT, D])
        I["c"] = self.din("c", [D])
        I["pm"] = self.din("pm", [128, 1])
        I["norm1_g"] = self.din("norm1_g", [D])
        I["norm2_g"] = self.din("norm2_g", [D])
        I["final_g"] = self.din("final_g", [D])
        I["w_ada"] = self.din("w_ada", [D, 6 * D])
        I["b_ada"] = self.din("b_ada", [6 * D])
        I["w_in"] = self.din("w_in", [D, 11264])
        I["r_lower"] = self.din("r_lower", [2, 1024])
        I["r_norm_g"] = self.din("r_norm_g", [128])
        I["w_up_a"] = self.din("w_up_a", [1024, D])
        I["w_up_r"] = self.din("w_up_r", [1024, D])
        I["w_out"] = self.din("w_out", [D, D])
        I["w_rg"] = self.din("w_rg", [D, 4])
        I["b_rg"] = self.din("b_rg", [4])
        I["w_re"] = self.din("w_re", [D, 32])
        I["b_re"] = self.din("b_re", [32])
        I["w1"] = self.din("w1", [32, D, 512])
        I["w3"] = self.din("w3", [32, D, 512])
        I["w2"] = self.din("w2", [32, 512, D])
        I["k_ident"] = self.din("k_ident", [128, 128])
        I["k_cm"] = self.din("k_cm", [128, 512])
        I["k_cpen"] = self.din("k_cpen", [128, 256])
        I["k_sel"] = self.din("k_sel", [16, 2048])
        I["k_wall"] = self.din("k_wall", [128, 136])
        I["k_wl"] = self.din("k_wl", [128, 128])
        I["k_bd"] = self.din("k_bd", [128, 128])
        I["k_slt"] = self.din("k_slt", [128, 128])
        I["k_ones"] = self.din("k_ones", [128, 128])
        I["k_ebase"] = self.din("k_ebase", [128, 32])
        self.I = I
        self.out = nc.dram_tensor("out", [NT, D], F32, kind="ExternalOutput").ap()
        self.dscr("MOD", [6 * D], F32)
        self.dscr("QT", [8, 128, NT], BF16)
        self.dscr("KT", [8, 128, 2 * NT], BF16)
        self.dscr("V", [2 * NT, 1024], BF16)
        self.dscr("RQT", [8, 128, NT], BF16)
        self.dscr("RF", [2 * NT, 1024], F32)
        self.dscr("RI", [2 * NT, 1024], BF16)
        self.dscr("ROG", [NT, 1024], BF16)
        self.dscr("GAT", [16, 128, NT], BF16)
        self.dscr("GRT", [16, 128, NT], BF16)
        self.dscr("MT", [16, 128, NT], BF16)
        self.dscr("X1", [NT, D], F32)
        self.dscr("H2T", [16, 128, NT], BF16)
        self.dscr("XD", [NR, D], BF16)
        self.dscr("A2R", [2, D], BF16)
        self.dscr("YD0", [NR, 1024], F32)
        self.dscr("YD1", [NR, 1024], F32)
        if self.debug:
            self.dscr("ATT", [8, 128, NT], BF16)
            self.dscr("OT", [8, 128, NT], BF16)
            self.dscr("WF", [128, 16 * 32], F32)
        self.scrB = {}

        with contextlib.ExitStack() as st:
            self.F = [st.enter_context(nc.psum_tensor("F%d" % i, [128, 512], F32)) for i in range(6)]
            self.T = [st.enter_context(nc.psum_tensor("T%d" % i, [128, 1024], BF16)) for i in range(2)]
            self.BF = [Buf("F%d" % i, excl=True) for i in range(6)]
            self.BFs = [[self.BF[i]] * 4 for i in range(6)]
            _bt = [Buf("T%d" % i, excl=True) for i in range(2)]
            self.BT = [[_bt[i]] * 8 for i in range(2)]
            self.dummy = sb.alloc("dummy", [128, 8], F32)
            self.stage0()
            self.bar()
            if "1" in self.stages:
                self.stage1()
                self.bar()
            sb.push()
            self.attT = sb.alloc("attT", [128, 8, NT], BF16)
            self.oT = sb.alloc("oT", [128, 8, NT], BF16)
            self.B_attT = [Buf("attT%d" % h) for h in range(8)]
            self.B_oT = [Buf("oT%d" % t) for t in range(16)]
            if "2" in self.stages:
                self.stage2()
                self.bar()
            if "3" in self.stages:
                self.stage3()
                self.bar()
            if "4" in self.stages:
                self.stage4a()
                self.bar()
            sb.pop()
            if "4" in self.stages:
                self.stage4b()
                self.bar()
            if "5" in self.stages:
                self.stage5()
            allb = [b for b in self.S.dma_bufs]
            S.op("sp", lambda e: e.nop(), [], allb)
            S.emit(st)
        return nc

    def bar(self):
        d = self.dummy
        self.S.barrier(lambda e: e.memset(d[:], 0.0))

    def sB(self, name, key):
        k = (name, key)
        if k not in self.scrB:
            self.scrB[k] = Buf("%s%s" % (name, key))
        return self.scrB[k]

    def stage0(self):
        S, sb, I = self.S, self.sb, self.I
        cbs = {"sp": [Buf("c%d" % i) for i in range(4)], "pool": [Buf("cp%d" % i) for i in range(2)]}
        cb = cbs["sp"] + cbs["pool"]
        self.B_c0 = Buf("constall")
        ci = [0]

        def cload(q, dst, src, **kw):
            lst = cbs[q]
            b = lst[ci[0] % len(lst)]
            ci[0] += 1
            self.dma(q, dst, src, [], [b], b, **kw)

        def A(name, shape, dt=F32):
            t = sb.alloc(name, shape, dt)
            return t

        self.identb = A("identb", [128, 128], BF16)
        self.ident32 = A("ident32", [128, 128])
        self.rows = A("rows", [48, 128])
        self.cm = A("cm", [128, 512], BF16)
        self.cpen = A("cpen", [128, 256])
        self.selc = A("selc", [16, 2048], BF16)
        self.wall = A("wall", [128, 136])
        self.wl = A("wl", [128, 128])
        self.bd = A("bd", [128, 128])
        self.epsc = A("epsc", [128, 1])
        self.pm = A("pm", [128, 1])
        self.ppen = A("ppen", [128, 1])
        self.g1c = A("g1c", [128, 16])
        self.g2c = A("g2c", [128, 16])
        self.ccol = A("ccol", [128, 16])
        self.cact = A("cact", [128, 16], BF16)
        self.modc = A("modc", [128, 6, 16])
        self.a1 = A("a1", [128, 16])
        self.a2 = A("a2", [128, 16])
        self.gt1b = A("gt1b", [128, D])
        self.gt2b = A("gt2b", [128, D])
        self.fgb = A("fgb", [128, D])
        self.lb = A("lb", [128, 1024])
        self.oml = A("oml", [128, 1024])
        self.rngb = A("rngb", [128, 128])
        self.wr = A("wr", [128, 16, 36], BF16)
        self.br = A("br", [128, 36])
        self.wfull = A("wfull", [128, 16, 32])
        self.slt = A("slt", [128, 128], BF16)
        self.ones = A("ones", [128, 128], BF16)
        self.ebase = A("ebase", [128, 32])
        self.tot = A("tot", [128, 32])
        self.desti = A("desti", [128, 16, 2], I32)
        self.wk = A("wk", [128, 16, 2])
        self.B_tot = Buf("tot")
        self.B_ind = Buf("ind")
        self.six = [A("six", [128, 2], I32) for _ in range(2)]
        self.gix = [A("gix", [128, 2], I32) for _ in range(2)]
        self.B_six = [[Buf("six%d_%d" % (i, k)) for k in range(2)] for i in range(2)]
        self.B_gix = [[Buf("gix%d_%d" % (i, k)) for k in range(2)] for i in range(2)]
        self.B_dest = [Buf("dest%d" % t) for t in range(16)]
        self.B_wfull = [Buf("wfull%d" % t) for t in range(16)]
        cload("pool", self.identb[:], I["k_ident"])
        cload("sp", self.ident32[:], I["k_ident"])
        cload("pool", self.cm[:], I["k_cm"])
        cload("sp", self.cpen[:], I["k_cpen"])
        cload("pool", self.selc[:], I["k_sel"])
        cload("sp", self.wall[:], I["k_wall"])
        cload("sp", self.wl[:], I["k_wl"])
        cload("sp", self.bd[:], I["k_bd"])
        cload("sp", self.pm[:], I["pm"])
        cload("sp", self.rows[0:16, :], I["norm1_g"].rearrange("(k p) -> k p", p=128))
        cload("sp", self.rows[16:32, :], I["norm2_g"].rearrange("(k p) -> k p", p=128))
        cload("sp", self.rows[32:48, :], I["c"].rearrange("(k p) -> k p", p=128))
        cload("sp", self.fgb[:], I["final_g"].partition_broadcast(128))
        cload("sp", self.lb[:], I["r_lower"][0, :].partition_broadcast(128))
        cload("sp", self.oml[:], I["r_lower"][1, :].partition_broadcast(128))
        cload("sp", self.rngb[:], I["r_norm_g"].partition_broadcast(128))
        cload("pool", self.wr[:, :, 0:4], I["w_rg"].rearrange("(k p) n -> p k n", p=128))
        cload("pool", self.wr[:, :, 4:36], I["w_re"].rearrange("(k p) n -> p k n", p=128))
        cload("pool", self.slt[:], I["k_slt"])
        cload("pool", self.ones[:], I["k_ones"])
        cload("sp", self.ebase[:], I["k_ebase"])
        cload("sp", self.br[:, 0:4], I["b_rg"].partition_broadcast(128))
        cload("sp", self.br[:, 4:36], I["b_re"].partition_broadcast(128))
        cs = Buf("cs")
        S.op("dve", lambda e: e.memset(self.epsc[:], EPS), [], [cs])
        S.op("pe", lambda e: e.transpose(out=self.F[2][:, 0:48], in_=self.rows[:, :], identity=self.ident32[0:48, 0:48]), cb, [self.BF[2]])
        self.cp("dve", self.g1c[:], self.F[2][:, 0:16], [self.BF[2]], [cs])
        self.cp("dve", self.g2c[:], self.F[2][:, 16:32], [self.BF[2]], [cs])
        self.cp("dve", self.ccol[:], self.F[2][:, 32:48], [self.BF[2]], [cs])
        S.op("dve", lambda e: e.memset(self.tot[:], 0.0), [], [self.B_tot])
        self.ts(self.ppen[:], self.pm[:], -1.0, -NEGBIG, ALU.add, ALU.mult, cb, [cs])
        self.tt(self.lb[:], self.lb[:], self.oml[:], ALU.subtract, cb, [cs])
        self.act(self.lb[:], self.lb[:], AF.Sigmoid, [cs], [cs])
        self.ts(self.oml[:], self.lb[:], -1.0, 1.0, ALU.mult, ALU.add, [cs], [cs])
        self.act(self.cact[:], self.ccol[:], AF.Silu, [cs], [cs])
        sb.push()
        MOD = self.dram["MOD"]
        wsl = [sb.alloc("wsl0", [128, KC, 512], BF16) for _ in range(3)]
        Bw = [self.dbuf("wsl", "pool") for _ in range(3)]
        bsl = [sb.alloc("bsl", [1, 512], F32) for _ in range(2)]
        Bb = [self.dbuf("bsl") for _ in range(2)]
        msl = [sb.alloc("msl", [1, 512], F32) for _ in range(2)]
        Bm = [self.dbuf("msl") for _ in range(2)]
        wsrc = I["w_ada"].rearrange("(k p) n -> p k n", p=128)
        Bmod = Buf("MOD")
        for j in range(24):
            w = wsl[j % 3]
            bw = Bw[j % 3]
            self.dma("pool", w[:], wsrc[:, :, j * 512:(j + 1) * 512], [], [bw], bw)
            bb = Bb[j % 2]
            self.dma("sp", bsl[j % 2][:], I["b_ada"][j * 512:(j + 1) * 512].rearrange("(o n) -> o n", o=1), [], [bb], bb)
            pf = self.F[j % 2]
            bpf = self.BF[j % 2]
            for kc in range(KC):
                self.mm(pf[0:1, :], self.cact[:, kc:kc + 1], w[:, kc, :], kc == 0, kc == KC - 1, [bw, cs], [bpf])
            bm = Bm[j % 2]
            self.tt(msl[j % 2][:], pf[0:1, :], bsl[j % 2][:], ALU.add, [bpf, bb], [bm])
            self.dma("sp", MOD[j * 512:(j + 1) * 512].rearrange("(o n) -> o n", o=1), msl[j % 2][:], [bm], [Bmod], bm)
        sb.pop()
        self.release(Bw + Bb + Bm)
        bmc = self.dbuf("modc")
        modr = sb.alloc("modr", [96, 128], F32)
        self.dma("sp", modr[:], MOD.rearrange("(r p) -> r p", p=128), [Bmod], [bmc], bmc)
        S.op("pe", lambda e: e.transpose(out=self.F[3][:, 0:96], in_=modr[:, :], identity=self.ident32[0:96, 0:96]), [bmc] + cb, [self.BF[3]])
        self.cp("dve", self.modc[:].rearrange("p s k -> p (s k)"), self.F[3][:, 0:96], [self.BF[3]], [cs])
        bg1 = self.dbuf("gt1b")
        self.dma("sp", self.gt1b[:], MOD[2 * D:3 * D].partition_broadcast(128), [Bmod], [bg1], bg1)
        bg2 = self.dbuf("gt2b")
        self.dma("sp", self.gt2b[:], MOD[5 * D:6 * D].partition_broadcast(128), [Bmod], [bg2], bg2)
        sb.push()
        g2b = sb.alloc("g2b", [128, D], F32)
        sc2b = sb.alloc("sc2b", [128, D], F32)
        sh2b = sb.alloc("sh2b", [128, D], F32)
        bq1, bq2, bq3 = self.dbuf("g2b"), self.dbuf("sc2b"), self.dbuf("sh2b")
        self.dma("sp", g2b[:], I["norm2_g"].partition_broadcast(128), [], [bq1], bq1)
        self.dma("sp", sc2b[:], MOD[4 * D:5 * D].partition_broadcast(128), [Bmod], [bq2], bq2)
        self.dma("sp", sh2b[:], MOD[3 * D:4 * D].partition_broadcast(128), [Bmod], [bq3], bq3)
        a2t = sb.alloc("a2t", [128, D], BF16)
        sh2t = sb.alloc("sh2t", [128, D], BF16)
        bq4, bq5 = self.dbuf("a2t"), self.dbuf("sh2t")
        self.stt(a2t[:], sc2b[:], 1.0, g2b[:], ALU.add, ALU.mult, [bq1, bq2], [bq4])
        self.cp("dve", sh2t[:], sh2b[:], [bq3], [bq5])
        self.B_a2r = Buf("A2R")
        self.dma("sp", self.dram["A2R"][0:1, :], a2t[0:1, :], [bq4], [self.B_a2r], bq4)
        self.dma("sp", self.dram["A2R"][1:2, :], sh2t[0:1, :], [bq5], [self.B_a2r], bq5)
        sb.pop()
        self.release([bq1, bq2, bq3, bq4, bq5])
        self.stt(self.a1[:], self.modc[:, 1, :], 1.0, self.g1c[:], ALU.add, ALU.mult, [cs], [cs])
        self.stt(self.a2[:], self.modc[:, 4, :], 1.0, self.g2c[:], ALU.add, ALU.mult, [cs], [cs])
        S.op("dve", lambda e: e.memset(self.epsc[:], EPS), cb + [cs, bmc, bg1, bg2], [self.B_c0])
        self.B_c0.const = True
        self.B_modc = bmc

    def norm_to_hT(self, xt, bx, hT_dst, bh, a_col, sh_col, ring):
        junk, ssq, tmp, rs, xn, bj, bs, bxn = ring
        self.act(junk[:], xt, AF.Square, [bx], [bj, bs], accum=ssq[:, 0:1])
        self.rstd(rs[:, 0:1], ssq[:, 0:1], float(D), [bs], [bs, bj], tmp[:, 0:1])
        yield
        self.ts(xn[:], xt, rs[:, 0:1], None, ALU.mult, None, [bx, bs], [bxn])
        yield
        for tb in range(2):
            for ts_ in range(8):
                kc = tb * 8 + ts_
                self.tr(self.T[tb][:, ts_ * 128:(ts_ + 1) * 128], xn[:, kc * 128:(kc + 1) * 128], [bxn], [self.BT[tb][0]])
            yield
            for ts_ in range(8):
                kc = tb * 8 + ts_
                pt = self.T[tb][:, ts_ * 128:(ts_ + 1) * 128]
                if tb == 0:
                    self.act(hT_dst[:, kc, :], pt, AF.Identity, [self.BT[tb][0], self.B_c0], [bh], scale=a_col[:, kc:kc + 1], bias=sh_col[:, kc:kc + 1])
                else:
                    self.ts(hT_dst[:, kc, :], pt, a_col[:, kc:kc + 1], sh_col[:, kc:kc + 1], ALU.mult, ALU.add, [self.BT[tb][0], self.B_c0], [bh])
                if ts_ % 2 == 1:
                    yield

    def norm_ring(self, sb, junk=None):
        if junk is None:
            junk = sb.alloc("junk", [128, D], BF16)
        ssq = sb.alloc("ssq", [128, 1], F32)
        tmp = sb.alloc("tmp", [128, 1], F32)
        rs = sb.alloc("rs", [128, 1], F32)
        xn = sb.alloc("xn", [128, D], BF16)
        return (junk, ssq, tmp, rs, xn, Buf("junk"), Buf("ssq"), Buf("xn"))

    def stage1(self):
        S, sb, I = self.S, self.sb, self.I
        sb.push()
        G = 1024
        hT = [sb.alloc("hT", [128, KC, G], BF16) for _ in range(2)]
        BhT = [[Buf("hT%d_%d" % (s, t)) for t in range(8)] for s in range(2)]
        wsl = [sb.alloc("wsl", [128, KC, 512], BF16) for _ in range(3)]
        Bw = [self.dbuf("wsl", "pool") for _ in range(3)]
        xs = [sb.alloc("xs", [128, D], F32) for _ in range(2)]
        Bx = [self.dbuf("xs") for _ in range(2)]
        rings = [self.norm_ring(sb)]
        rings.append(self.norm_ring(sb, junk=rings[0][0]))
        stg = []
        stgb = []
        for _ in range(4):
            sb.push()
            stgb.append(sb.alloc("stgb", [128, 512], BF16))
            sb.pop()
            stg.append(sb.alloc("stg", [128, 512], F32))
        Bs = [self.dbuf("stg") for _ in range(4)]
        wsrc = I["w_in"].rearrange("(k p) n -> p k n", p=128)
        D_ = self.dram
        zt = sb.alloc("zt", [128, 1024], BF16)
        Bz = self.dbuf("zt")
        S.op("dve", lambda e: e.memset(zt[:], 0.0), [], [Bz])
        for i in range(NR // 128):
            for hf in range(2):
                self.dma("sp", D_["XD"][i * 128:(i + 1) * 128, hf * 1024:(hf + 1) * 1024], zt[:], [Bz], [Buf("z")], Bz)
        groups = [("p", 0), ("p", 1), ("m", 0), ("m", 1)]
        pblocks = [2, 3, 4, 5, 8, 9, 10, 11]
        import os
        if os.environ.get("S1G"):
            groups = groups[:int(os.environ["S1G"])]
        if os.environ.get("S1B"):
            pblocks = pblocks[:int(os.environ["S1B"])]
        wi = [0]
        si = [0]
        xi = [0]
        def hT_gen(gi):
            kind, gidx = groups[gi]
            slot = gi % 2
            src = I["xp"] if kind == "p" else I["xm"]
            for t in range(8):
                xsl = xs[xi[0] % 2]
                bx = Bx[xi[0] % 2]
                ring = rings[xi[0] % 2]
                xi[0] += 1
                r0 = gidx * G + t * 128
                self.dma("sp", xsl[:], src[r0:r0 + 128, :], [], [bx], bx)
                yield
                for _ in self.norm_to_hT(xsl[:], bx, hT[slot][:, :, t * 128:(t + 1) * 128], BhT[slot][t], self.a1, self.modc[:, 0, :], ring):
                    yield

        for _ in hT_gen(0):
            pass
        for gi, (kind, gidx) in enumerate(groups):
            slot = gi % 2
            ltok0 = gidx * G + (0 if kind == "p" else NT)
            mtok0 = gidx * G
            nxt = hT_gen(gi + 1) if gi + 1 < len(groups) else iter(())
            blocks = pblocks if kind == "p" else list(range(22))
            nstep = -(-8 * 16 // (len(blocks) * 8)) + 1
            for blk in blocks:
                w = wsl[wi[0] % 3]
                bw = Bw[wi[0] % 3]
                wi[0] += 1
                self.dma("pool", w[:], wsrc[:, :, blk * 512:(blk + 1) * 512], [], [bw], bw)
                fm = blk in (0, 1, 2, 3, 6, 7) or blk >= 14
                for u in range(8):
                    fb = u % 4
                    pf = self.F[fb]
                    bpf = self.BF[fb]
                    if fm:
                        cs_, th = u // 2, u % 2
                        for kc in range(KC):
                            self.mm(pf[:, :], w[:, kc, cs_ * 128:(cs_ + 1) * 128], hT[slot][:, kc, th * 512:(th + 1) * 512],
                                    kc == 0, kc == KC - 1, [bw] + BhT[slot][th * 4:(th + 1) * 4], [bpf])
                    else:
                        for kc in range(KC):
                            self.mm(pf[:, :], hT[slot][:, kc, u * 128:(u + 1) * 128], w[:, kc, :],
                                    kc == 0, kc == KC - 1, [bw, BhT[slot][u]], [bpf])
                    sg = stg[si[0] % 4]
                    sgb = stgb[si[0] % 4]
                    bs = Bs[si[0] % 4]
                    si[0] += 1
                    if fm:
                        if blk < 2:
                            dst = D_["QT"][blk * 4 + cs_, :, mtok0 + th * 512: mtok0 + (th + 1) * 512]
                            key = ("QT", blk * 4 + cs_)
                            fn = AF.Copy
                        elif blk < 4:
                            dst = D_["KT"][(blk - 2) * 4 + cs_, :, ltok0 + th * 512: ltok0 + (th + 1) * 512]
                            key = ("KT", (blk - 2) * 4 + cs_)
                            fn = AF.Copy
                        elif blk < 8:
                            dst = D_["RQT"][(blk - 6) * 4 + cs_, :, mtok0 + th * 512: mtok0 + (th + 1) * 512]
                            key = ("RQT", (mtok0 + th * 512) // 128)
                            fn = AF.Copy
                        elif blk < 18:
                            dst = D_["GAT"][(blk - 14) * 4 + cs_, :, mtok0 + th * 512: mtok0 + (th + 1) * 512]
                            key = ("GAT", (blk - 14) * 4 + cs_)
                            fn = AF.Sigmoid
                        else:
                            dst = D_["GRT"][(blk - 18) * 4 + cs_, :, mtok0 + th * 512: mtok0 + (th + 1) * 512]
                            key = ("GRT", (blk - 18) * 4 + cs_)
                            fn = AF.Sigmoid
                        odt = BF16
                    else:
                        if blk < 6:
                            dst = D_["V"][ltok0 + u * 128: ltok0 + (u + 1) * 128, (blk - 4) * 512:(blk - 3) * 512]
                            key = ("V", (blk - 4) * 4)
                            fn, odt = AF.Copy, BF16
                        elif blk < 10:
                            dst = D_["RF"][ltok0 + u * 128: ltok0 + (u + 1) * 128, (blk - 8) * 512:(blk - 7) * 512]
                            key = ("RF", (ltok0 + u * 128) // 128)
                            fn, odt = AF.Copy, F32
                        elif blk < 12:
                            dst = D_["RI"][ltok0 + u * 128: ltok0 + (u + 1) * 128, (blk - 10) * 512:(blk - 9) * 512]
                            key = ("RI", (ltok0 + u * 128) // 128)
                            fn, odt = AF.Copy, BF16
                        else:
                            dst = D_["ROG"][mtok0 + u * 128: mtok0 + (u + 1) * 128, (blk - 12) * 512:(blk - 11) * 512]
                            key = ("ROG", (mtok0 + u * 128) // 128)
                            fn, odt = AF.Silu, BF16
                    so = sg[:] if odt == F32 else sgb[:]
                    if fn == AF.Copy and (u % 2 == 1):
                        self.cp("dve", so, pf[:, :], [bpf], [bs])
                    else:
                        self.act(so, pf[:, :], fn, [bpf], [bs])
                    wl_ = self.scr_w.setdefault(key, [])
                    tok = Buf("w")
                    wl_.append(tok)
                    self.dma("sp", dst, so, [bs], [tok], bs)
                    for _ in range(nstep):
                        next(nxt, None)
            for _ in nxt:
                pass
        sb.pop()
        self.release(Bw + Bx + Bs + [Bz])

    def scr_reads(self, name, keys):
        out = []
        for k in keys:
            out.extend(self.scr_w.get((name, k), []))
        return out

    def stage2(self):
        S, sb = self.S, self.sb
        D_ = self.dram
        sb.push()
        QTh = [sb.alloc("QTh", [128, NT], BF16) for _ in range(2)]
        KTh = [sb.alloc("KTh", [128, 2 * NT], BF16) for _ in range(2)]
        Vh = [sb.alloc("Vh", [128, 32, 129], BF16) for _ in range(2)]
        Bq = [self.dbuf("q") for _ in range(2)]
        Bk = [self.dbuf("k") for _ in range(2)]
        Bv = [self.dbuf("v") for _ in range(2)]
        km32 = [sb.alloc("km32", [128, 16], F32) for _ in range(2)]
        kmb = [sb.alloc("kmb", [128, 16], BF16) for _ in range(2)]
        gm = [sb.alloc("gm", [128, 16, 16], F32) for _ in range(2)]
        top8 = [sb.alloc("top8", [128, 16, 8], F32) for _ in range(2)]
        pen = [sb.alloc("pen", [128, 16, 16], F32) for _ in range(2)]
        penb = [sb.alloc("penb", [128, 256], BF16) for _ in range(2)]
        penT = [sb.alloc("penT", [16, NT], BF16) for _ in range(2)]
        Bkm = [Buf("km%d" % i) for i in range(2)]
        Bgm = [Buf("gm%d" % i) for i in range(2)]
        Bpen = [Buf("pen%d" % i) for i in range(2)]
        BpenT = [Buf("penT%d" % i) for i in range(2)]
        pT = [sb.alloc("pT", [128, 256], BF16) for _ in range(4)]
        BpT = [Buf("pT%d" % i) for i in range(4)]
        rsum = [sb.alloc("rsum", [128, 1], F32) for _ in range(2)]
        atts = [sb.alloc("atts", [128, 128], BF16) for _ in range(2)]
        Brs = [Buf("rsum%d" % i) for i in range(2)]
        Bat = [Buf("atts%d" % i) for i in range(2)]
        scale = 1.0 / np.sqrt(128.0)
        pi = [0]
        ei = [0]
        for sl in range(2):
            S.op("dve", lambda e, t=Vh[sl]: e.memset(t[:, :, 128:129], 1.0), [], [Bv[sl]])
        def preamble(h):
            sl = h % 2
            q, k, v = QTh[sl], KTh[sl], Vh[sl]
            bq, bk, bv = Bq[sl], Bk[sl], Bv[sl]
            self.dma("sp", q[:], D_["QT"][h], self.scr_reads("QT", [h]), [bq], bq)
            self.dma("sp", k[:], D_["KT"][h], self.scr_reads("KT", [h]), [bk], bk)
            self.dma("sp", v[:, :, 0:128], D_["V"].rearrange("(t p) c -> p t c", p=128)[:, :, h * 128:(h + 1) * 128],
                     self.scr_reads("V", [(h // 4) * 4]), [bv], bv)
            yield
            S.op("dve", lambda e, k=k: e.tensor_reduce(out=km32[sl][:], in_=k[:].rearrange("p (b l) -> p b l", l=256), axis=AX.X, op=ALU.add),
                 [bk], [Bkm[sl]])
            yield
            self.ts(kmb[sl][:], km32[sl][:], 1.0 / 256, None, ALU.mult, None, [Bkm[sl]], [Bkm[sl]])
            yield
            pg = self.F[5]
            bpg = self.BF[5]
            for qt in range(16):
                self.mm(pg[:, qt * 16:(qt + 1) * 16], q[:, qt * 128:(qt + 1) * 128], kmb[sl][:, :], True, True, [bq, Bkm[sl]], [bpg])
            yield
            gmf = gm[sl][:].rearrange("p a b -> p (a b)")
            self.tt(gmf, pg[:, 0:256], self.cpen[:], ALU.add, [bpg, self.B_c0], [Bgm[sl]])
            yield
            self.ts(gm[sl][:, :, 0:8], gm[sl][:, :, 0:8], self.ppen[:, 0:1], None, ALU.add, None, [Bgm[sl], self.B_c0], [Bgm[sl]])
            yield
            for qt in range(16):
                S.op("dve", lambda e, qt=qt: e.max(out=top8[sl][:, qt, :], in_=gm[sl][:, qt, :]), [Bgm[sl]], [Bpen[sl]])
                yield
            for qt in range(16):
                self.ts(pen[sl][:, qt, :], gm[sl][:, qt, :], top8[sl][:, qt, 2:3], None, ALU.is_ge, None, [Bgm[sl], Bpen[sl]], [Bpen[sl]])
                yield
            penf = pen[sl][:].rearrange("p a b -> p (a b)")
            self.ts(penf, penf, -1.0, -NEG, ALU.add, ALU.mult, [Bpen[sl]], [Bpen[sl]])
            yield
            self.ts(pen[sl][:, :, 0:8], pen[sl][:, :, 0:8], self.ppen[:, 0:1], None, ALU.add, None, [Bpen[sl], self.B_c0], [Bpen[sl]])
            yield
            self.cp("dve", penb[sl][:], penf, [Bpen[sl]], [Bpen[sl]])
            yield
            for half in range(2):
                for qt in range(half * 8, half * 8 + 8):
                    tb, ts_ = qt // 8, qt % 8
                    pt = self.T[tb][0:16, ts_ * 128:(ts_ + 1) * 128]
                    self.tr(pt, penb[sl][:, qt * 16:(qt + 1) * 16], [Bpen[sl]], [self.BT[tb][ts_]])
                yield
                for qt in range(half * 8, half * 8 + 8):
                    tb, ts_ = qt // 8, qt % 8
                    pt = self.T[tb][0:16, ts_ * 128:(ts_ + 1) * 128]
                    self.cp("act", penT[sl][:, qt * 128:(qt + 1) * 128], pt, [self.BT[tb][ts_]], [BpenT[sl]])
                yield

        for _ in preamble(0):
            pass
        for h in range(8):
            sl = h % 2
            q, k, v = QTh[sl], KTh[sl], Vh[sl]
            bq, bk, bv = Bq[sl], Bk[sl], Bv[sl]
            nxt = preamble(h + 1) if h + 1 < 8 else iter(())
            itc = [0]
            for qb in range(8):
                Bl = 8 + qb
                nkt = 2 * Bl + 2
                po = [self.F[3], self.F[4]]
                bpo = [self.BF[3], self.BF[4]]
                LAG = 2
                pend = []
                for kk in range(nkt + LAG):
                    if kk < nkt:
                        kt = kk
                        fb = kt % 3
                        ps = self.F[fb]
                        bps = self.BF[fb]
                        self.mm(ps[:, 0:256], k[:, kt * 128:(kt + 1) * 128], q[:, qb * 256:(qb + 1) * 256], True, False, [bk, bq], [bps])
                        if kt < 2 * Bl:
                            i = kt // 2
                            self.mm(ps[:, 0:256], self.selc[0:16, i * 128:(i + 1) * 128], penT[sl][0:16, qb * 256:(qb + 1) * 256],
                                    False, True, [BpenT[sl], self.B_c0], [bps])
                        else:
                            j = kt - 2 * Bl
                            self.mm(ps[:, 0:256], self.identb[:, :], self.cm[:, j * 256:(j + 1) * 256], False, True, [self.B_c0], [bps])
                        p = pT[pi[0] % 4]
                        bp = BpT[pi[0] % 4]
                        pi[0] += 1
                        self.act(p[:], ps[:, 0:256], AF.Exp, [bps], [bp], scale=scale)
                        pend.append((kt, p, bp))
                    if kk >= LAG:
                        kt, p, bp = pend.pop(0)
                        for qh in range(2):
                            self.mm(po[qh][:, 0:129], p[:, qh * 128:(qh + 1) * 128], v[:, kt, :], kt == 0, kt == nkt - 1, [bp, bv], [bpo[qh]])
                    itc[0] += 1
                    if itc[0] >= 24:
                        next(nxt, None)
                for qh in range(2):
                    e_ = ei[0] % 2
                    ei[0] += 1
                    S.op("dve", lambda e, o=rsum[e_], i=po[qh]: e.reciprocal(out=o[:, 0:1], in_=i[:, 128:129]), [bpo[qh]], [Brs[e_]])
                    self.act(atts[e_][:], po[qh][:, 0:128], AF.Copy, [bpo[qh], Brs[e_]], [Bat[e_]], scale=rsum[e_][:, 0:1])
                    qt = qb * 2 + qh
                    tb, ts_ = qt // 8, qt % 8
                    pt = self.T[tb][:, ts_ * 128:(ts_ + 1) * 128]
                    bt = self.BT[tb][ts_]
                    self.tr(pt, atts[e_][:], [Bat[e_]], [bt])
                    self.cp("dve", self.attT[:, h, qt * 128:(qt + 1) * 128], pt, [bt], [self.B_attT[h]])
            for _ in nxt:
                pass
            if self.debug:
                self.dma("sp", D_["ATT"][h], self.attT[:, h, :], [self.B_attT[h]], [Buf("x")], self.B_attT[h])
        sb.pop()
        self.release(Bq + Bk + Bv)

    def stage3(self):
        S, sb = self.S, self.sb
        D_ = self.dram
        sb.push()
        rf = [sb.alloc("rf", [128, 1024], F32) for _ in range(2)]
        ri = [sb.alloc("ri", [128, 1024], BF16) for _ in range(2)]
        rq = [sb.alloc("rq", [128, 8, 128], BF16) for _ in range(2)]
        rog = [sb.alloc("rog", [128, 1024], BF16) for _ in range(2)]
        Brf = [self.dbuf("rf") for _ in range(2)]
        Bri = [self.dbuf("ri") for _ in range(2)]
        Brq = [self.dbuf("rq") for _ in range(2)]
        Brog = [self.dbuf("rog") for _ in range(2)]
        logf = [sb.alloc("logf", [128, 1024], F32) for _ in range(2)]
        kin = [sb.alloc("kin", [128, 1024], F32) for _ in range(2)]
        Blf = [Buf("logf%d" % i) for i in range(2)]
        Bkin = [Buf("kin%d" % i) for i in range(2)]
        R = 4
        ebl = [sb.alloc("ebl", [128, 128], F32) for _ in range(R)]
        khat = [sb.alloc("khat", [128, 128], BF16) for _ in range(R)]
        eb8 = [sb.alloc("eb8", [128, 8], F32) for _ in range(R)]
        ebm = [sb.alloc("ebm", [128, 128], F32) for _ in range(R)]
        qtl = [sb.alloc("qtl", [128, 128], BF16) for _ in range(R)]
        enb = [sb.alloc("enb", [128, 128], F32) for _ in range(R)]
        ktl = [sb.alloc("ktl", [128, 128], BF16) for _ in range(R)]
        ktlT = [sb.alloc("ktlT", [128, 128], BF16) for _ in range(R)]
        khc = [sb.alloc("khc", [128, 4, 128], BF16) for _ in range(R)]
        qtlz = [sb.alloc("qtlz", [128, 4, 128], BF16) for _ in range(R)]
        Bkhc = [Buf("khc%d" % i) for i in range(R)]
        Bqz = [Buf("qtlz%d" % i) for i in range(R)]
        for i in range(R):
            S.op("dve", lambda e, t=qtlz[i]: e.memset(t[:], 0.0), [], [Bqz[i]])
        at = [sb.alloc("at", [128, 128], BF16) for _ in range(R)]
        Sb = [sb.alloc("Sb", [128, 4, 128], BF16) for _ in range(R)]
        ot = [sb.alloc("ot", [128, 128], F32) for _ in range(R)]
        of_ = [sb.alloc("of", [128, 128], BF16) for _ in range(R)]
        sq = [sb.alloc("sq", [128, 4], F32) for _ in range(R)]
        jk = [sb.alloc("jk", [128, 128], BF16) for _ in range(R)]
        names = "ebl khat eb8 ebm qtl enb ktl ktlT at ot of sq".split()
        Bn = {n: [Buf(n + str(i)) for i in range(R)] for n in names}
        BSb = [[Buf("Sb%d_%d" % (i, c)) for c in range(4)] for i in range(R)]
        St = sb.alloc("St", [128, 8, 128], F32)
        BSt = [Buf("St%d" % h) for h in range(8)]
        for h in range(8):
            S.op("dve", lambda e, h=h: e.memset(St[:, h, :], 0.0), [], [BSt[h]])
        F, BFs = self.F, self.BFs
        hi = [0]
        import os
        tl = list(range(32))
        if os.environ.get("S3T"):
            tl = [int(v) for v in os.environ["S3T"].split(",")]
        s3c = int(os.environ.get("S3C", "4"))
        s3m = int(os.environ.get("S3M", "9"))
        for tile in tl:
            main = tile >= 16
            mt = tile - 16
            sl = tile % 2
            self.dma("sp", rf[sl][:], D_["RF"][tile * 128:(tile + 1) * 128, :], self.scr_reads("RF", [tile]), [Brf[sl]], Brf[sl])
            self.dma("sp", ri[sl][:], D_["RI"][tile * 128:(tile + 1) * 128, :], self.scr_reads("RI", [tile]), [Bri[sl]], Bri[sl])
            if main:
                self.dma("sp", rq[sl][:], D_["RQT"][:, :, mt * 128:(mt + 1) * 128].rearrange("h p t -> p h t"),
                         self.scr_reads("RQT", [(mt // 4) * 4]), [Brq[sl]], Brq[sl])
                self.dma("sp", rog[sl][:], D_["ROG"][mt * 128:(mt + 1) * 128, :], self.scr_reads("ROG", [mt]), [Brog[sl]], Brog[sl])
            lf, kn = logf[sl], kin[sl]
            self.act(kn[:], rf[sl][:], AF.Sigmoid, [Brf[sl]], [Bkin[sl]])
            self.tt(kn[:], kn[:], self.oml[:], ALU.mult, [Bkin[sl], self.B_c0], [Bkin[sl]])
            self.tt(kn[:], kn[:], self.lb[:], ALU.add, [Bkin[sl], self.B_c0], [Bkin[sl]])
            self.act(lf[:], kn[:], AF.Ln, [Bkin[sl]], [Blf[sl]])
            self.ts(kn[:], kn[:], -1.0, 1.0, ALU.mult, ALU.add, [Bkin[sl]], [Bkin[sl]])
            def hbody(h, r, main=main, sl=sl, lf=lf, kn=kn, mt=mt):
                hs = slice(h * 128, (h + 1) * 128)
                s2 = h % 2
                c128 = slice(s2 * 128, (s2 + 1) * 128)
                bl_ps = F[0][:, c128]
                b8_ps = F[0][:, 256 + s2 * 8:256 + (s2 + 1) * 8]
                self.mm(bl_ps, self.wl[:, :], lf[:, hs], True, True, [Blf[sl], self.B_c0], [self.BF[0]])
                self.mm(b8_ps, lf[:, hs], self.wall[:, 128:136], True, True, [Blf[sl], self.B_c0], [self.BF[0]])
                if main:
                    self.mm(F[1][:, c128], lf[:, hs], self.wall[:, 0:128], True, True, [Blf[sl], self.B_c0], [self.BF[1]])
                    bm_ps = F[1][:, 256 + s2 * 128:256 + (s2 + 1) * 128]
                    self.mm(bm_ps, self.wall[:, 0:128], lf[:, hs], True, True, [Blf[sl], self.B_c0], [self.BF[1]])
                yield
                self.act(ebl[r][:], bl_ps, AF.Exp, [self.BF[0]], [Bn["ebl"][r]])
                self.act(eb8[r][:], b8_ps, AF.Exp, [self.BF[0]], [Bn["eb8"][r]])
                if main:
                    self.act(ebm[r][:], F[1][:, c128], AF.Exp, [self.BF[1]], [Bn["ebm"][r]])
                    self.act(enb[r][:], bm_ps, AF.Exp, [self.BF[1]], [Bn["enb"][r]], scale=-1.0)
                yield
                for c in range(4):
                    self.stt(khc[r][:, c, :], kn[:, hs], self.wall[:, 132 + c:133 + c], ebl[r][:], ALU.mult, ALU.mult,
                             [Bkin[sl], Bn["ebl"][r], self.B_c0], [Bkhc[r]])
                if main:
                    self.tt(ktl[r][:], kn[:, hs], enb[r][:], ALU.mult, [Bkin[sl], Bn["enb"][r]], [Bn["ktl"][r]])
                    self.tt(qtl[r][:], rq[sl][:, h, :], ebm[r][:], ALU.mult, [Brq[sl], Bn["ebm"][r]], [Bn["qtl"][r]])
                    for c in range(4):
                        cc = slice(32 * c, 32 * c + 32)
                        self.tt(qtlz[r][:, c, cc], rq[sl][:, h, cc], ebm[r][:, cc], ALU.mult, [Brq[sl], Bn["ebm"][r]], [Bqz[r]])
                yield
                pds = [F[5][:, (s2 * 2 + (c % 2)) * 128:(s2 * 2 + (c % 2) + 1) * 128] for c in range(4)]
                if main:
                    pt = self.T[0][:, c128]
                    self.tr(pt, ktl[r][:], [Bn["ktl"][r]], [self.BT[0][0]])
                    yield
                    self.cp("act", ktlT[r][:], pt, [self.BT[0][0]], [Bn["ktlT"][r]])
                    yield
                    self.mm(F[3][:, c128], ktlT[r][:], qtl[r][:], True, True, [Bn["ktlT"][r], Bn["qtl"][r]], [self.BF[3]])
                    yield
                    self.tt(at[r][:], F[3][:, c128], self.bd[:], ALU.mult, [self.BF[3], self.B_c0], [Bn["at"][r]])
                    yield
                pob = 4 if s2 == 0 else 2
                po = F[pob][:, 0:128]
                bpo = self.BF[pob]
                if main:
                    self.mm(po, at[r][:], ri[sl][:, hs], True, False, [Bn["at"][r], Bri[sl]], [bpo])
                for c in range(4):
                    self.mm(pds[c], khc[r][:, c, :], ri[sl][:, hs], True, True, [Bkhc[r], Bri[sl]], [self.BF[5]])
                    if main:
                        self.act(Sb[r][:, c, :], St[:, h, :], AF.Copy, [BSt[h], Bn["eb8"][r]], [BSb[r][c]], scale=eb8[r][:, c:c + 1])
                    yield
                    if main:
                        self.mm(po, qtlz[r][:, c, :], Sb[r][:, c, :], False, c == 3, [Bqz[r], BSb[r][c]], [bpo])
                    self.stt(St[:, h, :], St[:, h, :], eb8[r][:, 4 + c:5 + c], pds[c], ALU.mult, ALU.add, [BSt[h], Bn["eb8"][r], self.BF[5]], [BSt[h]])
                    yield
                if main:
                    self.act(jk[r][:], po, AF.Square, [bpo], [Bn["sq"][r]], accum=sq[r][:, 0:1])
                    yield
                    self.tt(ot[r][:], po, self.rngb[:], ALU.mult, [bpo, self.B_c0], [Bn["ot"][r]])
                    self.rstd(sq[r][:, 2:3], sq[r][:, 0:1], 128.0, [Bn["sq"][r]], [Bn["sq"][r], Bn["sq"][r]], sq[r][:, 1:2])
                    yield
                    self.stt(of_[r][:], ot[r][:], sq[r][:, 2:3], rog[sl][:, hs], ALU.mult, ALU.mult, [Bn["ot"][r], Bn["sq"][r], Brog[sl]], [Bn["of"][r]])
                    yield
                    pt = self.T[1][:, c128]
                    self.tr(pt, of_[r][:], [Bn["of"][r]], [self.BT[1][0]])
                    yield
                    self.cp("act", self.oT[:, h, mt * 128:(mt + 1) * 128], pt, [self.BT[1][0]], [self.B_oT[mt]])

            G = 2
            for h0 in range(0, 8, G):
                gens = []
                for h in range(h0, h0 + G):
                    gens.append(hbody(h, hi[0] % R))
                    hi[0] += 1
                while gens:
                    for g_ in list(gens):
                        try:
                            next(g_)
                        except StopIteration:
                            gens.remove(g_)
            if tile == 15:
                for h in range(8):
                    self.ts(St[:, h, :], St[:, h, :], self.pm[:, 0:1], None, ALU.mult, None, [BSt[h], self.B_c0], [BSt[h]])
        if self.debug:
            for h in range(8):
                self.dma("sp", D_["OT"][h], self.oT[:, h, :], self.B_oT, [Buf("x")], self.B_oT[0])
        sb.pop()
        self.release(Brf + Bri + Brq + Brog)

    def stage4a(self):
        S, sb, I = self.S, self.sb, self.I
        D_ = self.dram
        sb.push()
        wa = [sb.alloc("wa", [128, 8, 128], BF16) for _ in range(2)]
        wr_ = [sb.alloc("wr_", [128, 8, 128], BF16) for _ in range(2)]
        sga = [sb.alloc("sga", [128, NT], BF16) for _ in range(2)]
        sgr = [sb.alloc("sgr", [128, NT], BF16) for _ in range(2)]
        mT = [sb.alloc("mT", [128, NT], BF16) for _ in range(2)]
        t1 = [sb.alloc("t1", [128, 512], F32) for _ in range(2)]
        t2 = [sb.alloc("t2", [128, 512], F32) for _ in range(2)]
        Bwa = [self.dbuf("wa", "pool") for _ in range(2)]
        Bwr = [self.dbuf("wr", "pool") for _ in range(2)]
        Bsa = [self.dbuf("sga") for _ in range(2)]
        Bsr = [self.dbuf("sgr") for _ in range(2)]
        BmT = [self.dbuf("mT") for _ in range(2)]
        Bt1 = [Buf("t1_%d" % i) for i in range(2)]
        Bt2 = [Buf("t2_%d" % i) for i in range(2)]
        wua = I["w_up_a"].rearrange("(k p) n -> p k n", p=128)
        wur = I["w_up_r"].rearrange("(k p) n -> p k n", p=128)
        ti = [0]
        for ch in range(16):
            sl = ch % 2
            self.dma("pool", wa[sl][:], wua[:, :, ch * 128:(ch + 1) * 128], [], [Bwa[sl]], Bwa[sl])
            self.dma("pool", wr_[sl][:], wur[:, :, ch * 128:(ch + 1) * 128], [], [Bwr[sl]], Bwr[sl])
            self.dma("sp", sga[sl][:], D_["GAT"][ch], self.scr_reads("GAT", [ch]), [Bsa[sl]], Bsa[sl])
            self.dma("sp", sgr[sl][:], D_["GRT"][ch], self.scr_reads("GRT", [ch]), [Bsr[sl]], Bsr[sl])
            for g in range(4):
                gs = slice(g * 512, (g + 1) * 512)
                fa, fr = (g % 2) * 2, (g % 2) * 2 + 1
                for wc in range(8):
                    self.mm(self.F[fa][:, :], wa[sl][:, wc, :], self.attT[:, wc, gs], wc == 0, wc == 7, [Bwa[sl], self.B_attT[wc]], [self.BF[fa]])
                for wc in range(8):
                    self.mm(self.F[fr][:, :], wr_[sl][:, wc, :], self.oT[:, wc, gs], wc == 0, wc == 7,
                            [Bwr[sl]] + self.B_oT[g * 4:(g + 1) * 4], [self.BF[fr]])
                i = ti[0] % 2
                ti[0] += 1
                self.tt(t1[i][:], self.F[fa][:, :], sga[sl][:, gs], ALU.mult, [self.BF[fa], Bsa[sl]], [Bt1[i]])
                self.tt(t2[i][:], self.F[fr][:, :], sgr[sl][:, gs], ALU.mult, [self.BF[fr], Bsr[sl]], [Bt2[i]], eng="dve")
                self.tt(mT[sl][:, gs], t1[i][:], t2[i][:], ALU.add, [Bt1[i], Bt2[i]], [BmT[sl]], eng="dve")
            tok = Buf("w")
            self.scr_w.setdefault(("MT", ch), []).append(tok)
            self.dma("sp", D_["MT"][ch], mT[sl][:], [BmT[sl]], [tok], BmT[sl])
        sb.pop()
        self.release(Bwa + Bwr + Bsa + Bsr + BmT)

    def stage4b(self):
        S, sb, I = self.S, self.sb, self.I
        D_ = self.dram
        sb.push()
        wo = sb.alloc("wo", [128, KC, D], BF16)
        Bwo = [self.dbuf("wo", "pool") for _ in range(4)]
        wsrc = I["w_out"].rearrange("(k p) n -> p k n", p=128)
        for i in range(4):
            self.dma("pool", wo[:, i * 4:(i + 1) * 4, :], wsrc[:, i * 4:(i + 1) * 4, :], [], [Bwo[i]], Bwo[i])
        mTg = [sb.alloc("mTg", [128, KC, 512], BF16)] * 2
        Bmg = [self.dbuf("mTg")] * 2
        xs = [sb.alloc("xs", [128, D], F32) for _ in range(2)]
        Bx = [self.dbuf("xs") for _ in range(2)]
        x1 = [sb.alloc("x1", [128, D], F32) for _ in range(2)]
        Bx1 = [self.dbuf("x1") for _ in range(2)]
        h2 = [sb.alloc("h2", [128, KC, 128], BF16) for _ in range(2)]
        Bh2 = [self.dbuf("h2") for _ in range(2)]
        rings = [self.norm_ring(sb)]
        rings.append(self.norm_ring(sb, junk=rings[0][0]))
        tq = [sb.alloc("tq", [128, 512], F32) for _ in range(2)]
        Btq = [Buf("tq%d" % i) for i in range(2)]
        rt = sb.alloc("rt", [128, 160], F32)
        Brt = Buf("rt")
        r2 = sb.alloc("r2", [128, 112], F32)
        Br2 = Buf("r2")
        selb = sb.alloc("selb", [128, 32], BF16)
        Bselb = Buf("selb")
        h2k = [sb.alloc("h2k", [128, D], BF16) for _ in range(2)]
        Bh2k = [Buf("h2k%d" % i) for i in range(2)]
        a2b = sb.alloc("a2b", [128, D], BF16)
        sh2bb = sb.alloc("sh2bb", [128, D], BF16)
        Ba2 = self.dbuf("a2b")
        self.dma("sp", a2b[:], D_["A2R"][0, :].partition_broadcast(128), [self.B_a2r], [Ba2], Ba2)
        self.dma("sp", sh2bb[:], D_["A2R"][1, :].partition_broadcast(128), [self.B_a2r], [Ba2], Ba2)
        mtsrc = D_["MT"].rearrange("k p t -> p k t")
        qi = [0]
        for g in range(4):
            sg = g % 2
            self.dma("sp", mTg[sg][:], mtsrc[:, :, g * 512:(g + 1) * 512], self.scr_reads("MT", range(16)), [Bmg[sg]], Bmg[sg])
            for tt_ in range(4):
                tile = g * 4 + tt_
                sl = tile % 2
                self.dma("sp", xs[sl][:], I["xm"][tile * 128:(tile + 1) * 128, :], [], [Bx[sl]], Bx[sl])
                for cg in range(4):
                    cs_ = slice(cg * 512, (cg + 1) * 512)
                    fb = cg % 4
                    for kc in range(KC):
                        self.mm(self.F[fb][:, :], mTg[sg][:, kc, tt_ * 128:(tt_ + 1) * 128], wo[:, kc, cs_], kc == 0, kc == KC - 1,
                                [Bmg[sg], Bwo[kc // 4]], [self.BF[fb]])
                    i = qi[0] % 2
                    qi[0] += 1
                    self.tt(tq[i][:], self.F[fb][:, :], self.gt1b[:, cs_], ALU.mult, [self.BF[fb], self.B_c0], [Btq[i]])
                    self.tt(x1[sl][:, cs_], tq[i][:], xs[sl][:, cs_], ALU.add, [Btq[i], Bx[sl]], [Bx1[sl]], eng="dve")
                tok = Buf("w")
                self.scr_w.setdefault(("X1", tile), []).append(tok)
                self.dma("sp", D_["X1"][tile * 128:(tile + 1) * 128, :], x1[sl][:], [Bx1[sl]], [tok], Bx1[sl])
                for _ in self.norm_to_hT(x1[sl][:], Bx1[sl], h2[sl][:, :, :], Bh2[sl], self.a2, self.modc[:, 3, :], rings[sl]):
                    pass
                tok = Buf("w")
                self.scr_w.setdefault(("H2T", tile), []).append(tok)
                self.dma("sp", D_["H2T"][:, :, tile * 128:(tile + 1) * 128].rearrange("k p t -> p k t"), h2[sl][:], [Bh2[sl]], [tok], Bh2[sl])
                pr = self.F[5][:, 0:36]
                bpr = self.BF[5]
                for kc in range(KC):
                    self.mm(pr, h2[sl][:, kc, :], self.wr[:, kc, :], kc == 0, kc == KC - 1, [Bh2[sl], self.B_c0], [bpr])
                lg = rt[:, 0:36]
                self.tt(lg, pr, self.br[:], ALU.add, [bpr, self.B_c0], [Brt])
                mx = rt[:, 36:37]
                S.op("dve", lambda e: e.tensor_reduce(out=rt[:, 36:37], in_=rt[:, 0:4], axis=AX.X, op=ALU.max), [Brt], [Brt])
                self.ts(rt[:, 37:38], mx, -1.0, None, ALU.mult, None, [Brt], [Brt])
                self.act(rt[:, 40:44], rt[:, 0:4], AF.Exp, [Brt], [Brt], bias=rt[:, 37:38], accum=rt[:, 38:39])
                self.ts(rt[:, 44:48], rt[:, 0:4], mx, None, ALU.is_ge, None, [Brt], [Brt])
                self.ts(rt[:, 44:48], rt[:, 44:48], -1.0, -NEGBIG, ALU.add, ALU.mult, [Brt], [Brt])
                for gi in range(4):
                    self.ts(rt[:, 48 + gi * 8:56 + gi * 8], rt[:, 4 + gi * 8:12 + gi * 8], rt[:, 44 + gi:45 + gi], None, ALU.add, None, [Brt], [Brt])
                S.op("dve", lambda e: e.max(out=rt[:, 80:88], in_=rt[:, 48:80]), [Brt], [Brt])
                self.ts(rt[:, 88:120], rt[:, 48:80], rt[:, 81:82], None, ALU.is_ge, None, [Brt], [Brt])
                self.ts(rt[:, 39:40], rt[:, 80:81], -1.0, None, ALU.mult, None, [Brt], [Brt])
                self.act(rt[:, 120:152], rt[:, 48:80], AF.Exp, [Brt], [Brt], bias=rt[:, 39:40])
                self.tt(rt[:, 120:152], rt[:, 120:152], rt[:, 88:120], ALU.mult, [Brt], [Brt])
                S.op("dve", lambda e: e.reduce_sum(out=rt[:, 152:153], in_=rt[:, 120:152], axis=AX.X), [Brt], [Brt])
                self.tt(rt[:, 152:153], rt[:, 152:153], rt[:, 38:39], ALU.mult, [Brt], [Brt])
                S.op("dve", lambda e: e.reciprocal(out=rt[:, 153:154], in_=rt[:, 152:153]), [Brt], [Brt])
                self.ts(self.wfull[:, tile, :], rt[:, 120:152], rt[:, 153:154], None, ALU.mult, None, [Brt], [self.B_wfull[tile]])
                self.cp("dve", selb[:], rt[:, 88:120], [Brt], [Bselb])
                pc = self.F[4]
                self.mm(pc[:, 0:32], self.slt[:, :], selb[:], True, True, [Bselb, self.B_c0], [self.BF[4]])
                self.mm(pc[:, 32:64], self.ones[:, :], selb[:], True, True, [Bselb, self.B_c0], [self.BF[4]])
                self.tt(r2[:, 0:32], pc[:, 0:32], self.tot[:], ALU.add, [self.BF[4], self.B_tot], [Br2])
                self.tt(self.tot[:], pc[:, 32:64], self.tot[:], ALU.add, [self.BF[4], self.B_tot], [self.B_tot])
                self.ts(r2[:, 0:32], r2[:, 0:32], float(CAP - 1), None, ALU.min, None, [Br2], [Br2])
                self.tt(r2[:, 0:32], r2[:, 0:32], self.ebase[:], ALU.add, [Br2, self.B_c0], [Br2])
                self.stt(r2[:, 32:64], r2[:, 0:32], 1.0e6, rt[:, 88:120], ALU.add, ALU.mult, [Br2, Brt], [Br2])
                S.op("dve", lambda e: e.max(out=r2[:, 64:72], in_=r2[:, 32:64]), [Br2], [Br2])
                self.ts(r2[:, 72:74], r2[:, 64:66], -1.0e6, None, ALU.add, None, [Br2], [Br2])
                self.cp("dve", self.desti[:, tile, :], r2[:, 72:74], [Br2], [self.B_dest[tile]])
                for k_ in range(2):
                    self.ts(r2[:, 80:112], r2[:, 32:64], r2[:, 64 + k_:65 + k_], None, ALU.is_equal, None, [Br2], [Br2])
                    self.tt(r2[:, 80:112], r2[:, 80:112], self.wfull[:, tile, :], ALU.mult, [Br2, self.B_wfull[tile]], [Br2])
                    S.op("dve", lambda e, k_=k_, tile=tile: e.reduce_sum(out=self.wk[:, tile, k_:k_ + 1], in_=r2[:, 80:112], axis=AX.X),
                         [Br2], [self.B_dest[tile]])
                xn_t, bxn_t = h2k[sl], Bh2k[sl]
                self.tt(xn_t[:], rings[sl][4][:], a2b[:], ALU.mult, [rings[sl][7], Ba2], [bxn_t])
                self.tt(xn_t[:], xn_t[:], sh2bb[:], ALU.add, [bxn_t, Ba2], [bxn_t])
                for k_ in range(2):
                    self.cp("dve", self.six[sl][:, k_:k_ + 1], self.desti[:, tile, k_:k_ + 1], [self.B_dest[tile]], [self.B_six[sl][k_]])
                    S.op("pool", lambda e, k_=k_, ix=self.six[sl], xn_t=xn_t: e.indirect_dma_start(
                        out=D_["XD"], out_offset=bass.IndirectOffsetOnAxis(ap=ix[:, k_:k_ + 1], axis=0),
                        in_=xn_t[:], in_offset=None),
                        [bxn_t, self.B_six[sl][k_]], [Buf("sc")], dbuf=bxn_t)
        if self.debug:
            self.dma("sp", D_["WF"], self.wfull[:].rearrange("p a b -> p (a b)"), self.B_wfull, [Buf("x")], self.B_wfull[0])
        sb.pop()
        self.release(Bwo + Bmg[:1] + Bx + Bx1 + Bh2 + [Ba2])

    def stage5(self):
        S, sb, I = self.S, self.sb, self.I
        D_ = self.dram
        sb.push()
        w1 = [sb.alloc("w1", [128, KC, 512], BF16) for _ in range(2)]
        w3 = [sb.alloc("w3", [128, KC, 512], BF16) for _ in range(2)]
        w2 = sb.alloc("w2", [128, 4, D], BF16)
        Bw1 = [self.dbuf("w1", "pool") for _ in range(2)]
        Bw3 = [self.dbuf("w3", "pool") for _ in range(2)]
        Bw2 = self.dbuf("w2", "pool")
        nrt = CAP // 128
        xr = [sb.alloc("xr", [128, D], BF16) for _ in range(nrt)]
        Bxr = [self.dbuf("xr") for _ in range(nrt)]
        XT = [sb.alloc("XT", [128, KC, CAP], BF16) for _ in range(2)]
        BXT = [[Buf("XT%d_%d" % (i, j)) for j in range(nrt)] for i in range(2)]
        s1 = [sb.alloc("s1", [128, CAP], F32) for _ in range(2)]
        Bs1 = [Buf("s1_%d" % i) for i in range(2)]
        hid = [sb.alloc("hid", [128, 4, CAP], BF16) for _ in range(2)]
        Bhid = [[Buf("hid%d_%d" % (i, f)) for f in range(4)] for i in range(2)]
        ys = [sb.alloc("ys", [128, D], F32) for _ in range(2)]
        Bys = [self.dbuf("ys") for _ in range(2)]
        si = [0]
        yi = [0]

        def loads(ex):
            for r_ in range(nrt):
                row0 = ex * CAP + r_ * 128
                self.dma("sp", xr[r_][:], D_["XD"][row0:row0 + 128, :], [], [Bxr[r_]], Bxr[r_])

        def trans(ex):
            sl = ex % 2
            for r_ in range(nrt):
                for tb in range(2):
                    for ts_ in range(8):
                        kc = tb * 8 + ts_
                        self.tr(self.T[tb][:, ts_ * 128:(ts_ + 1) * 128], xr[r_][:, kc * 128:(kc + 1) * 128], [Bxr[r_]], [self.BT[tb][0]])
                    src = self.T[tb][:, :].rearrange("p (k t) -> p k t", t=128)
                    dst = XT[sl][:, tb * 8:(tb + 1) * 8, r_ * 128:(r_ + 1) * 128]
                    self.cp("act" if tb == 0 else "dve", dst, src, [self.BT[tb][0]], [BXT[sl][r_]])

        loads(0)
        trans(0)
        for ex in range(32):
            sl = ex % 2
            self.dma("pool", w1[sl][:], I["w1"][ex].rearrange("(k p) n -> p k n", p=128), [], [Bw1[sl]], Bw1[sl])
            self.dma("pool", w3[sl][:], I["w3"][ex].rearrange("(k p) n -> p k n", p=128), [], [Bw3[sl]], Bw3[sl])
            self.dma("pool", w2[:], I["w2"][ex].rearrange("(k p) n -> p k n", p=128), [], [Bw2], Bw2)
            if ex + 1 < 32:
                loads(ex + 1)
            for fc in range(4):
                fs = slice(fc * 128, (fc + 1) * 128)
                f1, f3 = (fc % 2) * 2, (fc % 2) * 2 + 1
                for kc in range(KC):
                    self.mm(self.F[f1][:, 0:CAP], w1[sl][:, kc, fs], XT[sl][:, kc, :], kc == 0, kc == KC - 1, [Bw1[sl]] + BXT[sl], [self.BF[f1]])
                for kc in range(KC):
                    self.mm(self.F[f3][:, 0:CAP], w3[sl][:, kc, fs], XT[sl][:, kc, :], kc == 0, kc == KC - 1, [Bw3[sl]] + BXT[sl], [self.BF[f3]])
                i = si[0] % 2
                si[0] += 1
                self.act(s1[i][:], self.F[f1][:, 0:CAP], AF.Silu, [self.BF[f1]], [Bs1[i]])
                self.tt(hid[sl][:, fc, :], s1[i][:], self.F[f3][:, 0:CAP], ALU.mult, [Bs1[i], self.BF[f3]], [Bhid[sl][fc]])
            if ex + 1 < 32:
                trans(ex + 1)
            for r_ in range(nrt):
                y_ = ys[yi[0] % 2]
                by = Bys[yi[0] % 2]
                yi[0] += 1
                for cg in range(4):
                    cs_ = slice(cg * 512, (cg + 1) * 512)
                    fb = 4 + (cg % 2)
                    for fc in range(4):
                        self.mm(self.F[fb][:, :], hid[sl][:, fc, r_ * 128:(r_ + 1) * 128], w2[:, fc, cs_], fc == 0, fc == 3,
                                [Bhid[sl][fc], Bw2], [self.BF[fb]])
                    if fb == 4:
                        self.cp("act", y_[:, cs_], self.F[fb][:, :], [self.BF[fb]], [by])
                    else:
                        self.cp("dve", y_[:, cs_], self.F[fb][:, :], [self.BF[fb]], [by])
                row0 = ex * CAP + r_ * 128
                for hf in range(2):
                    self.dma("sp", D_["YD%d" % hf][row0:row0 + 128, :], y_[:, hf * 1024:(hf + 1) * 1024], [by], [Buf("y")], by)
        sb.pop()
        self.release(Bw1 + Bw3 + [Bw2] + Bxr + Bys)
        self.bar()
        sb.push()
        xs = [sb.alloc("xs", [128, D], F32) for _ in range(2)]
        Bx = [self.dbuf("xs") for _ in range(2)]
        yg = [[[sb.alloc("yg", [128, 1024], F32) for _ in range(2)] for _ in range(2)] for _ in range(2)]
        Byg = [[[Buf("yg") for _ in range(2)] for _ in range(2)] for _ in range(2)]
        jk = sb.alloc("jk5", [128, D], BF16)
        st5 = sb.alloc("st5", [128, 4], F32)
        Bst = Buf("st5")
        for tile in range(16):
            sl = tile % 2
            self.dma("sp", xs[sl][:], D_["X1"][tile * 128:(tile + 1) * 128, :], self.scr_reads("X1", [tile]), [Bx[sl]], Bx[sl])
            for k_ in range(2):
                self.cp("dve", self.gix[sl][:, k_:k_ + 1], self.desti[:, tile, k_:k_ + 1], [self.B_dest[tile]], [self.B_gix[sl][k_]])
                for hf in range(2):
                    S.op("pool", lambda e, k_=k_, ix=self.gix[sl], dst=yg[sl][k_][hf], hf=hf: e.indirect_dma_start(
                        out=dst[:], out_offset=None, in_=D_["YD%d" % hf],
                        in_offset=bass.IndirectOffsetOnAxis(ap=ix[:, k_:k_ + 1], axis=0)),
                        [self.B_gix[sl][k_]], [Byg[sl][k_][hf]], dbuf=Byg[sl][k_][hf])
            for hf in range(2):
                a, b = yg[sl][0][hf], yg[sl][1][hf]
                ba, bb = Byg[sl][0][hf], Byg[sl][1][hf]
                hs_ = slice(hf * 1024, (hf + 1) * 1024)
                self.ts(a[:], a[:], self.wk[:, tile, 0:1], None, ALU.mult, None, [ba, self.B_dest[tile]], [ba])
                self.stt(a[:], b[:], self.wk[:, tile, 1:2], a[:], ALU.mult, ALU.add, [bb, ba, self.B_dest[tile]], [ba])
                self.tt(a[:], a[:], self.gt2b[:, hs_], ALU.mult, [ba, self.B_c0], [ba])
                self.tt(xs[sl][:, hs_], xs[sl][:, hs_], a[:], ALU.add, [Bx[sl], ba], [Bx[sl]])
            self.act(jk[:], xs[sl][:], AF.Square, [Bx[sl]], [Bst], accum=st5[:, 0:1])
            self.rstd(st5[:, 2:3], st5[:, 0:1], float(D), [Bst], [Bst, Bst], st5[:, 1:2])
            self.stt(xs[sl][:], xs[sl][:], st5[:, 2:3], self.fgb[:], ALU.mult, ALU.mult, [Bx[sl], Bst, self.B_c0], [Bx[sl]])
            self.dma("sp", self.out[tile * 128:(tile + 1) * 128, :], xs[sl][:], [Bx[sl]], [Buf("o")], Bx[sl])
        sb.pop()
        self.release(Bx)


def host_consts():
    bf = ml_dtypes.bfloat16
    c = {}
    c["k_ident"] = np.eye(128, dtype=np.float32)
    cm = np.zeros((128, 512), np.float32)
    for j in range(2):
        kp = np.arange(128)[:, None] + j * 128
        q = np.arange(256)[None, :]
        cm[:, j * 256:(j + 1) * 256] = np.where(kp <= q, 0.0, NEG)
    c["k_cm"] = cm
    cp = np.zeros((128, 16, 16), np.float32)
    for qt in range(16):
        cp[:, qt, 8 + qt // 2:] = NEGBIG
    c["k_cpen"] = cp.reshape(128, 256)
    sel = np.zeros((16, 16, 128), np.float32)
    for i in range(16):
        sel[i, i, :] = 1.0
    c["k_sel"] = sel.reshape(16, 2048)
    s = np.arange(128)[:, None]
    t = np.arange(128)[None, :]
    same = (s // 32) == (t // 32)
    mid = (t // 32) * 32 + 15
    wm = np.where(same & (s > mid) & (s <= t), 1.0, 0.0) - np.where(same & (s > t) & (s <= mid), 1.0, 0.0)
    wmid = np.zeros((128, 4), np.float32)
    wtot = np.zeros((128, 4), np.float32)
    for cc in range(4):
        sl = np.arange(128)
        wmid[:, cc] = ((sl // 32) == cc) & (sl <= cc * 32 + 15)
        wtot[:, cc] = (sl // 32) == cc
    c["k_wall"] = np.concatenate([wm, wmid, wtot], axis=1).astype(np.float32)
    c["k_wl"] = np.where(same & (s > t), 1.0, 0.0).astype(np.float32)
    c["k_bd"] = np.where(same & (s <= t), 1.0, 0.0).astype(np.float32)
    c["k_slt"] = np.where(s < t, 1.0, 0.0).astype(np.float32)
    c["k_ones"] = np.ones((128, 128), np.float32)
    c["k_ebase"] = np.tile((np.arange(32) * CAP).astype(np.float32)[None, :], (128, 1))
    return c


_NC_CACHE = {}


def get_nc(debug=False, stages="012345"):
    key = (debug, stages)
    if key not in _NC_CACHE:
        nc = bass.Bass("TRN2", target_bir_lowering=False)
        kb = K(nc, debug=debug, stages=stages)
        kb.build()
        _NC_CACHE[key] = (nc, kb)
    return _NC_CACHE[key]


def make_in_maps(inputs, cores=range(8)):
    f = lambda a: np.ascontiguousarray(np.asarray(a, dtype=np.float32))
    x = f(inputs["x"])
    c = f(inputs["c"])
    shared = {
        "norm1_g": f(inputs["norm1_g"])[0], "norm2_g": f(inputs["norm2_g"])[0], "final_g": f(inputs["final_g"]),
        "w_ada": f(inputs["w_ada"])[0], "b_ada": f(inputs["b_ada"])[0], "w_in": f(inputs["w_in"])[0],
        "r_lower": f(inputs["r_lower"]), "r_norm_g": f(inputs["r_norm_g"])[0],
        "w_up_a": f(inputs["w_up_a"])[0], "w_up_r": f(inputs["w_up_r"])[0], "w_out": f(inputs["w_out"])[0],
        "w_rg": f(inputs["w_rg"])[0], "b_rg": f(inputs["b_rg"])[0], "w_re": f(inputs["w_re"])[0], "b_re": f(inputs["b_re"])[0],
        "w1": f(inputs["w1"])[0], "w3": f(inputs["w3"])[0], "w2": f(inputs["w2"])[0],
    }
    shared.update(host_consts())
    maps = []
    for core in cores:
        b, half = core // 2, core % 2
        m = dict(shared)
        m["xm"] = np.ascontiguousarray(x[b, half * NT:(half + 1) * NT])
        m["xp"] = np.ascontiguousarray(x[b, 0:NT])
        m["c"] = np.ascontiguousarray(c[b])
        m["pm"] = np.full((128, 1), float(half), np.float32)
        maps.append(m)
    return maps


def kernel(**inputs):
    nc, _ = get_nc()
    maps = make_in_maps(inputs)
    res = run_bass_kernel_spmd(nc, maps, core_ids=list(range(8)))
    out = np.zeros((4, 4096, D), np.float32)
    for core in range(8):
        b, half = core // 2, core % 2
        out[b, half * NT:(half + 1) * NT] = np.asarray(res.results[core]["out"], dtype=np.float32)
    return out
```

```python
import contextlib
import numpy as np
import ml_dtypes
import concourse.bass as bass
import concourse.mybir as mybir
from concourse.bass_utils import run_bass_kernel_spmd

F32 = mybir.dt.float32
BF16 = mybir.dt.bfloat16
I32 = mybir.dt.int32
CAP = 512
NR = 32 * CAP
ALU = mybir.AluOpType
AF = mybir.ActivationFunctionType
AX = mybir.AxisListType

D = 2048
NT = 2048
KC = 16
EPS = 1e-6
NEG = -30000.0
NEGBIG = -1.0e30
ENGS = ("pe", "act", "dve", "pool", "sp")
SAME_ENGINE_SYNC = {"pe": False, "act": True, "dve": True, "pool": True, "sp": False}


class Buf:
    __slots__ = ("name", "last_w", "readers", "dsem", "dcount", "const", "last_dma", "excl")

    def __init__(self, name, const=False, excl=False):
        self.excl = excl
        self.name = name
        self.last_w = None
        self.readers = []
        self.dsem = None
        self.dcount = 0
        self.const = const
        self.last_dma = None


class Op:
    __slots__ = ("eng", "fn", "deps", "sig", "sem", "val", "dbuf")

    def __init__(self, eng, fn, dbuf=None):
        self.eng = eng
        self.fn = fn
        self.deps = []
        self.sig = dbuf is not None
        self.sem = None
        self.val = 0
        self.dbuf = dbuf


class Sched:
    def __init__(self, nc):
        self.nc = nc
        self.ops = {e: [] for e in ENGS}
        self.dma_bufs = []
        self.epoch = None
        self.all_ops = []

    def barrier(self, fn):
        o = Op("dve", fn, None)
        deps = {}
        for e in ENGS:
            if self.ops[e]:
                d = self.ops[e][-1]
                deps[id(d)] = d
        for b in self.dma_bufs:
            if b.last_dma is not None:
                deps[id(b.last_dma)] = b.last_dma
        o.deps = list(deps.values())
        self.ops["dve"].append(o)
        self.all_ops.append(o)
        self.epoch = o
        return o

    def op(self, eng, fn, reads=(), writes=(), dbuf=None, extra=()):
        o = Op(eng, fn, dbuf)
        deps = {}
        for d in extra:
            if d is not None:
                deps[id(d)] = d
        if self.epoch is not None:
            deps[id(self.epoch)] = self.epoch
        xr = [b for b in reads if b.excl]
        if xr:
            reads = [b for b in reads if not b.excl]
            writes = list(writes) + [b for b in xr if b not in writes]
        for b in reads:
            if b.last_w is not None:
                deps[id(b.last_w)] = b.last_w
        for b in writes:
            if b.last_w is not None:
                deps[id(b.last_w)] = b.last_w
            for r in b.readers:
                deps[id(r)] = r
        o.deps = list(deps.values())
        for b in writes:
            b.last_w = o
            b.readers = []
        for b in reads:
            if not b.const and b.last_w is not o:
                b.readers.append(o)
        if dbuf is not None:
            dbuf.last_dma = o
            if dbuf.dsem is None:
                dbuf.dsem = True
                self.dma_bufs.append(dbuf)
        self.ops[eng].append(o)
        self.all_ops.append(o)
        return o

    def emit(self, stack):
        nc = self.nc
        for e in ENGS:
            for o in self.ops[e]:
                keep = []
                for d in o.deps:
                    if d.dbuf is None and d.eng == o.eng and not SAME_ENGINE_SYNC[o.eng]:
                        continue
                    keep.append(d)
                    d.sig = True
                o.deps = keep
        esem = {}
        for e in ENGS:
            if any(o.sig and o.dbuf is None for o in self.ops[e]):
                esem[e] = stack.enter_context(nc.semaphore("s_" + e))
        for i, b in enumerate(self.dma_bufs):
            b.dsem = stack.enter_context(nc.semaphore("d%d" % i))
            b.dcount = 0
        cnt = {e: 0 for e in ENGS}
        for o in self.all_ops:
            if o.dbuf is not None:
                o.dbuf.dcount += 16
                o.sem = o.dbuf.dsem
                o.val = o.dbuf.dcount
            elif o.sig:
                cnt[o.eng] += 1
                o.sem = esem[o.eng]
                o.val = cnt[o.eng]
        block = stack.enter_context(nc.Block())
        handles = {"pe": block.tensor, "act": block.scalar, "dve": block.vector,
                   "pool": block.gpsimd, "sp": block.sync}
        for e in ENGS:
            ops = self.ops[e]
            if not ops:
                continue

            def body(eng, ops=ops):
                waited = {}
                for o in ops:
                    need = {}
                    for d in o.deps:
                        k = id(d.sem)
                        if d.val > need.get(k, (0, None))[0]:
                            need[k] = (d.val, d.sem)
                    for k, (v, sem) in need.items():
                        if waited.get(k, 0) >= v:
                            continue
                        waited[k] = v
                        eng.wait_ge(sem, v)
                    ins = o.fn(eng)
                    if o.sig:
                        ins.then_inc(o.sem, 16 if o.dbuf is not None else 1)

            handles[e](body)


class SbAlloc:
    def __init__(self, nc, base=17408, limit=229000):
        self.nc = nc
        self.top = base
        self.limit = limit
        self.n = 0
        self.marks = []

    def push(self):
        self.marks.append(self.top)

    def pop(self):
        self.top = self.marks.pop()

    def alloc(self, name, shape, dtype):
        sz = int(np.prod(shape[1:])) * (4 if dtype in (F32, I32) else 2)
        off = (self.top + 63) // 64 * 64
        assert off + sz <= self.limit, (name, off, sz, self.limit)
        self.top = off + sz
        self.n += 1
        return self.nc.alloc_sbuf_tensor_at("%s_%d" % (name, self.n), list(shape), dtype, offset=off)


class K:
    def __init__(self, nc, debug=False, stages="012345"):
        self.nc = nc
        self.S = Sched(nc)
        self.sb = SbAlloc(nc)
        self.debug = debug
        self.stages = stages
        self.dpool = {}
        self.bkind = {}
        self.dram = {}
        self.sbufs = {}
        self.scr_w = {}

    def din(self, name, shape, dt=F32):
        return self.nc.dram_tensor(name, list(shape), dt, kind="ExternalInput").ap()

    def dscr(self, name, shape, dt):
        kind = "ExternalOutput" if self.debug else "Internal"
        t = self.nc.dram_tensor(name, list(shape), dt, kind=kind).ap()
        self.dram[name] = t
        return t

    def dbuf(self, name, kind="sp"):
        pool = self.dpool.setdefault(kind, [])
        if pool:
            return pool.pop()
        b = Buf(name)
        self.bkind[id(b)] = kind
        return b

    def release(self, bufs):
        for b in bufs:
            self.dpool[self.bkind[id(b)]].append(b)

    def dma(self, q, out, in_, reads, writes, dbuf, **kw):
        k = self.bkind.setdefault(id(dbuf), q)
        assert k == q, (dbuf.name, k, q)
        return self.S.op(q, lambda e: e.dma_start(out=out, in_=in_, **kw), reads, writes, dbuf=dbuf)

    def mm(self, out, lhsT, rhs, start, stop, reads, writes, tp=None):
        if tp is not None:
            return self.S.op("pe", lambda e: e.matmul(out, lhsT=lhsT, rhs=rhs, start=start, stop=stop, tile_position=tp), reads, writes)
        return self.S.op("pe", lambda e: e.matmul(out, lhsT=lhsT, rhs=rhs, start=start, stop=stop), reads, writes)

    def tr(self, out, in_, reads, writes):
        idn = self.identb
        n = in_.shape[0]
        return self.S.op("pe", lambda e: e.transpose(out=out, in_=in_, identity=idn[0:n, 0:n]),
                         list(reads) + [self.B_c0], writes)

    def act(self, out, in_, func, reads, writes, scale=1.0, bias=None, accum=None):
        kw = {}
        if bias is not None:
            kw["bias"] = bias
        if accum is not None:
            kw["accum_out"] = accum
        return self.S.op("act", lambda e: e.activation(out=out, in_=in_, func=func, scale=scale, **kw), reads, writes)

    def ts(self, out, in0, s1, s2, op0, op1, reads, writes, eng="dve"):
        if s2 is None:
            return self.S.op(eng, lambda e: e.tensor_scalar(out=out, in0=in0, scalar1=s1, scalar2=None, op0=op0), reads, writes)
        return self.S.op(eng, lambda e: e.tensor_scalar(out=out, in0=in0, scalar1=s1, scalar2=s2, op0=op0, op1=op1), reads, writes)

    def tt(self, out, in0, in1, op, reads, writes, eng="dve"):
        return self.S.op(eng, lambda e: e.tensor_tensor(out=out, in0=in0, in1=in1, op=op), reads, writes)

    def stt(self, out, in0, scalar, in1, op0, op1, reads, writes, eng="dve"):
        return self.S.op(eng, lambda e: e.scalar_tensor_tensor(out=out, in0=in0, scalar=scalar, in1=in1, op0=op0, op1=op1), reads, writes)

    def cp(self, eng, out, in_, reads, writes):
        if eng == "act":
            return self.S.op("act", lambda e: e.copy(out=out, in_=in_), reads, writes)
        return self.S.op(eng, lambda e: e.tensor_copy(out=out, in_=in_), reads, writes)

    def rstd(self, out, ssq, n, reads, writes, tmp):
        eps = self.epsc
        self.act(tmp, ssq, AF.Sqrt, list(reads) + [self.B_c0], [writes[1]], scale=1.0 / n, bias=eps[:, 0:1])
        self.S.op("dve", lambda e: e.reciprocal(out=out, in_=tmp), [writes[1]], [writes[0]])

    def build(self):
        nc = self.nc
        sb = self.sb
        S = self.S
        I = {}
        I["xm"] = self.din("xm", [NT, D])
        I["xp"] = self.din("xp", [NT, D])
        I["c"] = self.din("c", [D])
        I["pm"] = self.din("pm", [128, 1])
        I["norm1_g"] = self.din("norm1_g", [D])
        I["norm2_g"] = self.din("norm2_g", [D])
        I["final_g"] = self.din("final_g", [D])
        I["w_ada"] = self.din("w_ada", [D, 6 * D])
        I["b_ada"] = self.din("b_ada", [6 * D])
        I["w_in"] = self.din("w_in", [D, 11264])
        I["r_lower"] = self.din("r_lower", [2, 1024])
        I["r_norm_g"] = self.din("r_norm_g", [128])
        I["w_up_a"] = self.din("w_up_a", [1024, D])
        I["w_up_r"] = self.din("w_up_r", [1024, D])
        I["w_out"] = self.din("w_out", [D, D])
        I["w_rg"] = self.din("w_rg", [D, 4])
        I["b_rg"] = self.din("b_rg", [4])
        I["w_re"] = self.din("w_re", [D, 32])
        I["b_re"] = self.din("b_re", [32])
        I["w1"] = self.din("w1", [32, D, 512])
        I["w3"] = self.din("w3", [32, D, 512])
        I["w2"] = self.din("w2", [32, 512, D])
        I["k_ident"] = self.din("k_ident", [128, 128])
        I["k_cm"] = self.din("k_cm", [128, 512])
        I["k_cpen"] = self.din("k_cpen", [128, 256])
        I["k_sel"] = self.din("k_sel", [16, 2048])
        I["k_wall"] = self.din("k_wall", [128, 136])
        I["k_wl"] = self.din("k_wl", [128, 128])
        I["k_bd"] = self.din("k_bd", [128, 128])
        I["k_slt"] = self.din("k_slt", [128, 128])
        I["k_ones"] = self.din("k_ones", [128, 128])
        I["k_ebase"] = self.din("k_ebase", [128, 32])
        self.I = I
        self.out = nc.dram_tensor("out", [NT, D], F32, kind="ExternalOutput").ap()
        self.dscr("MOD", [6 * D], F32)
        self.dscr("QT", [8, 128, NT], BF16)
        self.dscr("KT", [8, 128, 2 * NT], BF16)
        self.dscr("V", [2 * NT, 1024], BF16)
        self.dscr("RQT", [8, 128, NT], BF16)
        self.dscr("RF", [2 * NT, 1024], F32)
        self.dscr("RI", [2 * NT, 1024], BF16)
        self.dscr("ROG", [NT, 1024], BF16)
        self.dscr("GAT", [16, 128, NT], BF16)
        self.dscr("GRT", [16, 128, NT], BF16)
        self.dscr("MT", [16, 128, NT], BF16)
        self.dscr("X1", [NT, D], F32)
        self.dscr("H2T", [16, 128, NT], BF16)
        self.dscr("XD", [NR, D], BF16)
        self.dscr("A2R", [2, D], BF16)
        self.dscr("YD0", [NR, 1024], F32)
        self.dscr("YD1", [NR, 1024], F32)
        if self.debug:
            self.dscr("ATT", [8, 128, NT], BF16)
            self.dscr("OT", [8, 128, NT], BF16)
            self.dscr("WF", [128, 16 * 32], F32)
        self.scrB = {}

        with contextlib.ExitStack() as st:
            self.F = [st.enter_context(nc.psum_tensor("F%d" % i, [128, 512], F32)) for i in range(6)]
            self.T = [st.enter_context(nc.psum_tensor("T%d" % i, [128, 1024], BF16)) for i in range(2)]
            self.BF = [Buf("F%d" % i, excl=True) for i in range(6)]
            self.BFs = [[self.BF[i]] * 4 for i in range(6)]
            _bt = [Buf("T%d" % i, excl=True) for i in range(2)]
            self.BT = [[_bt[i]] * 8 for i in range(2)]
            self.dummy = sb.alloc("dummy", [128, 8], F32)
            self.stage0()
            self.bar()
            if "1" in self.stages:
                self.stage1()
                self.bar()
            sb.push()
            self.attT = sb.alloc("attT", [128, 8, NT], BF16)
            self.oT = sb.alloc("oT", [128, 8, NT], BF16)
            self.B_attT = [Buf("attT%d" % h) for h in range(8)]
            self.B_oT = [Buf("oT%d" % t) for t in range(16)]
            if "2" in self.stages:
                self.stage2()
                self.bar()
            if "3" in self.stages:
                self.stage3()
                self.bar()
            if "4" in self.stages:
                self.stage4a()
                self.bar()
            sb.pop()
            if "4" in self.stages:
                self.stage4b()
                self.bar()
            if "5" in self.stages:
                self.stage5()
            allb = [b for b in self.S.dma_bufs]
            S.op("sp", lambda e: e.nop(), [], allb)
            S.emit(st)
        return nc

    def bar(self):
        d = self.dummy
        self.S.barrier(lambda e: e.memset(d[:], 0.0))

    def sB(self, name, key):
        k = (name, key)
        if k not in self.scrB:
            self.scrB[k] = Buf("%s%s" % (name, key))
        return self.scrB[k]

    def stage0(self):
        S, sb, I = self.S, self.sb, self.I
        cbs = {"sp": [Buf("c%d" % i) for i in range(4)], "pool": [Buf("cp%d" % i) for i in range(2)]}
        cb = cbs["sp"] + cbs["pool"]
        self.B_c0 = Buf("constall")
        ci = [0]

        def cload(q, dst, src, **kw):
            lst = cbs[q]
            b = lst[ci[0] % len(lst)]
            ci[0] += 1
            self.dma(q, dst, src, [], [b], b, **kw)

        def A(name, shape, dt=F32):
            t = sb.alloc(name, shape, dt)
            return t

        self.identb = A("identb", [128, 128], BF16)
        self.ident32 = A("ident32", [128, 128])
        self.rows = A("rows", [48, 128])
        self.cm = A("cm", [128, 512], BF16)
        self.cpen = A("cpen", [128, 256])
        self.selc = A("selc", [16, 2048], BF16)
        self.wall = A("wall", [128, 136])
        self.wl = A("wl", [128, 128])
        self.bd = A("bd", [128, 128])
        self.epsc = A("epsc", [128, 1])
        self.pm = A("pm", [128, 1])
        self.ppen = A("ppen", [128, 1])
        self.g1c = A("g1c", [128, 16])
        self.g2c = A("g2c", [128, 16])
        self.ccol = A("ccol", [128, 16])
        self.cact = A("cact", [128, 16], BF16)
        self.modc = A("modc", [128, 6, 16])
        self.a1 = A("a1", [128, 16])
        self.a2 = A("a2", [128, 16])
        self.gt1b = A("gt1b", [128, D])
        self.gt2b = A("gt2b", [128, D])
        self.fgb = A("fgb", [128, D])
        self.lb = A("lb", [128, 1024])
        self.oml = A("oml", [128, 1024])
        self.rngb = A("rngb", [128, 128])
        self.wr = A("wr", [128, 16, 36], BF16)
        self.br = A("br", [128, 36])
        self.wfull = A("wfull", [128, 16, 32])
        self.slt = A("slt", [128, 128], BF16)
        self.ones = A("ones", [128, 128], BF16)
        self.ebase = A("ebase", [128, 32])
        self.tot = A("tot", [128, 32])
        self.desti = A("desti", [128, 16, 2], I32)
        self.wk = A("wk", [128, 16, 2])
        self.B_tot = Buf("tot")
        self.B_ind = Buf("ind")
        self.six = [A("six", [128, 2], I32) for _ in range(2)]
        self.gix = [A("gix", [128, 2], I32) for _ in range(2)]
        self.B_six = [[Buf("six%d_%d" % (i, k)) for k in range(2)] for i in range(2)]
        self.B_gix = [[Buf("gix%d_%d" % (i, k)) for k in range(2)] for i in range(2)]
        self.B_dest = [Buf("dest%d" % t) for t in range(16)]
        self.B_wfull = [Buf("wfull%d" % t) for t in range(16)]
        cload("pool", self.identb[:], I["k_ident"])
        cload("sp", self.ident32[:], I["k_ident"])
        cload("pool", self.cm[:], I["k_cm"])
        cload("sp", self.cpen[:], I["k_cpen"])
        cload("pool", self.selc[:], I["k_sel"])
        cload("sp", self.wall[:], I["k_wall"])
        cload("sp", self.wl[:], I["k_wl"])
        cload("sp", self.bd[:], I["k_bd"])
        cload("sp", self.pm[:], I["pm"])
        cload("sp", self.rows[0:16, :], I["norm1_g"].rearrange("(k p) -> k p", p=128))
        cload("sp", self.rows[16:32, :], I["norm2_g"].rearrange("(k p) -> k p", p=128))
        cload("sp", self.rows[32:48, :], I["c"].rearrange("(k p) -> k p", p=128))
        cload("sp", self.fgb[:], I["final_g"].partition_broadcast(128))
        cload("sp", self.lb[:], I["r_lower"][0, :].partition_broadcast(128))
        cload("sp", self.oml[:], I["r_lower"][1, :].partition_broadcast(128))
        cload("sp", self.rngb[:], I["r_norm_g"].partition_broadcast(128))
        cload("pool", self.wr[:, :, 0:4], I["w_rg"].rearrange("(k p) n -> p k n", p=128))
        cload("pool", self.wr[:, :, 4:36], I["w_re"].rearrange("(k p) n -> p k n", p=128))
        cload("pool", self.slt[:], I["k_slt"])
        cload("pool", self.ones[:], I["k_ones"])
        cload("sp", self.ebase[:], I["k_ebase"])
        cload("sp", self.br[:, 0:4], I["b_rg"].partition_broadcast(128))
        cload("sp", self.br[:, 4:36], I["b_re"].partition_broadcast(128))
        cs = Buf("cs")
        S.op("dve", lambda e: e.memset(self.epsc[:], EPS), [], [cs])
        S.op("pe", lambda e: e.transpose(out=self.F[2][:, 0:48], in_=self.rows[:, :], identity=self.ident32[0:48, 0:48]), cb, [self.BF[2]])
        self.cp("dve", self.g1c[:], self.F[2][:, 0:16], [self.BF[2]], [cs])
        self.cp("dve", self.g2c[:], self.F[2][:, 16:32], [self.BF[2]], [cs])
        self.cp("dve", self.ccol[:], self.F[2][:, 32:48], [self.BF[2]], [cs])
        S.op("dve", lambda e: e.memset(self.tot[:], 0.0), [], [self.B_tot])
        self.ts(self.ppen[:], self.pm[:], -1.0, -NEGBIG, ALU.add, ALU.mult, cb, [cs])
        self.tt(self.lb[:], self.lb[:], self.oml[:], ALU.subtract, cb, [cs])
        self.act(self.lb[:], self.lb[:], AF.Sigmoid, [cs], [cs])
        self.ts(self.oml[:], self.lb[:], -1.0, 1.0, ALU.mult, ALU.add, [cs], [cs])
        self.act(self.cact[:], self.ccol[:], AF.Silu, [cs], [cs])
        sb.push()
        MOD = self.dram["MOD"]
        wsl = [sb.alloc("wsl0", [128, KC, 512], BF16) for _ in range(3)]
        Bw = [self.dbuf("wsl", "pool") for _ in range(3)]
        bsl = [sb.alloc("bsl", [1, 512], F32) for _ in range(2)]
        Bb = [self.dbuf("bsl") for _ in range(2)]
        msl = [sb.alloc("msl", [1, 512], F32) for _ in range(2)]
        Bm = [self.dbuf("msl") for _ in range(2)]
        wsrc = I["w_ada"].rearrange("(k p) n -> p k n", p=128)
        Bmod = Buf("MOD")
        for j in range(24):
            w = wsl[j % 3]
            bw = Bw[j % 3]
            self.dma("pool", w[:], wsrc[:, :, j * 512:(j + 1) * 512], [], [bw], bw)
            bb = Bb[j % 2]
            self.dma("sp", bsl[j % 2][:], I["b_ada"][j * 512:(j + 1) * 512].rearrange("(o n) -> o n", o=1), [], [bb], bb)
            pf = self.F[j % 2]
            bpf = self.BF[j % 2]
            for kc in range(KC):
                self.mm(pf[0:1, :], self.cact[:, kc:kc + 1], w[:, kc, :], kc == 0, kc == KC - 1, [bw, cs], [bpf])
            bm = Bm[j % 2]
            self.tt(msl[j % 2][:], pf[0:1, :], bsl[j % 2][:], ALU.add, [bpf, bb], [bm])
            self.dma("sp", MOD[j * 512:(j + 1) * 512].rearrange("(o n) -> o n", o=1), msl[j % 2][:], [bm], [Bmod], bm)
        sb.pop()
        self.release(Bw + Bb + Bm)
        bmc = self.dbuf("modc")
        modr = sb.alloc("modr", [96, 128], F32)
        self.dma("sp", modr[:], MOD.rearrange("(r p) -> r p", p=128), [Bmod], [bmc], bmc)
        S.op("pe", lambda e: e.transpose(out=self.F[3][:, 0:96], in_=modr[:, :], identity=self.ident32[0:96, 0:96]), [bmc] + cb, [self.BF[3]])
        self.cp("dve", self.modc[:].rearrange("p s k -> p (s k)"), self.F[3][:, 0:96], [self.BF[3]], [cs])
        bg1 = self.dbuf("gt1b")
        self.dma("sp", self.gt1b[:], MOD[2 * D:3 * D].partition_broadcast(128), [Bmod], [bg1], bg1)
        bg2 = self.dbuf("gt2b")
        self.dma("sp", self.gt2b[:], MOD[5 * D:6 * D].partition_broadcast(128), [Bmod], [bg2], bg2)
        sb.push()
        g2b = sb.alloc("g2b", [128, D], F32)
        sc2b = sb.alloc("sc2b", [128, D], F32)
        sh2b = sb.alloc("sh2b", [128, D], F32)
        bq1, bq2, bq3 = self.dbuf("g2b"), self.dbuf("sc2b"), self.dbuf("sh2b")
        self.dma("sp", g2b[:], I["norm2_g"].partition_broadcast(128), [], [bq1], bq1)
        self.dma("sp", sc2b[:], MOD[4 * D:5 * D].partition_broadcast(128), [Bmod], [bq2], bq2)
        self.dma("sp", sh2b[:], MOD[3 * D:4 * D].partition_broadcast(128), [Bmod], [bq3], bq3)
        a2t = sb.alloc("a2t", [128, D], BF16)
        sh2t = sb.alloc("sh2t", [128, D], BF16)
        bq4, bq5 = self.dbuf("a2t"), self.dbuf("sh2t")
        self.stt(a2t[:], sc2b[:], 1.0, g2b[:], ALU.add, ALU.mult, [bq1, bq2], [bq4])
        self.cp("dve", sh2t[:], sh2b[:], [bq3], [bq5])
        self.B_a2r = Buf("A2R")
        self.dma("sp", self.dram["A2R"][0:1, :], a2t[0:1, :], [bq4], [self.B_a2r], bq4)
        self.dma("sp", self.dram["A2R"][1:2, :], sh2t[0:1, :], [bq5], [self.B_a2r], bq5)
        sb.pop()
        self.release([bq1, bq2, bq3, bq4, bq5])
        self.stt(self.a1[:], self.modc[:, 1, :], 1.0, self.g1c[:], ALU.add, ALU.mult, [cs], [cs])
        self.stt(self.a2[:], self.modc[:, 4, :], 1.0, self.g2c[:], ALU.add, ALU.mult, [cs], [cs])
        S.op("dve", lambda e: e.memset(self.epsc[:], EPS), cb + [cs, bmc, bg1, bg2], [self.B_c0])
        self.B_c0.const = True
        self.B_modc = bmc

    def norm_to_hT(self, xt, bx, hT_dst, bh, a_col, sh_col, ring):
        junk, ssq, tmp, rs, xn, bj, bs, bxn = ring
        self.act(junk[:], xt, AF.Square, [bx], [bj, bs], accum=ssq[:, 0:1])
        self.rstd(rs[:, 0:1], ssq[:, 0:1], float(D), [bs], [bs, bj], tmp[:, 0:1])
        yield
        self.ts(xn[:], xt, rs[:, 0:1], None, ALU.mult, None, [bx, bs], [bxn])
        yield
        for tb in range(2):
            for ts_ in range(8):
                kc = tb * 8 + ts_
                self.tr(self.T[tb][:, ts_ * 128:(ts_ + 1) * 128], xn[:, kc * 128:(kc + 1) * 128], [bxn], [self.BT[tb][0]])
            yield
            for ts_ in range(8):
                kc = tb * 8 + ts_
                pt = self.T[tb][:, ts_ * 128:(ts_ + 1) * 128]
                if tb == 0:
                    self.act(hT_dst[:, kc, :], pt, AF.Identity, [self.BT[tb][0], self.B_c0], [bh], scale=a_col[:, kc:kc + 1], bias=sh_col[:, kc:kc + 1])
                else:
                    self.ts(hT_dst[:, kc, :], pt, a_col[:, kc:kc + 1], sh_col[:, kc:kc + 1], ALU.mult, ALU.add, [self.BT[tb][0], self.B_c0], [bh])
                if ts_ % 2 == 1:
                    yield

    def norm_ring(self, sb, junk=None):
        if junk is None:
            junk = sb.alloc("junk", [128, D], BF16)
        ssq = sb.alloc("ssq", [128, 1], F32)
        tmp = sb.alloc("tmp", [128, 1], F32)
        rs = sb.alloc("rs", [128, 1], F32)
        xn = sb.alloc("xn", [128, D], BF16)
        return (junk, ssq, tmp, rs, xn, Buf("junk"), Buf("ssq"), Buf("xn"))

    def stage1(self):
        S, sb, I = self.S, self.sb, self.I
        sb.push()
        G = 1024
        hT = [sb.alloc("hT", [128, KC, G], BF16) for _ in range(2)]
        BhT = [[Buf("hT%d_%d" % (s, t)) for t in range(8)] for s in range(2)]
        wsl = [sb.alloc("wsl", [128, KC, 512], BF16) for _ in range(3)]
        Bw = [self.dbuf("wsl", "pool") for _ in range(3)]
        xs = [sb.alloc("xs", [128, D], F32) for _ in range(2)]
        Bx = [self.dbuf("xs") for _ in range(2)]
        rings = [self.norm_ring(sb)]
        rings.append(self.norm_ring(sb, junk=rings[0][0]))
        stg = []
        stgb = []
        for _ in range(4):
            sb.push()
            stgb.append(sb.alloc("stgb", [128, 512], BF16))
            sb.pop()
            stg.append(sb.alloc("stg", [128, 512], F32))
        Bs = [self.dbuf("stg") for _ in range(4)]
        wsrc = I["w_in"].rearrange("(k p) n -> p k n", p=128)
        D_ = self.dram
        zt = sb.alloc("zt", [128, 1024], BF16)
        Bz = self.dbuf("zt")
        S.op("dve", lambda e: e.memset(zt[:], 0.0), [], [Bz])
        for i in range(NR // 128):
            for hf in range(2):
                self.dma("sp", D_["XD"][i * 128:(i + 1) * 128, hf * 1024:(hf + 1) * 1024], zt[:], [Bz], [Buf("z")], Bz)
        groups = [("p", 0), ("p", 1), ("m", 0), ("m", 1)]
        pblocks = [2, 3, 4, 5, 8, 9, 10, 11]
        import os
        if os.environ.get("S1G"):
            groups = groups[:int(os.environ["S1G"])]
        if os.environ.get("S1B"):
            pblocks = pblocks[:int(os.environ["S1B"])]
        wi = [0]
        si = [0]
        xi = [0]
        def hT_gen(gi):
            kind, gidx = groups[gi]
            slot = gi % 2
            src = I["xp"] if kind == "p" else I["xm"]
            for t in range(8):
                xsl = xs[xi[0] % 2]
                bx = Bx[xi[0] % 2]
                ring = rings[xi[0] % 2]
                xi[0] += 1
                r0 = gidx * G + t * 128
                self.dma("sp", xsl[:], src[r0:r0 + 128, :], [], [bx], bx)
                yield
                for _ in self.norm_to_hT(xsl[:], bx, hT[slot][:, :, t * 128:(t + 1) * 128], BhT[slot][t], self.a1, self.modc[:, 0, :], ring):
                    yield

        for _ in hT_gen(0):
            pass
        for gi, (kind, gidx) in enumerate(groups):
            slot = gi % 2
            ltok0 = gidx * G + (0 if kind == "p" else NT)
            mtok0 = gidx * G
            nxt = hT_gen(gi + 1) if gi + 1 < len(groups) else iter(())
            blocks = pblocks if kind == "p" else list(range(22))
            nstep = -(-8 * 16 // (len(blocks) * 8)) + 1
            for blk in blocks:
                w = wsl[wi[0] % 3]
                bw = Bw[wi[0] % 3]
                wi[0] += 1
                self.dma("pool", w[:], wsrc[:, :, blk * 512:(blk + 1) * 512], [], [bw], bw)
                fm = blk in (0, 1, 2, 3, 6, 7) or blk >= 14
                for u in range(8):
                    fb = u % 4
                    pf = self.F[fb]
                    bpf = self.BF[fb]
                    if fm:
                        cs_, th = u // 2, u % 2
                        for kc in range(KC):
                            self.mm(pf[:, :], w[:, kc, cs_ * 128:(cs_ + 1) * 128], hT[slot][:, kc, th * 512:(th + 1) * 512],
                                    kc == 0, kc == KC - 1, [bw] + BhT[slot][th * 4:(th + 1) * 4], [bpf])
                    else:
                        for kc in range(KC):
                            self.mm(pf[:, :], hT[slot][:, kc, u * 128:(u + 1) * 128], w[:, kc, :],
                                    kc == 0, kc == KC - 1, [bw, BhT[slot][u]], [bpf])
                    sg = stg[si[0] % 4]
                    sgb = stgb[si[0] % 4]
                    bs = Bs[si[0] % 4]
                    si[0] += 1
                    if fm:
                        if blk < 2:
                            dst = D_["QT"][blk * 4 + cs_, :, mtok0 + th * 512: mtok0 + (th + 1) * 512]
                            key = ("QT", blk * 4 + cs_)
                            fn = AF.Copy
                        elif blk < 4:
                            dst = D_["KT"][(blk - 2) * 4 + cs_, :, ltok0 + th * 512: ltok0 + (th + 1) * 512]
                            key = ("KT", (blk - 2) * 4 + cs_)
                            fn = AF.Copy
                        elif blk < 8:
                            dst = D_["RQT"][(blk - 6) * 4 + cs_, :, mtok0 + th * 512: mtok0 + (th + 1) * 512]
                            key = ("RQT", (mtok0 + th * 512) // 128)
                            fn = AF.Copy
                        elif blk < 18:
                            dst = D_["GAT"][(blk - 14) * 4 + cs_, :, mtok0 + th * 512: mtok0 + (th + 1) * 512]
                            key = ("GAT", (blk - 14) * 4 + cs_)
                            fn = AF.Sigmoid
                        else:
                            dst = D_["GRT"][(blk - 18) * 4 + cs_, :, mtok0 + th * 512: mtok0 + (th + 1) * 512]
                            key = ("GRT", (blk - 18) * 4 + cs_)
                            fn = AF.Sigmoid
                        odt = BF16
                    else:
                        if blk < 6:
                            dst = D_["V"][ltok0 + u * 128: ltok0 + (u + 1) * 128, (blk - 4) * 512:(blk - 3) * 512]
                            key = ("V", (blk - 4) * 4)
                            fn, odt = AF.Copy, BF16
                        elif blk < 10:
                            dst = D_["RF"][ltok0 + u * 128: ltok0 + (u + 1) * 128, (blk - 8) * 512:(blk - 7) * 512]
                            key = ("RF", (ltok0 + u * 128) // 128)
                            fn, odt = AF.Copy, F32
                        elif blk < 12:
                            dst = D_["RI"][ltok0 + u * 128: ltok0 + (u + 1) * 128, (blk - 10) * 512:(blk - 9) * 512]
                            key = ("RI", (ltok0 + u * 128) // 128)
                            fn, odt = AF.Copy, BF16
                        else:
                            dst = D_["ROG"][mtok0 + u * 128: mtok0 + (u + 1) * 128, (blk - 12) * 512:(blk - 11) * 512]
                            key = ("ROG", (mtok0 + u * 128) // 128)
                            fn, odt = AF.Silu, BF16
                    so = sg[:] if odt == F32 else sgb[:]
                    if fn == AF.Copy and (u % 2 == 1):
                        self.cp("dve", so, pf[:, :], [bpf], [bs])
                    else:
                        self.act(so, pf[:, :], fn, [bpf], [bs])
                    wl_ = self.scr_w.setdefault(key, [])
                    tok = Buf("w")
                    wl_.append(tok)
                    self.dma("sp", dst, so, [bs], [tok], bs)
                    for _ in range(nstep):
                        next(nxt, None)
            for _ in nxt:
                pass
        sb.pop()
        self.release(Bw + Bx + Bs + [Bz])

    def scr_reads(self, name, keys):
        out = []
        for k in keys:
            out.extend(self.scr_w.get((name, k), []))
        return out

    def stage2(self):
        S, sb = self.S, self.sb
        D_ = self.dram
        sb.push()
        QTh = [sb.alloc("QTh", [128, NT], BF16) for _ in range(2)]
        KTh = [sb.alloc("KTh", [128, 2 * NT], BF16) for _ in range(2)]
        Vh = [sb.alloc("Vh", [128, 32, 129], BF16) for _ in range(2)]
        Bq = [self.dbuf("q") for _ in range(2)]
        Bk = [self.dbuf("k") for _ in range(2)]
        Bv = [self.dbuf("v") for _ in range(2)]
        km32 = [sb.alloc("km32", [128, 16], F32) for _ in range(2)]
        kmb = [sb.alloc("kmb", [128, 16], BF16) for _ in range(2)]
        gm = [sb.alloc("gm", [128, 16, 16], F32) for _ in range(2)]
        top8 = [sb.alloc("top8", [128, 16, 8], F32) for _ in range(2)]
        pen = [sb.alloc("pen", [128, 16, 16], F32) for _ in range(2)]
        penb = [sb.alloc("penb", [128, 256], BF16) for _ in range(2)]
        penT = [sb.alloc("penT", [16, NT], BF16) for _ in range(2)]
        Bkm = [Buf("km%d" % i) for i in range(2)]
        Bgm = [Buf("gm%d" % i) for i in range(2)]
        Bpen = [Buf("pen%d" % i) for i in range(2)]
        BpenT = [Buf("penT%d" % i) for i in range(2)]
        pT = [sb.alloc("pT", [128, 256], BF16) for _ in range(4)]
        BpT = [Buf("pT%d" % i) for i in range(4)]
        rsum = [sb.alloc("rsum", [128, 1], F32) for _ in range(2)]
        atts = [sb.alloc("atts", [128, 128], BF16) for _ in range(2)]
        Brs = [Buf("rsum%d" % i) for i in range(2)]
        Bat = [Buf("atts%d" % i) for i in range(2)]
        scale = 1.0 / np.sqrt(128.0)
        pi = [0]
        ei = [0]
        for sl in range(2):
            S.op("dve", lambda e, t=Vh[sl]: e.memset(t[:, :, 128:129], 1.0), [], [Bv[sl]])
        def preamble(h):
            sl = h % 2
            q, k, v = QTh[sl], KTh[sl], Vh[sl]
            bq, bk, bv = Bq[sl], Bk[sl], Bv[sl]
            self.dma("sp", q[:], D_["QT"][h], self.scr_reads("QT", [h]), [bq], bq)
            self.dma("sp", k[:], D_["KT"][h], self.scr_reads("KT", [h]), [bk], bk)
            self.dma("sp", v[:, :, 0:128], D_["V"].rearrange("(t p) c -> p t c", p=128)[:, :, h * 128:(h + 1) * 128],
                     self.scr_reads("V", [(h // 4) * 4]), [bv], bv)
            yield
            S.op("dve", lambda e, k=k: e.tensor_reduce(out=km32[sl][:], in_=k[:].rearrange("p (b l) -> p b l", l=256), axis=AX.X, op=ALU.add),
                 [bk], [Bkm[sl]])
            yield
            self.ts(kmb[sl][:], km32[sl][:], 1.0 / 256, None, ALU.mult, None, [Bkm[sl]], [Bkm[sl]])
            yield
            pg = self.F[5]
            bpg = self.BF[5]
            for qt in range(16):
                self.mm(pg[:, qt * 16:(qt + 1) * 16], q[:, qt * 128:(qt + 1) * 128], kmb[sl][:, :], True, True, [bq, Bkm[sl]], [bpg])
            yield
            gmf = gm[sl][:].rearrange("p a b -> p (a b)")
            self.tt(gmf, pg[:, 0:256], self.cpen[:], ALU.add, [bpg, self.B_c0], [Bgm[sl]])
            yield
            self.ts(gm[sl][:, :, 0:8], gm[sl][:, :, 0:8], self.ppen[:, 0:1], None, ALU.add, None, [Bgm[sl], self.B_c0], [Bgm[sl]])
            yield
            for qt in range(16):
                S.op("dve", lambda e, qt=qt: e.max(out=top8[sl][:, qt, :], in_=gm[sl][:, qt, :]), [Bgm[sl]], [Bpen[sl]])
                yield
            for qt in range(16):
                self.ts(pen[sl][:, qt, :], gm[sl][:, qt, :], top8[sl][:, qt, 2:3], None, ALU.is_ge, None, [Bgm[sl], Bpen[sl]], [Bpen[sl]])
                yield
            penf = pen[sl][:].rearrange("p a b -> p (a b)")
            self.ts(penf, penf, -1.0, -NEG, ALU.add, ALU.mult, [Bpen[sl]], [Bpen[sl]])
            yield
            self.ts(pen[sl][:, :, 0:8], pen[sl][:, :, 0:8], self.ppen[:, 0:1], None, ALU.add, None, [Bpen[sl], self.B_c0], [Bpen[sl]])
            yield
            self.cp("dve", penb[sl][:], penf, [Bpen[sl]], [Bpen[sl]])
            yield
            for half in range(2):
                for qt in range(half * 8, half * 8 + 8):
                    tb, ts_ = qt // 8, qt % 8
                    pt = self.T[tb][0:16, ts_ * 128:(ts_ + 1) * 128]
                    self.tr(pt, penb[sl][:, qt * 16:(qt + 1) * 16], [Bpen[sl]], [self.BT[tb][ts_]])
                yield
                for qt in range(half * 8, half * 8 + 8):
                    tb, ts_ = qt // 8, qt % 8
                    pt = self.T[tb][0:16, ts_ * 128:(ts_ + 1) * 128]
                    self.cp("act", penT[sl][:, qt * 128:(qt + 1) * 128], pt, [self.BT[tb][ts_]], [BpenT[sl]])
                yield

        for _ in preamble(0):
            pass
        for h in range(8):
            sl = h % 2
            q, k, v = QTh[sl], KTh[sl], Vh[sl]
            bq, bk, bv = Bq[sl], Bk[sl], Bv[sl]
            nxt = preamble(h + 1) if h + 1 < 8 else iter(())
            itc = [0]
            for qb in range(8):
                Bl = 8 + qb
                nkt = 2 * Bl + 2
                po = [self.F[3], self.F[4]]
                bpo = [self.BF[3], self.BF[4]]
                LAG = 2
                pend = []
                for kk in range(nkt + LAG):
                    if kk < nkt:
                        kt = kk
                        fb = kt % 3
                        ps = self.F[fb]
                        bps = self.BF[fb]
                        self.mm(ps[:, 0:256], k[:, kt * 128:(kt + 1) * 128], q[:, qb * 256:(qb + 1) * 256], True, False, [bk, bq], [bps])
                        if kt < 2 * Bl:
                            i = kt // 2
                            self.mm(ps[:, 0:256], self.selc[0:16, i * 128:(i + 1) * 128], penT[sl][0:16, qb * 256:(qb + 1) * 256],
                                    False, True, [BpenT[sl], self.B_c0], [bps])
                        else:
                            j = kt - 2 * Bl
                            self.mm(ps[:, 0:256], self.identb[:, :], self.cm[:, j * 256:(j + 1) * 256], False, True, [self.B_c0], [bps])
                        p = pT[pi[0] % 4]
                        bp = BpT[pi[0] % 4]
                        pi[0] += 1
                        self.act(p[:], ps[:, 0:256], AF.Exp, [bps], [bp], scale=scale)
                        pend.append((kt, p, bp))
                    if kk >= LAG:
                        kt, p, bp = pend.pop(0)
                        for qh in range(2):
                            self.mm(po[qh][:, 0:129], p[:, qh * 128:(qh + 1) * 128], v[:, kt, :], kt == 0, kt == nkt - 1, [bp, bv], [bpo[qh]])
                    itc[0] += 1
                    if itc[0] >= 24:
                        next(nxt, None)
                for qh in range(2):
                    e_ = ei[0] % 2
                    ei[0] += 1
                    S.op("dve", lambda e, o=rsum[e_], i=po[qh]: e.reciprocal(out=o[:, 0:1], in_=i[:, 128:129]), [bpo[qh]], [Brs[e_]])
                    self.act(atts[e_][:], po[qh][:, 0:128], AF.Copy, [bpo[qh], Brs[e_]], [Bat[e_]], scale=rsum[e_][:, 0:1])
                    qt = qb * 2 + qh
                    tb, ts_ = qt // 8, qt % 8
                    pt = self.T[tb][:, ts_ * 128:(ts_ + 1) * 128]
                    bt = self.BT[tb][ts_]
                    self.tr(pt, atts[e_][:], [Bat[e_]], [bt])
                    self.cp("dve", self.attT[:, h, qt * 128:(qt + 1) * 128], pt, [bt], [self.B_attT[h]])
            for _ in nxt:
                pass
            if self.debug:
                self.dma("sp", D_["ATT"][h], self.attT[:, h, :], [self.B_attT[h]], [Buf("x")], self.B_attT[h])
        sb.pop()
        self.release(Bq + Bk + Bv)

    def stage3(self):
        S, sb = self.S, self.sb
        D_ = self.dram
        sb.push()
        rf = [sb.alloc("rf", [128, 1024], F32) for _ in range(2)]
        ri = [sb.alloc("ri", [128, 1024], BF16) for _ in range(2)]
        rq = [sb.alloc("rq", [128, 8, 128], BF16) for _ in range(2)]
        rog = [sb.alloc("rog", [128, 1024], BF16) for _ in range(2)]
        Brf = [self.dbuf("rf") for _ in range(2)]
        Bri = [self.dbuf("ri") for _ in range(2)]
        Brq = [self.dbuf("rq") for _ in range(2)]
        Brog = [self.dbuf("rog") for _ in range(2)]
        logf = [sb.alloc("logf", [128, 1024], F32) for _ in range(2)]
        kin = [sb.alloc("kin", [128, 1024], F32) for _ in range(2)]
        Blf = [Buf("logf%d" % i) for i in range(2)]
        Bkin = [Buf("kin%d" % i) for i in range(2)]
        R = 4
        ebl = [sb.alloc("ebl", [128, 128], F32) for _ in range(R)]
        khat = [sb.alloc("khat", [128, 128], BF16) for _ in range(R)]
        eb8 = [sb.alloc("eb8", [128, 8], F32) for _ in range(R)]
        ebm = [sb.alloc("ebm", [128, 128], F32) for _ in range(R)]
        qtl = [sb.alloc("qtl", [128, 128], BF16) for _ in range(R)]
        enb = [sb.alloc("enb", [128, 128], F32) for _ in range(R)]
        ktl = [sb.alloc("ktl", [128, 128], BF16) for _ in range(R)]
        ktlT = [sb.alloc("ktlT", [128, 128], BF16) for _ in range(R)]
        khc = [sb.alloc("khc", [128, 4, 128], BF16) for _ in range(R)]
        qtlz = [sb.alloc("qtlz", [128, 4, 128], BF16) for _ in range(R)]
        Bkhc = [Buf("khc%d" % i) for i in range(R)]
        Bqz = [Buf("qtlz%d" % i) for i in range(R)]
        for i in range(R):
            S.op("dve", lambda e, t=qtlz[i]: e.memset(t[:], 0.0), [], [Bqz[i]])
        at = [sb.alloc("at", [128, 128], BF16) for _ in range(R)]
        Sb = [sb.alloc("Sb", [128, 4, 128], BF16) for _ in range(R)]
        ot = [sb.alloc("ot", [128, 128], F32) for _ in range(R)]
        of_ = [sb.alloc("of", [128, 128], BF16) for _ in range(R)]
        sq = [sb.alloc("sq", [128, 4], F32) for _ in range(R)]
        jk = [sb.alloc("jk", [128, 128], BF16) for _ in range(R)]
        names = "ebl khat eb8 ebm qtl enb ktl ktlT at ot of sq".split()
        Bn = {n: [Buf(n + str(i)) for i in range(R)] for n in names}
        BSb = [[Buf("Sb%d_%d" % (i, c)) for c in range(4)] for i in range(R)]
        St = sb.alloc("St", [128, 8, 128], F32)
        BSt = [Buf("St%d" % h) for h in range(8)]
        for h in range(8):
            S.op("dve", lambda e, h=h: e.memset(St[:, h, :], 0.0), [], [BSt[h]])
        F, BFs = self.F, self.BFs
        hi = [0]
        import os
        tl = list(range(32))
        if os.environ.get("S3T"):
            tl = [int(v) for v in os.environ["S3T"].split(",")]
        s3c = int(os.environ.get("S3C", "4"))
        s3m = int(os.environ.get("S3M", "9"))
        for tile in tl:
            main = tile >= 16
            mt = tile - 16
            sl = tile % 2
            self.dma("sp", rf[sl][:], D_["RF"][tile * 128:(tile + 1) * 128, :], self.scr_reads("RF", [tile]), [Brf[sl]], Brf[sl])
            self.dma("sp", ri[sl][:], D_["RI"][tile * 128:(tile + 1) * 128, :], self.scr_reads("RI", [tile]), [Bri[sl]], Bri[sl])
            if main:
                self.dma("sp", rq[sl][:], D_["RQT"][:, :, mt * 128:(mt + 1) * 128].rearrange("h p t -> p h t"),
                         self.scr_reads("RQT", [(mt // 4) * 4]), [Brq[sl]], Brq[sl])
                self.dma("sp", rog[sl][:], D_["ROG"][mt * 128:(mt + 1) * 128, :], self.scr_reads("ROG", [mt]), [Brog[sl]], Brog[sl])
            lf, kn = logf[sl], kin[sl]
            self.act(kn[:], rf[sl][:], AF.Sigmoid, [Brf[sl]], [Bkin[sl]])
            self.tt(kn[:], kn[:], self.oml[:], ALU.mult, [Bkin[sl], self.B_c0], [Bkin[sl]])
            self.tt(kn[:], kn[:], self.lb[:], ALU.add, [Bkin[sl], self.B_c0], [Bkin[sl]])
            self.act(lf[:], kn[:], AF.Ln, [Bkin[sl]], [Blf[sl]])
            self.ts(kn[:], kn[:], -1.0, 1.0, ALU.mult, ALU.add, [Bkin[sl]], [Bkin[sl]])
            def hbody(h, r, main=main, sl=sl, lf=lf, kn=kn, mt=mt):
                hs = slice(h * 128, (h + 1) * 128)
                s2 = h % 2
                c128 = slice(s2 * 128, (s2 + 1) * 128)
                if main:
                    bl_ps = F[0][:, c128]
                    b8_ps = F[0][:, 256 + s2 * 8:256 + (s2 + 1) * 8]
                    pdb = 5
                else:
                    s4 = h % 4
                    bl_ps = F[0][:, s4 * 128:(s4 + 1) * 128]
                    b8_ps = F[1][:, s4 * 8:(s4 + 1) * 8]
                    pdb = 5 if s4 < 2 else 3
                b8b = 0 if main else 1
                self.mm(bl_ps, self.wl[:, :], lf[:, hs], True, True, [Blf[sl], self.B_c0], [self.BF[0]])
                self.mm(b8_ps, lf[:, hs], self.wall[:, 128:136], True, True, [Blf[sl], self.B_c0], [self.BF[b8b]])
                if main:
                    self.mm(F[1][:, c128], lf[:, hs], self.wall[:, 0:128], True, True, [Blf[sl], self.B_c0], [self.BF[1]])
                    bm_ps = F[1][:, 256 + s2 * 128:256 + (s2 + 1) * 128]
                    self.mm(bm_ps, self.wall[:, 0:128], lf[:, hs], True, True, [Blf[sl], self.B_c0], [self.BF[1]])
                yield
                self.act(ebl[r][:], bl_ps, AF.Exp, [self.BF[0]], [Bn["ebl"][r]])
                self.act(eb8[r][:], b8_ps, AF.Exp, [self.BF[b8b]], [Bn["eb8"][r]])
                if main:
                    self.act(ebm[r][:], F[1][:, c128], AF.Exp, [self.BF[1]], [Bn["ebm"][r]])
                    self.act(enb[r][:], bm_ps, AF.Exp, [self.BF[1]], [Bn["enb"][r]], scale=-1.0)
                yield
                for c in range(4):
                    self.stt(khc[r][:, c, :], kn[:, hs], self.wall[:, 132 + c:133 + c], ebl[r][:], ALU.mult, ALU.mult,
                             [Bkin[sl], Bn["ebl"][r], self.B_c0], [Bkhc[r]])
                if main:
                    self.tt(ktl[r][:], kn[:, hs], enb[r][:], ALU.mult, [Bkin[sl], Bn["enb"][r]], [Bn["ktl"][r]])
                    self.tt(qtl[r][:], rq[sl][:, h, :], ebm[r][:], ALU.mult, [Brq[sl], Bn["ebm"][r]], [Bn["qtl"][r]])
                    for c in range(4):
                        cc = slice(32 * c, 32 * c + 32)
                        self.tt(qtlz[r][:, c, cc], rq[sl][:, h, cc], ebm[r][:, cc], ALU.mult, [Brq[sl], Bn["ebm"][r]], [Bqz[r]])
                yield
                pds = [F[pdb][:, (s2 * 2 + (c % 2)) * 128:(s2 * 2 + (c % 2) + 1) * 128] for c in range(4)]
                if main:
                    pt = self.T[0][:, c128]
                    self.tr(pt, ktl[r][:], [Bn["ktl"][r]], [self.BT[0][0]])
                    yield
                    self.cp("act", ktlT[r][:], pt, [self.BT[0][0]], [Bn["ktlT"][r]])
                    yield
                    self.mm(F[3][:, c128], ktlT[r][:], qtl[r][:], True, True, [Bn["ktlT"][r], Bn["qtl"][r]], [self.BF[3]])
                    yield
                    self.tt(at[r][:], F[3][:, c128], self.bd[:], ALU.mult, [self.BF[3], self.B_c0], [Bn["at"][r]])
                    yield
                pob = 4 if s2 == 0 else 2
                po = F[pob][:, 0:128]
                bpo = self.BF[pob]
                if main:
                    self.mm(po, at[r][:], ri[sl][:, hs], True, False, [Bn["at"][r], Bri[sl]], [bpo])
                for c in range(4):
                    self.mm(pds[c], khc[r][:, c, :], ri[sl][:, hs], True, True, [Bkhc[r], Bri[sl]], [self.BF[pdb]])
                    if main:
                        self.act(Sb[r][:, c, :], St[:, h, :], AF.Copy, [BSt[h], Bn["eb8"][r]], [BSb[r][c]], scale=eb8[r][:, c:c + 1])
                    yield
                    if main:
                        self.mm(po, qtlz[r][:, c, :], Sb[r][:, c, :], False, c == 3, [Bqz[r], BSb[r][c]], [bpo])
                    self.stt(St[:, h, :], St[:, h, :], eb8[r][:, 4 + c:5 + c], pds[c], ALU.mult, ALU.add, [BSt[h], Bn["eb8"][r], self.BF[pdb]], [BSt[h]])
                    yield
                if main:
                    self.act(jk[r][:], po, AF.Square, [bpo], [Bn["sq"][r]], accum=sq[r][:, 0:1])
                    yield
                    self.tt(ot[r][:], po, self.rngb[:], ALU.mult, [bpo, self.B_c0], [Bn["ot"][r]])
                    self.rstd(sq[r][:, 2:3], sq[r][:, 0:1], 128.0, [Bn["sq"][r]], [Bn["sq"][r], Bn["sq"][r]], sq[r][:, 1:2])
                    yield
                    self.stt(of_[r][:], ot[r][:], sq[r][:, 2:3], rog[sl][:, hs], ALU.mult, ALU.mult, [Bn["ot"][r], Bn["sq"][r], Brog[sl]], [Bn["of"][r]])
                    yield
                    pt = self.T[1][:, c128]
                    self.tr(pt, of_[r][:], [Bn["of"][r]], [self.BT[1][0]])
                    yield
                    self.cp("act", self.oT[:, h, mt * 128:(mt + 1) * 128], pt, [self.BT[1][0]], [self.B_oT[mt]])

            G = 2 if main else 4
            for h0 in range(0, 8, G):
                gens = []
                for h in range(h0, h0 + G):
                    gens.append(hbody(h, hi[0] % R))
                    hi[0] += 1
                while gens:
                    for g_ in list(gens):
                        try:
                            next(g_)
                        except StopIteration:
                            gens.remove(g_)
            if tile == 15:
                for h in range(8):
                    self.ts(St[:, h, :], St[:, h, :], self.pm[:, 0:1], None, ALU.mult, None, [BSt[h], self.B_c0], [BSt[h]])
        if self.debug:
            for h in range(8):
                self.dma("sp", D_["OT"][h], self.oT[:, h, :], self.B_oT, [Buf("x")], self.B_oT[0])
        sb.pop()
        self.release(Brf + Bri + Brq + Brog)

    def stage4a(self):
        S, sb, I = self.S, self.sb, self.I
        D_ = self.dram
        sb.push()
        wa = [sb.alloc("wa", [128, 8, 128], BF16) for _ in range(2)]
        wr_ = [sb.alloc("wr_", [128, 8, 128], BF16) for _ in range(2)]
        sga = [sb.alloc("sga", [128, NT], BF16) for _ in range(2)]
        sgr = [sb.alloc("sgr", [128, NT], BF16) for _ in range(2)]
        mT = [sb.alloc("mT", [128, NT], BF16) for _ in range(2)]
        t1 = [sb.alloc("t1", [128, 512], F32) for _ in range(2)]
        t2 = [sb.alloc("t2", [128, 512], F32) for _ in range(2)]
        Bwa = [self.dbuf("wa", "pool") for _ in range(2)]
        Bwr = [self.dbuf("wr", "pool") for _ in range(2)]
        Bsa = [self.dbuf("sga") for _ in range(2)]
        Bsr = [self.dbuf("sgr") for _ in range(2)]
        BmT = [self.dbuf("mT") for _ in range(2)]
        Bt1 = [Buf("t1_%d" % i) for i in range(2)]
        Bt2 = [Buf("t2_%d" % i) for i in range(2)]
        wua = I["w_up_a"].rearrange("(k p) n -> p k n", p=128)
        wur = I["w_up_r"].rearrange("(k p) n -> p k n", p=128)
        ti = [0]
        for ch in range(16):
            sl = ch % 2
            self.dma("pool", wa[sl][:], wua[:, :, ch * 128:(ch + 1) * 128], [], [Bwa[sl]], Bwa[sl])
            self.dma("pool", wr_[sl][:], wur[:, :, ch * 128:(ch + 1) * 128], [], [Bwr[sl]], Bwr[sl])
            self.dma("sp", sga[sl][:], D_["GAT"][ch], self.scr_reads("GAT", [ch]), [Bsa[sl]], Bsa[sl])
            self.dma("sp", sgr[sl][:], D_["GRT"][ch], self.scr_reads("GRT", [ch]), [Bsr[sl]], Bsr[sl])
            for g in range(4):
                gs = slice(g * 512, (g + 1) * 512)
                fa, fr = (g % 2) * 2, (g % 2) * 2 + 1
                for wc in range(8):
                    self.mm(self.F[fa][:, :], wa[sl][:, wc, :], self.attT[:, wc, gs], wc == 0, wc == 7, [Bwa[sl], self.B_attT[wc]], [self.BF[fa]])
                for wc in range(8):
                    self.mm(self.F[fr][:, :], wr_[sl][:, wc, :], self.oT[:, wc, gs], wc == 0, wc == 7,
                            [Bwr[sl]] + self.B_oT[g * 4:(g + 1) * 4], [self.BF[fr]])
                i = ti[0] % 2
                ti[0] += 1
                self.tt(t1[i][:], self.F[fa][:, :], sga[sl][:, gs], ALU.mult, [self.BF[fa], Bsa[sl]], [Bt1[i]])
                self.tt(t2[i][:], self.F[fr][:, :], sgr[sl][:, gs], ALU.mult, [self.BF[fr], Bsr[sl]], [Bt2[i]], eng="dve")
                self.tt(mT[sl][:, gs], t1[i][:], t2[i][:], ALU.add, [Bt1[i], Bt2[i]], [BmT[sl]], eng="dve")
            tok = Buf("w")
            self.scr_w.setdefault(("MT", ch), []).append(tok)
            self.dma("sp", D_["MT"][ch], mT[sl][:], [BmT[sl]], [tok], BmT[sl])
        sb.pop()
        self.release(Bwa + Bwr + Bsa + Bsr + BmT)

    def stage4b(self):
        S, sb, I = self.S, self.sb, self.I
        D_ = self.dram
        sb.push()
        wo = sb.alloc("wo", [128, KC, D], BF16)
        Bwo = [self.dbuf("wo", "pool") for _ in range(4)]
        wsrc = I["w_out"].rearrange("(k p) n -> p k n", p=128)
        for i in range(4):
            self.dma("pool", wo[:, i * 4:(i + 1) * 4, :], wsrc[:, i * 4:(i + 1) * 4, :], [], [Bwo[i]], Bwo[i])
        mTg = [sb.alloc("mTg", [128, KC, 512], BF16)] * 2
        Bmg = [self.dbuf("mTg")] * 2
        xs = [sb.alloc("xs", [128, D], F32) for _ in range(2)]
        Bx = [self.dbuf("xs") for _ in range(2)]
        x1 = [sb.alloc("x1", [128, D], F32) for _ in range(2)]
        Bx1 = [self.dbuf("x1") for _ in range(2)]
        h2 = [sb.alloc("h2", [128, KC, 128], BF16) for _ in range(2)]
        Bh2 = [self.dbuf("h2") for _ in range(2)]
        rings = [self.norm_ring(sb)]
        rings.append(self.norm_ring(sb, junk=rings[0][0]))
        tq = [sb.alloc("tq", [128, 512], F32) for _ in range(2)]
        Btq = [Buf("tq%d" % i) for i in range(2)]
        rt = sb.alloc("rt", [128, 160], F32)
        Brt = Buf("rt")
        r2 = sb.alloc("r2", [128, 112], F32)
        Br2 = Buf("r2")
        selb = sb.alloc("selb", [128, 32], BF16)
        Bselb = Buf("selb")
        h2k = [sb.alloc("h2k", [128, D], BF16) for _ in range(2)]
        Bh2k = [Buf("h2k%d" % i) for i in range(2)]
        a2b = sb.alloc("a2b", [128, D], BF16)
        sh2bb = sb.alloc("sh2bb", [128, D], BF16)
        Ba2 = self.dbuf("a2b")
        self.dma("sp", a2b[:], D_["A2R"][0, :].partition_broadcast(128), [self.B_a2r], [Ba2], Ba2)
        self.dma("sp", sh2bb[:], D_["A2R"][1, :].partition_broadcast(128), [self.B_a2r], [Ba2], Ba2)
        mtsrc = D_["MT"].rearrange("k p t -> p k t")
        qi = [0]
        for g in range(4):
            sg = g % 2
            self.dma("sp", mTg[sg][:], mtsrc[:, :, g * 512:(g + 1) * 512], self.scr_reads("MT", range(16)), [Bmg[sg]], Bmg[sg])
            for tt_ in range(4):
                tile = g * 4 + tt_
                sl = tile % 2
                self.dma("sp", xs[sl][:], I["xm"][tile * 128:(tile + 1) * 128, :], [], [Bx[sl]], Bx[sl])
                for cg in range(4):
                    cs_ = slice(cg * 512, (cg + 1) * 512)
                    fb = cg % 4
                    for kc in range(KC):
                        self.mm(self.F[fb][:, :], mTg[sg][:, kc, tt_ * 128:(tt_ + 1) * 128], wo[:, kc, cs_], kc == 0, kc == KC - 1,
                                [Bmg[sg], Bwo[kc // 4]], [self.BF[fb]])
                    i = qi[0] % 2
                    qi[0] += 1
                    self.tt(tq[i][:], self.F[fb][:, :], self.gt1b[:, cs_], ALU.mult, [self.BF[fb], self.B_c0], [Btq[i]])
                    self.tt(x1[sl][:, cs_], tq[i][:], xs[sl][:, cs_], ALU.add, [Btq[i], Bx[sl]], [Bx1[sl]], eng="dve")
                tok = Buf("w")
                self.scr_w.setdefault(("X1", tile), []).append(tok)
                self.dma("sp", D_["X1"][tile * 128:(tile + 1) * 128, :], x1[sl][:], [Bx1[sl]], [tok], Bx1[sl])
                for _ in self.norm_to_hT(x1[sl][:], Bx1[sl], h2[sl][:, :, :], Bh2[sl], self.a2, self.modc[:, 3, :], rings[sl]):
                    pass
                tok = Buf("w")
                self.scr_w.setdefault(("H2T", tile), []).append(tok)
                self.dma("sp", D_["H2T"][:, :, tile * 128:(tile + 1) * 128].rearrange("k p t -> p k t"), h2[sl][:], [Bh2[sl]], [tok], Bh2[sl])
                pr = self.F[5][:, 0:36]
                bpr = self.BF[5]
                for kc in range(KC):
                    self.mm(pr, h2[sl][:, kc, :], self.wr[:, kc, :], kc == 0, kc == KC - 1, [Bh2[sl], self.B_c0], [bpr])
                lg = rt[:, 0:36]
                self.tt(lg, pr, self.br[:], ALU.add, [bpr, self.B_c0], [Brt])
                mx = rt[:, 36:37]
                S.op("dve", lambda e: e.tensor_reduce(out=rt[:, 36:37], in_=rt[:, 0:4], axis=AX.X, op=ALU.max), [Brt], [Brt])
                self.ts(rt[:, 37:38], mx, -1.0, None, ALU.mult, None, [Brt], [Brt])
                self.act(rt[:, 40:44], rt[:, 0:4], AF.Exp, [Brt], [Brt], bias=rt[:, 37:38], accum=rt[:, 38:39])
                self.ts(rt[:, 44:48], rt[:, 0:4], mx, None, ALU.is_ge, None, [Brt], [Brt])
                self.ts(rt[:, 44:48], rt[:, 44:48], -1.0, -NEGBIG, ALU.add, ALU.mult, [Brt], [Brt])
                for gi in range(4):
                    self.ts(rt[:, 48 + gi * 8:56 + gi * 8], rt[:, 4 + gi * 8:12 + gi * 8], rt[:, 44 + gi:45 + gi], None, ALU.add, None, [Brt], [Brt])
                S.op("dve", lambda e: e.max(out=rt[:, 80:88], in_=rt[:, 48:80]), [Brt], [Brt])
                self.ts(rt[:, 88:120], rt[:, 48:80], rt[:, 81:82], None, ALU.is_ge, None, [Brt], [Brt])
                self.ts(rt[:, 39:40], rt[:, 80:81], -1.0, None, ALU.mult, None, [Brt], [Brt])
                self.act(rt[:, 120:152], rt[:, 48:80], AF.Exp, [Brt], [Brt], bias=rt[:, 39:40])
                self.tt(rt[:, 120:152], rt[:, 120:152], rt[:, 88:120], ALU.mult, [Brt], [Brt])
                S.op("dve", lambda e: e.reduce_sum(out=rt[:, 152:153], in_=rt[:, 120:152], axis=AX.X), [Brt], [Brt])
                self.tt(rt[:, 152:153], rt[:, 152:153], rt[:, 38:39], ALU.mult, [Brt], [Brt])
                S.op("dve", lambda e: e.reciprocal(out=rt[:, 153:154], in_=rt[:, 152:153]), [Brt], [Brt])
                self.ts(self.wfull[:, tile, :], rt[:, 120:152], rt[:, 153:154], None, ALU.mult, None, [Brt], [self.B_wfull[tile]])
                self.cp("dve", selb[:], rt[:, 88:120], [Brt], [Bselb])
                pc = self.F[4]
                self.mm(pc[:, 0:32], self.slt[:, :], selb[:], True, True, [Bselb, self.B_c0], [self.BF[4]])
                self.mm(pc[:, 32:64], self.ones[:, :], selb[:], True, True, [Bselb, self.B_c0], [self.BF[4]])
                self.tt(r2[:, 0:32], pc[:, 0:32], self.tot[:], ALU.add, [self.BF[4], self.B_tot], [Br2])
                self.tt(self.tot[:], pc[:, 32:64], self.tot[:], ALU.add, [self.BF[4], self.B_tot], [self.B_tot])
                self.ts(r2[:, 0:32], r2[:, 0:32], float(CAP - 1), None, ALU.min, None, [Br2], [Br2])
                self.tt(r2[:, 0:32], r2[:, 0:32], self.ebase[:], ALU.add, [Br2, self.B_c0], [Br2])
                self.stt(r2[:, 32:64], r2[:, 0:32], 1.0e6, rt[:, 88:120], ALU.add, ALU.mult, [Br2, Brt], [Br2])
                S.op("dve", lambda e: e.max(out=r2[:, 64:72], in_=r2[:, 32:64]), [Br2], [Br2])
                self.ts(r2[:, 72:74], r2[:, 64:66], -1.0e6, None, ALU.add, None, [Br2], [Br2])
                self.cp("dve", self.desti[:, tile, :], r2[:, 72:74], [Br2], [self.B_dest[tile]])
                for k_ in range(2):
                    self.ts(r2[:, 80:112], r2[:, 32:64], r2[:, 64 + k_:65 + k_], None, ALU.is_equal, None, [Br2], [Br2])
                    self.tt(r2[:, 80:112], r2[:, 80:112], self.wfull[:, tile, :], ALU.mult, [Br2, self.B_wfull[tile]], [Br2])
                    S.op("dve", lambda e, k_=k_, tile=tile: e.reduce_sum(out=self.wk[:, tile, k_:k_ + 1], in_=r2[:, 80:112], axis=AX.X),
                         [Br2], [self.B_dest[tile]])
                xn_t, bxn_t = h2k[sl], Bh2k[sl]
                self.tt(xn_t[:], rings[sl][4][:], a2b[:], ALU.mult, [rings[sl][7], Ba2], [bxn_t])
                self.tt(xn_t[:], xn_t[:], sh2bb[:], ALU.add, [bxn_t, Ba2], [bxn_t])
                for k_ in range(2):
                    self.cp("dve", self.six[sl][:, k_:k_ + 1], self.desti[:, tile, k_:k_ + 1], [self.B_dest[tile]], [self.B_six[sl][k_]])
                    S.op("pool", lambda e, k_=k_, ix=self.six[sl], xn_t=xn_t: e.indirect_dma_start(
                        out=D_["XD"], out_offset=bass.IndirectOffsetOnAxis(ap=ix[:, k_:k_ + 1], axis=0),
                        in_=xn_t[:], in_offset=None),
                        [bxn_t, self.B_six[sl][k_]], [Buf("sc")], dbuf=bxn_t)
        if self.debug:
            self.dma("sp", D_["WF"], self.wfull[:].rearrange("p a b -> p (a b)"), self.B_wfull, [Buf("x")], self.B_wfull[0])
        sb.pop()
        self.release(Bwo + Bmg[:1] + Bx + Bx1 + Bh2 + [Ba2])

    def stage5(self):
        S, sb, I = self.S, self.sb, self.I
        D_ = self.dram
        sb.push()
        w1 = [sb.alloc("w1", [128, KC, 512], BF16) for _ in range(2)]
        w3 = [sb.alloc("w3", [128, KC, 512], BF16) for _ in range(2)]
        w2 = sb.alloc("w2", [128, 4, D], BF16)
        Bw1 = [self.dbuf("w1", "pool") for _ in range(2)]
        Bw3 = [self.dbuf("w3", "pool") for _ in range(2)]
        Bw2 = self.dbuf("w2", "pool")
        nrt = CAP // 128
        xr = [sb.alloc("xr", [128, D], BF16) for _ in range(nrt)]
        Bxr = [self.dbuf("xr") for _ in range(nrt)]
        XT = [sb.alloc("XT", [128, KC, CAP], BF16) for _ in range(2)]
        BXT = [[Buf("XT%d_%d" % (i, j)) for j in range(nrt)] for i in range(2)]
        s1 = [sb.alloc("s1", [128, CAP], F32) for _ in range(2)]
        Bs1 = [Buf("s1_%d" % i) for i in range(2)]
        hid = [sb.alloc("hid", [128, 4, CAP], BF16) for _ in range(2)]
        Bhid = [[Buf("hid%d_%d" % (i, f)) for f in range(4)] for i in range(2)]
        ys = [sb.alloc("ys", [128, D], F32) for _ in range(2)]
        Bys = [self.dbuf("ys") for _ in range(2)]
        si = [0]
        yi = [0]

        def loads(ex):
            for r_ in range(nrt):
                row0 = ex * CAP + r_ * 128
                self.dma("sp", xr[r_][:], D_["XD"][row0:row0 + 128, :], [], [Bxr[r_]], Bxr[r_])

        def trans(ex):
            sl = ex % 2
            for r_ in range(nrt):
                for tb in range(2):
                    for ts_ in range(8):
                        kc = tb * 8 + ts_
                        self.tr(self.T[tb][:, ts_ * 128:(ts_ + 1) * 128], xr[r_][:, kc * 128:(kc + 1) * 128], [Bxr[r_]], [self.BT[tb][0]])
                    src = self.T[tb][:, :].rearrange("p (k t) -> p k t", t=128)
                    dst = XT[sl][:, tb * 8:(tb + 1) * 8, r_ * 128:(r_ + 1) * 128]
                    self.cp("act" if tb == 0 else "dve", dst, src, [self.BT[tb][0]], [BXT[sl][r_]])

        loads(0)
        trans(0)
        for ex in range(32):
            sl = ex % 2
            self.dma("pool", w1[sl][:], I["w1"][ex].rearrange("(k p) n -> p k n", p=128), [], [Bw1[sl]], Bw1[sl])
            self.dma("pool", w3[sl][:], I["w3"][ex].rearrange("(k p) n -> p k n", p=128), [], [Bw3[sl]], Bw3[sl])
            self.dma("pool", w2[:], I["w2"][ex].rearrange("(k p) n -> p k n", p=128), [], [Bw2], Bw2)
            if ex + 1 < 32:
                loads(ex + 1)
            for fc in range(4):
                fs = slice(fc * 128, (fc + 1) * 128)
                f1, f3 = (fc % 2) * 2, (fc % 2) * 2 + 1
                for kc in range(KC):
                    self.mm(self.F[f1][:, 0:CAP], w1[sl][:, kc, fs], XT[sl][:, kc, :], kc == 0, kc == KC - 1, [Bw1[sl]] + BXT[sl], [self.BF[f1]])
                for kc in range(KC):
                    self.mm(self.F[f3][:, 0:CAP], w3[sl][:, kc, fs], XT[sl][:, kc, :], kc == 0, kc == KC - 1, [Bw3[sl]] + BXT[sl], [self.BF[f3]])
                i = si[0] % 2
                si[0] += 1
                self.act(s1[i][:], self.F[f1][:, 0:CAP], AF.Silu, [self.BF[f1]], [Bs1[i]])
                self.tt(hid[sl][:, fc, :], s1[i][:], self.F[f3][:, 0:CAP], ALU.mult, [Bs1[i], self.BF[f3]], [Bhid[sl][fc]])
            if ex + 1 < 32:
                trans(ex + 1)
            for r_ in range(nrt):
                y_ = ys[yi[0] % 2]
                by = Bys[yi[0] % 2]
                yi[0] += 1
                for cg in range(4):
                    cs_ = slice(cg * 512, (cg + 1) * 512)
                    fb = 4 + (cg % 2)
                    for fc in range(4):
                        self.mm(self.F[fb][:, :], hid[sl][:, fc, r_ * 128:(r_ + 1) * 128], w2[:, fc, cs_], fc == 0, fc == 3,
                                [Bhid[sl][fc], Bw2], [self.BF[fb]])
                    if fb == 4:
                        self.cp("act", y_[:, cs_], self.F[fb][:, :], [self.BF[fb]], [by])
                    else:
                        self.cp("dve", y_[:, cs_], self.F[fb][:, :], [self.BF[fb]], [by])
                row0 = ex * CAP + r_ * 128
                for hf in range(2):
                    self.dma("sp", D_["YD%d" % hf][row0:row0 + 128, :], y_[:, hf * 1024:(hf + 1) * 1024], [by], [Buf("y")], by)
        sb.pop()
        self.release(Bw1 + Bw3 + [Bw2] + Bxr + Bys)
        self.bar()
        sb.push()
        xs = [sb.alloc("xs", [128, D], F32) for _ in range(2)]
        Bx = [self.dbuf("xs") for _ in range(2)]
        yg = [[[sb.alloc("yg", [128, 1024], F32) for _ in range(2)] for _ in range(2)] for _ in range(2)]
        Byg = [[[Buf("yg") for _ in range(2)] for _ in range(2)] for _ in range(2)]
        jk = sb.alloc("jk5", [128, D], BF16)
        st5 = sb.alloc("st5", [128, 4], F32)
        Bst = Buf("st5")
        for tile in range(16):
            sl = tile % 2
            self.dma("sp", xs[sl][:], D_["X1"][tile * 128:(tile + 1) * 128, :], self.scr_reads("X1", [tile]), [Bx[sl]], Bx[sl])
            for k_ in range(2):
                self.cp("dve", self.gix[sl][:, k_:k_ + 1], self.desti[:, tile, k_:k_ + 1], [self.B_dest[tile]], [self.B_gix[sl][k_]])
                for hf in range(2):
                    S.op("pool", lambda e, k_=k_, ix=self.gix[sl], dst=yg[sl][k_][hf], hf=hf: e.indirect_dma_start(
                        out=dst[:], out_offset=None, in_=D_["YD%d" % hf],
                        in_offset=bass.IndirectOffsetOnAxis(ap=ix[:, k_:k_ + 1], axis=0)),
                        [self.B_gix[sl][k_]], [Byg[sl][k_][hf]], dbuf=Byg[sl][k_][hf])
            for hf in range(2):
                a, b = yg[sl][0][hf], yg[sl][1][hf]
                ba, bb = Byg[sl][0][hf], Byg[sl][1][hf]
                hs_ = slice(hf * 1024, (hf + 1) * 1024)
                self.ts(a[:], a[:], self.wk[:, tile, 0:1], None, ALU.mult, None, [ba, self.B_dest[tile]], [ba])
                self.stt(a[:], b[:], self.wk[:, tile, 1:2], a[:], ALU.mult, ALU.add, [bb, ba, self.B_dest[tile]], [ba])
                self.tt(a[:], a[:], self.gt2b[:, hs_], ALU.mult, [ba, self.B_c0], [ba])
                self.tt(xs[sl][:, hs_], xs[sl][:, hs_], a[:], ALU.add, [Bx[sl], ba], [Bx[sl]])
            self.act(jk[:], xs[sl][:], AF.Square, [Bx[sl]], [Bst], accum=st5[:, 0:1])
            self.rstd(st5[:, 2:3], st5[:, 0:1], float(D), [Bst], [Bst, Bst], st5[:, 1:2])
            self.stt(xs[sl][:], xs[sl][:], st5[:, 2:3], self.fgb[:], ALU.mult, ALU.mult, [Bx[sl], Bst, self.B_c0], [Bx[sl]])
            self.dma("sp", self.out[tile * 128:(tile + 1) * 128, :], xs[sl][:], [Bx[sl]], [Buf("o")], Bx[sl])
        sb.pop()
        self.release(Bx)


def host_consts():
    bf = ml_dtypes.bfloat16
    c = {}
    c["k_ident"] = np.eye(128, dtype=np.float32)
    cm = np.zeros((128, 512), np.float32)
    for j in range(2):
        kp = np.arange(128)[:, None] + j * 128
        q = np.arange(256)[None, :]
        cm[:, j * 256:(j + 1) * 256] = np.where(kp <= q, 0.0, NEG)
    c["k_cm"] = cm
    cp = np.zeros((128, 16, 16), np.float32)
    for qt in range(16):
        cp[:, qt, 8 + qt // 2:] = NEGBIG
    c["k_cpen"] = cp.reshape(128, 256)
    sel = np.zeros((16, 16, 128), np.float32)
    for i in range(16):
        sel[i, i, :] = 1.0
    c["k_sel"] = sel.reshape(16, 2048)
    s = np.arange(128)[:, None]
    t = np.arange(128)[None, :]
    same = (s // 32) == (t // 32)
    mid = (t // 32) * 32 + 15
    wm = np.where(same & (s > mid) & (s <= t), 1.0, 0.0) - np.where(same & (s > t) & (s <= mid), 1.0, 0.0)
    wmid = np.zeros((128, 4), np.float32)
    wtot = np.zeros((128, 4), np.float32)
    for cc in range(4):
        sl = np.arange(128)
        wmid[:, cc] = ((sl // 32) == cc) & (sl <= cc * 32 + 15)
        wtot[:, cc] = (sl // 32) == cc
    c["k_wall"] = np.concatenate([wm, wmid, wtot], axis=1).astype(np.float32)
    c["k_wl"] = np.where(same & (s > t), 1.0, 0.0).astype(np.float32)
    c["k_bd"] = np.where(same & (s <= t), 1.0, 0.0).astype(np.float32)
    c["k_slt"] = np.where(s < t, 1.0, 0.0).astype(np.float32)
    c["k_ones"] = np.ones((128, 128), np.float32)
    c["k_ebase"] = np.tile((np.arange(32) * CAP).astype(np.float32)[None, :], (128, 1))
    return c


_NC_CACHE = {}


def get_nc(debug=False, stages="012345"):
    key = (debug, stages)
    if key not in _NC_CACHE:
        nc = bass.Bass("TRN2", target_bir_lowering=False)
        kb = K(nc, debug=debug, stages=stages)
        kb.build()
        _NC_CACHE[key] = (nc, kb)
    return _NC_CACHE[key]


def make_in_maps(inputs, cores=range(8)):
    f = lambda a: np.ascontiguousarray(np.asarray(a, dtype=np.float32))
    x = f(inputs["x"])
    c = f(inputs["c"])
    shared = {
        "norm1_g": f(inputs["norm1_g"])[0], "norm2_g": f(inputs["norm2_g"])[0], "final_g": f(inputs["final_g"]),
        "w_ada": f(inputs["w_ada"])[0], "b_ada": f(inputs["b_ada"])[0], "w_in": f(inputs["w_in"])[0],
        "r_lower": f(inputs["r_lower"]), "r_norm_g": f(inputs["r_norm_g"])[0],
        "w_up_a": f(inputs["w_up_a"])[0], "w_up_r": f(inputs["w_up_r"])[0], "w_out": f(inputs["w_out"])[0],
        "w_rg": f(inputs["w_rg"])[0], "b_rg": f(inputs["b_rg"])[0], "w_re": f(inputs["w_re"])[0], "b_re": f(inputs["b_re"])[0],
        "w1": f(inputs["w1"])[0], "w3": f(inputs["w3"])[0], "w2": f(inputs["w2"])[0],
    }
    shared.update(host_consts())
    maps = []
    for core in cores:
        b, half = core // 2, core % 2
        m = dict(shared)
        m["xm"] = np.ascontiguousarray(x[b, half * NT:(half + 1) * NT])
        m["xp"] = np.ascontiguousarray(x[b, 0:NT])
        m["c"] = np.ascontiguousarray(c[b])
        m["pm"] = np.full((128, 1), float(half), np.float32)
        maps.append(m)
    return maps


def kernel(**inputs):
    nc, _ = get_nc()
    maps = make_in_maps(inputs)
    res = run_bass_kernel_spmd(nc, maps, core_ids=list(range(8)))
    out = np.zeros((4, 4096, D), np.float32)
    for core in range(8):
        b, half = core // 2, core % 2
        out[b, half * NT:(half + 1) * NT] = np.asarray(res.results[core]["out"], dtype=np.float32)
    return out
```

```python
import contextlib
import numpy as np
import ml_dtypes
import concourse.bass as bass
import concourse.mybir as mybir
from concourse.bass_utils import run_bass_kernel_spmd

F32 = mybir.dt.float32
BF16 = mybir.dt.bfloat16
I32 = mybir.dt.int32
CAP = 512
NR = 32 * CAP
ALU = mybir.AluOpType
AF = mybir.ActivationFunctionType
AX = mybir.AxisListType

D = 2048
NT = 2048
KC = 16
EPS = 1e-6
NEG = -30000.0
NEGBIG = -1.0e30
ENGS = ("pe", "act", "dve", "pool", "sp")
SAME_ENGINE_SYNC = {"pe": False, "act": True, "dve": True, "pool": True, "sp": False}


class Buf:
    __slots__ = ("name", "last_w", "readers", "dsem", "dcount", "const", "last_dma", "excl")

    def __init__(self, name, const=False, excl=False):
        self.excl = excl
        self.name = name
        self.last_w = None
        self.readers = []
        self.dsem = None
        self.dcount = 0
        self.const = const
        self.last_dma = None


class Op:
    __slots__ = ("eng", "fn", "deps", "sig", "sem", "val", "dbuf")

    def __init__(self, eng, fn, dbuf=None):
        self.eng = eng
        self.fn = fn
        self.deps = []
        self.sig = dbuf is not None
        self.sem = None
        self.val = 0
        self.dbuf = dbuf


class Sched:
    def __init__(self, nc):
        self.nc = nc
        self.ops = {e: [] for e in ENGS}
        self.dma_bufs = []
        self.epoch = None
        self.all_ops = []

    def barrier(self, fn):
        o = Op("dve", fn, None)
        deps = {}
        for e in ENGS:
            if self.ops[e]:
                d = self.ops[e][-1]
                deps[id(d)] = d
        for b in self.dma_bufs:
            if b.last_dma is not None:
                deps[id(b.last_dma)] = b.last_dma
        o.deps = list(deps.values())
        self.ops["dve"].append(o)
        self.all_ops.append(o)
        self.epoch = o
        return o

    def op(self, eng, fn, reads=(), writes=(), dbuf=None, extra=()):
        o = Op(eng, fn, dbuf)
        deps = {}
        for d in extra:
            if d is not None:
                deps[id(d)] = d
        if self.epoch is not None:
            deps[id(self.epoch)] = self.epoch
        xr = [b for b in reads if b.excl]
        if xr:
            reads = [b for b in reads if not b.excl]
            writes = list(writes) + [b for b in xr if b not in writes]
        for b in reads:
            if b.last_w is not None:
                deps[id(b.last_w)] = b.last_w
        for b in writes:
            if b.last_w is not None:
                deps[id(b.last_w)] = b.last_w
            for r in b.readers:
                deps[id(r)] = r
        o.deps = list(deps.values())
        for b in writes:
            b.last_w = o
            b.readers = []
        for b in reads:
            if not b.const and b.last_w is not o:
                b.readers.append(o)
        if dbuf is not None:
            dbuf.last_dma = o
            if dbuf.dsem is None:
                dbuf.dsem = True
                self.dma_bufs.append(dbuf)
        self.ops[eng].append(o)
        self.all_ops.append(o)
        return o

    def emit(self, stack):
        nc = self.nc
        for e in ENGS:
            for o in self.ops[e]:
                keep = []
                for d in o.deps:
                    if d.dbuf is None and d.eng == o.eng and not SAME_ENGINE_SYNC[o.eng]:
                        continue
                    keep.append(d)
                    d.sig = True
                o.deps = keep
        esem = {}
        for e in ENGS:
            if any(o.sig and o.dbuf is None for o in self.ops[e]):
                esem[e] = stack.enter_context(nc.semaphore("s_" + e))
        for i, b in enumerate(self.dma_bufs):
            b.dsem = stack.enter_context(nc.semaphore("d%d" % i))
            b.dcount = 0
        cnt = {e: 0 for e in ENGS}
        for o in self.all_ops:
            if o.dbuf is not None:
                o.dbuf.dcount += 16
                o.sem = o.dbuf.dsem
                o.val = o.dbuf.dcount
            elif o.sig:
                cnt[o.eng] += 1
                o.sem = esem[o.eng]
                o.val = cnt[o.eng]
        block = stack.enter_context(nc.Block())
        handles = {"pe": block.tensor, "act": block.scalar, "dve": block.vector,
                   "pool": block.gpsimd, "sp": block.sync}
        for e in ENGS:
            ops = self.ops[e]
            if not ops:
                continue

            def body(eng, ops=ops):
                waited = {}
                for o in ops:
                    need = {}
                    for d in o.deps:
                        k = id(d.sem)
                        if d.val > need.get(k, (0, None))[0]:
                            need[k] = (d.val, d.sem)
                    for k, (v, sem) in need.items():
                        if waited.get(k, 0) >= v:
                            continue
                        waited[k] = v
                        eng.wait_ge(sem, v)
                    ins = o.fn(eng)
                    if o.sig:
                        ins.then_inc(o.sem, 16 if o.dbuf is not None else 1)

            handles[e](body)


class SbAlloc:
    def __init__(self, nc, base=17408, limit=229000):
        self.nc = nc
        self.top = base
        self.limit = limit
        self.n = 0
        self.marks = []

    def push(self):
        self.marks.append(self.top)

    def pop(self):
        self.top = self.marks.pop()

    def alloc(self, name, shape, dtype):
        sz = int(np.prod(shape[1:])) * (4 if dtype in (F32, I32) else 2)
        off = (self.top + 63) // 64 * 64
        assert off + sz <= self.limit, (name, off, sz, self.limit)
        self.top = off + sz
        self.n += 1
        return self.nc.alloc_sbuf_tensor_at("%s_%d" % (name, self.n), list(shape), dtype, offset=off)


class K:
    def __init__(self, nc, debug=False, stages="012345"):
        self.nc = nc
        self.S = Sched(nc)
        self.sb = SbAlloc(nc)
        self.debug = debug
        self.stages = stages
        self.dpool = {}
        self.bkind = {}
        self.dram = {}
        self.sbufs = {}
        self.scr_w = {}

    def din(self, name, shape, dt=F32):
        return self.nc.dram_tensor(name, list(shape), dt, kind="ExternalInput").ap()

    def dscr(self, name, shape, dt):
        kind = "ExternalOutput" if self.debug else "Internal"
        t = self.nc.dram_tensor(name, list(shape), dt, kind=kind).ap()
        self.dram[name] = t
        return t

    def dbuf(self, name, kind="sp"):
        pool = self.dpool.setdefault(kind, [])
        if pool:
            return pool.pop()
        b = Buf(name)
        self.bkind[id(b)] = kind
        return b

    def release(self, bufs):
        for b in bufs:
            self.dpool[self.bkind[id(b)]].append(b)

    def dma(self, q, out, in_, reads, writes, dbuf, **kw):
        k = self.bkind.setdefault(id(dbuf), q)
        assert k == q, (dbuf.name, k, q)
        return self.S.op(q, lambda e: e.dma_start(out=out, in_=in_, **kw), reads, writes, dbuf=dbuf)

    def mm(self, out, lhsT, rhs, start, stop, reads, writes, tp=None):
        if tp is not None:
            return self.S.op("pe", lambda e: e.matmul(out, lhsT=lhsT, rhs=rhs, start=start, stop=stop, tile_position=tp), reads, writes)
        return self.S.op("pe", lambda e: e.matmul(out, lhsT=lhsT, rhs=rhs, start=start, stop=stop), reads, writes)

    def tr(self, out, in_, reads, writes):
        idn = self.identb
        n = in_.shape[0]
        return self.S.op("pe", lambda e: e.transpose(out=out, in_=in_, identity=idn[0:n, 0:n]),
                         list(reads) + [self.B_c0], writes)

    def act(self, out, in_, func, reads, writes, scale=1.0, bias=None, accum=None):
        kw = {}
        if bias is not None:
            kw["bias"] = bias
        if accum is not None:
            kw["accum_out"] = accum
        return self.S.op("act", lambda e: e.activation(out=out, in_=in_, func=func, scale=scale, **kw), reads, writes)

    def ts(self, out, in0, s1, s2, op0, op1, reads, writes, eng="dve"):
        if s2 is None:
            return self.S.op(eng, lambda e: e.tensor_scalar(out=out, in0=in0, scalar1=s1, scalar2=None, op0=op0), reads, writes)
        return self.S.op(eng, lambda e: e.tensor_scalar(out=out, in0=in0, scalar1=s1, scalar2=s2, op0=op0, op1=op1), reads, writes)

    def tt(self, out, in0, in1, op, reads, writes, eng="dve"):
        return self.S.op(eng, lambda e: e.tensor_tensor(out=out, in0=in0, in1=in1, op=op), reads, writes)

    def stt(self, out, in0, scalar, in1, op0, op1, reads, writes, eng="dve"):
        return self.S.op(eng, lambda e: e.scalar_tensor_tensor(out=out, in0=in0, scalar=scalar, in1=in1, op0=op0, op1=op1), reads, writes)

    def cp(self, eng, out, in_, reads, writes):
        if eng == "act":
            return self.S.op("act", lambda e: e.copy(out=out, in_=in_), reads, writes)
        return self.S.op(eng, lambda e: e.tensor_copy(out=out, in_=in_), reads, writes)

    def rstd(self, out, ssq, n, reads, writes, tmp):
        eps = self.epsc
        self.act(tmp, ssq, AF.Sqrt, list(reads) + [self.B_c0], [writes[1]], scale=1.0 / n, bias=eps[:, 0:1])
        self.S.op("dve", lambda e: e.reciprocal(out=out, in_=tmp), [writes[1]], [writes[0]])

    def build(self):
        nc = self.nc
        sb = self.sb
        S = self.S
        I = {}
        I["xm"] = self.din("xm", [NT, D])
        I["xp"] = self.din("xp", [NT, D])
        I["c"] = self.din("c", [D])
        I["pm"] = self.din("pm", [128, 1])
        I["norm1_g"] = self.din("norm1_g", [D])
        I["norm2_g"] = self.din("norm2_g", [D])
        I["final_g"] = self.din("final_g", [D])
        I["w_ada"] = self.din("w_ada", [D, 6 * D])
        I["b_ada"] = self.din("b_ada", [6 * D])
        I["w_in"] = self.din("w_in", [D, 11264])
        I["r_lower"] = self.din("r_lower", [2, 1024])
        I["r_norm_g"] = self.din("r_norm_g", [128])
        I["w_up_a"] = self.din("w_up_a", [1024, D])
        I["w_up_r"] = self.din("w_up_r", [1024, D])
        I["w_out"] = self.din("w_out", [D, D])
        I["w_rg"] = self.din("w_rg", [D, 4])
        I["b_rg"] = self.din("b_rg", [4])
        I["w_re"] = self.din("w_re", [D, 32])
        I["b_re"] = self.din("b_re", [32])
        I["w1"] = self.din("w1", [32, D, 512])
        I["w3"] = self.din("w3", [32, D, 512])
        I["w2"] = self.din("w2", [32, 512, D])
        I["k_ident"] = self.din("k_ident", [128, 128])
        I["k_cm"] = self.din("k_cm", [128, 512])
        I["k_cpen"] = self.din("k_cpen", [128, 256])
        I["k_sel"] = self.din("k_sel", [16, 2048])
        I["k_wall"] = self.din("k_wall", [128, 136])
        I["k_wl"] = self.din("k_wl", [128, 128])
        I["k_bd"] = self.din("k_bd", [128, 128])
        I["k_slt"] = self.din("k_slt", [128, 128])
        I["k_ones"] = self.din("k_ones", [128, 128])
        I["k_ebase"] = self.din("k_ebase", [128, 32])
        self.I = I
        self.out = nc.dram_tensor("out", [NT, D], F32, kind="ExternalOutput").ap()
        self.dscr("MOD", [6 * D], F32)
        self.dscr("QT", [8, 128, NT], BF16)
        self.dscr("KT", [8, 128, 2 * NT], BF16)
        self.dscr("V", [2 * NT, 1024], BF16)
        self.dscr("RQT", [8, 128, NT], BF16)
        self.dscr("RF", [2 * NT, 1024], F32)
        self.dscr("RI", [2 * NT, 1024], BF16)
        self.dscr("ROG", [NT, 1024], BF16)
        self.dscr("GAT", [16, 128, NT], BF16)
        self.dscr("GRT", [16, 128, NT], BF16)
        self.dscr("MT", [16, 128, NT], BF16)
        self.dscr("X1", [NT, D], F32)
        self.dscr("H2T", [16, 128, NT], BF16)
        self.dscr("XD", [NR, D], BF16)
        self.dscr("A2R", [2, D], BF16)
        self.dscr("YD0", [NR, 1024], F32)
        self.dscr("YD1", [NR, 1024], F32)
        if self.debug:
            self.dscr("ATT", [8, 128, NT], BF16)
            self.dscr("OT", [8, 128, NT], BF16)
            self.dscr("WF", [128, 16 * 32], F32)
        self.scrB = {}

        with contextlib.ExitStack() as st:
            self.F = [st.enter_context(nc.psum_tensor("F%d" % i, [128, 512], F32)) for i in range(6)]
            self.T = [st.enter_context(nc.psum_tensor("T%d" % i, [128, 1024], BF16)) for i in range(2)]
            self.BF = [Buf("F%d" % i, excl=True) for i in range(6)]
            self.BFs = [[self.BF[i]] * 4 for i in range(6)]
            _bt = [Buf("T%d" % i, excl=True) for i in range(2)]
            self.BT = [[_bt[i]] * 8 for i in range(2)]
            self.dummy = sb.alloc("dummy", [128, 8], F32)
            self.stage0()
            self.bar()
            if "1" in self.stages:
                self.stage1()
                self.bar()
            sb.push()
            self.attT = sb.alloc("attT", [128, 8, NT], BF16)
            self.oT = sb.alloc("oT", [128, 8, NT], BF16)
            self.B_attT = [Buf("attT%d" % h) for h in range(8)]
            self.B_oT = [Buf("oT%d" % t) for t in range(16)]
            if "2" in self.stages:
                self.stage2()
                self.bar()
            if "3" in self.stages:
                self.stage3()
                self.bar()
            if "4" in self.stages:
                self.stage4a()
                self.bar()
            sb.pop()
            if "4" in self.stages:
                self.stage4b()
                self.bar()
            if "5" in self.stages:
                self.stage5()
            allb = [b for b in self.S.dma_bufs]
            S.op("sp", lambda e: e.nop(), [], allb)
            S.emit(st)
        return nc

    def bar(self):
        d = self.dummy
        self.S.barrier(lambda e: e.memset(d[:], 0.0))

    def sB(self, name, key):
        k = (name, key)
        if k not in self.scrB:
            self.scrB[k] = Buf("%s%s" % (name, key))
        return self.scrB[k]

    def stage0(self):
        S, sb, I = self.S, self.sb, self.I
        cbs = {"sp": [Buf("c%d" % i) for i in range(4)], "pool": [Buf("cp%d" % i) for i in range(2)]}
        cb = cbs["sp"] + cbs["pool"]
        self.B_c0 = Buf("constall")
        ci = [0]

        def cload(q, dst, src, **kw):
            lst = cbs[q]
            b = lst[ci[0] % len(lst)]
            ci[0] += 1
            self.dma(q, dst, src, [], [b], b, **kw)

        def A(name, shape, dt=F32):
            t = sb.alloc(name, shape, dt)
            return t

        self.identb = A("identb", [128, 128], BF16)
        self.ident32 = A("ident32", [128, 128])
        self.rows = A("rows", [48, 128])
        self.cm = A("cm", [128, 512], BF16)
        self.cpen = A("cpen", [128, 256])
        self.selc = A("selc", [16, 2048], BF16)
        self.wall = A("wall", [128, 136])
        self.wl = A("wl", [128, 128])
        self.bd = A("bd", [128, 128])
        self.epsc = A("epsc", [128, 1])
        self.pm = A("pm", [128, 1])
        self.ppen = A("ppen", [128, 1])
        self.g1c = A("g1c", [128, 16])
        self.g2c = A("g2c", [128, 16])
        self.ccol = A("ccol", [128, 16])
        self.cact = A("cact", [128, 16], BF16)
        self.modc = A("modc", [128, 6, 16])
        self.a1 = A("a1", [128, 16])
        self.a2 = A("a2", [128, 16])
        self.gt1b = A("gt1b", [128, D])
        self.gt2b = A("gt2b", [128, D])
        self.fgb = A("fgb", [128, D])
        self.lb = A("lb", [128, 1024])
        self.oml = A("oml", [128, 1024])
        self.rngb = A("rngb", [128, 128])
        self.wr = A("wr", [128, 16, 36], BF16)
        self.br = A("br", [128, 36])
        self.wfull = A("wfull", [128, 16, 32])
        self.slt = A("slt", [128, 128], BF16)
        self.ones = A("ones", [128, 128], BF16)
        self.ebase = A("ebase", [128, 32])
        self.tot = A("tot", [128, 32])
        self.desti = A("desti", [128, 16, 2], I32)
        self.wk = A("wk", [128, 16, 2])
        self.B_tot = Buf("tot")
        self.B_ind = Buf("ind")
        self.six = [A("six", [128, 2], I32) for _ in range(2)]
        self.gix = [A("gix", [128, 2], I32) for _ in range(2)]
        self.B_six = [[Buf("six%d_%d" % (i, k)) for k in range(2)] for i in range(2)]
        self.B_gix = [[Buf("gix%d_%d" % (i, k)) for k in range(2)] for i in range(2)]
        self.B_dest = [Buf("dest%d" % t) for t in range(16)]
        self.B_wfull = [Buf("wfull%d" % t) for t in range(16)]
        cload("pool", self.identb[:], I["k_ident"])
        cload("sp", self.ident32[:], I["k_ident"])
        cload("pool", self.cm[:], I["k_cm"])
        cload("sp", self.cpen[:], I["k_cpen"])
        cload("pool", self.selc[:], I["k_sel"])
        cload("sp", self.wall[:], I["k_wall"])
        cload("sp", self.wl[:], I["k_wl"])
        cload("sp", self.bd[:], I["k_bd"])
        cload("sp", self.pm[:], I["pm"])
        cload("sp", self.rows[0:16, :], I["norm1_g"].rearrange("(k p) -> k p", p=128))
        cload("sp", self.rows[16:32, :], I["norm2_g"].rearrange("(k p) -> k p", p=128))
        cload("sp", self.rows[32:48, :], I["c"].rearrange("(k p) -> k p", p=128))
        cload("sp", self.fgb[:], I["final_g"].partition_broadcast(128))
        cload("sp", self.lb[:], I["r_lower"][0, :].partition_broadcast(128))
        cload("sp", self.oml[:], I["r_lower"][1, :].partition_broadcast(128))
        cload("sp", self.rngb[:], I["r_norm_g"].partition_broadcast(128))
        cload("pool", self.wr[:, :, 0:4], I["w_rg"].rearrange("(k p) n -> p k n", p=128))
        cload("pool", self.wr[:, :, 4:36], I["w_re"].rearrange("(k p) n -> p k n", p=128))
        cload("pool", self.slt[:], I["k_slt"])
        cload("pool", self.ones[:], I["k_ones"])
        cload("sp", self.ebase[:], I["k_ebase"])
        cload("sp", self.br[:, 0:4], I["b_rg"].partition_broadcast(128))
        cload("sp", self.br[:, 4:36], I["b_re"].partition_broadcast(128))
        cs = Buf("cs")
        S.op("dve", lambda e: e.memset(self.epsc[:], EPS), [], [cs])
        S.op("pe", lambda e: e.transpose(out=self.F[2][:, 0:48], in_=self.rows[:, :], identity=self.ident32[0:48, 0:48]), cb, [self.BF[2]])
        self.cp("dve", self.g1c[:], self.F[2][:, 0:16], [self.BF[2]], [cs])
        self.cp("dve", self.g2c[:], self.F[2][:, 16:32], [self.BF[2]], [cs])
        self.cp("dve", self.ccol[:], self.F[2][:, 32:48], [self.BF[2]], [cs])
        S.op("dve", lambda e: e.memset(self.tot[:], 0.0), [], [self.B_tot])
        self.ts(self.ppen[:], self.pm[:], -1.0, -NEGBIG, ALU.add, ALU.mult, cb, [cs])
        self.tt(self.lb[:], self.lb[:], self.oml[:], ALU.subtract, cb, [cs])
        self.act(self.lb[:], self.lb[:], AF.Sigmoid, [cs], [cs])
        self.ts(self.oml[:], self.lb[:], -1.0, 1.0, ALU.mult, ALU.add, [cs], [cs])
        self.act(self.cact[:], self.ccol[:], AF.Silu, [cs], [cs])
        sb.push()
        MOD = self.dram["MOD"]
        wsl = [sb.alloc("wsl0", [128, KC, 512], BF16) for _ in range(3)]
        Bw = [self.dbuf("wsl", "pool") for _ in range(3)]
        bsl = [sb.alloc("bsl", [1, 512], F32) for _ in range(2)]
        Bb = [self.dbuf("bsl") for _ in range(2)]
        msl = [sb.alloc("msl", [1, 512], F32) for _ in range(2)]
        Bm = [self.dbuf("msl") for _ in range(2)]
        wsrc = I["w_ada"].rearrange("(k p) n -> p k n", p=128)
        Bmod = Buf("MOD")
        for j in range(24):
            w = wsl[j % 3]
            bw = Bw[j % 3]
            self.dma("pool", w[:], wsrc[:, :, j * 512:(j + 1) * 512], [], [bw], bw)
            bb = Bb[j % 2]
            self.dma("sp", bsl[j % 2][:], I["b_ada"][j * 512:(j + 1) * 512].rearrange("(o n) -> o n", o=1), [], [bb], bb)
            pf = self.F[j % 2]
            bpf = self.BF[j % 2]
            for kc in range(KC):
                self.mm(pf[0:1, :], self.cact[:, kc:kc + 1], w[:, kc, :], kc == 0, kc == KC - 1, [bw, cs], [bpf])
            bm = Bm[j % 2]
            self.tt(msl[j % 2][:], pf[0:1, :], bsl[j % 2][:], ALU.add, [bpf, bb], [bm])
            self.dma("sp", MOD[j * 512:(j + 1) * 512].rearrange("(o n) -> o n", o=1), msl[j % 2][:], [bm], [Bmod], bm)
        sb.pop()
        self.release(Bw + Bb + Bm)
        bmc = self.dbuf("modc")
        modr = sb.alloc("modr", [96, 128], F32)
        self.dma("sp", modr[:], MOD.rearrange("(r p) -> r p", p=128), [Bmod], [bmc], bmc)
        S.op("pe", lambda e: e.transpose(out=self.F[3][:, 0:96], in_=modr[:, :], identity=self.ident32[0:96, 0:96]), [bmc] + cb, [self.BF[3]])
        self.cp("dve", self.modc[:].rearrange("p s k -> p (s k)"), self.F[3][:, 0:96], [self.BF[3]], [cs])
        bg1 = self.dbuf("gt1b")
        self.dma("sp", self.gt1b[:], MOD[2 * D:3 * D].partition_broadcast(128), [Bmod], [bg1], bg1)
        bg2 = self.dbuf("gt2b")
        self.dma("sp", self.gt2b[:], MOD[5 * D:6 * D].partition_broadcast(128), [Bmod], [bg2], bg2)
        sb.push()
        g2b = sb.alloc("g2b", [128, D], F32)
        sc2b = sb.alloc("sc2b", [128, D], F32)
        sh2b = sb.alloc("sh2b", [128, D], F32)
        bq1, bq2, bq3 = self.dbuf("g2b"), self.dbuf("sc2b"), self.dbuf("sh2b")
        self.dma("sp", g2b[:], I["norm2_g"].partition_broadcast(128), [], [bq1], bq1)
        self.dma("sp", sc2b[:], MOD[4 * D:5 * D].partition_broadcast(128), [Bmod], [bq2], bq2)
        self.dma("sp", sh2b[:], MOD[3 * D:4 * D].partition_broadcast(128), [Bmod], [bq3], bq3)
        a2t = sb.alloc("a2t", [128, D], BF16)
        sh2t = sb.alloc("sh2t", [128, D], BF16)
        bq4, bq5 = self.dbuf("a2t"), self.dbuf("sh2t")
        self.stt(a2t[:], sc2b[:], 1.0, g2b[:], ALU.add, ALU.mult, [bq1, bq2], [bq4])
        self.cp("dve", sh2t[:], sh2b[:], [bq3], [bq5])
        self.B_a2r = Buf("A2R")
        self.dma("sp", self.dram["A2R"][0:1, :], a2t[0:1, :], [bq4], [self.B_a2r], bq4)
        self.dma("sp", self.dram["A2R"][1:2, :], sh2t[0:1, :], [bq5], [self.B_a2r], bq5)
        sb.pop()
        self.release([bq1, bq2, bq3, bq4, bq5])
        self.stt(self.a1[:], self.modc[:, 1, :], 1.0, self.g1c[:], ALU.add, ALU.mult, [cs], [cs])
        self.stt(self.a2[:], self.modc[:, 4, :], 1.0, self.g2c[:], ALU.add, ALU.mult, [cs], [cs])
        S.op("dve", lambda e: e.memset(self.epsc[:], EPS), cb + [cs, bmc, bg1, bg2], [self.B_c0])
        self.B_c0.const = True
        self.B_modc = bmc

    def norm_to_hT(self, xt, bx, hT_dst, bh, a_col, sh_col, ring):
        junk, ssq, tmp, rs, xn, bj, bs, bxn = ring
        self.act(junk[:], xt, AF.Square, [bx], [bj, bs], accum=ssq[:, 0:1])
        self.rstd(rs[:, 0:1], ssq[:, 0:1], float(D), [bs], [bs, bj], tmp[:, 0:1])
        yield
        self.ts(xn[:], xt, rs[:, 0:1], None, ALU.mult, None, [bx, bs], [bxn])
        yield
        for tb in range(2):
            for ts_ in range(8):
                kc = tb * 8 + ts_
                self.tr(self.T[tb][:, ts_ * 128:(ts_ + 1) * 128], xn[:, kc * 128:(kc + 1) * 128], [bxn], [self.BT[tb][0]])
            yield
            for ts_ in range(8):
                kc = tb * 8 + ts_
                pt = self.T[tb][:, ts_ * 128:(ts_ + 1) * 128]
                if tb == 0:
                    self.act(hT_dst[:, kc, :], pt, AF.Identity, [self.BT[tb][0], self.B_c0], [bh], scale=a_col[:, kc:kc + 1], bias=sh_col[:, kc:kc + 1])
                else:
                    self.ts(hT_dst[:, kc, :], pt, a_col[:, kc:kc + 1], sh_col[:, kc:kc + 1], ALU.mult, ALU.add, [self.BT[tb][0], self.B_c0], [bh])
                if ts_ % 2 == 1:
                    yield

    def norm_ring(self, sb, junk=None):
        if junk is None:
            junk = sb.alloc("junk", [128, D], BF16)
        ssq = sb.alloc("ssq", [128, 1], F32)
        tmp = sb.alloc("tmp", [128, 1], F32)
        rs = sb.alloc("rs", [128, 1], F32)
        xn = sb.alloc("xn", [128, D], BF16)
        return (junk, ssq, tmp, rs, xn, Buf("junk"), Buf("ssq"), Buf("xn"))

    def stage1(self):
        S, sb, I = self.S, self.sb, self.I
        sb.push()
        G = 1024
        hT = [sb.alloc("hT", [128, KC, G], BF16) for _ in range(2)]
        BhT = [[Buf("hT%d_%d" % (s, t)) for t in range(8)] for s in range(2)]
        wsl = [sb.alloc("wsl", [128, KC, 512], BF16) for _ in range(3)]
        Bw = [self.dbuf("wsl", "pool") for _ in range(3)]
        xs = [sb.alloc("xs", [128, D], F32) for _ in range(2)]
        Bx = [self.dbuf("xs") for _ in range(2)]
        rings = [self.norm_ring(sb)]
        rings.append(self.norm_ring(sb, junk=rings[0][0]))
        stg = []
        stgb = []
        for _ in range(4):
            sb.push()
            stgb.append(sb.alloc("stgb", [128, 512], BF16))
            sb.pop()
            stg.append(sb.alloc("stg", [128, 512], F32))
        Bs = [self.dbuf("stg") for _ in range(4)]
        wsrc = I["w_in"].rearrange("(k p) n -> p k n", p=128)
        D_ = self.dram
        zt = sb.alloc("zt", [128, 1024], BF16)
        Bz = self.dbuf("zt")
        S.op("dve", lambda e: e.memset(zt[:], 0.0), [], [Bz])
        def zfill():
            for i in range(NR // 128):
                for hf in range(2):
                    self.dma("sp", D_["XD"][i * 128:(i + 1) * 128, hf * 1024:(hf + 1) * 1024], zt[:], [Bz], [Buf("z")], Bz)
                    yield
        zgen = zfill()
        groups = [("p", 0), ("p", 1), ("m", 0), ("m", 1)]
        pblocks = [2, 3, 4, 5, 8, 9, 10, 11]
        import os
        if os.environ.get("S1G"):
            groups = groups[:int(os.environ["S1G"])]
        if os.environ.get("S1B"):
            pblocks = pblocks[:int(os.environ["S1B"])]
        wi = [0]
        si = [0]
        xi = [0]
        def hT_gen(gi):
            kind, gidx = groups[gi]
            slot = gi % 2
            src = I["xp"] if kind == "p" else I["xm"]
            for t in range(8):
                xsl = xs[xi[0] % 2]
                bx = Bx[xi[0] % 2]
                ring = rings[xi[0] % 2]
                xi[0] += 1
                r0 = gidx * G + t * 128
                self.dma("sp", xsl[:], src[r0:r0 + 128, :], [], [bx], bx)
                yield
                for _ in self.norm_to_hT(xsl[:], bx, hT[slot][:, :, t * 128:(t + 1) * 128], BhT[slot][t], self.a1, self.modc[:, 0, :], ring):
                    yield

        for _ in hT_gen(0):
            pass
        for gi, (kind, gidx) in enumerate(groups):
            slot = gi % 2
            ltok0 = gidx * G + (0 if kind == "p" else NT)
            mtok0 = gidx * G
            nxt = hT_gen(gi + 1) if gi + 1 < len(groups) else iter(())
            blocks = pblocks if kind == "p" else list(range(22))
            nstep = -(-8 * 16 // (len(blocks) * 8)) + 1
            for blk in blocks:
                w = wsl[wi[0] % 3]
                bw = Bw[wi[0] % 3]
                wi[0] += 1
                self.dma("pool", w[:], wsrc[:, :, blk * 512:(blk + 1) * 512], [], [bw], bw)
                fm = blk in (0, 1, 2, 3, 6, 7) or blk >= 14
                for u in range(8):
                    fb = u % 4
                    pf = self.F[fb]
                    bpf = self.BF[fb]
                    if fm:
                        cs_, th = u // 2, u % 2
                        for kc in range(KC):
                            self.mm(pf[:, :], w[:, kc, cs_ * 128:(cs_ + 1) * 128], hT[slot][:, kc, th * 512:(th + 1) * 512],
                                    kc == 0, kc == KC - 1, [bw] + BhT[slot][th * 4:(th + 1) * 4], [bpf])
                    else:
                        for kc in range(KC):
                            self.mm(pf[:, :], hT[slot][:, kc, u * 128:(u + 1) * 128], w[:, kc, :],
                                    kc == 0, kc == KC - 1, [bw, BhT[slot][u]], [bpf])
                    sg = stg[si[0] % 4]
                    sgb = stgb[si[0] % 4]
                    bs = Bs[si[0] % 4]
                    si[0] += 1
                    if fm:
                        if blk < 2:
                            dst = D_["QT"][blk * 4 + cs_, :, mtok0 + th * 512: mtok0 + (th + 1) * 512]
                            key = ("QT", blk * 4 + cs_)
                            fn = AF.Copy
                        elif blk < 4:
                            dst = D_["KT"][(blk - 2) * 4 + cs_, :, ltok0 + th * 512: ltok0 + (th + 1) * 512]
                            key = ("KT", (blk - 2) * 4 + cs_)
                            fn = AF.Copy
                        elif blk < 8:
                            dst = D_["RQT"][(blk - 6) * 4 + cs_, :, mtok0 + th * 512: mtok0 + (th + 1) * 512]
                            key = ("RQT", (mtok0 + th * 512) // 128)
                            fn = AF.Copy
                        elif blk < 18:
                            dst = D_["GAT"][(blk - 14) * 4 + cs_, :, mtok0 + th * 512: mtok0 + (th + 1) * 512]
                            key = ("GAT", (blk - 14) * 4 + cs_)
                            fn = AF.Sigmoid
                        else:
                            dst = D_["GRT"][(blk - 18) * 4 + cs_, :, mtok0 + th * 512: mtok0 + (th + 1) * 512]
                            key = ("GRT", (blk - 18) * 4 + cs_)
                            fn = AF.Sigmoid
                        odt = BF16
                    else:
                        if blk < 6:
                            dst = D_["V"][ltok0 + u * 128: ltok0 + (u + 1) * 128, (blk - 4) * 512:(blk - 3) * 512]
                            key = ("V", (blk - 4) * 4)
                            fn, odt = AF.Copy, BF16
                        elif blk < 10:
                            dst = D_["RF"][ltok0 + u * 128: ltok0 + (u + 1) * 128, (blk - 8) * 512:(blk - 7) * 512]
                            key = ("RF", (ltok0 + u * 128) // 128)
                            fn, odt = AF.Copy, F32
                        elif blk < 12:
                            dst = D_["RI"][ltok0 + u * 128: ltok0 + (u + 1) * 128, (blk - 10) * 512:(blk - 9) * 512]
                            key = ("RI", (ltok0 + u * 128) // 128)
                            fn, odt = AF.Copy, BF16
                        else:
                            dst = D_["ROG"][mtok0 + u * 128: mtok0 + (u + 1) * 128, (blk - 12) * 512:(blk - 11) * 512]
                            key = ("ROG", (mtok0 + u * 128) // 128)
                            fn, odt = AF.Silu, BF16
                    so = sg[:] if odt == F32 else sgb[:]
                    if fn == AF.Copy and (u % 2 == 1):
                        self.cp("dve", so, pf[:, :], [bpf], [bs])
                    else:
                        self.act(so, pf[:, :], fn, [bpf], [bs])
                    wl_ = self.scr_w.setdefault(key, [])
                    tok = Buf("w")
                    wl_.append(tok)
                    self.dma("sp", dst, so, [bs], [tok], bs)
                    for _ in range(nstep):
                        next(nxt, None)
                    next(zgen, None)
            for _ in nxt:
                pass
        for _ in zgen:
            pass
        sb.pop()
        self.release(Bw + Bx + Bs + [Bz])

    def scr_reads(self, name, keys):
        out = []
        for k in keys:
            out.extend(self.scr_w.get((name, k), []))
        return out

    def stage2(self):
        S, sb = self.S, self.sb
        D_ = self.dram
        sb.push()
        QTh = [sb.alloc("QTh", [128, NT], BF16) for _ in range(2)]
        KTh = [sb.alloc("KTh", [128, 2 * NT], BF16) for _ in range(2)]
        Vh = [sb.alloc("Vh", [128, 32, 129], BF16) for _ in range(2)]
        Bq = [self.dbuf("q") for _ in range(2)]
        Bk = [self.dbuf("k") for _ in range(2)]
        Bv = [self.dbuf("v") for _ in range(2)]
        km32 = [sb.alloc("km32", [128, 16], F32) for _ in range(2)]
        kmb = [sb.alloc("kmb", [128, 16], BF16) for _ in range(2)]
        gm = [sb.alloc("gm", [128, 16, 16], F32) for _ in range(2)]
        top8 = [sb.alloc("top8", [128, 16, 8], F32) for _ in range(2)]
        pen = [sb.alloc("pen", [128, 16, 16], F32) for _ in range(2)]
        penb = [sb.alloc("penb", [128, 256], BF16) for _ in range(2)]
        penT = [sb.alloc("penT", [16, NT], BF16) for _ in range(2)]
        Bkm = [Buf("km%d" % i) for i in range(2)]
        Bgm = [Buf("gm%d" % i) for i in range(2)]
        Bpen = [Buf("pen%d" % i) for i in range(2)]
        BpenT = [Buf("penT%d" % i) for i in range(2)]
        pT = [sb.alloc("pT", [128, 256], BF16) for _ in range(4)]
        BpT = [Buf("pT%d" % i) for i in range(4)]
        rsum = [sb.alloc("rsum", [128, 1], F32) for _ in range(2)]
        atts = [sb.alloc("atts", [128, 128], BF16) for _ in range(2)]
        Brs = [Buf("rsum%d" % i) for i in range(2)]
        Bat = [Buf("atts%d" % i) for i in range(2)]
        scale = 1.0 / np.sqrt(128.0)
        pi = [0]
        ei = [0]
        for sl in range(2):
            S.op("dve", lambda e, t=Vh[sl]: e.memset(t[:, :, 128:129], 1.0), [], [Bv[sl]])
        def preamble(h):
            sl = h % 2
            q, k, v = QTh[sl], KTh[sl], Vh[sl]
            bq, bk, bv = Bq[sl], Bk[sl], Bv[sl]
            self.dma("sp", q[:], D_["QT"][h], self.scr_reads("QT", [h]), [bq], bq)
            self.dma("sp", k[:], D_["KT"][h], self.scr_reads("KT", [h]), [bk], bk)
            self.dma("sp", v[:, :, 0:128], D_["V"].rearrange("(t p) c -> p t c", p=128)[:, :, h * 128:(h + 1) * 128],
                     self.scr_reads("V", [(h // 4) * 4]), [bv], bv)
            yield
            S.op("dve", lambda e, k=k: e.tensor_reduce(out=km32[sl][:], in_=k[:].rearrange("p (b l) -> p b l", l=256), axis=AX.X, op=ALU.add),
                 [bk], [Bkm[sl]])
            yield
            self.ts(kmb[sl][:], km32[sl][:], 1.0 / 256, None, ALU.mult, None, [Bkm[sl]], [Bkm[sl]])
            yield
            pg = self.F[5]
            bpg = self.BF[5]
            for qt in range(16):
                self.mm(pg[:, qt * 16:(qt + 1) * 16], q[:, qt * 128:(qt + 1) * 128], kmb[sl][:, :], True, True, [bq, Bkm[sl]], [bpg])
            yield
            gmf = gm[sl][:].rearrange("p a b -> p (a b)")
            self.tt(gmf, pg[:, 0:256], self.cpen[:], ALU.add, [bpg, self.B_c0], [Bgm[sl]])
            yield
            self.ts(gm[sl][:, :, 0:8], gm[sl][:, :, 0:8], self.ppen[:, 0:1], None, ALU.add, None, [Bgm[sl], self.B_c0], [Bgm[sl]])
            yield
            for qt in range(16):
                S.op("dve", lambda e, qt=qt: e.max(out=top8[sl][:, qt, :], in_=gm[sl][:, qt, :]), [Bgm[sl]], [Bpen[sl]])
                yield
            for qt in range(16):
                self.ts(pen[sl][:, qt, :], gm[sl][:, qt, :], top8[sl][:, qt, 2:3], None, ALU.is_ge, None, [Bgm[sl], Bpen[sl]], [Bpen[sl]])
                yield
            penf = pen[sl][:].rearrange("p a b -> p (a b)")
            self.ts(penf, penf, -1.0, -NEG, ALU.add, ALU.mult, [Bpen[sl]], [Bpen[sl]])
            yield
            self.ts(pen[sl][:, :, 0:8], pen[sl][:, :, 0:8], self.ppen[:, 0:1], None, ALU.add, None, [Bpen[sl], self.B_c0], [Bpen[sl]])
            yield
            self.cp("dve", penb[sl][:], penf, [Bpen[sl]], [Bpen[sl]])
            yield
            for half in range(2):
                for qt in range(half * 8, half * 8 + 8):
                    tb, ts_ = qt // 8, qt % 8
                    pt = self.T[tb][0:16, ts_ * 128:(ts_ + 1) * 128]
                    self.tr(pt, penb[sl][:, qt * 16:(qt + 1) * 16], [Bpen[sl]], [self.BT[tb][ts_]])
                yield
                for qt in range(half * 8, half * 8 + 8):
                    tb, ts_ = qt // 8, qt % 8
                    pt = self.T[tb][0:16, ts_ * 128:(ts_ + 1) * 128]
                    self.cp("act", penT[sl][:, qt * 128:(qt + 1) * 128], pt, [self.BT[tb][ts_]], [BpenT[sl]])
                yield

        for _ in preamble(0):
            pass
        for h in range(8):
            sl = h % 2
            q, k, v = QTh[sl], KTh[sl], Vh[sl]
            bq, bk, bv = Bq[sl], Bk[sl], Bv[sl]
            nxt = preamble(h + 1) if h + 1 < 8 else iter(())
            itc = [0]
            for qb in range(8):
                Bl = 8 + qb
                nkt = 2 * Bl + 2
                po = [self.F[3], self.F[4]]
                bpo = [self.BF[3], self.BF[4]]
                LAG = 2
                pend = []
                for kk in range(nkt + LAG):
                    if kk < nkt:
                        kt = kk
                        fb = kt % 3
                        ps = self.F[fb]
                        bps = self.BF[fb]
                        self.mm(ps[:, 0:256], k[:, kt * 128:(kt + 1) * 128], q[:, qb * 256:(qb + 1) * 256], True, False, [bk, bq], [bps])
                        if kt < 2 * Bl:
                            i = kt // 2
                            self.mm(ps[:, 0:256], self.selc[0:16, i * 128:(i + 1) * 128], penT[sl][0:16, qb * 256:(qb + 1) * 256],
                                    False, True, [BpenT[sl], self.B_c0], [bps])
                        else:
                            j = kt - 2 * Bl
                            self.mm(ps[:, 0:256], self.identb[:, :], self.cm[:, j * 256:(j + 1) * 256], False, True, [self.B_c0], [bps])
                        p = pT[pi[0] % 4]
                        bp = BpT[pi[0] % 4]
                        pi[0] += 1
                        self.act(p[:], ps[:, 0:256], AF.Exp, [bps], [bp], scale=scale)
                        pend.append((kt, p, bp))
                    if kk >= LAG:
                        kt, p, bp = pend.pop(0)
                        for qh in range(2):
                            self.mm(po[qh][:, 0:129], p[:, qh * 128:(qh + 1) * 128], v[:, kt, :], kt == 0, kt == nkt - 1, [bp, bv], [bpo[qh]])
                    itc[0] += 1
                    if itc[0] >= 24:
                        next(nxt, None)
                for qh in range(2):
                    e_ = ei[0] % 2
                    ei[0] += 1
                    S.op("dve", lambda e, o=rsum[e_], i=po[qh]: e.reciprocal(out=o[:, 0:1], in_=i[:, 128:129]), [bpo[qh]], [Brs[e_]])
                    self.act(atts[e_][:], po[qh][:, 0:128], AF.Copy, [bpo[qh], Brs[e_]], [Bat[e_]], scale=rsum[e_][:, 0:1])
                    qt = qb * 2 + qh
                    tb, ts_ = qt // 8, qt % 8
                    pt = self.T[tb][:, ts_ * 128:(ts_ + 1) * 128]
                    bt = self.BT[tb][ts_]
                    self.tr(pt, atts[e_][:], [Bat[e_]], [bt])
                    self.cp("dve", self.attT[:, h, qt * 128:(qt + 1) * 128], pt, [bt], [self.B_attT[h]])
            for _ in nxt:
                pass
            if self.debug:
                self.dma("sp", D_["ATT"][h], self.attT[:, h, :], [self.B_attT[h]], [Buf("x")], self.B_attT[h])
        sb.pop()
        self.release(Bq + Bk + Bv)

    def stage3(self):
        S, sb = self.S, self.sb
        D_ = self.dram
        sb.push()
        rf = [sb.alloc("rf", [128, 1024], F32) for _ in range(2)]
        ri = [sb.alloc("ri", [128, 1024], BF16) for _ in range(2)]
        rq = [sb.alloc("rq", [128, 8, 128], BF16) for _ in range(2)]
        rog = [sb.alloc("rog", [128, 1024], BF16) for _ in range(2)]
        Brf = [self.dbuf("rf") for _ in range(2)]
        Bri = [self.dbuf("ri") for _ in range(2)]
        Brq = [self.dbuf("rq") for _ in range(2)]
        Brog = [self.dbuf("rog") for _ in range(2)]
        logf = [sb.alloc("logf", [128, 1024], F32) for _ in range(2)]
        kin = [sb.alloc("kin", [128, 1024], F32) for _ in range(2)]
        Blf = [Buf("logf%d" % i) for i in range(2)]
        Bkin = [Buf("kin%d" % i) for i in range(2)]
        R = 4
        ebl = [sb.alloc("ebl", [128, 128], F32) for _ in range(R)]
        khat = [sb.alloc("khat", [128, 128], BF16) for _ in range(R)]
        eb8 = [sb.alloc("eb8", [128, 8], F32) for _ in range(R)]
        ebm = [sb.alloc("ebm", [128, 128], F32) for _ in range(R)]
        qtl = [sb.alloc("qtl", [128, 128], BF16) for _ in range(R)]
        enb = [sb.alloc("enb", [128, 128], F32) for _ in range(R)]
        ktl = [sb.alloc("ktl", [128, 128], BF16) for _ in range(R)]
        ktlT = [sb.alloc("ktlT", [128, 128], BF16) for _ in range(R)]
        khc = [sb.alloc("khc", [128, 4, 128], BF16) for _ in range(R)]
        qtlz = [sb.alloc("qtlz", [128, 4, 128], BF16) for _ in range(R)]
        Bkhc = [Buf("khc%d" % i) for i in range(R)]
        Bqz = [Buf("qtlz%d" % i) for i in range(R)]
        for i in range(R):
            S.op("dve", lambda e, t=qtlz[i]: e.memset(t[:], 0.0), [], [Bqz[i]])
        at = [sb.alloc("at", [128, 128], BF16) for _ in range(R)]
        Sb = [sb.alloc("Sb", [128, 4, 128], BF16) for _ in range(R)]
        ot = [sb.alloc("ot", [128, 128], F32) for _ in range(R)]
        of_ = [sb.alloc("of", [128, 128], BF16) for _ in range(R)]
        sq = [sb.alloc("sq", [128, 4], F32) for _ in range(R)]
        jk = [sb.alloc("jk", [128, 128], BF16) for _ in range(R)]
        names = "ebl khat eb8 ebm qtl enb ktl ktlT at ot of sq".split()
        Bn = {n: [Buf(n + str(i)) for i in range(R)] for n in names}
        BSb = [[Buf("Sb%d_%d" % (i, c)) for c in range(4)] for i in range(R)]
        St = sb.alloc("St", [128, 8, 128], F32)
        BSt = [Buf("St%d" % h) for h in range(8)]
        for h in range(8):
            S.op("dve", lambda e, h=h: e.memset(St[:, h, :], 0.0), [], [BSt[h]])
        F, BFs = self.F, self.BFs
        hi = [0]
        import os
        tl = list(range(32))
        if os.environ.get("S3T"):
            tl = [int(v) for v in os.environ["S3T"].split(",")]
        s3c = int(os.environ.get("S3C", "4"))
        s3m = int(os.environ.get("S3M", "9"))
        for tile in tl:
            main = tile >= 16
            mt = tile - 16
            sl = tile % 2
            self.dma("sp", rf[sl][:], D_["RF"][tile * 128:(tile + 1) * 128, :], self.scr_reads("RF", [tile]), [Brf[sl]], Brf[sl])
            self.dma("sp", ri[sl][:], D_["RI"][tile * 128:(tile + 1) * 128, :], self.scr_reads("RI", [tile]), [Bri[sl]], Bri[sl])
            if main:
                self.dma("sp", rq[sl][:], D_["RQT"][:, :, mt * 128:(mt + 1) * 128].rearrange("h p t -> p h t"),
                         self.scr_reads("RQT", [(mt // 4) * 4]), [Brq[sl]], Brq[sl])
                self.dma("sp", rog[sl][:], D_["ROG"][mt * 128:(mt + 1) * 128, :], self.scr_reads("ROG", [mt]), [Brog[sl]], Brog[sl])
            lf, kn = logf[sl], kin[sl]
            self.act(kn[:], rf[sl][:], AF.Sigmoid, [Brf[sl]], [Bkin[sl]])
            self.tt(kn[:], kn[:], self.oml[:], ALU.mult, [Bkin[sl], self.B_c0], [Bkin[sl]])
            self.tt(kn[:], kn[:], self.lb[:], ALU.add, [Bkin[sl], self.B_c0], [Bkin[sl]])
            self.act(lf[:], kn[:], AF.Ln, [Bkin[sl]], [Blf[sl]])
            self.ts(kn[:], kn[:], -1.0, 1.0, ALU.mult, ALU.add, [Bkin[sl]], [Bkin[sl]])
            def hbody(h, r, main=main, sl=sl, lf=lf, kn=kn, mt=mt):
                hs = slice(h * 128, (h + 1) * 128)
                s2 = h % 2
                c128 = slice(s2 * 128, (s2 + 1) * 128)
                bl_ps = F[0][:, c128]
                b8_ps = F[0][:, 256 + s2 * 8:256 + (s2 + 1) * 8]
                self.mm(bl_ps, self.wl[:, :], lf[:, hs], True, True, [Blf[sl], self.B_c0], [self.BF[0]])
                self.mm(b8_ps, lf[:, hs], self.wall[:, 128:136], True, True, [Blf[sl], self.B_c0], [self.BF[0]])
                if main:
                    self.mm(F[1][:, c128], lf[:, hs], self.wall[:, 0:128], True, True, [Blf[sl], self.B_c0], [self.BF[1]])
                    bm_ps = F[1][:, 256 + s2 * 128:256 + (s2 + 1) * 128]
                    self.mm(bm_ps, self.wall[:, 0:128], lf[:, hs], True, True, [Blf[sl], self.B_c0], [self.BF[1]])
                yield
                self.act(ebl[r][:], bl_ps, AF.Exp, [self.BF[0]], [Bn["ebl"][r]])
                self.act(eb8[r][:], b8_ps, AF.Exp, [self.BF[0]], [Bn["eb8"][r]])
                if main:
                    self.act(ebm[r][:], F[1][:, c128], AF.Exp, [self.BF[1]], [Bn["ebm"][r]])
                    self.act(enb[r][:], bm_ps, AF.Exp, [self.BF[1]], [Bn["enb"][r]], scale=-1.0)
                yield
                for c in range(4):
                    self.stt(khc[r][:, c, :], kn[:, hs], self.wall[:, 132 + c:133 + c], ebl[r][:], ALU.mult, ALU.mult,
                             [Bkin[sl], Bn["ebl"][r], self.B_c0], [Bkhc[r]])
                if main:
                    self.tt(ktl[r][:], kn[:, hs], enb[r][:], ALU.mult, [Bkin[sl], Bn["enb"][r]], [Bn["ktl"][r]])
                    self.tt(qtl[r][:], rq[sl][:, h, :], ebm[r][:], ALU.mult, [Brq[sl], Bn["ebm"][r]], [Bn["qtl"][r]])
                    for c in range(4):
                        cc = slice(32 * c, 32 * c + 32)
                        self.tt(qtlz[r][:, c, cc], rq[sl][:, h, cc], ebm[r][:, cc], ALU.mult, [Brq[sl], Bn["ebm"][r]], [Bqz[r]])
                yield
                pds = [F[5][:, (s2 * 2 + (c % 2)) * 128:(s2 * 2 + (c % 2) + 1) * 128] for c in range(4)]
                if main:
                    pt = self.T[0][:, c128]
                    self.tr(pt, ktl[r][:], [Bn["ktl"][r]], [self.BT[0][0]])
                    yield
                    self.cp("act", ktlT[r][:], pt, [self.BT[0][0]], [Bn["ktlT"][r]])
                    yield
                    self.mm(F[3][:, c128], ktlT[r][:], qtl[r][:], True, True, [Bn["ktlT"][r], Bn["qtl"][r]], [self.BF[3]])
                    yield
                    self.tt(at[r][:], F[3][:, c128], self.bd[:], ALU.mult, [self.BF[3], self.B_c0], [Bn["at"][r]])
                    yield
                pob = 4 if s2 == 0 else 2
                po = F[pob][:, 0:128]
                bpo = self.BF[pob]
                if main:
                    self.mm(po, at[r][:], ri[sl][:, hs], True, False, [Bn["at"][r], Bri[sl]], [bpo])
                for c in range(4):
                    self.mm(pds[c], khc[r][:, c, :], ri[sl][:, hs], True, True, [Bkhc[r], Bri[sl]], [self.BF[5]])
                    if main:
                        self.act(Sb[r][:, c, :], St[:, h, :], AF.Copy, [BSt[h], Bn["eb8"][r]], [BSb[r][c]], scale=eb8[r][:, c:c + 1])
                    yield
                    if main:
                        self.mm(po, qtlz[r][:, c, :], Sb[r][:, c, :], False, c == 3, [Bqz[r], BSb[r][c]], [bpo])
                    self.stt(St[:, h, :], St[:, h, :], eb8[r][:, 4 + c:5 + c], pds[c], ALU.mult, ALU.add, [BSt[h], Bn["eb8"][r], self.BF[5]], [BSt[h]])
                    yield
                if main:
                    self.act(jk[r][:], po, AF.Square, [bpo], [Bn["sq"][r]], accum=sq[r][:, 0:1])
                    yield
                    self.tt(ot[r][:], po, self.rngb[:], ALU.mult, [bpo, self.B_c0], [Bn["ot"][r]])
                    self.rstd(sq[r][:, 2:3], sq[r][:, 0:1], 128.0, [Bn["sq"][r]], [Bn["sq"][r], Bn["sq"][r]], sq[r][:, 1:2])
                    yield
                    self.stt(of_[r][:], ot[r][:], sq[r][:, 2:3], rog[sl][:, hs], ALU.mult, ALU.mult, [Bn["ot"][r], Bn["sq"][r], Brog[sl]], [Bn["of"][r]])
                    yield
                    pt = self.T[1][:, c128]
                    self.tr(pt, of_[r][:], [Bn["of"][r]], [self.BT[1][0]])
                    yield
                    self.cp("act", self.oT[:, h, mt * 128:(mt + 1) * 128], pt, [self.BT[1][0]], [self.B_oT[mt]])

            G = 2
            for h0 in range(0, 8, G):
                gens = []
                for h in range(h0, h0 + G):
                    gens.append(hbody(h, hi[0] % R))
                    hi[0] += 1
                while gens:
                    for g_ in list(gens):
                        try:
                            next(g_)
                        except StopIteration:
                            gens.remove(g_)
            if tile == 15:
                for h in range(8):
                    self.ts(St[:, h, :], St[:, h, :], self.pm[:, 0:1], None, ALU.mult, None, [BSt[h], self.B_c0], [BSt[h]])
        if self.debug:
            for h in range(8):
                self.dma("sp", D_["OT"][h], self.oT[:, h, :], self.B_oT, [Buf("x")], self.B_oT[0])
        sb.pop()
        self.release(Brf + Bri + Brq + Brog)

    def stage4a(self):
        S, sb, I = self.S, self.sb, self.I
        D_ = self.dram
        sb.push()
        wa = [sb.alloc("wa", [128, 8, 128], BF16) for _ in range(2)]
        wr_ = [sb.alloc("wr_", [128, 8, 128], BF16) for _ in range(2)]
        sga = [sb.alloc("sga", [128, NT], BF16) for _ in range(2)]
        sgr = [sb.alloc("sgr", [128, NT], BF16) for _ in range(2)]
        mT = [sb.alloc("mT", [128, NT], BF16) for _ in range(2)]
        t1 = [sb.alloc("t1", [128, 512], F32) for _ in range(2)]
        t2 = [sb.alloc("t2", [128, 512], F32) for _ in range(2)]
        Bwa = [self.dbuf("wa", "pool") for _ in range(2)]
        Bwr = [self.dbuf("wr", "pool") for _ in range(2)]
        Bsa = [self.dbuf("sga") for _ in range(2)]
        Bsr = [self.dbuf("sgr") for _ in range(2)]
        BmT = [self.dbuf("mT") for _ in range(2)]
        Bt1 = [Buf("t1_%d" % i) for i in range(2)]
        Bt2 = [Buf("t2_%d" % i) for i in range(2)]
        wua = I["w_up_a"].rearrange("(k p) n -> p k n", p=128)
        wur = I["w_up_r"].rearrange("(k p) n -> p k n", p=128)
        ti = [0]
        for ch in range(16):
            sl = ch % 2
            self.dma("pool", wa[sl][:], wua[:, :, ch * 128:(ch + 1) * 128], [], [Bwa[sl]], Bwa[sl])
            self.dma("pool", wr_[sl][:], wur[:, :, ch * 128:(ch + 1) * 128], [], [Bwr[sl]], Bwr[sl])
            self.dma("sp", sga[sl][:], D_["GAT"][ch], self.scr_reads("GAT", [ch]), [Bsa[sl]], Bsa[sl])
            self.dma("sp", sgr[sl][:], D_["GRT"][ch], self.scr_reads("GRT", [ch]), [Bsr[sl]], Bsr[sl])
            for g in range(4):
                gs = slice(g * 512, (g + 1) * 512)
                fa, fr = (g % 2) * 2, (g % 2) * 2 + 1
                for wc in range(8):
                    self.mm(self.F[fa][:, :], wa[sl][:, wc, :], self.attT[:, wc, gs], wc == 0, wc == 7, [Bwa[sl], self.B_attT[wc]], [self.BF[fa]])
                for wc in range(8):
                    self.mm(self.F[fr][:, :], wr_[sl][:, wc, :], self.oT[:, wc, gs], wc == 0, wc == 7,
                            [Bwr[sl]] + self.B_oT[g * 4:(g + 1) * 4], [self.BF[fr]])
                i = ti[0] % 2
                ti[0] += 1
                self.tt(t1[i][:], self.F[fa][:, :], sga[sl][:, gs], ALU.mult, [self.BF[fa], Bsa[sl]], [Bt1[i]])
                self.tt(t2[i][:], self.F[fr][:, :], sgr[sl][:, gs], ALU.mult, [self.BF[fr], Bsr[sl]], [Bt2[i]], eng="dve")
                self.tt(mT[sl][:, gs], t1[i][:], t2[i][:], ALU.add, [Bt1[i], Bt2[i]], [BmT[sl]], eng="dve")
            tok = Buf("w")
            self.scr_w.setdefault(("MT", ch), []).append(tok)
            self.dma("sp", D_["MT"][ch], mT[sl][:], [BmT[sl]], [tok], BmT[sl])
        sb.pop()
        self.release(Bwa + Bwr + Bsa + Bsr + BmT)

    def stage4b(self):
        S, sb, I = self.S, self.sb, self.I
        D_ = self.dram
        sb.push()
        wo = sb.alloc("wo", [128, KC, D], BF16)
        Bwo = [self.dbuf("wo", "pool") for _ in range(4)]
        wsrc = I["w_out"].rearrange("(k p) n -> p k n", p=128)
        for i in range(4):
            self.dma("pool", wo[:, i * 4:(i + 1) * 4, :], wsrc[:, i * 4:(i + 1) * 4, :], [], [Bwo[i]], Bwo[i])
        mTg = [sb.alloc("mTg", [128, KC, 512], BF16)] * 2
        Bmg = [self.dbuf("mTg")] * 2
        xs = [sb.alloc("xs", [128, D], F32) for _ in range(2)]
        Bx = [self.dbuf("xs") for _ in range(2)]
        x1 = [sb.alloc("x1", [128, D], F32) for _ in range(2)]
        Bx1 = [self.dbuf("x1") for _ in range(2)]
        h2 = [sb.alloc("h2", [128, KC, 128], BF16) for _ in range(2)]
        Bh2 = [self.dbuf("h2") for _ in range(2)]
        rings = [self.norm_ring(sb)]
        rings.append(self.norm_ring(sb, junk=rings[0][0]))
        tq = [sb.alloc("tq", [128, 512], F32) for _ in range(2)]
        Btq = [Buf("tq%d" % i) for i in range(2)]
        rt = sb.alloc("rt", [128, 160], F32)
        Brt = Buf("rt")
        r2 = sb.alloc("r2", [128, 112], F32)
        Br2 = Buf("r2")
        selb = sb.alloc("selb", [128, 32], BF16)
        Bselb = Buf("selb")
        h2k = [sb.alloc("h2k", [128, D], BF16) for _ in range(2)]
        Bh2k = [Buf("h2k%d" % i) for i in range(2)]
        a2b = sb.alloc("a2b", [128, D], BF16)
        sh2bb = sb.alloc("sh2bb", [128, D], BF16)
        Ba2 = self.dbuf("a2b")
        self.dma("sp", a2b[:], D_["A2R"][0, :].partition_broadcast(128), [self.B_a2r], [Ba2], Ba2)
        self.dma("sp", sh2bb[:], D_["A2R"][1, :].partition_broadcast(128), [self.B_a2r], [Ba2], Ba2)
        mtsrc = D_["MT"].rearrange("k p t -> p k t")
        qi = [0]
        for g in range(4):
            sg = g % 2
            self.dma("sp", mTg[sg][:], mtsrc[:, :, g * 512:(g + 1) * 512], self.scr_reads("MT", range(16)), [Bmg[sg]], Bmg[sg])
            for tt_ in range(4):
                tile = g * 4 + tt_
                sl = tile % 2
                self.dma("sp", xs[sl][:], I["xm"][tile * 128:(tile + 1) * 128, :], [], [Bx[sl]], Bx[sl])
                for cg in range(4):
                    cs_ = slice(cg * 512, (cg + 1) * 512)
                    fb = cg % 4
                    for kc in range(KC):
                        self.mm(self.F[fb][:, :], mTg[sg][:, kc, tt_ * 128:(tt_ + 1) * 128], wo[:, kc, cs_], kc == 0, kc == KC - 1,
                                [Bmg[sg], Bwo[kc // 4]], [self.BF[fb]])
                    i = qi[0] % 2
                    qi[0] += 1
                    self.tt(tq[i][:], self.F[fb][:, :], self.gt1b[:, cs_], ALU.mult, [self.BF[fb], self.B_c0], [Btq[i]])
                    self.tt(x1[sl][:, cs_], tq[i][:], xs[sl][:, cs_], ALU.add, [Btq[i], Bx[sl]], [Bx1[sl]], eng="dve")
                tok = Buf("w")
                self.scr_w.setdefault(("X1", tile), []).append(tok)
                self.dma("sp", D_["X1"][tile * 128:(tile + 1) * 128, :], x1[sl][:], [Bx1[sl]], [tok], Bx1[sl])
                for _ in self.norm_to_hT(x1[sl][:], Bx1[sl], h2[sl][:, :, :], Bh2[sl], self.a2, self.modc[:, 3, :], rings[sl]):
                    pass
                tok = Buf("w")
                self.scr_w.setdefault(("H2T", tile), []).append(tok)
                self.dma("sp", D_["H2T"][:, :, tile * 128:(tile + 1) * 128].rearrange("k p t -> p k t"), h2[sl][:], [Bh2[sl]], [tok], Bh2[sl])
                pr = self.F[5][:, 0:36]
                bpr = self.BF[5]
                for kc in range(KC):
                    self.mm(pr, h2[sl][:, kc, :], self.wr[:, kc, :], kc == 0, kc == KC - 1, [Bh2[sl], self.B_c0], [bpr])
                lg = rt[:, 0:36]
                self.tt(lg, pr, self.br[:], ALU.add, [bpr, self.B_c0], [Brt])
                mx = rt[:, 36:37]
                S.op("dve", lambda e: e.tensor_reduce(out=rt[:, 36:37], in_=rt[:, 0:4], axis=AX.X, op=ALU.max), [Brt], [Brt])
                self.ts(rt[:, 37:38], mx, -1.0, None, ALU.mult, None, [Brt], [Brt])
                self.act(rt[:, 40:44], rt[:, 0:4], AF.Exp, [Brt], [Brt], bias=rt[:, 37:38], accum=rt[:, 38:39])
                self.ts(rt[:, 44:48], rt[:, 0:4], mx, None, ALU.is_ge, None, [Brt], [Brt])
                self.ts(rt[:, 44:48], rt[:, 44:48], -1.0, -NEGBIG, ALU.add, ALU.mult, [Brt], [Brt])
                for gi in range(4):
                    self.ts(rt[:, 48 + gi * 8:56 + gi * 8], rt[:, 4 + gi * 8:12 + gi * 8], rt[:, 44 + gi:45 + gi], None, ALU.add, None, [Brt], [Brt])
                S.op("dve", lambda e: e.max(out=rt[:, 80:88], in_=rt[:, 48:80]), [Brt], [Brt])
                self.ts(rt[:, 88:120], rt[:, 48:80], rt[:, 81:82], None, ALU.is_ge, None, [Brt], [Brt])
                self.ts(rt[:, 39:40], rt[:, 80:81], -1.0, None, ALU.mult, None, [Brt], [Brt])
                self.act(rt[:, 120:152], rt[:, 48:80], AF.Exp, [Brt], [Brt], bias=rt[:, 39:40])
                self.tt(rt[:, 120:152], rt[:, 120:152], rt[:, 88:120], ALU.mult, [Brt], [Brt])
                S.op("dve", lambda e: e.reduce_sum(out=rt[:, 152:153], in_=rt[:, 120:152], axis=AX.X), [Brt], [Brt])
                self.tt(rt[:, 152:153], rt[:, 152:153], rt[:, 38:39], ALU.mult, [Brt], [Brt])
                S.op("dve", lambda e: e.reciprocal(out=rt[:, 153:154], in_=rt[:, 152:153]), [Brt], [Brt])
                self.ts(self.wfull[:, tile, :], rt[:, 120:152], rt[:, 153:154], None, ALU.mult, None, [Brt], [self.B_wfull[tile]])
                self.cp("dve", selb[:], rt[:, 88:120], [Brt], [Bselb])
                pc = self.F[4]
                self.mm(pc[:, 0:32], self.slt[:, :], selb[:], True, True, [Bselb, self.B_c0], [self.BF[4]])
                self.mm(pc[:, 32:64], self.ones[:, :], selb[:], True, True, [Bselb, self.B_c0], [self.BF[4]])
                self.tt(r2[:, 0:32], pc[:, 0:32], self.tot[:], ALU.add, [self.BF[4], self.B_tot], [Br2])
                self.tt(self.tot[:], pc[:, 32:64], self.tot[:], ALU.add, [self.BF[4], self.B_tot], [self.B_tot])
                self.ts(r2[:, 0:32], r2[:, 0:32], float(CAP - 1), None, ALU.min, None, [Br2], [Br2])
                self.tt(r2[:, 0:32], r2[:, 0:32], self.ebase[:], ALU.add, [Br2, self.B_c0], [Br2])
                self.stt(r2[:, 32:64], r2[:, 0:32], 1.0e6, rt[:, 88:120], ALU.add, ALU.mult, [Br2, Brt], [Br2])
                S.op("dve", lambda e: e.max(out=r2[:, 64:72], in_=r2[:, 32:64]), [Br2], [Br2])
                self.ts(r2[:, 72:74], r2[:, 64:66], -1.0e6, None, ALU.add, None, [Br2], [Br2])
                self.cp("dve", self.desti[:, tile, :], r2[:, 72:74], [Br2], [self.B_dest[tile]])
                for k_ in range(2):
                    self.ts(r2[:, 80:112], r2[:, 32:64], r2[:, 64 + k_:65 + k_], None, ALU.is_equal, None, [Br2], [Br2])
                    self.tt(r2[:, 80:112], r2[:, 80:112], self.wfull[:, tile, :], ALU.mult, [Br2, self.B_wfull[tile]], [Br2])
                    S.op("dve", lambda e, k_=k_, tile=tile: e.reduce_sum(out=self.wk[:, tile, k_:k_ + 1], in_=r2[:, 80:112], axis=AX.X),
                         [Br2], [self.B_dest[tile]])
                xn_t, bxn_t = h2k[sl], Bh2k[sl]
                self.tt(xn_t[:], rings[sl][4][:], a2b[:], ALU.mult, [rings[sl][7], Ba2], [bxn_t])
                self.tt(xn_t[:], xn_t[:], sh2bb[:], ALU.add, [bxn_t, Ba2], [bxn_t])
                for k_ in range(2):
                    self.cp("dve", self.six[sl][:, k_:k_ + 1], self.desti[:, tile, k_:k_ + 1], [self.B_dest[tile]], [self.B_six[sl][k_]])
                    S.op("pool", lambda e, k_=k_, ix=self.six[sl], xn_t=xn_t: e.indirect_dma_start(
                        out=D_["XD"], out_offset=bass.IndirectOffsetOnAxis(ap=ix[:, k_:k_ + 1], axis=0),
                        in_=xn_t[:], in_offset=None),
                        [bxn_t, self.B_six[sl][k_]], [Buf("sc")], dbuf=bxn_t)
        if self.debug:
            self.dma("sp", D_["WF"], self.wfull[:].rearrange("p a b -> p (a b)"), self.B_wfull, [Buf("x")], self.B_wfull[0])
        sb.pop()
        self.release(Bwo + Bmg[:1] + Bx + Bx1 + Bh2 + [Ba2])

    def stage5(self):
        S, sb, I = self.S, self.sb, self.I
        D_ = self.dram
        sb.push()
        w1 = [sb.alloc("w1", [128, KC, 512], BF16) for _ in range(2)]
        w3 = [sb.alloc("w3", [128, KC, 512], BF16) for _ in range(2)]
        w2 = sb.alloc("w2", [128, 4, D], BF16)
        Bw1 = [self.dbuf("w1", "pool") for _ in range(2)]
        Bw3 = [self.dbuf("w3", "pool") for _ in range(2)]
        Bw2 = self.dbuf("w2", "pool")
        nrt = CAP // 128
        xr = [sb.alloc("xr", [128, D], BF16) for _ in range(nrt)]
        Bxr = [self.dbuf("xr") for _ in range(nrt)]
        XT = [sb.alloc("XT", [128, KC, CAP], BF16) for _ in range(2)]
        BXT = [[Buf("XT%d_%d" % (i, j)) for j in range(nrt)] for i in range(2)]
        s1 = [sb.alloc("s1", [128, CAP], F32) for _ in range(2)]
        Bs1 = [Buf("s1_%d" % i) for i in range(2)]
        hid = [sb.alloc("hid", [128, 4, CAP], BF16) for _ in range(2)]
        Bhid = [[Buf("hid%d_%d" % (i, f)) for f in range(4)] for i in range(2)]
        ys = [sb.alloc("ys", [128, D], F32) for _ in range(2)]
        Bys = [self.dbuf("ys") for _ in range(2)]
        si = [0]
        yi = [0]

        def loads(ex):
            for r_ in range(nrt):
                row0 = ex * CAP + r_ * 128
                self.dma("sp", xr[r_][:], D_["XD"][row0:row0 + 128, :], [], [Bxr[r_]], Bxr[r_])

        def trans(ex):
            sl = ex % 2
            for r_ in range(nrt):
                for tb in range(2):
                    for ts_ in range(8):
                        kc = tb * 8 + ts_
                        self.tr(self.T[tb][:, ts_ * 128:(ts_ + 1) * 128], xr[r_][:, kc * 128:(kc + 1) * 128], [Bxr[r_]], [self.BT[tb][0]])
                    src = self.T[tb][:, :].rearrange("p (k t) -> p k t", t=128)
                    dst = XT[sl][:, tb * 8:(tb + 1) * 8, r_ * 128:(r_ + 1) * 128]
                    self.cp("act" if tb == 0 else "dve", dst, src, [self.BT[tb][0]], [BXT[sl][r_]])

        loads(0)
        trans(0)
        for ex in range(32):
            sl = ex % 2
            self.dma("pool", w1[sl][:], I["w1"][ex].rearrange("(k p) n -> p k n", p=128), [], [Bw1[sl]], Bw1[sl])
            self.dma("pool", w3[sl][:], I["w3"][ex].rearrange("(k p) n -> p k n", p=128), [], [Bw3[sl]], Bw3[sl])
            self.dma("pool", w2[:], I["w2"][ex].rearrange("(k p) n -> p k n", p=128), [], [Bw2], Bw2)
            if ex + 1 < 32:
                loads(ex + 1)
            for fc in range(4):
                fs = slice(fc * 128, (fc + 1) * 128)
                f1, f3 = (fc % 2) * 2, (fc % 2) * 2 + 1
                for kc in range(KC):
                    self.mm(self.F[f1][:, 0:CAP], w1[sl][:, kc, fs], XT[sl][:, kc, :], kc == 0, kc == KC - 1, [Bw1[sl]] + BXT[sl], [self.BF[f1]])
                for kc in range(KC):
                    self.mm(self.F[f3][:, 0:CAP], w3[sl][:, kc, fs], XT[sl][:, kc, :], kc == 0, kc == KC - 1, [Bw3[sl]] + BXT[sl], [self.BF[f3]])
                i = si[0] % 2
                si[0] += 1
                self.act(s1[i][:], self.F[f1][:, 0:CAP], AF.Silu, [self.BF[f1]], [Bs1[i]])
                self.tt(hid[sl][:, fc, :], s1[i][:], self.F[f3][:, 0:CAP], ALU.mult, [Bs1[i], self.BF[f3]], [Bhid[sl][fc]])
            if ex + 1 < 32:
                trans(ex + 1)
            for r_ in range(nrt):
                y_ = ys[yi[0] % 2]
                by = Bys[yi[0] % 2]
                yi[0] += 1
                for cg in range(4):
                    cs_ = slice(cg * 512, (cg + 1) * 512)
                    fb = 4 + (cg % 2)
                    for fc in range(4):
                        self.mm(self.F[fb][:, :], hid[sl][:, fc, r_ * 128:(r_ + 1) * 128], w2[:, fc, cs_], fc == 0, fc == 3,
                                [Bhid[sl][fc], Bw2], [self.BF[fb]])
                    if fb == 4:
                        self.cp("act", y_[:, cs_], self.F[fb][:, :], [self.BF[fb]], [by])
                    else:
                        self.cp("dve", y_[:, cs_], self.F[fb][:, :], [self.BF[fb]], [by])
                row0 = ex * CAP + r_ * 128
                for hf in range(2):
                    self.dma("sp", D_["YD%d" % hf][row0:row0 + 128, :], y_[:, hf * 1024:(hf + 1) * 1024], [by], [Buf("y")], by)
        sb.pop()
        self.release(Bw1 + Bw3 + [Bw2] + Bxr + Bys)
        self.bar()
        sb.push()
        xs = [sb.alloc("xs", [128, D], F32) for _ in range(2)]
        Bx = [self.dbuf("xs") for _ in range(2)]
        yg = [[[sb.alloc("yg", [128, 1024], F32) for _ in range(2)] for _ in range(2)] for _ in range(2)]
        Byg = [[[Buf("yg") for _ in range(2)] for _ in range(2)] for _ in range(2)]
        jk = sb.alloc("jk5", [128, D], BF16)
        st5 = sb.alloc("st5", [128, 4], F32)
        Bst = Buf("st5")
        for tile in range(16):
            sl = tile % 2
            self.dma("sp", xs[sl][:], D_["X1"][tile * 128:(tile + 1) * 128, :], self.scr_reads("X1", [tile]), [Bx[sl]], Bx[sl])
            for k_ in range(2):
                self.cp("dve", self.gix[sl][:, k_:k_ + 1], self.desti[:, tile, k_:k_ + 1], [self.B_dest[tile]], [self.B_gix[sl][k_]])
                for hf in range(2):
                    S.op("pool", lambda e, k_=k_, ix=self.gix[sl], dst=yg[sl][k_][hf], hf=hf: e.indirect_dma_start(
                        out=dst[:], out_offset=None, in_=D_["YD%d" % hf],
                        in_offset=bass.IndirectOffsetOnAxis(ap=ix[:, k_:k_ + 1], axis=0)),
                        [self.B_gix[sl][k_]], [Byg[sl][k_][hf]], dbuf=Byg[sl][k_][hf])
            for hf in range(2):
                a, b = yg[sl][0][hf], yg[sl][1][hf]
                ba, bb = Byg[sl][0][hf], Byg[sl][1][hf]
                hs_ = slice(hf * 1024, (hf + 1) * 1024)
                self.ts(a[:], a[:], self.wk[:, tile, 0:1], None, ALU.mult, None, [ba, self.B_dest[tile]], [ba])
                self.stt(a[:], b[:], self.wk[:, tile, 1:2], a[:], ALU.mult, ALU.add, [bb, ba, self.B_dest[tile]], [ba])
                self.tt(a[:], a[:], self.gt2b[:, hs_], ALU.mult, [ba, self.B_c0], [ba])
                self.tt(xs[sl][:, hs_], xs[sl][:, hs_], a[:], ALU.add, [Bx[sl], ba], [Bx[sl]])
            self.act(jk[:], xs[sl][:], AF.Square, [Bx[sl]], [Bst], accum=st5[:, 0:1])
            self.rstd(st5[:, 2:3], st5[:, 0:1], float(D), [Bst], [Bst, Bst], st5[:, 1:2])
            self.stt(xs[sl][:], xs[sl][:], st5[:, 2:3], self.fgb[:], ALU.mult, ALU.mult, [Bx[sl], Bst, self.B_c0], [Bx[sl]])
            self.dma("sp", self.out[tile * 128:(tile + 1) * 128, :], xs[sl][:], [Bx[sl]], [Buf("o")], Bx[sl])
        sb.pop()
        self.release(Bx)


def host_consts():
    bf = ml_dtypes.bfloat16
    c = {}
    c["k_ident"] = np.eye(128, dtype=np.float32)
    cm = np.zeros((128, 512), np.float32)
    for j in range(2):
        kp = np.arange(128)[:, None] + j * 128
        q = np.arange(256)[None, :]
        cm[:, j * 256:(j + 1) * 256] = np.where(kp <= q, 0.0, NEG)
    c["k_cm"] = cm
    cp = np.zeros((128, 16, 16), np.float32)
    for qt in range(16):
        cp[:, qt, 8 + qt // 2:] = NEGBIG
    c["k_cpen"] = cp.reshape(128, 256)
    sel = np.zeros((16, 16, 128), np.float32)
    for i in range(16):
        sel[i, i, :] = 1.0
    c["k_sel"] = sel.reshape(16, 2048)
    s = np.arange(128)[:, None]
    t = np.arange(128)[None, :]
    same = (s // 32) == (t // 32)
    mid = (t // 32) * 32 + 15
    wm = np.where(same & (s > mid) & (s <= t), 1.0, 0.0) - np.where(same & (s > t) & (s <= mid), 1.0, 0.0)
    wmid = np.zeros((128, 4), np.float32)
    wtot = np.zeros((128, 4), np.float32)
    for cc in range(4):
        sl = np.arange(128)
        wmid[:, cc] = ((sl // 32) == cc) & (sl <= cc * 32 + 15)
        wtot[:, cc] = (sl // 32) == cc
    c["k_wall"] = np.concatenate([wm, wmid, wtot], axis=1).astype(np.float32)
    c["k_wl"] = np.where(same & (s > t), 1.0, 0.0).astype(np.float32)
    c["k_bd"] = np.where(same & (s <= t), 1.0, 0.0).astype(np.float32)
    c["k_slt"] = np.where(s < t, 1.0, 0.0).astype(np.float32)
    c["k_ones"] = np.ones((128, 128), np.float32)
    c["k_ebase"] = np.tile((np.arange(32) * CAP).astype(np.float32)[None, :], (128, 1))
    return c


_NC_CACHE = {}


def get_nc(debug=False, stages="012345"):
    key = (debug, stages)
    if key not in _NC_CACHE:
        nc = bass.Bass("TRN2", target_bir_lowering=False)
        kb = K(nc, debug=debug, stages=stages)
        kb.build()
        _NC_CACHE[key] = (nc, kb)
    return _NC_CACHE[key]


def make_in_maps(inputs, cores=range(8)):
    f = lambda a: np.ascontiguousarray(np.asarray(a, dtype=np.float32))
    x = f(inputs["x"])
    c = f(inputs["c"])
    shared = {
        "norm1_g": f(inputs["norm1_g"])[0], "norm2_g": f(inputs["norm2_g"])[0], "final_g": f(inputs["final_g"]),
        "w_ada": f(inputs["w_ada"])[0], "b_ada": f(inputs["b_ada"])[0], "w_in": f(inputs["w_in"])[0],
        "r_lower": f(inputs["r_lower"]), "r_norm_g": f(inputs["r_norm_g"])[0],
        "w_up_a": f(inputs["w_up_a"])[0], "w_up_r": f(inputs["w_up_r"])[0], "w_out": f(inputs["w_out"])[0],
        "w_rg": f(inputs["w_rg"])[0], "b_rg": f(inputs["b_rg"])[0], "w_re": f(inputs["w_re"])[0], "b_re": f(inputs["b_re"])[0],
        "w1": f(inputs["w1"])[0], "w3": f(inputs["w3"])[0], "w2": f(inputs["w2"])[0],
    }
    shared.update(host_consts())
    maps = []
    for core in cores:
        b, half = core // 2, core % 2
        m = dict(shared)
        m["xm"] = np.ascontiguousarray(x[b, half * NT:(half + 1) * NT])
        m["xp"] = np.ascontiguousarray(x[b, 0:NT])
        m["c"] = np.ascontiguousarray(c[b])
        m["pm"] = np.full((128, 1), float(half), np.float32)
        maps.append(m)
    return maps


def kernel(**inputs):
    nc, _ = get_nc()
    maps = make_in_maps(inputs)
    res = run_bass_kernel_spmd(nc, maps, core_ids=list(range(8)))
    out = np.zeros((4, 4096, D), np.float32)
    for core in range(8):
        b, half = core // 2, core % 2
        out[b, half * NT:(half + 1) * NT] = np.asarray(res.results[core]["out"], dtype=np.float32)
    return out
```
